# Optimizing a Trainium2 kernel written in Bass

```python
import math
import jax, jax.numpy as jnp
from jax import lax
import numpy as np

D_MODEL = 1024
BATCH = 8
SEQ = 4096
DEPTH = 2

ATT_HEADS = 8
HEAD_DIM = 64
ATT_WIDTH = ATT_HEADS * 2 * HEAD_DIM
Q_BLOCK = 128
ROPE_THETA = 10000.0
MAX_POS_OFFSET = 1024
ML_HEADS = 8
ML_QK_DIM = D_MODEL // 16
ML_V_DIM = D_MODEL // 8
ML_QK = ML_HEADS * ML_QK_DIM
ML_V = ML_HEADS * ML_V_DIM
ML_PROJ = 2 * ML_QK + 2 * ML_V + 2 * ML_HEADS
ML_CHUNK = 64
GATE_SOFTCAP = 15.0
N_EXPERTS = 32
TOP_K = 4
D_FF = D_MODEL
SWIGLU_ALPHA = 1.702
SWIGLU_LIMIT = 7.0
EXPERT_BLOCK = 512
RMS_EPS = 1e-6
N_MIXERS = 2
N_ATT_LAYERS = (DEPTH + 1) // 2
N_ML_LAYERS = DEPTH // 2

kernel_name = 'hybrid_diffattn_mlstm_moe_adaln'


def rms_norm(x, gain):
    xf = x.astype(jnp.float32)
    y = xf * lax.rsqrt(jnp.mean(xf * xf, axis=-1, keepdims=True) + RMS_EPS)
    return (y * gain.astype(jnp.float32)).astype(x.dtype)


def rope_tables(positions):
    inv_freq = ROPE_THETA ** (-jnp.arange(0, HEAD_DIM, 2, dtype=jnp.float32) / HEAD_DIM)
    ang = positions.astype(jnp.float32)[..., None] * inv_freq
    return jnp.cos(ang), jnp.sin(ang)


def apply_rope(x, cos, sin):
    xf = x.astype(jnp.float32)
    x1, x2 = jnp.split(xf, 2, axis=-1)
    return jnp.concatenate([x1 * cos - x2 * sin, x2 * cos + x1 * sin], axis=-1).astype(x.dtype)


def softcap(z):
    return GATE_SOFTCAP * jnp.tanh(z / GATE_SOFTCAP)


def diff_attention(h, cos, sin, w_in, w_out, q_gain, k_gain, lam_q1, lam_k1, lam_q2, lam_k2, sub_gain, lambda_init):
    B, S, _ = h.shape
    f32 = jnp.float32
    nq = S // Q_BLOCK
    q, k, v = jnp.split(h @ w_in, 3, axis=-1)
    q = q.reshape(B, S, ATT_HEADS, 2, HEAD_DIM)
    k = k.reshape(B, S, ATT_HEADS, 2, HEAD_DIM)
    v = v.reshape(B, S, ATT_HEADS, 2 * HEAD_DIM)
    cs, sn = cos[:, :, None, None, :], sin[:, :, None, None, :]
    q = apply_rope(rms_norm(q, q_gain), cs, sn)
    k = apply_rope(rms_norm(k, k_gain), cs, sn)
    lam = (jnp.exp(jnp.sum(lam_q1.astype(f32) * lam_k1.astype(f32)))
           - jnp.exp(jnp.sum(lam_q2.astype(f32) * lam_k2.astype(f32))) + lambda_init)
    q_blocks = q.reshape(B, nq, Q_BLOCK, ATT_HEADS, 2, HEAD_DIM).transpose(1, 0, 3, 4, 2, 5)
    k_t = k.transpose(0, 2, 3, 1, 4)
    v_t = v.transpose(0, 2, 1, 3)
    key_pos = jnp.arange(S)
    scale = HEAD_DIM ** -0.5

    def attend_block(args):
        q_blk, blk = args
        s = jnp.einsum('bhcqd,bhckd->bhcqk', q_blk, k_t).astype(f32) * scale
        q_pos = blk * Q_BLOCK + jnp.arange(Q_BLOCK)
        s = jnp.where(key_pos[None, :] <= q_pos[:, None], s, -jnp.inf)
        p = jax.nn.softmax(s, axis=-1)
        a = p[:, :, 0] - lam * p[:, :, 1]
        return jnp.einsum('bhqk,bhkd->bhqd', a.astype(v_t.dtype), v_t)

    o = lax.map(attend_block, (q_blocks, jnp.arange(nq)))
    o = o.transpose(1, 0, 3, 2, 4).reshape(B, S, ATT_HEADS, 2 * HEAD_DIM)
    o = rms_norm(o, sub_gain) * (1.0 - lambda_init)
    return o.reshape(B, S, ATT_WIDTH) @ w_out


def mlstm_chunkwise(q, k, v, i_pre, log_f):
    f32 = jnp.float32
    B, H, S, dk = q.shape
    dv = v.shape[-1]
    L = ML_CHUNK
    NC = S // L

    def to_chunks(a):
        a = a.astype(f32).reshape(a.shape[:2] + (NC, L) + a.shape[3:])
        return jnp.moveaxis(a, 2, 0)

    xs = tuple(to_chunks(a) for a in (q, k, v, i_pre, log_f))
    causal = jnp.tril(jnp.ones((L, L), dtype=bool))

    def step(carry, inp):
        C, n, m = carry
        qb, kb, vb, ib, fb = inp
        b = jnp.cumsum(fb, axis=-1)
        g = b[..., -1]
        log_d = b[..., :, None] - b[..., None, :] + ib[..., None, :]
        log_d = jnp.where(causal, log_d, -jnp.inf)
        inter = b + m[..., None]
        m_row = jnp.maximum(inter, jnp.max(log_d, axis=-1))
        s = jnp.einsum('bhld,bhsd->bhls', qb, kb) * jnp.exp(log_d - m_row[..., None])
        w_inter = jnp.exp(inter - m_row)
        num = (w_inter[..., None] * jnp.einsum('bhld,bhdv->bhlv', qb, C)
               + jnp.einsum('bhls,bhsv->bhlv', s, vb))
        den = w_inter * jnp.einsum('bhld,bhd->bhl', qb, n) + jnp.sum(s, axis=-1)
        h = num / jnp.maximum(jnp.abs(den), jnp.exp(-m_row))[..., None]
        log_w = g[..., None] - b + ib
        m_new = jnp.maximum(g + m, jnp.max(log_w, axis=-1))
        ws = jnp.exp(log_w - m_new[..., None])
        decay = jnp.exp(g + m - m_new)
        C_new = decay[..., None, None] * C + jnp.einsum('bhl,bhld,bhlv->bhdv', ws, kb, vb)
        n_new = decay[..., None] * n + jnp.einsum('bhl,bhld->bhd', ws, kb)
        return (C_new, n_new, m_new), h

    init = (jnp.zeros((B, H, dk, dv), f32), jnp.zeros((B, H, dk), f32), jnp.zeros((B, H), f32))
    _, hs = lax.scan(step, init, xs)
    return jnp.moveaxis(hs, 0, 2).reshape(B, H, S, dv)


def mlstm_mixer(h, w_in, b_igate, b_fgate, out_gain, w_out):
    B, S, _ = h.shape
    f32 = jnp.float32
    splits = [ML_QK, 2 * ML_QK, 2 * ML_QK + ML_V, 2 * ML_QK + 2 * ML_V, 2 * ML_QK + 2 * ML_V + ML_HEADS]
    q, k, v, o, i_pre, f_pre = jnp.split(h @ w_in, splits, axis=-1)
    q = q.reshape(B, S, ML_HEADS, ML_QK_DIM).transpose(0, 2, 1, 3)
    k = (k * (ML_QK_DIM ** -0.5)).reshape(B, S, ML_HEADS, ML_QK_DIM).transpose(0, 2, 1, 3)
    v = v.reshape(B, S, ML_HEADS, ML_V_DIM).transpose(0, 2, 1, 3)
    i_pre = softcap(i_pre.astype(f32) + b_igate.astype(f32)).transpose(0, 2, 1)
    log_f = jax.nn.log_sigmoid(softcap(f_pre.astype(f32) + b_fgate.astype(f32))).transpose(0, 2, 1)
    ht = mlstm_chunkwise(q, k, v, i_pre, log_f).transpose(0, 2, 1, 3)
    ht = rms_norm(ht, out_gain.reshape(ML_HEADS, ML_V_DIM)).astype(h.dtype)
    y = jax.nn.sigmoid(o).reshape(B, S, ML_HEADS, ML_V_DIM) * ht
    return y.reshape(B, S, ML_V) @ w_out


def moe_ffn(h, w_router, b_router, w_gate_up, b_gate_up, w_down, b_down):
    B, S, D = h.shape
    T = B * S
    G = EXPERT_BLOCK
    xt = h.reshape(T, D)
    logits = (xt @ w_router).astype(jnp.float32) + b_router.astype(jnp.float32)
    top_val, top_idx = lax.top_k(logits, TOP_K)
    gates = jax.nn.softmax(top_val, axis=-1)
    A = T * TOP_K
    flat_e = top_idx.reshape(A).astype(jnp.int32)
    flat_t = (jnp.arange(A, dtype=jnp.int32) // TOP_K)
    flat_w = gates.reshape(A)
    order = jnp.argsort(flat_e)
    se, st, sw = flat_e[order], flat_t[order], flat_w[order]
    counts = jnp.bincount(flat_e, length=N_EXPERTS)
    start = jnp.cumsum(counts) - counts
    padded = (counts + G - 1) // G * G
    pend = jnp.cumsum(padded)
    pstart = pend - padded
    slot = pstart[se] + (jnp.arange(A) - start[se])
    n_blocks = -(-A // G) + N_EXPERTS
    P = n_blocks * G
    slot_tok = jnp.full((P,), T, dtype=jnp.int32).at[slot].set(st)
    slot_w = jnp.zeros((P,), h.dtype).at[slot].set(sw.astype(h.dtype))
    block_e = jnp.minimum(jnp.searchsorted(pend, jnp.arange(n_blocks) * G, side='right'), N_EXPERTS - 1)
    xpad = jnp.concatenate([xt, jnp.zeros((1, D), xt.dtype)], axis=0)

    def expert_block(args):
        idx, e = args
        xb = xpad[idx]
        gu = xb @ w_gate_up[e] + b_gate_up[e]
        glu = jnp.minimum(gu[:, ::2], SWIGLU_LIMIT)
        lin = jnp.clip(gu[:, 1::2], -SWIGLU_LIMIT, SWIGLU_LIMIT)
        act = glu * jax.nn.sigmoid(SWIGLU_ALPHA * glu) * (lin + 1.0)
        return act @ w_down[e] + b_down[e]

    ys = lax.map(expert_block, (slot_tok.reshape(n_blocks, G), block_e)).reshape(P, D)
    out = jax.ops.segment_sum(ys * slot_w[:, None], slot_tok, num_segments=T + 1)[:T]
    return out.reshape(B, S, D)


def setup_inputs(seed: int = 0) -> dict:
    key = jax.random.key(seed)
    ks = jax.random.split(key, 32)
    D = D_MODEL
    f32 = jnp.float32

    def nrm(k, shape, scale):
        return jax.random.normal(k, shape, f32) * scale

    na, nm = N_ATT_LAYERS, N_ML_LAYERS
    positions = (jax.random.randint(ks[2], (BATCH, 1), 0, MAX_POS_OFFSET, dtype=jnp.int32)
                 + jnp.arange(SEQ, dtype=jnp.int32)[None, :])
    return {
        'x': nrm(ks[0], (BATCH, SEQ, D), 1.0),
        'c': nrm(ks[1], (BATCH, D), 1.0),
        'positions': positions,
        'ada_w': nrm(ks[3], (DEPTH, D, 6 * D), 0.5 * D ** -0.5),
        'ada_b': nrm(ks[4], (DEPTH, 6 * D), 0.02),
        'mix_norm': 1.0 + nrm(ks[5], (DEPTH, D), 0.02),
        'ffn_norm': 1.0 + nrm(ks[6], (DEPTH, D), 0.02),
        'att_w_in': nrm(ks[7], (na, D, 3 * ATT_WIDTH), D ** -0.5),
        'att_w_out': nrm(ks[8], (na, ATT_WIDTH, D), ATT_WIDTH ** -0.5),
        'att_q_norm': 1.0 + nrm(ks[9], (na, HEAD_DIM), 0.02),
        'att_k_norm': 1.0 + nrm(ks[10], (na, HEAD_DIM), 0.02),
        'att_lam_q1': nrm(ks[11], (na, HEAD_DIM), 0.1),
        'att_lam_k1': nrm(ks[12], (na, HEAD_DIM), 0.1),
        'att_lam_q2': nrm(ks[13], (na, HEAD_DIM), 0.1),
        'att_lam_k2': nrm(ks[14], (na, HEAD_DIM), 0.1),
        'att_sub_norm': 1.0 + nrm(ks[15], (na, 2 * HEAD_DIM), 0.02),
        'ml_w_in': nrm(ks[16], (nm, D, ML_PROJ), D ** -0.5),
        'ml_b_igate': nrm(ks[17], (nm, ML_HEADS), 0.1),
        'ml_b_fgate': jnp.linspace(3.0, 6.0, ML_HEADS, dtype=f32)[None, :] + nrm(ks[18], (nm, ML_HEADS), 0.1),
        'ml_out_norm': 1.0 + nrm(ks[19], (nm, ML_V), 0.02),
        'ml_w_out': nrm(ks[20], (nm, ML_V, D), ML_V ** -0.5),
        'router_w': nrm(ks[21], (DEPTH, D, N_EXPERTS), D ** -0.5),
        'router_b': nrm(ks[22], (DEPTH, N_EXPERTS), 0.01),
        'moe_w_gate_up': nrm(ks[23], (DEPTH, N_EXPERTS, D, 2 * D_FF), D ** -0.5),
        'moe_b_gate_up': nrm(ks[24], (DEPTH, N_EXPERTS, 2 * D_FF), 0.02),
        'moe_w_down': nrm(ks[25], (DEPTH, N_EXPERTS, D_FF, D), D_FF ** -0.5),
        'moe_b_down': nrm(ks[26], (DEPTH, N_EXPERTS, D), 0.02),
    }


def reference(x, c, positions, ada_w, ada_b, mix_norm, ffn_norm,
              att_w_in, att_w_out, att_q_norm, att_k_norm, att_lam_q1, att_lam_k1, att_lam_q2, att_lam_k2, att_sub_norm,
              ml_w_in, ml_b_igate, ml_b_fgate, ml_out_norm, ml_w_out,
              router_w, router_b, moe_w_gate_up, moe_b_gate_up, moe_w_down, moe_b_down):
    cos, sin = rope_tables(positions)
    c_act = jax.nn.silu(c)
    for layer in range(DEPTH):
        mod = (c_act @ ada_w[layer] + ada_b[layer])[:, None, :]
        sh1, sc1, g1, sh2, sc2, g2 = jnp.split(mod, 6, axis=-1)
        h = rms_norm(x, mix_norm[layer]) * (1.0 + sc1) + sh1
        j = layer // N_MIXERS
        if layer % N_MIXERS == 0:
            lambda_init = 0.8 - 0.6 * math.exp(-0.3 * layer)
            y = diff_attention(h, cos, sin, att_w_in[j], att_w_out[j], att_q_norm[j], att_k_norm[j],
                               att_lam_q1[j], att_lam_k1[j], att_lam_q2[j], att_lam_k2[j], att_sub_norm[j], lambda_init)
        else:
            y = mlstm_mixer(h, ml_w_in[j], ml_b_igate[j], ml_b_fgate[j], ml_out_norm[j], ml_w_out[j])
        x = x + g1 * y
        h = rms_norm(x, ffn_norm[layer]) * (1.0 + sc2) + sh2
        x = x + g2 * moe_ffn(h, router_w[layer], router_b[layer], moe_w_gate_up[layer],
                             moe_b_gate_up[layer], moe_w_down[layer], moe_b_down[layer])
    return x
```

```python
import numpy as np
import concourse.bass as bass
import concourse.mybir as mybir
from concourse.bass_utils import run_bass_kernel_spmd

F32 = mybir.dt.float32
BF16 = mybir.dt.bfloat16
I32 = mybir.dt.int32
AF = mybir.ActivationFunctionType
ALU = mybir.AluOpType
AX = mybir.AxisListType

ENGMAP = {"pe": "tensor", "act": "scalar", "dve": "vector", "pool": "gpsimd", "sp": "sync"}


class Sched:
    def __init__(self, nc, stack, ndma=8):
        self.nc = nc
        self.prog = {k: [] for k in ENGMAP}
        self.sem = {}
        self.cnt = {k: 0 for k in ENGMAP}
        self.dsem = {}
        self.drr = {k: 0 for k in ENGMAP}
        for k in ENGMAP:
            self.sem[k] = stack.enter_context(nc.semaphore("s_" + k))
        for k in ("sp", "pool", "act"):
            self.dsem[k] = [[stack.enter_context(nc.semaphore("d_%s%d" % (k, j))), 0] for j in range(ndma)]
        self.lastw = {}
        self.readers = {}
        self.seen = {}
        self.nwait = 0
        self.nins = 0

    def _deps(self, eng, reads, writes):
        best = {}
        def add(tok):
            s, v, src = tok
            if src == eng and eng == "pe":
                return
            key = id(s)
            if key not in best or best[key][1] < v:
                best[key] = (s, v)
        for k in reads:
            if k in self.lastw:
                add(self.lastw[k])
        for k in writes:
            if k in self.lastw:
                add(self.lastw[k])
            for t in self.readers.get(k, ()):
                add(t)
        for key, (s, v) in best.items():
            sk = (eng, key)
            if self.seen.get(sk, 0) >= v:
                continue
            self.seen[sk] = v
            self.prog[eng].append(lambda e, s=s, v=v: e.wait_ge(s, v))
            self.nwait += 1

    def _commit(self, tok, reads, writes):
        for k in reads:
            self.readers.setdefault(k, []).append(tok)
        for k in writes:
            self.lastw[k] = tok
            self.readers[k] = []

    def op(self, eng, fn, reads=(), writes=(), sig=True):
        self._deps(eng, reads, writes)
        sem = self.sem[eng]
        if sig:
            self.cnt[eng] += 1
            tok = (sem, self.cnt[eng], eng)
            self.prog[eng].append(lambda e, fn=fn, sem=sem: fn(e).then_inc(sem, 1))
        else:
            tok = (sem, self.cnt[eng] + 1, eng)
            self.prog[eng].append(lambda e, fn=fn: fn(e))
        self.nins += 1
        self._commit(tok, reads, writes)
        return tok

    def dma(self, q, out, in_, reads=(), writes=(), **kw):
        self._deps(q, reads, writes)
        j = self.drr[q]
        self.drr[q] = (j + 1) % len(self.dsem[q])
        ent = self.dsem[q][j]
        sem, c = ent
        if c > 0:
            sk = (q, id(sem))
            if self.seen.get(sk, 0) < 16 * c:
                self.seen[sk] = 16 * c
                self.prog[q].append(lambda e, s=sem, v=16 * c: e.wait_ge(s, v))
        ent[1] = c + 1
        tok = (sem, 16 * (c + 1), "dma_" + q)
        self.prog[q].append(lambda e, out=out, in_=in_, sem=sem, kw=kw: e.dma_start(out=out, in_=in_, **kw).then_inc(sem, 16))
        self.nins += 1
        self._commit(tok, reads, writes)
        return tok

    def barrier(self):
        toks = []
        for k in ENGMAP:
            if self.cnt[k] > 0:
                toks.append((self.sem[k], self.cnt[k], k))
        for q in self.dsem:
            for sem, c in self.dsem[q]:
                if c > 0:
                    toks.append((sem, 16 * c, "dma"))
        for eng in ENGMAP:
            for s_, v, src in toks:
                if src == eng and eng == "pe":
                    continue
                sk = (eng, id(s_))
                if self.seen.get(sk, 0) >= v:
                    continue
                self.seen[sk] = v
                self.prog[eng].append(lambda e, s_=s_, v=v: e.wait_ge(s_, v))
        self.lastw = {}
        self.readers = {}

    def wait_all(self, eng, keys):
        self._deps(eng, list(keys), [])

    def emit(self, block):
        for k, name in ENGMAP.items():
            lst = self.prog[k]
            def body(e, lst=lst):
                for f in lst:
                    f(e)
            getattr(block, name)(body)


import math
from contextlib import ExitStack
import numpy as np

S_LEN = 4096
D = 1024
NT = S_LEN // 128
EPS = 1e-6
PI = math.pi


def make_consts():
    c = np.zeros((128, 1024), np.float32)
    c[:, 0:128] = np.eye(128, dtype=np.float32)
    s = np.arange(128)[:, None]
    l = np.arange(128)[None, :]
    c[:, 128:256] = (s <= l).astype(np.float32)
    c[:, 256:384] = np.where(s <= l, 0.0, -30000.0)
    inv = (10000.0 ** (-np.arange(0, 64, 2, dtype=np.float32) / 64)).astype(np.float32)
    c[:, 384:416] = inv[None, :]
    for h in range(8):
        for j in range(3):
            c[8 * j + h, 416 + 0:416 + 0] = 0
    return c


def make_sel():
    sel = np.zeros((128, 8, 128), np.float32)
    for h in range(8):
        for j in range(3):
            sel[8 * j + h, h, :] = 1.0
    return sel.reshape(128, 1024)


class Ctx:
    pass


_uid = [0]


def uname(name):
    _uid[0] += 1
    return "s%d_%s" % (_uid[0], name)


def T(cx, name, shape, dt=F32):
    return cx.stack.enter_context(cx.nc.sbuf_tensor(uname(name), shape, dt))


def common_setup(cx, layer_half, c_d, adaw_d, adab_d, norm_d):
    S, nc = cx.S, cx.nc
    with ExitStack() as st:
        def TT(name, shape, dt=F32):
            return st.enter_context(nc.sbuf_tensor(uname(name), shape, dt))
        rowc = TT("rowc", [128, 1024])
        crep = TT("crep", [128, 8, 128])
        stg = [TT("adst%d" % i, [128, 3072]) for i in range(2)]
        adab = TT("adab", [128, 3072])
        nrm = TT("nrmb", [128, 1024])
        c0 = layer_half * 3072
        S.dma("sp", rowc[:], c_d.partition_broadcast(128), writes=["rowc"])
        S.dma("sp", adab[:], adab_d[:, c0:c0 + 3072].partition_broadcast(128), writes=["adab"])
        S.dma("sp", nrm[:], norm_d.partition_broadcast(128), writes=["nrmb"])
        S.op("act", lambda e: e.activation(out=crep[:].rearrange("p a b -> p (a b)"), in_=rowc[:], func=AF.Sigmoid), reads=["rowc"], writes=["crep"])
        S.op("dve", lambda e: e.tensor_tensor(out=rowc[:], in0=rowc[:], in1=crep[:].rearrange("p a b -> p (a b)"), op=ALU.mult), reads=["crep", "rowc"], writes=["rowc"])
        for kc in range(8):
            b = cx.ps[kc // 4]
            S.op("pe", lambda e, kc=kc, b=b: e.transpose(b[:, (kc % 4) * 128:(kc % 4 + 1) * 128], rowc[:, kc * 128:(kc + 1) * 128], cx.ident[:]),
                 reads=["rowc", "consts"], writes=["ps%d" % (kc // 4)])
        for hb in range(2):
            S.op("dve", lambda e, hb=hb: e.tensor_copy(out=crep[:, hb * 4:(hb + 1) * 4, :].rearrange("p a b -> p (a b)"), in_=cx.ps[hb][:, :]),
                 reads=[], writes=["crep", "ps%d" % hb])
        for kc in range(8):
            sb = stg[kc % 2]
            S.dma("sp" if kc % 2 == 0 else "pool", sb[:], adaw_d[kc * 128:(kc + 1) * 128, c0:c0 + 3072], writes=["adst%d" % (kc % 2)])
            for n in range(6):
                S.op("pe", lambda e, kc=kc, n=n, sb=sb: e.matmul(cx.ps[n][:, :], lhsT=crep[:, kc, :], rhs=sb[:, n * 512:(n + 1) * 512], start=(kc == 0), stop=(kc == 7)),
                     reads=["crep", "adst%d" % (kc % 2)], writes=["ps%d" % n], sig=(kc == 7 or n == 5))
        for n in range(6):
            dst = (cx.SH, cx.A, cx.G)[n // 2]
            S.op("dve", lambda e, n=n, dst=dst: e.tensor_tensor(out=dst[:, (n % 2) * 512:(n % 2 + 1) * 512], in0=cx.ps[n][:, :], in1=adab[:, n * 512:(n + 1) * 512], op=ALU.add),
                 reads=["adab"], writes=["mod%d" % (n // 2), "ps%d" % n])
        S.op("dve", lambda e: e.scalar_tensor_tensor(out=cx.A[:], in0=cx.A[:], scalar=1.0, in1=nrm[:], op0=ALU.add, op1=ALU.mult),
             reads=["mod1", "nrmb"], writes=["mod1"])
        S.barrier()


def norm_tile(cx, t, x_d, hT, tok0, want_f32T=None):
    S, nc = cx.S, cx.nc
    i = cx.ncnt % 2
    cx.ncnt += 1
    xt, hh, jk = cx.xt[i], cx.hh[i], cx.jk
    ss = cx.nss[i]
    kx, kh, ks = "xt%d" % i, "hh%d" % i, "nss%d" % i
    S.dma("sp", xt[:], x_d[t * 128:(t + 1) * 128, :], writes=[kx])
    S.op("act", lambda e: e.activation(out=jk[:], in_=xt[:], func=AF.Square, accum_out=ss[:, 0:1]), reads=[kx], writes=["jk", ks])
    S.op("dve", lambda e: e.tensor_scalar(out=ss[:, 1:2], in0=ss[:, 0:1], scalar1=1.0 / D, scalar2=EPS, op0=ALU.mult, op1=ALU.add), reads=[ks], writes=[ks])
    S.op("act", lambda e: e.activation(out=ss[:, 2:3], in_=ss[:, 1:2], func=AF.Sqrt), reads=[ks], writes=[ks])
    S.op("dve", lambda e: e.reciprocal(out=ss[:, 3:4], in_=ss[:, 2:3]), reads=[ks], writes=[ks])
    S.op("dve", lambda e: e.scalar_tensor_tensor(out=hh[:], in0=xt[:], scalar=ss[:, 3:4], in1=cx.A[:], op0=ALU.mult, op1=ALU.mult), reads=[kx, ks, "mod1"], writes=[kh])
    S.op("pool", lambda e: e.tensor_tensor(out=hh[:], in0=hh[:], in1=cx.SH[:], op=ALU.add), reads=[kh, "mod0"], writes=[kh])
    for kc in range(8):
        b = 6 + kc // 4
        S.op("pe", lambda e, kc=kc, b=b: e.transpose(cx.ps[b][:, (kc % 4) * 128:(kc % 4 + 1) * 128], hh[:, kc * 128:(kc + 1) * 128], cx.ident[:]),
             reads=[kh, "consts"], writes=["ps%d" % b], sig=(kc % 4 == 3))
    for hb in range(2):
        eng = "act" if hb == 0 else "dve"
        outap = hT[:, hb * 4:(hb + 1) * 4, tok0:tok0 + 128]
        inap = cx.ps[6 + hb][:, :].rearrange("p (a b) -> p a b", a=4)
        if eng == "act":
            S.op("act", lambda e, o=outap, i_=inap: e.activation(out=o, in_=i_, func=AF.Copy), reads=[], writes=[("hT", tok0 // 128), "ps%d" % (6 + hb)])
        else:
            S.op("dve", lambda e, o=outap, i_=inap: e.tensor_copy(out=o, in_=i_), reads=[], writes=[("hT", tok0 // 128), "ps%d" % (6 + hb)])
        if want_f32T is not None:
            o2 = want_f32T[:, hb * 4:(hb + 1) * 4, :]
            S.op("pool" if False else "dve", lambda e, o=o2, i_=inap: e.tensor_copy(out=o, in_=i_), reads=[], writes=["hTf", "ps%d" % (6 + hb)])


def flash_head(cx, h, mode, QT, KT, V, finish_tile):
    S = cx.S
    ncomp = 2 if mode == "att" else 1
    for qb in range(8):
        for c in range(ncomp):
            pb = 0 if mode == "ml" else c * 64
            nk = 4 * qb + 4
            for kt in range(nk):
                j0 = max(0, kt - 4 * qb)
                c0 = j0 * 128
                sb = cx.stcnt % 2
                cx.stcnt += 1
                stp = cx.ps[sb]
                pt = cx.PT[sb]
                kst, kpt = "ps%d" % sb, "PT%d" % sb
                S.op("pe", lambda e, stp=stp, pb=pb, kt=kt, qb=qb, c0=c0: e.matmul(
                    stp[:, c0:512], lhsT=KT[pb:pb + 64, kt * 128:(kt + 1) * 128], rhs=QT[pb:pb + 64, qb * 512 + c0:qb * 512 + 512], start=True, stop=True),
                    reads=[("QK", h % 2)], writes=[kst])
                if mode == "att":
                    S.op("act", lambda e, stp=stp, pt=pt, c0=c0: e.activation(out=pt[:, c0:512], in_=stp[:, c0:512], func=AF.Exp, scale=0.125),
                         reads=[], writes=[kpt, kst])
                    if kt >= 4 * qb:
                        S.op("pool", lambda e, pt=pt, c0=c0: e.tensor_tensor(out=pt[:, c0:c0 + 128], in0=pt[:, c0:c0 + 128], in1=cx.mask01b[:], op=ALU.mult),
                             reads=[kpt, "consts2"], writes=[kpt])
                else:
                    rb = cx.ps[4 + sb]
                    krb, kdt = "ps%d" % (4 + sb), "DT%d" % sb
                    dt_ = cx.DT[sb]
                    S.op("pe", lambda e, rb=rb, qb=qb, c0=c0: e.matmul(rb[:, c0:512], lhsT=cx.sel3[0:24, h, :], rhs=cx.r3[0:24, qb * 512 + c0:qb * 512 + 512], start=True, stop=True),
                         reads=["r3", "sel3"], writes=[krb])
                    ucol = cx.ucol[:, kt * 8 + h:kt * 8 + h + 1]
                    if kt >= 4 * qb:
                        S.op("dve", lambda e, rb=rb, dt_=dt_, c0=c0: e.tensor_tensor(out=dt_[:, c0:c0 + 128], in0=rb[:, c0:c0 + 128], in1=cx.maskneg[:], op=ALU.add),
                             reads=["consts"], writes=[kdt, krb])
                        S.op("act", lambda e, dt_=dt_, c0=c0, ucol=ucol: e.activation(out=dt_[:, c0:c0 + 128], in_=dt_[:, c0:c0 + 128], func=AF.Exp, bias=ucol),
                             reads=[kdt, "ucol"], writes=[kdt])
                        if c0 + 128 < 512:
                            S.op("act", lambda e, rb=rb, dt_=dt_, c0=c0, ucol=ucol: e.activation(out=dt_[:, c0 + 128:512], in_=rb[:, c0 + 128:512], func=AF.Exp, bias=ucol),
                                 reads=["ucol"], writes=[kdt, krb])
                    else:
                        S.op("act", lambda e, rb=rb, dt_=dt_, ucol=ucol: e.activation(out=dt_[:, 0:512], in_=rb[:, 0:512], func=AF.Exp, bias=ucol),
                             reads=["ucol"], writes=[kdt, krb])
                    S.op("dve", lambda e, stp=stp, pt=pt, dt_=dt_, c0=c0: e.scalar_tensor_tensor(out=pt[:, c0:512], in0=stp[:, c0:512], scalar=0.125, in1=dt_[:, c0:512], op0=ALU.mult, op1=ALU.mult),
                         reads=[kdt], writes=[kpt, kst])
                for i in range(j0, 4):
                    ob = cx.ps[2 + i // 2]
                    oap = ob[:, (i % 2) * 256:(i % 2) * 256 + 129]
                    last = (kt == 4 * qb + i)
                    S.op("pe", lambda e, oap=oap, pt=pt, i=i, kt=kt, last=last: e.matmul(oap, lhsT=pt[:, i * 128:(i + 1) * 128], rhs=V[:, kt, 0:129], start=(kt == 0 and i % 2 == 0), stop=last, skip_group_check=True),
                         reads=[kpt, ("V", h % 2)], writes=["ps%d" % (2 + i // 2)], sig=(last or i == 3))
            for i in range(4):
                ob = cx.ps[2 + i // 2]
                oap = ob[:, (i % 2) * 256:(i % 2) * 256 + 129]
                finish_tile(4 * qb + i, c, oap, "ps%d" % (2 + i // 2))


def rstd_ops(cx, ssap, n, inv_n, key):
    S = cx.S
    S.op("dve", lambda e: e.tensor_scalar(out=ssap[:, n:2 * n], in0=ssap[:, 0:n], scalar1=inv_n, scalar2=EPS, op0=ALU.mult, op1=ALU.add), reads=[key], writes=[key])
    S.op("act", lambda e: e.activation(out=ssap[:, n:2 * n], in_=ssap[:, n:2 * n], func=AF.Sqrt), reads=[key], writes=[key])
    S.op("dve", lambda e: e.reciprocal(out=ssap[:, 2 * n:3 * n], in_=ssap[:, n:2 * n]), reads=[key], writes=[key])


def load_cast_w(cx, dst, stage, kstage, kdst, srcs, q="sp", cast_eng="pool"):
    S = cx.S
    for (ap, off, w) in srcs:
        S.dma(q, stage[:, :, off:off + w], ap.rearrange("(kc p) j -> p kc j", p=128), writes=[kstage])
    if cast_eng == "act":
        S.op("act", lambda e: e.activation(out=dst[:], in_=stage[:], func=AF.Copy), reads=[kstage], writes=[kdst])
    else:
        S.op(cast_eng, lambda e: e.tensor_copy(out=dst[:], in_=stage[:]), reads=[kstage], writes=[kdst])


def phase_attn(cx, x_d, xo_d, c_d, pos_d, adaw_d, adab_d, mixn_d, win_d, wout_d, qn_d, kn_d, lam_ds, subn_d, oT_d):
    S, nc = cx.S, cx.nc
    st = cx.stack
    lam_init = 0.2
    common_setup(cx, 0, c_d, adaw_d, adab_d, mixn_d)
    hT = T(cx, "hT", [128, 8, S_LEN], BF16)
    GQK = T(cx, "GQK", [128, 256])
    for j, d_ in enumerate((qn_d, qn_d, kn_d, kn_d)):
        S.dma("pool", GQK[:, j * 64:(j + 1) * 64], d_.partition_broadcast(128), writes=["GQK"])
    lamt = T(cx, "lamt", [128, 4, 64])
    for j, d_ in enumerate(lam_ds):
        S.dma("pool", lamt[:, j, :], d_.partition_broadcast(128), writes=["lamt"])
    lams = T(cx, "lams", [128, 8])
    S.op("dve", lambda e: e.tensor_tensor(out=lamt[:, 0, :], in0=lamt[:, 0, :], in1=lamt[:, 1, :], op=ALU.mult), reads=["lamt"], writes=["lamt"])
    S.op("dve", lambda e: e.tensor_tensor(out=lamt[:, 2, :], in0=lamt[:, 2, :], in1=lamt[:, 3, :], op=ALU.mult), reads=["lamt"], writes=["lamt"])
    S.op("dve", lambda e: e.reduce_sum(out=lams[:, 0:1], in_=lamt[:, 0, :], axis=AX.X), reads=["lamt"], writes=["lams"])
    S.op("dve", lambda e: e.reduce_sum(out=lams[:, 1:2], in_=lamt[:, 2, :], axis=AX.X), reads=["lamt"], writes=["lams"])
    S.op("act", lambda e: e.activation(out=lams[:, 2:4], in_=lams[:, 0:2], func=AF.Exp), reads=["lams"], writes=["lams"])
    S.op("dve", lambda e: e.tensor_tensor(out=lams[:, 4:5], in0=lams[:, 3:4], in1=lams[:, 2:3], op=ALU.subtract), reads=["lams"], writes=["lams"])
    S.op("dve", lambda e: e.tensor_scalar(out=lams[:, 5:6], in0=lams[:, 4:5], scalar1=-lam_init, scalar2=None, op0=ALU.add), reads=["lams"], writes=["lams"])
    neglam = lams[:, 5:6]
    subg = T(cx, "subg", [128, 2])
    S.dma("pool", subg[:, 0:1], subn_d.rearrange("o (p a) -> (o p) a", a=1), writes=["subg"])
    S.op("dve", lambda e: e.tensor_scalar(out=subg[:, 1:2], in0=subg[:, 0:1], scalar1=1.0 - lam_init, scalar2=None, op0=ALU.mult), reads=["subg"], writes=["subg"])
    cosT = T(cx, "cosT", [128, NT, 32])
    sinT = T(cx, "sinT", [128, NT, 32])
    with ExitStack() as st2:
        posi = st2.enter_context(nc.sbuf_tensor(uname("posi"), [32, 128], I32))
        posf = st2.enter_context(nc.sbuf_tensor(uname("posf"), [32, 128], F32))
        post = st2.enter_context(nc.sbuf_tensor(uname("post"), [128, 32], F32))
        ang = st2.enter_context(nc.sbuf_tensor(uname("ang"), [128, NT, 32], F32))
        tmpf = st2.enter_context(nc.sbuf_tensor(uname("tmpf"), [128, NT, 32], F32))
        tmpi = st2.enter_context(nc.sbuf_tensor(uname("tmpi"), [128, NT, 32], I32))
        S.dma("sp", posi[:], pos_d.rearrange("o (t p) -> (o t) p", p=128), writes=["posi"])
        S.op("dve", lambda e: e.tensor_copy(out=posf[:], in_=posi[:]), reads=["posi"], writes=["posf"])
        S.op("pe", lambda e: e.transpose(cx.ps[0][:, 0:32], posf[:], cx.ident[0:32, 0:32]), reads=["posf", "consts"], writes=["ps0"])
        S.op("dve", lambda e: e.tensor_copy(out=post[:], in_=cx.ps[0][:, 0:32]), reads=[], writes=["post", "ps0"])
        S.op("dve", lambda e: e.tensor_tensor(out=ang[:], in0=post[:].unsqueeze(2).to_broadcast([128, NT, 32]), in1=cx.invf[:].unsqueeze(1).to_broadcast([128, NT, 32]), op=ALU.mult),
             reads=["post", "consts"], writes=["ang"])
        for which, dstT in ((0, sinT), (1, cosT)):
            off = 0.0 if which == 0 else PI / 2
            S.op("dve", lambda e, off=off: e.tensor_scalar(out=tmpf[:], in0=ang[:], scalar1=off, scalar2=1.0 / (2 * PI), op0=ALU.add, op1=ALU.mult), reads=["ang"], writes=["tmpf"])
            S.op("dve", lambda e: e.tensor_copy(out=tmpi[:], in_=tmpf[:]), reads=["tmpf"], writes=["tmpi"])
            S.op("dve", lambda e: e.tensor_copy(out=tmpf[:], in_=tmpi[:]), reads=["tmpi"], writes=["tmpf"])
            S.op("dve", lambda e: e.scalar_tensor_tensor(out=tmpf[:], in0=tmpf[:], scalar=-2 * PI, in1=ang[:], op0=ALU.mult, op1=ALU.add), reads=["tmpf", "ang"], writes=["tmpf"])
            S.op("dve", lambda e, off=off: e.tensor_scalar(out=tmpf[:], in0=tmpf[:], scalar1=off, scalar2=PI, op0=ALU.add, op1=ALU.min), reads=["tmpf"], writes=["tmpf"])
            S.op("dve", lambda e: e.tensor_scalar(out=tmpf[:], in0=tmpf[:], scalar1=-PI, scalar2=None, op0=ALU.max), reads=["tmpf"], writes=["tmpf"])
            S.op("act", lambda e, dstT=dstT: e.activation(out=dstT[:], in_=tmpf[:], func=AF.Sin), reads=["tmpf"], writes=["rope"])
        S.barrier()
    with ExitStack() as st2:
        cx.xt = [st2.enter_context(nc.sbuf_tensor(uname("xt%d" % i), [128, 1024], F32)) for i in range(2)]
        cx.hh = [st2.enter_context(nc.sbuf_tensor(uname("hh%d" % i), [128, 1024], F32)) for i in range(2)]
        cx.jk = st2.enter_context(nc.sbuf_tensor(uname("jk"), [128, 1024], F32))
        cx.nss = [st2.enter_context(nc.sbuf_tensor(uname("nss%d" % i), [128, 4], F32)) for i in range(2)]
        cx.ncnt = 0
        for t in range(NT):
            norm_tile(cx, t, x_d, hT, t * 128)
        S.barrier()
    with ExitStack() as st2:
        def TT(name, shape, dt=F32):
            return st2.enter_context(nc.sbuf_tensor(uname(name), shape, dt))
        wst = TT("wst", [128, 8, 384])
        wb = [TT("wb%d" % i, [128, 8, 384], BF16) for i in range(2)]
        QKT = [TT("QKT%d" % i, [128, 2, S_LEN], BF16) for i in range(2)]
        V = [TT("V%d" % i, [128, NT, 132], BF16) for i in range(2)]
        cx.PT = [TT("PT%d" % i, [128, 512], BF16) for i in range(2)]
        cx.mask01b = TT("mask01b", [128, 128], BF16)
        sq = [TT("sq%d" % i, [128, 256]) for i in range(2)]
        qn = [TT("qn%d" % i, [128, 256]) for i in range(2)]
        qr = [TT("qr%d" % i, [128, 256]) for i in range(2)]
        t1 = [TT("t1_%d" % i, [128, 128]) for i in range(2)]
        t2 = [TT("t2_%d" % i, [128, 128]) for i in range(2)]
        pss = [TT("pss%d" % i, [128, 12]) for i in range(2)]
        o0 = TT("o0", [128, 4, 128])
        ot = [TT("ot%d" % i, [128, 128]) for i in range(2)]
        ojk = TT("ojk", [128, 128])
        osb = [TT("osb%d" % i, [128, 132]) for i in range(2)]
        oss = [TT("oss%d" % i, [128, 4]) for i in range(2)]
        oTb = [TT("oTb%d" % i, [128, S_LEN], BF16) for i in range(2)]
        S.op("dve", lambda e: e.tensor_copy(out=cx.mask01b[:], in_=cx.mask01[:]), reads=["consts"], writes=["consts2"])
        for i in range(2):
            S.op("pool", lambda e, i=i: e.memset(V[i][:, :, 128:129], 1.0), writes=[("V", i)])
        cx.stcnt = 0
        fcnt = [0]
        for h in range(8):
            hb = h % 2
            load_cast_w(cx, wb[hb], wst, "wst", "wb%d" % hb,
                        [(win_d[:, o_ * 1024 + h * 128:o_ * 1024 + (h + 1) * 128], o_ * 128, 128) for o_ in range(3)], q="pool", cast_eng="pool")
            for t in range(NT):
                p = t % 2
                P1 = cx.ps[4 + p]
                kp = "ps%d" % (4 + p)
                for kc in range(8):
                    S.op("pe", lambda e, P1=P1, kc=kc, t=t, hb=hb: e.matmul(P1[:, 0:384], lhsT=hT[:, kc, t * 128:(t + 1) * 128], rhs=wb[hb][:, kc, :], start=(kc == 0), stop=(kc == 7)),
                         reads=[("hT", t), "wb%d" % hb], writes=[kp], sig=(kc == 7))
                ksq, kqn, kqr, kt1, kt2, kss = "sq%d" % p, "qn%d" % p, "qr%d" % p, "t1_%d" % p, "t2_%d" % p, "pss%d" % p
                S.op("act", lambda e, P1=P1, p=p: e.activation(out=sq[p][:], in_=P1[:, 0:256], func=AF.Square), reads=[], writes=[ksq, kp])
                S.op("dve", lambda e, p=p: e.reduce_sum(out=pss[p][:, 0:4], in_=sq[p][:].rearrange("p (a b) -> p a b", a=4), axis=AX.X), reads=[ksq], writes=[kss])
                rstd_ops(cx, pss[p], 4, 1.0 / 64, kss)
                S.op("dve", lambda e, P1=P1, p=p: e.tensor_tensor(out=qn[p][:].rearrange("p (a b) -> p a b", a=4), in0=P1[:, 0:256].rearrange("p (a b) -> p a b", a=4),
                                                             in1=pss[p][:, 8:12].unsqueeze(2).to_broadcast([128, 4, 64]), op=ALU.mult), reads=[kss], writes=[kqn, kp])
                S.op("pool", lambda e, p=p: e.tensor_tensor(out=qn[p][:], in0=qn[p][:], in1=GQK[:], op=ALU.mult), reads=[kqn, "GQK"], writes=[kqn])
                q4 = qn[p][:].rearrange("p (a b) -> p a b", a=4)
                r4 = qr[p][:].rearrange("p (a b) -> p a b", a=4)
                cb = cosT[:, t, :].unsqueeze(1).to_broadcast([128, 4, 32])
                sb_ = sinT[:, t, :].unsqueeze(1).to_broadcast([128, 4, 32])
                a4 = t1[p][:].rearrange("p (a b) -> p a b", a=4)
                b4 = t2[p][:].rearrange("p (a b) -> p a b", a=4)
                S.op("dve", lambda e, a4=a4, q4=q4, cb=cb: e.tensor_tensor(out=a4, in0=q4[:, :, 0:32], in1=cb, op=ALU.mult), reads=[kqn, "rope"], writes=[kt1])
                S.op("pool", lambda e, b4=b4, q4=q4, sb_=sb_: e.tensor_tensor(out=b4, in0=q4[:, :, 32:64], in1=sb_, op=ALU.mult), reads=[kqn, "rope"], writes=[kt2])
                S.op("dve", lambda e, a4=a4, b4=b4, r4=r4: e.tensor_tensor(out=r4[:, :, 0:32], in0=a4, in1=b4, op=ALU.subtract), reads=[kt1, kt2], writes=[kqr])
                S.op("pool", lambda e, a4=a4, q4=q4, cb=cb: e.tensor_tensor(out=a4, in0=q4[:, :, 32:64], in1=cb, op=ALU.mult), reads=[kqn, "rope", kqr], writes=[kt1])
                S.op("dve", lambda e, b4=b4, q4=q4, sb_=sb_: e.tensor_tensor(out=b4, in0=q4[:, :, 0:32], in1=sb_, op=ALU.mult), reads=[kqn, "rope", kqr], writes=[kt2])
                S.op("pool", lambda e, a4=a4, b4=b4, r4=r4: e.tensor_tensor(out=r4[:, :, 32:64], in0=a4, in1=b4, op=ALU.add), reads=[kt1, kt2], writes=[kqr])
                for j in range(2):
                    S.op("pe", lambda e, j=j, p=p: e.transpose(cx.ps[6][:, j * 128:(j + 1) * 128], qr[p][:, j * 128:(j + 1) * 128], cx.ident[:]), reads=[kqr, "consts"], writes=["ps6"], sig=(j == 1))
                S.op("act", lambda e, t=t, hb=hb: e.activation(out=QKT[hb][:, :, t * 128:(t + 1) * 128], in_=cx.ps[6][:, 0:256].rearrange("p (a b) -> p a b", a=2), func=AF.Copy),
                     reads=[], writes=[("QK", hb), "ps6"])
                S.op("dve", lambda e, P1=P1, t=t, hb=hb: e.tensor_copy(out=V[hb][:, t, 0:128], in_=P1[:, 256:384]), reads=[], writes=[("V", hb), kp])

            def finish(Tq, c, oap_ps, okey, h=h, hb=hb):
                i = Tq % 4
                f = fcnt[0] % 2
                fcnt[0] += 1
                kos, kot, kob = "oss%d" % f, "ot%d" % f, "osb%d" % f
                oap = osb[f]
                if f == 0:
                    S.op("act", lambda e: e.activation(out=oap[:, 0:129], in_=oap_ps, func=AF.Copy), reads=[], writes=[kob, okey])
                else:
                    S.op("dve", lambda e: e.tensor_copy(out=oap[:, 0:129], in_=oap_ps), reads=[], writes=[kob, okey])
                S.op("dve", lambda e: e.reciprocal(out=oss[f][:, 0:1], in_=oap[:, 128:129]), reads=[kob], writes=[kos])
                if c == 0:
                    S.op("act", lambda e: e.activation(out=o0[:, i, :], in_=oap[:, 0:128], func=AF.Copy, scale=oss[f][:, 0:1]), reads=[kob, kos], writes=[("o0", i)])
                    return
                S.op("act", lambda e: e.activation(out=ot[f][:], in_=oap[:, 0:128], func=AF.Copy, scale=oss[f][:, 0:1]), reads=[kob, kos], writes=[kot])
                S.op("dve", lambda e: e.scalar_tensor_tensor(out=ot[f][:], in0=ot[f][:], scalar=neglam, in1=o0[:, i, :], op0=ALU.mult, op1=ALU.add), reads=[kot, ("o0", i), "lams"], writes=[kot])
                S.op("act", lambda e: e.activation(out=ojk[:], in_=ot[f][:], func=AF.Square, accum_out=oss[f][:, 1:2]), reads=[kot], writes=["ojk", kos])
                S.op("dve", lambda e: e.tensor_scalar(out=oss[f][:, 2:3], in0=oss[f][:, 1:2], scalar1=1.0 / 128, scalar2=EPS, op0=ALU.mult, op1=ALU.add), reads=[kos], writes=[kos])
                S.op("act", lambda e: e.activation(out=oss[f][:, 2:3], in_=oss[f][:, 2:3], func=AF.Sqrt), reads=[kos], writes=[kos])
                S.op("dve", lambda e: e.reciprocal(out=oss[f][:, 3:4], in_=oss[f][:, 2:3]), reads=[kos], writes=[kos])
                S.op("dve", lambda e: e.tensor_scalar(out=ot[f][:], in0=ot[f][:], scalar1=oss[f][:, 3:4], scalar2=None, op0=ALU.mult), reads=[kot, kos], writes=[kot])
                S.op("pe", lambda e: e.transpose(cx.ps[7][:, 0:128], ot[f][:], cx.ident[:]), reads=[kot, "consts"], writes=["ps7"])
                S.op("act", lambda e: e.activation(out=oTb[hb][:, Tq * 128:(Tq + 1) * 128], in_=cx.ps[7][:, 0:128], func=AF.Copy, scale=subg[:, 1:2]), reads=["subg"], writes=[("oTb", hb), "ps7"])

            flash_head(cx, h, "att", QKT[hb][:, 0, :], QKT[hb][:, 1, :], V[hb], finish)
            S.dma("sp", oT_d[h], oTb[hb][:], reads=[("oTb", hb)], writes=["oT_d"])
        S.barrier()
    out_proj(cx, x_d, xo_d, wout_d, oT_d)


def out_proj(cx, x_d, xo_d, wout_d, oT_d):
    S, nc = cx.S, cx.nc
    with ExitStack() as st2:
        def TT(name, shape, dt=F32):
            return st2.enter_context(nc.sbuf_tensor(uname(name), shape, dt))
        wst = TT("wost", [128, 8, 1024])
        wo = TT("wo", [128, 8, 1024], BF16)
        oTt = [TT("oTt%d" % i, [128, 8, 128], BF16) for i in range(2)]
        xt = [TT("xo%d" % i, [128, 1024]) for i in range(2)]
        yt = [TT("yo%d" % i, [128, 1024]) for i in range(2)]
        load_cast_w(cx, wo, wst, "wost", "wo", [(wout_d, 0, 1024)], q="pool", cast_eng="pool")
        for t in range(NT):
            p = t % 2
            S.dma("sp", oTt[p][:], oT_d[:, :, t * 128:(t + 1) * 128].rearrange("h p j -> p h j"), reads=["oT_d"], writes=["oTt%d" % p])
            S.dma("sp", xt[p][:], x_d[t * 128:(t + 1) * 128, :], writes=["xo%d" % p])
            for n in range(2):
                pb = cx.ps[2 * p + n]
                kp = "ps%d" % (2 * p + n)
                for h in range(8):
                    S.op("pe", lambda e, pb=pb, h=h, p=p, n=n: e.matmul(pb[:, :], lhsT=oTt[p][:, h, :], rhs=wo[:, h, n * 512:(n + 1) * 512], start=(h == 0), stop=(h == 7)),
                         reads=["oTt%d" % p, "wo"], writes=[kp], sig=(h == 7))
                S.op("dve", lambda e, pb=pb, p=p, n=n: e.tensor_tensor(out=yt[p][:, n * 512:(n + 1) * 512], in0=pb[:, :], in1=cx.G[:, n * 512:(n + 1) * 512], op=ALU.mult),
                     reads=["mod2"], writes=["yo%d" % p, kp])
            S.op("pool", lambda e, p=p: e.tensor_tensor(out=yt[p][:], in0=yt[p][:], in1=xt[p][:], op=ALU.add), reads=["yo%d" % p, "xo%d" % p], writes=["yo%d" % p])
            S.dma("sp", xo_d[t * 128:(t + 1) * 128, :], yt[p][:], reads=["yo%d" % p], writes=["xo_d"])
        S.barrier()


def new_ctx(nc, stack):
    cx = Ctx()
    cx.nc = nc
    cx.stack = stack
    cx.S = Sched(nc, stack)
    cx.ps = [stack.enter_context(nc.psum_tensor("psb%d" % i, [128, 512], F32)) for i in range(8)]
    cx.consts = T(cx, "consts", [128, 1024])
    cx.ident = cx.consts[:, 0:128]
    cx.mask01 = cx.consts[:, 128:256]
    cx.maskneg = cx.consts[:, 256:384]
    cx.invf = cx.consts[:, 384:416]
    cx.SH = T(cx, "SH", [128, 1024])
    cx.A = T(cx, "A", [128, 1024])
    cx.G = T(cx, "G", [128, 1024])
    return cx


def dram_in(nc, name, shape, dt=F32):
    return nc.dram_tensor(name, list(shape), dt, kind="ExternalInput").ap()


def build_attn_prog():
    nc = bass.Bass("TRN2", target_bir_lowering=False)
    x_d = dram_in(nc, "x", [S_LEN, D])
    c_d = dram_in(nc, "c", [1, D])
    pos_d = dram_in(nc, "pos", [1, S_LEN], I32)
    adaw_d = dram_in(nc, "adaw", [D, 6 * D])
    adab_d = dram_in(nc, "adab", [1, 6 * D])
    mixn_d = dram_in(nc, "mixn", [1, D])
    win_d = dram_in(nc, "win", [D, 3 * D])
    wout_d = dram_in(nc, "wout", [D, D])
    qn_d = dram_in(nc, "qn", [1, 64])
    kn_d = dram_in(nc, "kn", [1, 64])
    lam_ds = [dram_in(nc, "lam%d" % i, [1, 64]) for i in range(4)]
    subn_d = dram_in(nc, "subn", [1, 128])
    consts_d = dram_in(nc, "consts", [128, 1024])
    xo_d = nc.dram_tensor("xo", [S_LEN, D], F32, kind="ExternalOutput").ap()
    oT_d = nc.dram_tensor("oT_d", [8, 128, S_LEN], BF16, kind="Internal").ap()
    with ExitStack() as stack:
        cx = new_ctx(nc, stack)
        block = stack.enter_context(nc.Block())
        cx.S.dma("sp", cx.consts[:], consts_d, writes=["consts"])
        phase_attn(cx, x_d, xo_d, c_d, pos_d, adaw_d, adab_d, mixn_d, win_d, wout_d, qn_d, kn_d, lam_ds, subn_d, oT_d)
        print("instructions", cx.S.nins, "waits", cx.S.nwait)
        cx.S.emit(block)
    return nc


def phase_moe(cx, x_d, xo_d, c_d, adaw_d, adab_d, ffnn_d, wr_d, br_d, wgu_d, bgu_d, wd_d, bd_d, n_exp=32):
    S, nc = cx.S, cx.nc
    common_setup(cx, 1, c_d, adaw_d, adab_d, ffnn_d)
    QT_ = 1024
    NQ = S_LEN // QT_
    with ExitStack() as st2:
        def TT(name, shape, dt=F32):
            return st2.enter_context(nc.sbuf_tensor(uname(name), shape, dt))
        biasT = TT("biasT", [128, 16, 32])
        with ExitStack() as st3:
            bgr = st3.enter_context(nc.sbuf_tensor(uname("bgr"), [32, 2048], F32))
            S.dma("pool", bgr[:], bgu_d, writes=["bgr"])
            bgr3 = bgr[:].rearrange("e (f two) -> e f two", two=2)
            for j in range(16):
                src = bgr3[:, (j % 8) * 128:(j % 8 + 1) * 128, j // 8]
                S.op("pe", lambda e, j=j, src=src: e.transpose(cx.ps[j // 8][:, (j % 8) * 32:(j % 8) * 32 + 32], src, cx.ident[0:32, 0:32]), reads=["bgr", "consts"], writes=["ps%d" % (j // 8)])
            for g in range(2):
                S.op("dve", lambda e, g=g: e.tensor_copy(out=biasT[:, g * 8:(g + 1) * 8, :], in_=cx.ps[g][:, 0:256].rearrange("p (a b) -> p a b", a=8)), reads=[], writes=["biasT", "ps%d" % g])
            S.barrier()
        hT = TT("hTm", [128, 8, QT_], BF16)
        hTf = TT("hTf", [128, 8, 128])
        acc = TT("acc", [128, 8, 1024])
        wr = TT("wr", [128, 8, 32])
        brb = TT("brb", [128, 32])
        bd = TT("bd", [32, 1024])
        gates = TT("gates", [128, 8, 32])
        gT = TT("gT", [32, 128])
        rt = TT("rt", [128, 96])
        r8 = TT("r8", [128, 16])
        wgu = [TT("wgu%d" % i, [128, 8, 1024], BF16) for i in range(2)]
        wdb = [TT("wdb%d" % i, [128, 4, 1024], BF16) for i in range(2)]
        NSTG = 4
        stg = [TT("stg%d" % i, [128, 1024]) for i in range(NSTG)]
        actT = TT("actT", [128, 4, 512], BF16)
        gt = [TT("gt%d" % i, [128, 512]) for i in range(2)]
        sg = [TT("sg%d" % i, [128, 512]) for i in range(2)]
        lt = [TT("lt%d" % i, [128, 512]) for i in range(2)]
        cx.xt = [TT("xt%d" % i, [128, 1024]) for i in range(2)]
        cx.hh = [TT("hh%d" % i, [128, 1024]) for i in range(2)]
        cx.jk = TT("jk", [128, 1024])
        cx.nss = [TT("nss%d" % i, [128, 4]) for i in range(2)]
        cx.ncnt = 0
        S.dma("pool", wr[:], wr_d.rearrange("(kc p) e -> p kc e", p=128), writes=["wr"])
        S.dma("pool", brb[:], br_d.partition_broadcast(128), writes=["brb"])
        S.dma("pool", bd[:], bd_d, writes=["bd"])
        NU = n_exp * 2

        def load_dma(seq, s_):
            u = seq % NU
            ex_, hf = u // 2, u % 2
            sb = (seq * 12 + s_) % NSTG
            ks = "stg%d" % sb
            if s_ < 8:
                S.dma("sp", stg[sb][:], wgu_d[ex_, s_ * 128:(s_ + 1) * 128, hf * 1024:(hf + 1) * 1024], writes=[ks])
            else:
                fc = s_ - 8
                S.dma("sp", stg[sb][:], wd_d[ex_, hf * 512 + fc * 128:hf * 512 + (fc + 1) * 128, :], writes=[ks])

        def load_cast(seq, s_):
            wb2 = seq % 2
            sb = (seq * 12 + s_) % NSTG
            ks = "stg%d" % sb
            if s_ < 8:
                s3 = stg[sb][:].rearrange("p (f two) -> p f two", two=2)
                S.op("pool", lambda e: e.tensor_copy(out=wgu[wb2][:, s_, 0:512], in_=s3[:, :, 0]), reads=[ks], writes=["wgu%d" % wb2])
                S.op("pool", lambda e: e.tensor_copy(out=wgu[wb2][:, s_, 512:1024], in_=s3[:, :, 1]), reads=[ks], writes=["wgu%d" % wb2])
            else:
                fc = s_ - 8
                S.op("pool", lambda e: e.tensor_copy(out=wdb[wb2][:, fc, :], in_=stg[sb][:]), reads=[ks], writes=["wdb%d" % wb2])

        for qtr in range(NQ):
            for tl in range(8):
                t = qtr * 8 + tl
                norm_tile(cx, t, x_d, hT, tl * 128, want_f32T=hTf)
                for kc in range(8):
                    S.op("pe", lambda e, kc=kc: e.matmul(cx.ps[5][:, 0:32], lhsT=hTf[:, kc, :], rhs=wr[:, kc, :], start=(kc == 0), stop=(kc == 7)), reads=["hTf", "wr"], writes=["ps5"], sig=(kc == 7))
                S.op("dve", lambda e: e.tensor_tensor(out=rt[:, 0:32], in0=cx.ps[5][:, 0:32], in1=brb[:], op=ALU.add), reads=["brb"], writes=["rt", "ps5"])
                S.op("dve", lambda e: e.max(out=r8[:, 0:8], in_=rt[:, 0:32]), reads=["rt"], writes=["r8"])
                S.op("dve", lambda e: e.tensor_scalar(out=rt[:, 32:64], in0=rt[:, 0:32], scalar1=r8[:, 3:4], scalar2=None, op0=ALU.is_ge), reads=["rt", "r8"], writes=["rt"])
                S.op("dve", lambda e: e.tensor_scalar(out=r8[:, 8:9], in0=r8[:, 0:1], scalar1=-1.0, scalar2=None, op0=ALU.mult), reads=["r8"], writes=["r8"])
                S.op("act", lambda e: e.activation(out=rt[:, 64:96], in_=rt[:, 0:32], func=AF.Exp, bias=r8[:, 8:9]), reads=["rt", "r8"], writes=["rt"])
                S.op("dve", lambda e: e.tensor_tensor(out=rt[:, 64:96], in0=rt[:, 64:96], in1=rt[:, 32:64], op=ALU.mult), reads=["rt"], writes=["rt"])
                S.op("dve", lambda e: e.reduce_sum(out=r8[:, 9:10], in_=rt[:, 64:96], axis=AX.X), reads=["rt"], writes=["r8"])
                S.op("dve", lambda e: e.reciprocal(out=r8[:, 10:11], in_=r8[:, 9:10]), reads=["r8"], writes=["r8"])
                S.op("dve", lambda e, tl=tl: e.tensor_scalar(out=gates[:, tl, :], in0=rt[:, 64:96], scalar1=r8[:, 10:11], scalar2=None, op0=ALU.mult), reads=["rt", "r8"], writes=["gates"])
                S.op("pe", lambda e, tl=tl: e.transpose(cx.ps[5][0:32, 128:256], gates[:, tl, :], cx.ident[:]), reads=["gates", "consts"], writes=["ps5"])
                S.op("dve", lambda e: e.tensor_copy(out=gT[:], in_=cx.ps[5][0:32, 128:256]), reads=[], writes=["gT", "ps5"])
                for n in range(2):
                    S.op("pe", lambda e, n=n: e.matmul(cx.ps[4][:, :], lhsT=gT[:], rhs=bd[:, n * 512:(n + 1) * 512], start=True, stop=True), reads=["gT", "bd"], writes=["ps4"])
                    S.op("act", lambda e, tl=tl, n=n: e.activation(out=acc[:, tl, n * 512:(n + 1) * 512], in_=cx.ps[4][:, :], func=AF.Copy), reads=[], writes=[("acc", tl), "ps4"])
            for u in range(NU):
                ex, hf = u // 2, u % 2
                seq = qtr * NU + u
                wb_ = seq % 2
                if seq == 0:
                    for s_ in range(12):
                        load_dma(0, s_)
                        load_cast(0, s_)
                nxt = seq + 1 if seq + 1 < NQ * NU else None
                slot = [0]

                def tick(nxt=nxt, slot=slot):
                    s_ = slot[0]
                    slot[0] += 1
                    if nxt is None:
                        return
                    if s_ < 12:
                        load_dma(nxt, s_)
                    if 2 <= s_ < 14:
                        load_cast(nxt, s_ - 2)
                for tb in range(QT_ // 512):
                    for j in range(4):
                        p = j % 2
                        jj = hf * 4 + j
                        psg, psl = cx.ps[2 * p], cx.ps[2 * p + 1]
                        kg, kl = "ps%d" % (2 * p), "ps%d" % (2 * p + 1)
                        for (pp, kk, off) in ((psg, kg, 0), (psl, kl, 512)):
                            for kc in range(8):
                                S.op("pe", lambda e, pp=pp, kc=kc, off=off, j=j, tb=tb, wb_=wb_: e.matmul(pp[:, :], lhsT=wgu[wb_][:, kc, off + j * 128:off + (j + 1) * 128], rhs=hT[:, kc, tb * 512:(tb + 1) * 512], start=(kc == 0), stop=(kc == 7)),
                                     reads=["wgu%d" % wb_] + [("hT", tb * 4 + q_) for q_ in range(4)], writes=[kk], sig=(kc == 7))
                        S.op("dve", lambda e, p=p, psg=psg, jj=jj, ex=ex: e.tensor_scalar(out=gt[p][:], in0=psg[:, :], scalar1=biasT[:, jj, ex:ex + 1], scalar2=7.0, op0=ALU.add, op1=ALU.min), reads=["biasT"], writes=["gt%d" % p, kg])
                        S.op("act", lambda e, p=p: e.activation(out=sg[p][:], in_=gt[p][:], func=AF.Sigmoid, scale=1.702), reads=["gt%d" % p], writes=["sg%d" % p])
                        S.op("dve", lambda e, p=p, psl=psl, jj=jj, ex=ex: e.tensor_scalar(out=lt[p][:], in0=psl[:, :], scalar1=biasT[:, 8 + jj, ex:ex + 1], scalar2=7.0, op0=ALU.add, op1=ALU.min), reads=["biasT"], writes=["lt%d" % p, kl])
                        S.op("pool", lambda e, p=p: e.tensor_scalar(out=lt[p][:], in0=lt[p][:], scalar1=-7.0, scalar2=1.0, op0=ALU.max, op1=ALU.add), reads=["lt%d" % p], writes=["lt%d" % p])
                        S.op("pool", lambda e, p=p: e.tensor_tensor(out=gt[p][:], in0=gt[p][:], in1=sg[p][:], op=ALU.mult), reads=["gt%d" % p, "sg%d" % p], writes=["gt%d" % p])
                        S.op("dve", lambda e, p=p, j=j: e.tensor_tensor(out=actT[:, j, :], in0=gt[p][:], in1=lt[p][:], op=ALU.mult), reads=["gt%d" % p, "lt%d" % p], writes=[("actT", j)])
                        tick()
                    for tt in range(4):
                        tl = tb * 4 + tt
                        for n in range(2):
                            pb = cx.ps[4 + n]
                            kp = "ps%d" % (4 + n)
                            for fc in range(4):
                                S.op("pe", lambda e, pb=pb, fc=fc, tt=tt, n=n, wb_=wb_: e.matmul(pb[:, :], lhsT=actT[:, fc, tt * 128:(tt + 1) * 128], rhs=wdb[wb_][:, fc, n * 512:(n + 1) * 512], start=(fc == 0), stop=(fc == 3)),
                                     reads=[("actT", fc), "wdb%d" % wb_], writes=[kp], sig=(fc == 3))
                            S.op("dve", lambda e, pb=pb, tl=tl, n=n, ex=ex: e.scalar_tensor_tensor(out=acc[:, tl, n * 512:(n + 1) * 512], in0=pb[:, :], scalar=gates[:, tl, ex:ex + 1], in1=acc[:, tl, n * 512:(n + 1) * 512], op0=ALU.mult, op1=ALU.add),
                                 reads=["gates"], writes=[("acc", tl), kp])
                        tick()
            for tl in range(8):
                t = qtr * 8 + tl
                p = tl % 2
                S.dma("sp", cx.xt[p][:], x_d[t * 128:(t + 1) * 128, :], writes=["xt%d" % p])
                S.op("dve", lambda e, tl=tl: e.tensor_tensor(out=acc[:, tl, :], in0=acc[:, tl, :], in1=cx.G[:], op=ALU.mult), reads=["mod2"], writes=[("acc", tl)])
                S.op("pool", lambda e, tl=tl, p=p: e.tensor_tensor(out=acc[:, tl, :], in0=acc[:, tl, :], in1=cx.xt[p][:], op=ALU.add), reads=["xt%d" % p], writes=[("acc", tl)])
                S.dma("sp", xo_d[t * 128:(t + 1) * 128, :], acc[:, tl, :], reads=[("acc", tl)], writes=["xo_d"])
        S.barrier()


def build_moe_prog(n_exp=32):
    nc = bass.Bass("TRN2", target_bir_lowering=False)
    x_d = dram_in(nc, "x", [S_LEN, D])
    c_d = dram_in(nc, "c", [1, D])
    adaw_d = dram_in(nc, "adaw", [D, 6 * D])
    adab_d = dram_in(nc, "adab", [1, 6 * D])
    ffnn_d = dram_in(nc, "ffnn", [1, D])
    wr_d = dram_in(nc, "wr", [D, 32])
    br_d = dram_in(nc, "br", [1, 32])
    wgu_d = dram_in(nc, "wgu", [32, D, 2 * D])
    bgu_d = dram_in(nc, "bgu", [32, 2 * D])
    wd_d = dram_in(nc, "wd", [32, D, D])
    bd_d = dram_in(nc, "bd", [32, D])
    consts_d = dram_in(nc, "consts", [128, 1024])
    xo_d = nc.dram_tensor("xo", [S_LEN, D], F32, kind="ExternalOutput").ap()
    with ExitStack() as stack:
        cx = new_ctx(nc, stack)
        block = stack.enter_context(nc.Block())
        cx.S.dma("sp", cx.consts[:], consts_d, writes=["consts"])
        phase_moe(cx, x_d, xo_d, c_d, adaw_d, adab_d, ffnn_d, wr_d, br_d, wgu_d, bgu_d, wd_d, bd_d, n_exp=n_exp)
        print("instructions", cx.S.nins, "waits", cx.S.nwait)
        cx.S.emit(block)
    return nc


def phase_mlstm(cx, x_d, xo_d, c_d, adaw_d, adab_d, mixn_d, win_d, bi_d, bf_d, outn_d, wout_d, oT_d, sel_d):
    S, nc = cx.S, cx.nc
    common_setup(cx, 0, c_d, adaw_d, adab_d, mixn_d)
    hT = T(cx, "hT", [128, 8, S_LEN], BF16)
    OG = T(cx, "OG", [128, 1024])
    S.dma("pool", OG[:], outn_d.partition_broadcast(128), writes=["OG"])
    cx.sel3 = T(cx, "sel3", [128, 8, 128], BF16)
    cx.r3 = T(cx, "r3", [32, S_LEN], BF16)
    cx.ucol = T(cx, "ucol", [128, 256])
    emcol = T(cx, "emcol", [128, 256])
    with ExitStack() as st2:
        cx.xt = [st2.enter_context(nc.sbuf_tensor(uname("xt%d" % i), [128, 1024], F32)) for i in range(2)]
        cx.hh = [st2.enter_context(nc.sbuf_tensor(uname("hh%d" % i), [128, 1024], F32)) for i in range(2)]
        cx.jk = st2.enter_context(nc.sbuf_tensor(uname("jk"), [128, 1024], F32))
        cx.nss = [st2.enter_context(nc.sbuf_tensor(uname("nss%d" % i), [128, 4], F32)) for i in range(2)]
        cx.ncnt = 0
        S.dma("pool", cx.jk[:], sel_d, writes=["jk"])
        S.op("dve", lambda e: e.tensor_copy(out=cx.sel3[:].rearrange("p a b -> p (a b)"), in_=cx.jk[:]), reads=["jk"], writes=["sel3"])
        for t in range(NT):
            norm_tile(cx, t, x_d, hT, t * 128)
        S.barrier()
    with ExitStack() as st2:
        def TT(name, shape, dt=F32):
            return st2.enter_context(nc.sbuf_tensor(uname(name), shape, dt))
        wif = TT("wif", [128, 8, 16])
        wifb = TT("wifb", [128, 8, 16], BF16)
        bcol = TT("bcol", [8, 4])
        gi = TT("gi", [8, S_LEN])
        gf = TT("gf", [8, S_LEN])
        Ft = TT("Ft", [8, S_LEN])
        ones = TT("ones", [8, S_LEN])
        cm = TT("cm", [8, S_LEN])
        rb = [TT("rb%d" % i, [8, S_LEN], BF16) for i in range(3)]
        S.dma("sp", wif[:], win_d[:, 3072:3088].rearrange("(kc p) j -> p kc j", p=128), writes=["wif"])
        S.op("dve", lambda e: e.tensor_copy(out=wifb[:], in_=wif[:]), reads=["wif"], writes=["wifb"])
        S.dma("sp", bcol[:, 0:1], bi_d.rearrange("o (p a) -> (o p) a", a=1), writes=["bcol"])
        S.dma("sp", bcol[:, 1:2], bf_d.rearrange("o (p a) -> (o p) a", a=1), writes=["bcol"])
        S.op("dve", lambda e: e.tensor_scalar(out=bcol[:, 2:4], in0=bcol[:, 0:2], scalar1=1.0 / 15, scalar2=None, op0=ALU.mult), reads=["bcol"], writes=["bcol"])
        S.op("pool", lambda e: e.memset(ones[:], 1.0), writes=["ones"])
        for blk in range(8):
            for g in range(2):
                for kc in range(8):
                    S.op("pe", lambda e, g=g, kc=kc, blk=blk: e.matmul(cx.ps[g][0:8, :], lhsT=wifb[:, kc, g * 8:(g + 1) * 8], rhs=hT[:, kc, blk * 512:(blk + 1) * 512], start=(kc == 0), stop=(kc == 7)),
                         reads=["wifb"] + [("hT", blk * 4 + q_) for q_ in range(4)], writes=["ps%d" % g], sig=(kc == 7))
                dst = gi if g == 0 else gf
                S.op("act", lambda e, g=g, blk=blk, dst=dst: e.activation(out=dst[:, blk * 512:(blk + 1) * 512], in_=cx.ps[g][0:8, :], func=AF.Tanh, scale=1.0 / 15, bias=bcol[:, 2 + g:3 + g]),
                     reads=["bcol"], writes=["g%d" % g, "ps%d" % g])
        S.op("dve", lambda e: e.tensor_scalar(out=gi[:], in0=gi[:], scalar1=15.0, scalar2=None, op0=ALU.mult), reads=["g0"], writes=["g0"])
        S.op("act", lambda e: e.activation(out=gf[:], in_=gf[:], func=AF.Exp, scale=-15.0), reads=["g1"], writes=["g1"])
        S.op("act", lambda e: e.activation(out=gf[:], in_=gf[:], func=AF.Ln, bias=1.0), reads=["g1"], writes=["g1"])
        S.op("dve", lambda e: e.tensor_tensor_scan(out=Ft[:], data0=ones[:], data1=gf[:], initial=0.0, op0=ALU.mult, op1=ALU.subtract), reads=["ones", "g1"], writes=["Ft"])
        S.op("dve", lambda e: e.tensor_tensor(out=gi[:], in0=gi[:], in1=Ft[:], op=ALU.subtract), reads=["g0", "Ft"], writes=["g0"])
        S.op("dve", lambda e: e.tensor_tensor_scan(out=cm[:], data0=ones[:], data1=gi[:], initial=0.0, op0=ALU.mult, op1=ALU.max), reads=["ones", "g0"], writes=["cm"])
        S.op("dve", lambda e: e.tensor_tensor(out=Ft[:], in0=Ft[:], in1=cm[:], op=ALU.add), reads=["Ft", "cm"], writes=["Ft"])
        S.op("act", lambda e: e.activation(out=Ft[:], in_=Ft[:], func=AF.Exp, scale=-1.0), reads=["Ft"], writes=["Ft"])
        S.op("dve", lambda e: e.tensor_scalar(out=cm[:], in0=cm[:], scalar1=-1.0, scalar2=None, op0=ALU.mult), reads=["cm"], writes=["cm"])
        for j in range(3):
            S.op("dve", lambda e, j=j: e.tensor_copy(out=rb[j][:], in_=cm[:]), reads=["cm"], writes=["rb%d" % j])
            if j < 2:
                S.op("dve", lambda e, j=j: e.tensor_tensor(out=cm[:], in0=cm[:], in1=rb[j][:], op=ALU.subtract), reads=["cm", "rb%d" % j], writes=["cm"])
            S.dma("sp", cx.r3[8 * j:8 * j + 8, :], rb[j][:], reads=["rb%d" % j], writes=["r3"])
        for which, src, dst, key in ((0, gi, cx.ucol, "ucol"), (1, Ft, emcol, "emcol")):
            for t in range(NT):
                S.op("pe", lambda e, which=which, src=src, t=t: e.transpose(cx.ps[2 + which][:, t * 8:(t + 1) * 8], src[:, t * 128:(t + 1) * 128], cx.ident[0:8, 0:8]),
                     reads=["g0" if which == 0 else "Ft", "consts"], writes=["ps%d" % (2 + which)])
            S.op("dve", lambda e, which=which, dst=dst: e.tensor_copy(out=dst[:], in_=cx.ps[2 + which][:, 0:256]), reads=[], writes=[key, "ps%d" % (2 + which)])
        S.barrier()
    with ExitStack() as st2:
        def TT(name, shape, dt=F32):
            return st2.enter_context(nc.sbuf_tensor(uname(name), shape, dt))
        wst = TT("wst", [128, 8, 384])
        wb = [TT("wb%d" % i, [128, 8, 384], BF16) for i in range(2)]
        QKT = [TT("QKT%d" % i, [128, 2, S_LEN], BF16) for i in range(2)]
        V = [TT("V%d" % i, [128, NT, 132], BF16) for i in range(2)]
        og = [TT("og0", [128, NT, 128], BF16)] * 2
        cx.PT = [TT("PT%d" % i, [128, 512], BF16) for i in range(2)]
        cx.DT = [TT("DT%d" % i, [128, 512]) for i in range(2)]
        qk = [TT("qk%d" % i, [128, 128]) for i in range(2)]
        ot = [TT("ot%d" % i, [128, 128]) for i in range(2)]
        ojk = TT("ojk", [128, 128])
        osb = [TT("osb%d" % i, [128, 132]) for i in range(2)]
        oss = [TT("oss%d" % i, [128, 8]) for i in range(2)]
        oTb = [TT("oTb0", [128, S_LEN], BF16)] * 2
        for i in range(2):
            S.op("pool", lambda e, i=i: e.memset(V[i][:, :, 128:129], 1.0), writes=[("V", i)])
        cx.stcnt = 0
        fcnt = [0]
        for h in range(8):
            hb = h % 2
            load_cast_w(cx, wb[hb], wst, "wst", "wb%d" % hb,
                        [(win_d[:, h * 64:(h + 1) * 64], 0, 64), (win_d[:, 512 + h * 64:512 + (h + 1) * 64], 64, 64),
                         (win_d[:, 1024 + h * 128:1024 + (h + 1) * 128], 128, 128), (win_d[:, 2048 + h * 128:2048 + (h + 1) * 128], 256, 128)], q="pool", cast_eng="pool")
            for t in range(NT):
                p = t % 2
                P1 = cx.ps[4 + p]
                kp = "ps%d" % (4 + p)
                for kc in range(8):
                    S.op("pe", lambda e, P1=P1, kc=kc, t=t, hb=hb: e.matmul(P1[:, 0:384], lhsT=hT[:, kc, t * 128:(t + 1) * 128], rhs=wb[hb][:, kc, :], start=(kc == 0), stop=(kc == 7)),
                         reads=[("hT", t), "wb%d" % hb], writes=[kp], sig=(kc == 7))
                S.op("dve", lambda e, P1=P1, p=p: e.tensor_copy(out=qk[p][:], in_=P1[:, 0:128]), reads=[], writes=["qk%d" % p, kp])
                S.op("dve", lambda e, P1=P1, t=t, hb=hb: e.tensor_copy(out=V[hb][:, t, 0:128], in_=P1[:, 128:256]), reads=[], writes=[("V", hb), kp])
                S.op("act", lambda e, P1=P1, t=t, hb=hb: e.activation(out=og[hb][:, t, :], in_=P1[:, 256:384], func=AF.Sigmoid), reads=[], writes=[("og", 0), kp])
                for j in range(2):
                    S.op("pe", lambda e, j=j, p=p: e.transpose(cx.ps[6][0:64, j * 128:(j + 1) * 128], qk[p][:, j * 64:(j + 1) * 64], cx.ident[:]), reads=["qk%d" % p, "consts"], writes=["ps6"], sig=(j == 1))
                S.op("act", lambda e, t=t, hb=hb: e.activation(out=QKT[hb][0:64, :, t * 128:(t + 1) * 128], in_=cx.ps[6][0:64, 0:256].rearrange("p (a b) -> p a b", a=2), func=AF.Copy),
                     reads=[], writes=[("QK", hb), "ps6"])

            def finish(Tq, c, oap_ps, okey, h=h, hb=hb):
                f = fcnt[0] % 2
                fcnt[0] += 1
                kos, kot, kob = "oss%d" % f, "ot%d" % f, "osb%d" % f
                oap = osb[f]
                if f == 0:
                    S.op("act", lambda e: e.activation(out=oap[:, 0:129], in_=oap_ps, func=AF.Copy), reads=[], writes=[kob, okey])
                else:
                    S.op("dve", lambda e: e.tensor_copy(out=oap[:, 0:129], in_=oap_ps), reads=[], writes=[kob, okey])
                S.op("act", lambda e: e.activation(out=oss[f][:, 0:1], in_=oap[:, 128:129], func=AF.Abs), reads=[kob], writes=[kos])
                S.op("dve", lambda e: e.tensor_tensor(out=oss[f][:, 0:1], in0=oss[f][:, 0:1], in1=emcol[:, Tq * 8 + h:Tq * 8 + h + 1], op=ALU.max), reads=[kos, "emcol"], writes=[kos])
                S.op("dve", lambda e: e.reciprocal(out=oss[f][:, 1:2], in_=oss[f][:, 0:1]), reads=[kos], writes=[kos])
                S.op("act", lambda e: e.activation(out=ot[f][:], in_=oap[:, 0:128], func=AF.Copy, scale=oss[f][:, 1:2]), reads=[kob, kos], writes=[kot])
                S.op("act", lambda e: e.activation(out=ojk[:], in_=ot[f][:], func=AF.Square, accum_out=oss[f][:, 2:3]), reads=[kot], writes=["ojk", kos])
                S.op("dve", lambda e: e.tensor_scalar(out=oss[f][:, 3:4], in0=oss[f][:, 2:3], scalar1=1.0 / 128, scalar2=EPS, op0=ALU.mult, op1=ALU.add), reads=[kos], writes=[kos])
                S.op("act", lambda e: e.activation(out=oss[f][:, 3:4], in_=oss[f][:, 3:4], func=AF.Sqrt), reads=[kos], writes=[kos])
                S.op("dve", lambda e: e.reciprocal(out=oss[f][:, 4:5], in_=oss[f][:, 3:4]), reads=[kos], writes=[kos])
                S.op("dve", lambda e: e.scalar_tensor_tensor(out=ot[f][:], in0=ot[f][:], scalar=oss[f][:, 4:5], in1=OG[:, h * 128:(h + 1) * 128], op0=ALU.mult, op1=ALU.mult), reads=[kot, kos, "OG"], writes=[kot])
                S.op("pool", lambda e: e.tensor_tensor(out=ot[f][:], in0=ot[f][:], in1=og[hb][:, Tq, :], op=ALU.mult), reads=[kot, ("og", 0)], writes=[kot])
                S.op("pe", lambda e: e.transpose(cx.ps[7][:, 0:128], ot[f][:], cx.ident[:]), reads=[kot, "consts"], writes=["ps7"])
                S.op("act", lambda e: e.activation(out=oTb[hb][:, Tq * 128:(Tq + 1) * 128], in_=cx.ps[7][:, 0:128], func=AF.Copy), reads=[], writes=[("oTb", 0), "ps7"])

            flash_head(cx, h, "ml", QKT[hb][:, 0, :], QKT[hb][:, 1, :], V[hb], finish)
            S.dma("sp", oT_d[h], oTb[hb][:], reads=[("oTb", 0)], writes=["oT_d"])
        S.barrier()
    out_proj(cx, x_d, xo_d, wout_d, oT_d)


def build_ml_prog():
    nc = bass.Bass("TRN2", target_bir_lowering=False)
    x_d = dram_in(nc, "x", [S_LEN, D])
    c_d = dram_in(nc, "c", [1, D])
    adaw_d = dram_in(nc, "adaw", [D, 6 * D])
    adab_d = dram_in(nc, "adab", [1, 6 * D])
    mixn_d = dram_in(nc, "mixn", [1, D])
    win_d = dram_in(nc, "win", [D, 3088])
    bi_d = dram_in(nc, "bi", [1, 8])
    bf_d = dram_in(nc, "bf", [1, 8])
    outn_d = dram_in(nc, "outn", [1, D])
    wout_d = dram_in(nc, "wout", [D, D])
    consts_d = dram_in(nc, "consts", [128, 1024])
    sel_d = dram_in(nc, "sel", [128, 1024])
    xo_d = nc.dram_tensor("xo", [S_LEN, D], F32, kind="ExternalOutput").ap()
    oT_d = nc.dram_tensor("oT_d", [8, 128, S_LEN], BF16, kind="Internal").ap()
    with ExitStack() as stack:
        cx = new_ctx(nc, stack)
        block = stack.enter_context(nc.Block())
        cx.S.dma("sp", cx.consts[:], consts_d, writes=["consts"])
        phase_mlstm(cx, x_d, xo_d, c_d, adaw_d, adab_d, mixn_d, win_d, bi_d, bf_d, outn_d, wout_d, oT_d, sel_d)
        print("instructions", cx.S.nins, "waits", cx.S.nwait)
        cx.S.emit(block)
    return nc


PHASES = ("attn", "moe0", "mlstm", "moe1")


def build_prog(phases, n_exp=32, sliced=False):
    nc = bass.Bass("TRN2", target_bir_lowering=False)
    d = {}
    def inp(name, shape, dt=F32):
        d[name] = dram_in(nc, name, shape, dt)
        return d[name]
    x_d = inp("x", [S_LEN, D])
    c_d = inp("c", [1, D])
    NL = 1 if sliced else 2
    adaw_d = inp("ada_w", [NL, D, 6 * D])
    adab_d = inp("ada_b", [NL, 6 * D])
    consts_d = inp("consts", [128, 1024])
    if "attn" in phases:
        pos_d = inp("positions", [1, S_LEN], I32)
        inp("mix_norm", [NL, D])
        inp("att_w_in", [1, D, 3 * D]); inp("att_w_out", [1, D, D])
        for nm in ("att_q_norm", "att_k_norm", "att_lam_q1", "att_lam_k1", "att_lam_q2", "att_lam_k2"):
            inp(nm, [1, 64])
        inp("att_sub_norm", [1, 128])
    if "mlstm" in phases:
        if "mix_norm" not in d:
            inp("mix_norm", [NL, D])
        inp("ml_w_in", [1, D, 3088]); inp("ml_b_igate", [1, 8]); inp("ml_b_fgate", [1, 8])
        inp("ml_out_norm", [1, D]); inp("ml_w_out", [1, D, D]); inp("sel", [128, 1024])
    if "moe0" in phases or "moe1" in phases:
        inp("ffn_norm", [NL, D]); inp("router_w", [NL, D, 32]); inp("router_b", [NL, 32])
        inp("moe_w_gate_up", [NL, 32, D, 2 * D]); inp("moe_b_gate_up", [NL, 32, 2 * D])
        inp("moe_w_down", [NL, 32, D, D]); inp("moe_b_down", [NL, 32, D])
    xo_d = nc.dram_tensor("xo", [S_LEN, D], F32, kind="ExternalOutput").ap()
    oT_d = nc.dram_tensor("oT_d", [8, 128, S_LEN], BF16, kind="Internal").ap()
    xs = [x_d]
    for i in range(len(phases) - 1):
        xs.append(nc.dram_tensor("xmid%d" % i, [S_LEN, D], F32, kind="Internal").ap())
    xs.append(xo_d)
    with ExitStack() as stack:
        cx = new_ctx(nc, stack)
        block = stack.enter_context(nc.Block())
        cx.S.dma("sp", cx.consts[:], consts_d, writes=["consts"])
        for i, ph in enumerate(phases):
            xin, xout = xs[i], xs[i + 1]
            with ExitStack() as pst:
                cx.stack = pst
                if ph == "attn":
                    phase_attn(cx, xin, xout, c_d, pos_d, adaw_d[0], adab_d[0:1, :], d["mix_norm"][0:1, :], d["att_w_in"][0], d["att_w_out"][0],
                               d["att_q_norm"], d["att_k_norm"], [d["att_lam_q1"], d["att_lam_k1"], d["att_lam_q2"], d["att_lam_k2"]], d["att_sub_norm"], oT_d)
                elif ph == "mlstm":
                    L = 0 if sliced else 1
                    phase_mlstm(cx, xin, xout, c_d, adaw_d[L], adab_d[L:L + 1, :], d["mix_norm"][L:L + 1, :], d["ml_w_in"][0], d["ml_b_igate"], d["ml_b_fgate"],
                                d["ml_out_norm"], d["ml_w_out"][0], oT_d, d["sel"])
                else:
                    L = 0 if sliced else int(ph[-1])
                    phase_moe(cx, xin, xout, c_d, adaw_d[L], adab_d[L:L + 1, :], d["ffn_norm"][L:L + 1, :], d["router_w"][L], d["router_b"][L:L + 1, :],
                              d["moe_w_gate_up"][L], d["moe_b_gate_up"][L], d["moe_w_down"][L], d["moe_b_down"][L], n_exp=n_exp)
                cx.S.barrier()
            cx.stack = stack
        cx.S.emit(block)
    return nc, list(d.keys())


LAUNCH_GROUPS = [("attn",), ("moe0",), ("mlstm",), ("moe1",)]
PHASE_LAYER = {"attn": 0, "moe0": 0, "mlstm": 1, "moe1": 1}
PER_LAYER = ("ada_w", "ada_b", "mix_norm", "ffn_norm", "router_w", "router_b", "moe_w_gate_up", "moe_b_gate_up", "moe_w_down", "moe_b_down")
FUSED = False


def kernel(**inputs):
    n = 8
    shared = {k: np.ascontiguousarray(v) for k, v in inputs.items() if k not in ("x", "c", "positions")}
    shared["consts"] = make_consts()
    shared["sel"] = make_sel()
    xcur = [np.ascontiguousarray(inputs["x"][b]) for b in range(n)]
    groups = [PHASES] if FUSED else LAUNCH_GROUPS
    progs = {}
    for grp in groups:
        sliced = not FUSED
        pkey = ("moe0",) if (sliced and grp[0].startswith("moe")) else grp
        if pkey not in progs:
            progs[pkey] = build_prog(pkey, sliced=sliced)
        nc, names = progs[pkey]
        L = PHASE_LAYER[grp[0]]
        in_maps = []
        for b in range(n):
            m = {}
            for k in names:
                if k == "x":
                    m[k] = xcur[b]
                elif k == "c":
                    m[k] = np.ascontiguousarray(inputs["c"][b:b + 1])
                elif k == "positions":
                    m[k] = np.ascontiguousarray(inputs["positions"][b:b + 1]).astype(np.int32)
                elif sliced and k in PER_LAYER:
                    m[k] = np.ascontiguousarray(shared[k][L:L + 1])
                else:
                    m[k] = shared[k]
            in_maps.append(m)
        res = run_bass_kernel_spmd(nc, in_maps, core_ids=list(range(n)))
        xcur = [np.ascontiguousarray(res.results[b]["xo"]) for b in range(n)]
    return np.stack(xcur, axis=0).astype(np.float32)
```

```python
import numpy as np
import concourse.bass as bass
import concourse.mybir as mybir
from concourse.bass_utils import run_bass_kernel_spmd

F32 = mybir.dt.float32
BF16 = mybir.dt.bfloat16
I32 = mybir.dt.int32
AF = mybir.ActivationFunctionType
ALU = mybir.AluOpType
AX = mybir.AxisListType

ENGMAP = {"pe": "tensor", "act": "scalar", "dve": "vector", "pool": "gpsimd", "sp": "sync"}


class Sched:
    def __init__(self, nc, stack, ndma=8):
        self.nc = nc
        self.prog = {k: [] for k in ENGMAP}
        self.sem = {}
        self.cnt = {k: 0 for k in ENGMAP}
        self.dsem = {}
        self.drr = {k: 0 for k in ENGMAP}
        for k in ENGMAP:
            self.sem[k] = stack.enter_context(nc.semaphore("s_" + k))
        for k in ("sp", "pool", "act"):
            self.dsem[k] = [[stack.enter_context(nc.semaphore("d_%s%d" % (k, j))), 0] for j in range(ndma)]
        self.lastw = {}
        self.readers = {}
        self.seen = {}
        self.nwait = 0
        self.nins = 0

    def _deps(self, eng, reads, writes):
        best = {}
        def add(tok):
            s, v, src = tok
            if src == eng and eng == "pe":
                return
            key = id(s)
            if key not in best or best[key][1] < v:
                best[key] = (s, v)
        for k in reads:
            if k in self.lastw:
                add(self.lastw[k])
        for k in writes:
            if k in self.lastw:
                add(self.lastw[k])
            for t in self.readers.get(k, ()):
                add(t)
        for key, (s, v) in best.items():
            sk = (eng, key)
            if self.seen.get(sk, 0) >= v:
                continue
            self.seen[sk] = v
            self.prog[eng].append(lambda e, s=s, v=v: e.wait_ge(s, v))
            self.nwait += 1

    def _commit(self, tok, reads, writes):
        for k in reads:
            self.readers.setdefault(k, []).append(tok)
        for k in writes:
            self.lastw[k] = tok
            self.readers[k] = []

    def op(self, eng, fn, reads=(), writes=(), sig=True):
        self._deps(eng, reads, writes)
        sem = self.sem[eng]
        if sig:
            self.cnt[eng] += 1
            tok = (sem, self.cnt[eng], eng)
            self.prog[eng].append(lambda e, fn=fn, sem=sem: fn(e).then_inc(sem, 1))
        else:
            tok = (sem, self.cnt[eng] + 1, eng)
            self.prog[eng].append(lambda e, fn=fn: fn(e))
        self.nins += 1
        self._commit(tok, reads, writes)
        return tok

    def dma(self, q, out, in_, reads=(), writes=(), **kw):
        self._deps(q, reads, writes)
        j = self.drr[q]
        self.drr[q] = (j + 1) % len(self.dsem[q])
        ent = self.dsem[q][j]
        sem, c = ent
        if c > 0:
            sk = (q, id(sem))
            if self.seen.get(sk, 0) < 16 * c:
                self.seen[sk] = 16 * c
                self.prog[q].append(lambda e, s=sem, v=16 * c: e.wait_ge(s, v))
        ent[1] = c + 1
        tok = (sem, 16 * (c + 1), "dma_" + q)
        self.prog[q].append(lambda e, out=out, in_=in_, sem=sem, kw=kw: e.dma_start(out=out, in_=in_, **kw).then_inc(sem, 16))
        self.nins += 1
        self._commit(tok, reads, writes)
        return tok

    def barrier(self):
        toks = []
        for k in ENGMAP:
            if self.cnt[k] > 0:
                toks.append((self.sem[k], self.cnt[k], k))
        for q in self.dsem:
            for sem, c in self.dsem[q]:
                if c > 0:
                    toks.append((sem, 16 * c, "dma"))
        for eng in ENGMAP:
            for s_, v, src in toks:
                if src == eng and eng == "pe":
                    continue
                sk = (eng, id(s_))
                if self.seen.get(sk, 0) >= v:
                    continue
                self.seen[sk] = v
                self.prog[eng].append(lambda e, s_=s_, v=v: e.wait_ge(s_, v))
        self.lastw = {}
        self.readers = {}

    def wait_all(self, eng, keys):
        self._deps(eng, list(keys), [])

    def emit(self, block):
        for k, name in ENGMAP.items():
            lst = self.prog[k]
            def body(e, lst=lst):
                for f in lst:
                    f(e)
            getattr(block, name)(body)


import math
from contextlib import ExitStack
import numpy as np

S_LEN = 4096
D = 1024
NT = S_LEN // 128
EPS = 1e-6
PI = math.pi


def make_consts():
    c = np.zeros((128, 1024), np.float32)
    c[:, 0:128] = np.eye(128, dtype=np.float32)
    s = np.arange(128)[:, None]
    l = np.arange(128)[None, :]
    c[:, 128:256] = (s <= l).astype(np.float32)
    c[:, 256:384] = np.where(s <= l, 0.0, -30000.0)
    inv = (10000.0 ** (-np.arange(0, 64, 2, dtype=np.float32) / 64)).astype(np.float32)
    c[:, 384:416] = inv[None, :]
    for h in range(8):
        for j in range(3):
            c[8 * j + h, 416 + 0:416 + 0] = 0
    return c


def make_sel():
    sel = np.zeros((128, 8, 128), np.float32)
    for h in range(8):
        for j in range(3):
            sel[8 * j + h, h, :] = 1.0
    return sel.reshape(128, 1024)


class Ctx:
    pass


_uid = [0]


def uname(name):
    _uid[0] += 1
    return "s%d_%s" % (_uid[0], name)


def T(cx, name, shape, dt=F32):
    return cx.stack.enter_context(cx.nc.sbuf_tensor(uname(name), shape, dt))


def common_setup(cx, layer_half, c_d, adaw_d, adab_d, norm_d):
    S, nc = cx.S, cx.nc
    with ExitStack() as st:
        def TT(name, shape, dt=F32):
            return st.enter_context(nc.sbuf_tensor(uname(name), shape, dt))
        rowc = TT("rowc", [128, 1024])
        crep = TT("crep", [128, 8, 128])
        stg = [TT("adst%d" % i, [128, 3072]) for i in range(2)]
        adab = TT("adab", [128, 3072])
        nrm = TT("nrmb", [128, 1024])
        c0 = layer_half * 3072
        S.dma("sp", rowc[:], c_d.partition_broadcast(128), writes=["rowc"])
        S.dma("sp", adab[:], adab_d[:, c0:c0 + 3072].partition_broadcast(128), writes=["adab"])
        S.dma("sp", nrm[:], norm_d.partition_broadcast(128), writes=["nrmb"])
        S.op("act", lambda e: e.activation(out=crep[:].rearrange("p a b -> p (a b)"), in_=rowc[:], func=AF.Sigmoid), reads=["rowc"], writes=["crep"])
        S.op("dve", lambda e: e.tensor_tensor(out=rowc[:], in0=rowc[:], in1=crep[:].rearrange("p a b -> p (a b)"), op=ALU.mult), reads=["crep", "rowc"], writes=["rowc"])
        for kc in range(8):
            b = cx.ps[kc // 4]
            S.op("pe", lambda e, kc=kc, b=b: e.transpose(b[:, (kc % 4) * 128:(kc % 4 + 1) * 128], rowc[:, kc * 128:(kc + 1) * 128], cx.ident[:]),
                 reads=["rowc", "consts"], writes=["ps%d" % (kc // 4)])
        for hb in range(2):
            S.op("dve", lambda e, hb=hb: e.tensor_copy(out=crep[:, hb * 4:(hb + 1) * 4, :].rearrange("p a b -> p (a b)"), in_=cx.ps[hb][:, :]),
                 reads=[], writes=["crep", "ps%d" % hb])
        for kc in range(8):
            sb = stg[kc % 2]
            S.dma("sp" if kc % 2 == 0 else "pool", sb[:], adaw_d[kc * 128:(kc + 1) * 128, c0:c0 + 3072], writes=["adst%d" % (kc % 2)])
            for n in range(6):
                S.op("pe", lambda e, kc=kc, n=n, sb=sb: e.matmul(cx.ps[n][:, :], lhsT=crep[:, kc, :], rhs=sb[:, n * 512:(n + 1) * 512], start=(kc == 0), stop=(kc == 7)),
                     reads=["crep", "adst%d" % (kc % 2)], writes=["ps%d" % n], sig=(kc == 7 or n == 5))
        for n in range(6):
            dst = (cx.SH, cx.A, cx.G)[n // 2]
            S.op("dve", lambda e, n=n, dst=dst: e.tensor_tensor(out=dst[:, (n % 2) * 512:(n % 2 + 1) * 512], in0=cx.ps[n][:, :], in1=adab[:, n * 512:(n + 1) * 512], op=ALU.add),
                 reads=["adab"], writes=["mod%d" % (n // 2), "ps%d" % n])
        S.op("dve", lambda e: e.scalar_tensor_tensor(out=cx.A[:], in0=cx.A[:], scalar=1.0, in1=nrm[:], op0=ALU.add, op1=ALU.mult),
             reads=["mod1", "nrmb"], writes=["mod1"])
        S.barrier()


def norm_tile(cx, t, x_d, hT, tok0, want_f32T=None):
    S, nc = cx.S, cx.nc
    i = cx.ncnt % 2
    cx.ncnt += 1
    xt, hh, jk = cx.xt[i], cx.hh[i], cx.jk
    ss = cx.nss[i]
    kx, kh, ks = "xt%d" % i, "hh%d" % i, "nss%d" % i
    S.dma("sp", xt[:], x_d[t * 128:(t + 1) * 128, :], writes=[kx])
    S.op("act", lambda e: e.activation(out=jk[:], in_=xt[:], func=AF.Square, accum_out=ss[:, 0:1]), reads=[kx], writes=["jk", ks])
    S.op("dve", lambda e: e.tensor_scalar(out=ss[:, 1:2], in0=ss[:, 0:1], scalar1=1.0 / D, scalar2=EPS, op0=ALU.mult, op1=ALU.add), reads=[ks], writes=[ks])
    S.op("act", lambda e: e.activation(out=ss[:, 2:3], in_=ss[:, 1:2], func=AF.Sqrt), reads=[ks], writes=[ks])
    S.op("dve", lambda e: e.reciprocal(out=ss[:, 3:4], in_=ss[:, 2:3]), reads=[ks], writes=[ks])
    S.op("dve", lambda e: e.scalar_tensor_tensor(out=hh[:], in0=xt[:], scalar=ss[:, 3:4], in1=cx.A[:], op0=ALU.mult, op1=ALU.mult), reads=[kx, ks, "mod1"], writes=[kh])
    S.op("pool", lambda e: e.tensor_tensor(out=hh[:], in0=hh[:], in1=cx.SH[:], op=ALU.add), reads=[kh, "mod0"], writes=[kh])
    for kc in range(8):
        b = 6 + kc // 4
        S.op("pe", lambda e, kc=kc, b=b: e.transpose(cx.ps[b][:, (kc % 4) * 128:(kc % 4 + 1) * 128], hh[:, kc * 128:(kc + 1) * 128], cx.ident[:]),
             reads=[kh, "consts"], writes=["ps%d" % b], sig=(kc % 4 == 3))
    for hb in range(2):
        eng = "act" if hb == 0 else "dve"
        outap = hT[:, hb * 4:(hb + 1) * 4, tok0:tok0 + 128]
        inap = cx.ps[6 + hb][:, :].rearrange("p (a b) -> p a b", a=4)
        if eng == "act":
            S.op("act", lambda e, o=outap, i_=inap: e.activation(out=o, in_=i_, func=AF.Copy), reads=[], writes=[("hT", tok0 // 128), "ps%d" % (6 + hb)])
        else:
            S.op("dve", lambda e, o=outap, i_=inap: e.tensor_copy(out=o, in_=i_), reads=[], writes=[("hT", tok0 // 128), "ps%d" % (6 + hb)])
        if want_f32T is not None:
            o2 = want_f32T[:, hb * 4:(hb + 1) * 4, :]
            S.op("pool" if False else "dve", lambda e, o=o2, i_=inap: e.tensor_copy(out=o, in_=i_), reads=[], writes=["hTf", "ps%d" % (6 + hb)])


def flash_head(cx, h, mode, QT, KT, V, finish_tile):
    S = cx.S
    ncomp = 2 if mode == "att" else 1
    for qb in range(8):
        for c in range(ncomp):
            pb = 0 if mode == "ml" else c * 64
            nk = 4 * qb + 4
            for kt in range(nk):
                j0 = max(0, kt - 4 * qb)
                c0 = j0 * 128
                sb = cx.stcnt % 2
                cx.stcnt += 1
                stp = cx.ps[sb]
                pt = cx.PT[sb]
                kst, kpt = "ps%d" % sb, "PT%d" % sb
                S.op("pe", lambda e, stp=stp, pb=pb, kt=kt, qb=qb, c0=c0: e.matmul(
                    stp[:, c0:512], lhsT=KT[pb:pb + 64, kt * 128:(kt + 1) * 128], rhs=QT[pb:pb + 64, qb * 512 + c0:qb * 512 + 512], start=True, stop=True),
                    reads=[("QK", h % 2)], writes=[kst])
                if mode == "att":
                    S.op("act", lambda e, stp=stp, pt=pt, c0=c0: e.activation(out=pt[:, c0:512], in_=stp[:, c0:512], func=AF.Exp, scale=0.125),
                         reads=[], writes=[kpt, kst])
                    if kt >= 4 * qb:
                        S.op("pool", lambda e, pt=pt, c0=c0: e.tensor_tensor(out=pt[:, c0:c0 + 128], in0=pt[:, c0:c0 + 128], in1=cx.mask01b[:], op=ALU.mult),
                             reads=[kpt, "consts2"], writes=[kpt])
                else:
                    rb = cx.ps[4 + sb]
                    krb, kdt = "ps%d" % (4 + sb), "DT%d" % sb
                    dt_ = cx.DT[sb]
                    S.op("pe", lambda e, rb=rb, qb=qb, c0=c0: e.matmul(rb[:, c0:512], lhsT=cx.sel3[0:24, h, :], rhs=cx.r3[0:24, qb * 512 + c0:qb * 512 + 512], start=True, stop=True),
                         reads=["r3", "sel3"], writes=[krb])
                    ucol = cx.ucol[:, kt * 8 + h:kt * 8 + h + 1]
                    if kt >= 4 * qb:
                        S.op("dve", lambda e, rb=rb, dt_=dt_, c0=c0: e.tensor_tensor(out=dt_[:, c0:c0 + 128], in0=rb[:, c0:c0 + 128], in1=cx.maskneg[:], op=ALU.add),
                             reads=["consts"], writes=[kdt, krb])
                        S.op("act", lambda e, dt_=dt_, c0=c0, ucol=ucol: e.activation(out=dt_[:, c0:c0 + 128], in_=dt_[:, c0:c0 + 128], func=AF.Exp, bias=ucol),
                             reads=[kdt, "ucol"], writes=[kdt])
                        if c0 + 128 < 512:
                            S.op("act", lambda e, rb=rb, dt_=dt_, c0=c0, ucol=ucol: e.activation(out=dt_[:, c0 + 128:512], in_=rb[:, c0 + 128:512], func=AF.Exp, bias=ucol),
                                 reads=["ucol"], writes=[kdt, krb])
                    else:
                        S.op("act", lambda e, rb=rb, dt_=dt_, ucol=ucol: e.activation(out=dt_[:, 0:512], in_=rb[:, 0:512], func=AF.Exp, bias=ucol),
                             reads=["ucol"], writes=[kdt, krb])
                    dbg = getattr(cx, "dbg", None)
                    if dbg is not None and h == 0 and qb == 3 and kt in (0, 11):
                        S.dma("sp", dbg["DT%d" % kt], dt_[:, :], reads=[kdt], writes=["dbgDT%d" % kt])
                        S.op("dve", lambda e, rb=rb: e.tensor_copy(out=cx.dbgt[:], in_=rb[:, :]), reads=[], writes=["dbgt", krb])
                        S.dma("sp", dbg["RB%d" % kt], cx.dbgt[:], reads=["dbgt"], writes=["dbgRB%d" % kt])
                        if kt == 0:
                            S.dma("sp", dbg["sel3"], cx.sel3[0:32, 0, :], reads=["sel3"], writes=["dbgsel"])
                    S.op("dve", lambda e, stp=stp, pt=pt, dt_=dt_, c0=c0: e.scalar_tensor_tensor(out=pt[:, c0:512], in0=stp[:, c0:512], scalar=0.125, in1=dt_[:, c0:512], op0=ALU.mult, op1=ALU.mult),
                         reads=[kdt], writes=[kpt, kst])
                for i in range(j0, 4):
                    ob = cx.ps[2 + i // 2]
                    oap = ob[:, (i % 2) * 256:(i % 2) * 256 + 129]
                    last = (kt == 4 * qb + i)
                    S.op("pe", lambda e, oap=oap, pt=pt, i=i, kt=kt, last=last: e.matmul(oap, lhsT=pt[:, i * 128:(i + 1) * 128], rhs=V[:, kt, 0:129], start=(kt == 0 and i % 2 == 0), stop=last, skip_group_check=True),
                         reads=[kpt, ("V", h % 2)], writes=["ps%d" % (2 + i // 2)], sig=(last or i == 3))
            for i in range(4):
                ob = cx.ps[2 + i // 2]
                oap = ob[:, (i % 2) * 256:(i % 2) * 256 + 129]
                finish_tile(4 * qb + i, c, oap, "ps%d" % (2 + i // 2))


def rstd_ops(cx, ssap, n, inv_n, key):
    S = cx.S
    S.op("dve", lambda e: e.tensor_scalar(out=ssap[:, n:2 * n], in0=ssap[:, 0:n], scalar1=inv_n, scalar2=EPS, op0=ALU.mult, op1=ALU.add), reads=[key], writes=[key])
    S.op("act", lambda e: e.activation(out=ssap[:, n:2 * n], in_=ssap[:, n:2 * n], func=AF.Sqrt), reads=[key], writes=[key])
    S.op("dve", lambda e: e.reciprocal(out=ssap[:, 2 * n:3 * n], in_=ssap[:, n:2 * n]), reads=[key], writes=[key])


def load_cast_w(cx, dst, stage, kstage, kdst, srcs, q="sp", cast_eng="pool"):
    S = cx.S
    for (ap, off, w) in srcs:
        S.dma(q, stage[:, :, off:off + w], ap.rearrange("(kc p) j -> p kc j", p=128), writes=[kstage])
    if cast_eng == "act":
        S.op("act", lambda e: e.activation(out=dst[:], in_=stage[:], func=AF.Copy), reads=[kstage], writes=[kdst])
    else:
        S.op(cast_eng, lambda e: e.tensor_copy(out=dst[:], in_=stage[:]), reads=[kstage], writes=[kdst])


def phase_attn(cx, x_d, xo_d, c_d, pos_d, adaw_d, adab_d, mixn_d, win_d, wout_d, qn_d, kn_d, lam_ds, subn_d, oT_d):
    S, nc = cx.S, cx.nc
    st = cx.stack
    lam_init = 0.2
    common_setup(cx, 0, c_d, adaw_d, adab_d, mixn_d)
    hT = T(cx, "hT", [128, 8, S_LEN], BF16)
    GQK = T(cx, "GQK", [128, 256])
    for j, d_ in enumerate((qn_d, qn_d, kn_d, kn_d)):
        S.dma("pool", GQK[:, j * 64:(j + 1) * 64], d_.partition_broadcast(128), writes=["GQK"])
    lamt = T(cx, "lamt", [128, 4, 64])
    for j, d_ in enumerate(lam_ds):
        S.dma("pool", lamt[:, j, :], d_.partition_broadcast(128), writes=["lamt"])
    lams = T(cx, "lams", [128, 8])
    S.op("dve", lambda e: e.tensor_tensor(out=lamt[:, 0, :], in0=lamt[:, 0, :], in1=lamt[:, 1, :], op=ALU.mult), reads=["lamt"], writes=["lamt"])
    S.op("dve", lambda e: e.tensor_tensor(out=lamt[:, 2, :], in0=lamt[:, 2, :], in1=lamt[:, 3, :], op=ALU.mult), reads=["lamt"], writes=["lamt"])
    S.op("dve", lambda e: e.reduce_sum(out=lams[:, 0:1], in_=lamt[:, 0, :], axis=AX.X), reads=["lamt"], writes=["lams"])
    S.op("dve", lambda e: e.reduce_sum(out=lams[:, 1:2], in_=lamt[:, 2, :], axis=AX.X), reads=["lamt"], writes=["lams"])
    S.op("act", lambda e: e.activation(out=lams[:, 2:4], in_=lams[:, 0:2], func=AF.Exp), reads=["lams"], writes=["lams"])
    S.op("dve", lambda e: e.tensor_tensor(out=lams[:, 4:5], in0=lams[:, 3:4], in1=lams[:, 2:3], op=ALU.subtract), reads=["lams"], writes=["lams"])
    S.op("dve", lambda e: e.tensor_scalar(out=lams[:, 5:6], in0=lams[:, 4:5], scalar1=-lam_init, scalar2=None, op0=ALU.add), reads=["lams"], writes=["lams"])
    neglam = lams[:, 5:6]
    subg = T(cx, "subg", [128, 2])
    S.dma("pool", subg[:, 0:1], subn_d.rearrange("o (p a) -> (o p) a", a=1), writes=["subg"])
    S.op("dve", lambda e: e.tensor_scalar(out=subg[:, 1:2], in0=subg[:, 0:1], scalar1=1.0 - lam_init, scalar2=None, op0=ALU.mult), reads=["subg"], writes=["subg"])
    cosT = T(cx, "cosT", [128, NT, 32])
    sinT = T(cx, "sinT", [128, NT, 32])
    with ExitStack() as st2:
        posi = st2.enter_context(nc.sbuf_tensor(uname("posi"), [32, 128], I32))
        posf = st2.enter_context(nc.sbuf_tensor(uname("posf"), [32, 128], F32))
        post = st2.enter_context(nc.sbuf_tensor(uname("post"), [128, 32], F32))
        ang = st2.enter_context(nc.sbuf_tensor(uname("ang"), [128, NT, 32], F32))
        tmpf = st2.enter_context(nc.sbuf_tensor(uname("tmpf"), [128, NT, 32], F32))
        tmpi = st2.enter_context(nc.sbuf_tensor(uname("tmpi"), [128, NT, 32], I32))
        S.dma("sp", posi[:], pos_d.rearrange("o (t p) -> (o t) p", p=128), writes=["posi"])
        S.op("dve", lambda e: e.tensor_copy(out=posf[:], in_=posi[:]), reads=["posi"], writes=["posf"])
        S.op("pe", lambda e: e.transpose(cx.ps[0][:, 0:32], posf[:], cx.ident[0:32, 0:32]), reads=["posf", "consts"], writes=["ps0"])
        S.op("dve", lambda e: e.tensor_copy(out=post[:], in_=cx.ps[0][:, 0:32]), reads=[], writes=["post", "ps0"])
        S.op("dve", lambda e: e.tensor_tensor(out=ang[:], in0=post[:].unsqueeze(2).to_broadcast([128, NT, 32]), in1=cx.invf[:].unsqueeze(1).to_broadcast([128, NT, 32]), op=ALU.mult),
             reads=["post", "consts"], writes=["ang"])
        for which, dstT in ((0, sinT), (1, cosT)):
            off = 0.0 if which == 0 else PI / 2
            S.op("dve", lambda e, off=off: e.tensor_scalar(out=tmpf[:], in0=ang[:], scalar1=off, scalar2=1.0 / (2 * PI), op0=ALU.add, op1=ALU.mult), reads=["ang"], writes=["tmpf"])
            S.op("dve", lambda e: e.tensor_copy(out=tmpi[:], in_=tmpf[:]), reads=["tmpf"], writes=["tmpi"])
            S.op("dve", lambda e: e.tensor_copy(out=tmpf[:], in_=tmpi[:]), reads=["tmpi"], writes=["tmpf"])
            S.op("dve", lambda e: e.scalar_tensor_tensor(out=tmpf[:], in0=tmpf[:], scalar=-2 * PI, in1=ang[:], op0=ALU.mult, op1=ALU.add), reads=["tmpf", "ang"], writes=["tmpf"])
            S.op("dve", lambda e, off=off: e.tensor_scalar(out=tmpf[:], in0=tmpf[:], scalar1=off, scalar2=PI, op0=ALU.add, op1=ALU.min), reads=["tmpf"], writes=["tmpf"])
            S.op("dve", lambda e: e.tensor_scalar(out=tmpf[:], in0=tmpf[:], scalar1=-PI, scalar2=None, op0=ALU.max), reads=["tmpf"], writes=["tmpf"])
            S.op("act", lambda e, dstT=dstT: e.activation(out=dstT[:], in_=tmpf[:], func=AF.Sin), reads=["tmpf"], writes=["rope"])
        S.barrier()
    with ExitStack() as st2:
        cx.xt = [st2.enter_context(nc.sbuf_tensor(uname("xt%d" % i), [128, 1024], F32)) for i in range(2)]
        cx.hh = [st2.enter_context(nc.sbuf_tensor(uname("hh%d" % i), [128, 1024], F32)) for i in range(2)]
        cx.jk = st2.enter_context(nc.sbuf_tensor(uname("jk"), [128, 1024], F32))
        cx.nss = [st2.enter_context(nc.sbuf_tensor(uname("nss%d" % i), [128, 4], F32)) for i in range(2)]
        cx.ncnt = 0
        for t in range(NT):
            norm_tile(cx, t, x_d, hT, t * 128)
        S.barrier()
    with ExitStack() as st2:
        def TT(name, shape, dt=F32):
            return st2.enter_context(nc.sbuf_tensor(uname(name), shape, dt))
        wst = TT("wst", [128, 8, 384])
        wb = [TT("wb%d" % i, [128, 8, 384], BF16) for i in range(2)]
        QKT = [TT("QKT%d" % i, [128, 2, S_LEN], BF16) for i in range(2)]
        V = [TT("V%d" % i, [128, NT, 132], BF16) for i in range(2)]
        cx.PT = [TT("PT%d" % i, [128, 512], BF16) for i in range(2)]
        cx.mask01b = TT("mask01b", [128, 128], BF16)
        sq = [TT("sq%d" % i, [128, 256]) for i in range(2)]
        qn = [TT("qn%d" % i, [128, 256]) for i in range(2)]
        qr = [TT("qr%d" % i, [128, 256]) for i in range(2)]
        t1 = [TT("t1_%d" % i, [128, 128]) for i in range(2)]
        t2 = [TT("t2_%d" % i, [128, 128]) for i in range(2)]
        pss = [TT("pss%d" % i, [128, 12]) for i in range(2)]
        o0 = TT("o0", [128, 4, 128])
        ot = [TT("ot%d" % i, [128, 128]) for i in range(2)]
        ojk = TT("ojk", [128, 128])
        osb = [TT("osb%d" % i, [128, 132]) for i in range(2)]
        oss = [TT("oss%d" % i, [128, 4]) for i in range(2)]
        oTb = [TT("oTb%d" % i, [128, S_LEN], BF16) for i in range(2)]
        S.op("dve", lambda e: e.tensor_copy(out=cx.mask01b[:], in_=cx.mask01[:]), reads=["consts"], writes=["consts2"])
        for i in range(2):
            S.op("pool", lambda e, i=i: e.memset(V[i][:, :, 128:129], 1.0), writes=[("V", i)])
        cx.stcnt = 0
        fcnt = [0]
        for h in range(8):
            hb = h % 2
            load_cast_w(cx, wb[hb], wst, "wst", "wb%d" % hb,
                        [(win_d[:, o_ * 1024 + h * 128:o_ * 1024 + (h + 1) * 128], o_ * 128, 128) for o_ in range(3)], q="pool", cast_eng="pool")
            for t in range(NT):
                p = t % 2
                P1 = cx.ps[4 + p]
                kp = "ps%d" % (4 + p)
                for kc in range(8):
                    S.op("pe", lambda e, P1=P1, kc=kc, t=t, hb=hb: e.matmul(P1[:, 0:384], lhsT=hT[:, kc, t * 128:(t + 1) * 128], rhs=wb[hb][:, kc, :], start=(kc == 0), stop=(kc == 7)),
                         reads=[("hT", t), "wb%d" % hb], writes=[kp], sig=(kc == 7))
                ksq, kqn, kqr, kt1, kt2, kss = "sq%d" % p, "qn%d" % p, "qr%d" % p, "t1_%d" % p, "t2_%d" % p, "pss%d" % p
                S.op("act", lambda e, P1=P1, p=p: e.activation(out=sq[p][:], in_=P1[:, 0:256], func=AF.Square), reads=[], writes=[ksq, kp])
                S.op("dve", lambda e, p=p: e.reduce_sum(out=pss[p][:, 0:4], in_=sq[p][:].rearrange("p (a b) -> p a b", a=4), axis=AX.X), reads=[ksq], writes=[kss])
                rstd_ops(cx, pss[p], 4, 1.0 / 64, kss)
                S.op("dve", lambda e, P1=P1, p=p: e.tensor_tensor(out=qn[p][:].rearrange("p (a b) -> p a b", a=4), in0=P1[:, 0:256].rearrange("p (a b) -> p a b", a=4),
                                                             in1=pss[p][:, 8:12].unsqueeze(2).to_broadcast([128, 4, 64]), op=ALU.mult), reads=[kss], writes=[kqn, kp])
                S.op("pool", lambda e, p=p: e.tensor_tensor(out=qn[p][:], in0=qn[p][:], in1=GQK[:], op=ALU.mult), reads=[kqn, "GQK"], writes=[kqn])
                q4 = qn[p][:].rearrange("p (a b) -> p a b", a=4)
                r4 = qr[p][:].rearrange("p (a b) -> p a b", a=4)
                cb = cosT[:, t, :].unsqueeze(1).to_broadcast([128, 4, 32])
                sb_ = sinT[:, t, :].unsqueeze(1).to_broadcast([128, 4, 32])
                a4 = t1[p][:].rearrange("p (a b) -> p a b", a=4)
                b4 = t2[p][:].rearrange("p (a b) -> p a b", a=4)
                S.op("dve", lambda e, a4=a4, q4=q4, cb=cb: e.tensor_tensor(out=a4, in0=q4[:, :, 0:32], in1=cb, op=ALU.mult), reads=[kqn, "rope"], writes=[kt1])
                S.op("pool", lambda e, b4=b4, q4=q4, sb_=sb_: e.tensor_tensor(out=b4, in0=q4[:, :, 32:64], in1=sb_, op=ALU.mult), reads=[kqn, "rope"], writes=[kt2])
                S.op("dve", lambda e, a4=a4, b4=b4, r4=r4: e.tensor_tensor(out=r4[:, :, 0:32], in0=a4, in1=b4, op=ALU.subtract), reads=[kt1, kt2], writes=[kqr])
                S.op("pool", lambda e, a4=a4, q4=q4, cb=cb: e.tensor_tensor(out=a4, in0=q4[:, :, 32:64], in1=cb, op=ALU.mult), reads=[kqn, "rope", kqr], writes=[kt1])
                S.op("dve", lambda e, b4=b4, q4=q4, sb_=sb_: e.tensor_tensor(out=b4, in0=q4[:, :, 0:32], in1=sb_, op=ALU.mult), reads=[kqn, "rope", kqr], writes=[kt2])
                S.op("pool", lambda e, a4=a4, b4=b4, r4=r4: e.tensor_tensor(out=r4[:, :, 32:64], in0=a4, in1=b4, op=ALU.add), reads=[kt1, kt2], writes=[kqr])
                for j in range(2):
                    S.op("pe", lambda e, j=j, p=p: e.transpose(cx.ps[6][:, j * 128:(j + 1) * 128], qr[p][:, j * 128:(j + 1) * 128], cx.ident[:]), reads=[kqr, "consts"], writes=["ps6"], sig=(j == 1))
                S.op("act", lambda e, t=t, hb=hb: e.activation(out=QKT[hb][:, :, t * 128:(t + 1) * 128], in_=cx.ps[6][:, 0:256].rearrange("p (a b) -> p a b", a=2), func=AF.Copy),
                     reads=[], writes=[("QK", hb), "ps6"])
                S.op("dve", lambda e, P1=P1, t=t, hb=hb: e.tensor_copy(out=V[hb][:, t, 0:128], in_=P1[:, 256:384]), reads=[], writes=[("V", hb), kp])

            def finish(Tq, c, oap_ps, okey, h=h, hb=hb):
                i = Tq % 4
                f = fcnt[0] % 2
                fcnt[0] += 1
                kos, kot, kob = "oss%d" % f, "ot%d" % f, "osb%d" % f
                oap = osb[f]
                if f == 0:
                    S.op("act", lambda e: e.activation(out=oap[:, 0:129], in_=oap_ps, func=AF.Copy), reads=[], writes=[kob, okey])
                else:
                    S.op("dve", lambda e: e.tensor_copy(out=oap[:, 0:129], in_=oap_ps), reads=[], writes=[kob, okey])
                S.op("dve", lambda e: e.reciprocal(out=oss[f][:, 0:1], in_=oap[:, 128:129]), reads=[kob], writes=[kos])
                if c == 0:
                    S.op("act", lambda e: e.activation(out=o0[:, i, :], in_=oap[:, 0:128], func=AF.Copy, scale=oss[f][:, 0:1]), reads=[kob, kos], writes=[("o0", i)])
                    return
                S.op("act", lambda e: e.activation(out=ot[f][:], in_=oap[:, 0:128], func=AF.Copy, scale=oss[f][:, 0:1]), reads=[kob, kos], writes=[kot])
                S.op("dve", lambda e: e.scalar_tensor_tensor(out=ot[f][:], in0=ot[f][:], scalar=neglam, in1=o0[:, i, :], op0=ALU.mult, op1=ALU.add), reads=[kot, ("o0", i), "lams"], writes=[kot])
                S.op("act", lambda e: e.activation(out=ojk[:], in_=ot[f][:], func=AF.Square, accum_out=oss[f][:, 1:2]), reads=[kot], writes=["ojk", kos])
                S.op("dve", lambda e: e.tensor_scalar(out=oss[f][:, 2:3], in0=oss[f][:, 1:2], scalar1=1.0 / 128, scalar2=EPS, op0=ALU.mult, op1=ALU.add), reads=[kos], writes=[kos])
                S.op("act", lambda e: e.activation(out=oss[f][:, 2:3], in_=oss[f][:, 2:3], func=AF.Sqrt), reads=[kos], writes=[kos])
                S.op("dve", lambda e: e.reciprocal(out=oss[f][:, 3:4], in_=oss[f][:, 2:3]), reads=[kos], writes=[kos])
                S.op("dve", lambda e: e.tensor_scalar(out=ot[f][:], in0=ot[f][:], scalar1=oss[f][:, 3:4], scalar2=None, op0=ALU.mult), reads=[kot, kos], writes=[kot])
                S.op("pe", lambda e: e.transpose(cx.ps[7][:, 0:128], ot[f][:], cx.ident[:]), reads=[kot, "consts"], writes=["ps7"])
                S.op("act", lambda e: e.activation(out=oTb[hb][:, Tq * 128:(Tq + 1) * 128], in_=cx.ps[7][:, 0:128], func=AF.Copy, scale=subg[:, 1:2]), reads=["subg"], writes=[("oTb", hb), "ps7"])

            flash_head(cx, h, "att", QKT[hb][:, 0, :], QKT[hb][:, 1, :], V[hb], finish)
            S.dma("sp", oT_d[h], oTb[hb][:], reads=[("oTb", hb)], writes=["oT_d"])
        S.barrier()
    out_proj(cx, x_d, xo_d, wout_d, oT_d)


def out_proj(cx, x_d, xo_d, wout_d, oT_d):
    S, nc = cx.S, cx.nc
    with ExitStack() as st2:
        def TT(name, shape, dt=F32):
            return st2.enter_context(nc.sbuf_tensor(uname(name), shape, dt))
        wst = TT("wost", [128, 8, 1024])
        wo = TT("wo", [128, 8, 1024], BF16)
        oTt = [TT("oTt%d" % i, [128, 8, 128], BF16) for i in range(2)]
        xt = [TT("xo%d" % i, [128, 1024]) for i in range(2)]
        yt = [TT("yo%d" % i, [128, 1024]) for i in range(2)]
        load_cast_w(cx, wo, wst, "wost", "wo", [(wout_d, 0, 1024)], q="pool", cast_eng="pool")
        for t in range(NT):
            p = t % 2
            S.dma("sp", oTt[p][:], oT_d[:, :, t * 128:(t + 1) * 128].rearrange("h p j -> p h j"), reads=["oT_d"], writes=["oTt%d" % p])
            S.dma("sp", xt[p][:], x_d[t * 128:(t + 1) * 128, :], writes=["xo%d" % p])
            for n in range(2):
                pb = cx.ps[2 * p + n]
                kp = "ps%d" % (2 * p + n)
                for h in range(8):
                    S.op("pe", lambda e, pb=pb, h=h, p=p, n=n: e.matmul(pb[:, :], lhsT=oTt[p][:, h, :], rhs=wo[:, h, n * 512:(n + 1) * 512], start=(h == 0), stop=(h == 7)),
                         reads=["oTt%d" % p, "wo"], writes=[kp], sig=(h == 7))
                S.op("dve", lambda e, pb=pb, p=p, n=n: e.tensor_tensor(out=yt[p][:, n * 512:(n + 1) * 512], in0=pb[:, :], in1=cx.G[:, n * 512:(n + 1) * 512], op=ALU.mult),
                     reads=["mod2"], writes=["yo%d" % p, kp])
            S.op("pool", lambda e, p=p: e.tensor_tensor(out=yt[p][:], in0=yt[p][:], in1=xt[p][:], op=ALU.add), reads=["yo%d" % p, "xo%d" % p], writes=["yo%d" % p])
            S.dma("sp", xo_d[t * 128:(t + 1) * 128, :], yt[p][:], reads=["yo%d" % p], writes=["xo_d"])
        S.barrier()


def new_ctx(nc, stack):
    cx = Ctx()
    cx.nc = nc
    cx.stack = stack
    cx.S = Sched(nc, stack)
    cx.ps = [stack.enter_context(nc.psum_tensor("psb%d" % i, [128, 512], F32)) for i in range(8)]
    cx.consts = T(cx, "consts", [128, 1024])
    cx.ident = cx.consts[:, 0:128]
    cx.mask01 = cx.consts[:, 128:256]
    cx.maskneg = cx.consts[:, 256:384]
    cx.invf = cx.consts[:, 384:416]
    cx.SH = T(cx, "SH", [128, 1024])
    cx.A = T(cx, "A", [128, 1024])
    cx.G = T(cx, "G", [128, 1024])
    return cx


def dram_in(nc, name, shape, dt=F32):
    return nc.dram_tensor(name, list(shape), dt, kind="ExternalInput").ap()


def build_attn_prog():
    nc = bass.Bass("TRN2", target_bir_lowering=False)
    x_d = dram_in(nc, "x", [S_LEN, D])
    c_d = dram_in(nc, "c", [1, D])
    pos_d = dram_in(nc, "pos", [1, S_LEN], I32)
    adaw_d = dram_in(nc, "adaw", [D, 6 * D])
    adab_d = dram_in(nc, "adab", [1, 6 * D])
    mixn_d = dram_in(nc, "mixn", [1, D])
    win_d = dram_in(nc, "win", [D, 3 * D])
    wout_d = dram_in(nc, "wout", [D, D])
    qn_d = dram_in(nc, "qn", [1, 64])
    kn_d = dram_in(nc, "kn", [1, 64])
    lam_ds = [dram_in(nc, "lam%d" % i, [1, 64]) for i in range(4)]
    subn_d = dram_in(nc, "subn", [1, 128])
    consts_d = dram_in(nc, "consts", [128, 1024])
    xo_d = nc.dram_tensor("xo", [S_LEN, D], F32, kind="ExternalOutput").ap()
    oT_d = nc.dram_tensor("oT_d", [8, 128, S_LEN], BF16, kind="Internal").ap()
    with ExitStack() as stack:
        cx = new_ctx(nc, stack)
        block = stack.enter_context(nc.Block())
        cx.S.dma("sp", cx.consts[:], consts_d, writes=["consts"])
        phase_attn(cx, x_d, xo_d, c_d, pos_d, adaw_d, adab_d, mixn_d, win_d, wout_d, qn_d, kn_d, lam_ds, subn_d, oT_d)
        print("instructions", cx.S.nins, "waits", cx.S.nwait)
        cx.S.emit(block)
    return nc


def phase_moe(cx, x_d, xo_d, c_d, adaw_d, adab_d, ffnn_d, wr_d, br_d, wgu_d, bgu_d, wd_d, bd_d, n_exp=32):
    S, nc = cx.S, cx.nc
    common_setup(cx, 1, c_d, adaw_d, adab_d, ffnn_d)
    QT_ = 1024
    NQ = S_LEN // QT_
    with ExitStack() as st2:
        def TT(name, shape, dt=F32):
            return st2.enter_context(nc.sbuf_tensor(uname(name), shape, dt))
        biasT = TT("biasT", [128, 16, 32])
        with ExitStack() as st3:
            bgr = st3.enter_context(nc.sbuf_tensor(uname("bgr"), [32, 2048], F32))
            S.dma("pool", bgr[:], bgu_d, writes=["bgr"])
            bgr3 = bgr[:].rearrange("e (f two) -> e f two", two=2)
            for j in range(16):
                src = bgr3[:, (j % 8) * 128:(j % 8 + 1) * 128, j // 8]
                S.op("pe", lambda e, j=j, src=src: e.transpose(cx.ps[j // 8][:, (j % 8) * 32:(j % 8) * 32 + 32], src, cx.ident[0:32, 0:32]), reads=["bgr", "consts"], writes=["ps%d" % (j // 8)])
            for g in range(2):
                S.op("dve", lambda e, g=g: e.tensor_copy(out=biasT[:, g * 8:(g + 1) * 8, :], in_=cx.ps[g][:, 0:256].rearrange("p (a b) -> p a b", a=8)), reads=[], writes=["biasT", "ps%d" % g])
            S.barrier()
        hT = TT("hTm", [128, 8, QT_], BF16)
        hTf = TT("hTf", [128, 8, 128])
        acc = TT("acc", [128, 8, 1024])
        wr = TT("wr", [128, 8, 32])
        brb = TT("brb", [128, 32])
        bd = TT("bd", [32, 1024])
        gates = TT("gates", [128, 8, 32])
        gT = TT("gT", [32, 128])
        rt = TT("rt", [128, 96])
        r8 = TT("r8", [128, 16])
        wgu = [TT("wgu%d" % i, [128, 8, 1024], BF16) for i in range(2)]
        wdb = [TT("wdb%d" % i, [128, 4, 1024], BF16) for i in range(2)]
        NSTG = 4
        stg = [TT("stg%d" % i, [128, 1024]) for i in range(NSTG)]
        actT = TT("actT", [128, 4, 512], BF16)
        gt = [TT("gt%d" % i, [128, 512]) for i in range(2)]
        sg = [TT("sg%d" % i, [128, 512]) for i in range(2)]
        lt = [TT("lt%d" % i, [128, 512]) for i in range(2)]
        cx.xt = [TT("xt%d" % i, [128, 1024]) for i in range(2)]
        cx.hh = [TT("hh%d" % i, [128, 1024]) for i in range(2)]
        cx.jk = TT("jk", [128, 1024])
        cx.nss = [TT("nss%d" % i, [128, 4]) for i in range(2)]
        cx.ncnt = 0
        S.dma("pool", wr[:], wr_d.rearrange("(kc p) e -> p kc e", p=128), writes=["wr"])
        S.dma("pool", brb[:], br_d.partition_broadcast(128), writes=["brb"])
        S.dma("pool", bd[:], bd_d, writes=["bd"])
        NU = n_exp * 2

        def load_dma(seq, s_):
            u = seq % NU
            ex_, hf = u // 2, u % 2
            sb = (seq * 12 + s_) % NSTG
            ks = "stg%d" % sb
            if s_ < 8:
                S.dma("sp", stg[sb][:], wgu_d[ex_, s_ * 128:(s_ + 1) * 128, hf * 1024:(hf + 1) * 1024], writes=[ks])
            else:
                fc = s_ - 8
                S.dma("sp", stg[sb][:], wd_d[ex_, hf * 512 + fc * 128:hf * 512 + (fc + 1) * 128, :], writes=[ks])

        def load_cast(seq, s_):
            wb2 = seq % 2
            sb = (seq * 12 + s_) % NSTG
            ks = "stg%d" % sb
            if s_ < 8:
                s3 = stg[sb][:].rearrange("p (f two) -> p f two", two=2)
                S.op("pool", lambda e: e.tensor_copy(out=wgu[wb2][:, s_, 0:512], in_=s3[:, :, 0]), reads=[ks], writes=["wgu%d" % wb2])
                S.op("pool", lambda e: e.tensor_copy(out=wgu[wb2][:, s_, 512:1024], in_=s3[:, :, 1]), reads=[ks], writes=["wgu%d" % wb2])
            else:
                fc = s_ - 8
                S.op("pool", lambda e: e.tensor_copy(out=wdb[wb2][:, fc, :], in_=stg[sb][:]), reads=[ks], writes=["wdb%d" % wb2])

        for qtr in range(NQ):
            for tl in range(8):
                t = qtr * 8 + tl
                norm_tile(cx, t, x_d, hT, tl * 128, want_f32T=hTf)
                for kc in range(8):
                    S.op("pe", lambda e, kc=kc: e.matmul(cx.ps[5][:, 0:32], lhsT=hTf[:, kc, :], rhs=wr[:, kc, :], start=(kc == 0), stop=(kc == 7)), reads=["hTf", "wr"], writes=["ps5"], sig=(kc == 7))
                S.op("dve", lambda e: e.tensor_tensor(out=rt[:, 0:32], in0=cx.ps[5][:, 0:32], in1=brb[:], op=ALU.add), reads=["brb"], writes=["rt", "ps5"])
                S.op("dve", lambda e: e.max(out=r8[:, 0:8], in_=rt[:, 0:32]), reads=["rt"], writes=["r8"])
                S.op("dve", lambda e: e.tensor_scalar(out=rt[:, 32:64], in0=rt[:, 0:32], scalar1=r8[:, 3:4], scalar2=None, op0=ALU.is_ge), reads=["rt", "r8"], writes=["rt"])
                S.op("dve", lambda e: e.tensor_scalar(out=r8[:, 8:9], in0=r8[:, 0:1], scalar1=-1.0, scalar2=None, op0=ALU.mult), reads=["r8"], writes=["r8"])
                S.op("act", lambda e: e.activation(out=rt[:, 64:96], in_=rt[:, 0:32], func=AF.Exp, bias=r8[:, 8:9]), reads=["rt", "r8"], writes=["rt"])
                S.op("dve", lambda e: e.tensor_tensor(out=rt[:, 64:96], in0=rt[:, 64:96], in1=rt[:, 32:64], op=ALU.mult), reads=["rt"], writes=["rt"])
                S.op("dve", lambda e: e.reduce_sum(out=r8[:, 9:10], in_=rt[:, 64:96], axis=AX.X), reads=["rt"], writes=["r8"])
                S.op("dve", lambda e: e.reciprocal(out=r8[:, 10:11], in_=r8[:, 9:10]), reads=["r8"], writes=["r8"])
                S.op("dve", lambda e, tl=tl: e.tensor_scalar(out=gates[:, tl, :], in0=rt[:, 64:96], scalar1=r8[:, 10:11], scalar2=None, op0=ALU.mult), reads=["rt", "r8"], writes=["gates"])
                S.op("pe", lambda e, tl=tl: e.transpose(cx.ps[5][0:32, 128:256], gates[:, tl, :], cx.ident[:]), reads=["gates", "consts"], writes=["ps5"])
                S.op("dve", lambda e: e.tensor_copy(out=gT[:], in_=cx.ps[5][0:32, 128:256]), reads=[], writes=["gT", "ps5"])
                for n in range(2):
                    S.op("pe", lambda e, n=n: e.matmul(cx.ps[4][:, :], lhsT=gT[:], rhs=bd[:, n * 512:(n + 1) * 512], start=True, stop=True), reads=["gT", "bd"], writes=["ps4"])
                    S.op("act", lambda e, tl=tl, n=n: e.activation(out=acc[:, tl, n * 512:(n + 1) * 512], in_=cx.ps[4][:, :], func=AF.Copy), reads=[], writes=[("acc", tl), "ps4"])
            for u in range(NU):
                ex, hf = u // 2, u % 2
                seq = qtr * NU + u
                wb_ = seq % 2
                if seq == 0:
                    for s_ in range(12):
                        load_dma(0, s_)
                        load_cast(0, s_)
                nxt = seq + 1 if seq + 1 < NQ * NU else None
                slot = [0]

                def tick(nxt=nxt, slot=slot):
                    s_ = slot[0]
                    slot[0] += 1
                    if nxt is None:
                        return
                    if s_ < 12:
                        load_dma(nxt, s_)
                    if 2 <= s_ < 14:
                        load_cast(nxt, s_ - 2)
                for tb in range(QT_ // 512):
                    for j in range(4):
                        p = j % 2
                        jj = hf * 4 + j
                        psg, psl = cx.ps[2 * p], cx.ps[2 * p + 1]
                        kg, kl = "ps%d" % (2 * p), "ps%d" % (2 * p + 1)
                        for (pp, kk, off) in ((psg, kg, 0), (psl, kl, 512)):
                            for kc in range(8):
                                S.op("pe", lambda e, pp=pp, kc=kc, off=off, j=j, tb=tb, wb_=wb_: e.matmul(pp[:, :], lhsT=wgu[wb_][:, kc, off + j * 128:off + (j + 1) * 128], rhs=hT[:, kc, tb * 512:(tb + 1) * 512], start=(kc == 0), stop=(kc == 7)),
                                     reads=["wgu%d" % wb_] + [("hT", tb * 4 + q_) for q_ in range(4)], writes=[kk], sig=(kc == 7))
                        S.op("dve", lambda e, p=p, psg=psg, jj=jj, ex=ex: e.tensor_scalar(out=gt[p][:], in0=psg[:, :], scalar1=biasT[:, jj, ex:ex + 1], scalar2=7.0, op0=ALU.add, op1=ALU.min), reads=["biasT"], writes=["gt%d" % p, kg])
                        S.op("act", lambda e, p=p: e.activation(out=sg[p][:], in_=gt[p][:], func=AF.Sigmoid, scale=1.702), reads=["gt%d" % p], writes=["sg%d" % p])
                        S.op("dve", lambda e, p=p, psl=psl, jj=jj, ex=ex: e.tensor_scalar(out=lt[p][:], in0=psl[:, :], scalar1=biasT[:, 8 + jj, ex:ex + 1], scalar2=7.0, op0=ALU.add, op1=ALU.min), reads=["biasT"], writes=["lt%d" % p, kl])
                        S.op("pool", lambda e, p=p: e.tensor_scalar(out=lt[p][:], in0=lt[p][:], scalar1=-7.0, scalar2=1.0, op0=ALU.max, op1=ALU.add), reads=["lt%d" % p], writes=["lt%d" % p])
                        S.op("pool", lambda e, p=p: e.tensor_tensor(out=gt[p][:], in0=gt[p][:], in1=sg[p][:], op=ALU.mult), reads=["gt%d" % p, "sg%d" % p], writes=["gt%d" % p])
                        S.op("dve", lambda e, p=p, j=j: e.tensor_tensor(out=actT[:, j, :], in0=gt[p][:], in1=lt[p][:], op=ALU.mult), reads=["gt%d" % p, "lt%d" % p], writes=[("actT", j)])
                        tick()
                    for tt in range(4):
                        tl = tb * 4 + tt
                        for n in range(2):
                            pb = cx.ps[4 + n]
                            kp = "ps%d" % (4 + n)
                            for fc in range(4):
                                S.op("pe", lambda e, pb=pb, fc=fc, tt=tt, n=n, wb_=wb_: e.matmul(pb[:, :], lhsT=actT[:, fc, tt * 128:(tt + 1) * 128], rhs=wdb[wb_][:, fc, n * 512:(n + 1) * 512], start=(fc == 0), stop=(fc == 3)),
                                     reads=[("actT", fc), "wdb%d" % wb_], writes=[kp], sig=(fc == 3))
                            S.op("dve", lambda e, pb=pb, tl=tl, n=n, ex=ex: e.scalar_tensor_tensor(out=acc[:, tl, n * 512:(n + 1) * 512], in0=pb[:, :], scalar=gates[:, tl, ex:ex + 1], in1=acc[:, tl, n * 512:(n + 1) * 512], op0=ALU.mult, op1=ALU.add),
                                 reads=["gates"], writes=[("acc", tl), kp])
                        tick()
            for tl in range(8):
                t = qtr * 8 + tl
                p = tl % 2
                xtp = cx.xt[p]
                S.dma("sp", xtp[:], x_d[t * 128:(t + 1) * 128, :], writes=["xt%d" % p])
                S.op("dve", lambda e, tl=tl: e.tensor_tensor(out=acc[:, tl, :], in0=acc[:, tl, :], in1=cx.G[:], op=ALU.mult), reads=["mod2"], writes=[("acc", tl)])
                S.op("pool", lambda e, tl=tl, xtp=xtp: e.tensor_tensor(out=acc[:, tl, :], in0=acc[:, tl, :], in1=xtp[:], op=ALU.add), reads=["xt%d" % p], writes=[("acc", tl)])
                S.dma("sp", xo_d[t * 128:(t + 1) * 128, :], acc[:, tl, :], reads=[("acc", tl)], writes=["xo_d"])
        S.barrier()


def build_moe_prog(n_exp=32):
    nc = bass.Bass("TRN2", target_bir_lowering=False)
    x_d = dram_in(nc, "x", [S_LEN, D])
    c_d = dram_in(nc, "c", [1, D])
    adaw_d = dram_in(nc, "adaw", [D, 6 * D])
    adab_d = dram_in(nc, "adab", [1, 6 * D])
    ffnn_d = dram_in(nc, "ffnn", [1, D])
    wr_d = dram_in(nc, "wr", [D, 32])
    br_d = dram_in(nc, "br", [1, 32])
    wgu_d = dram_in(nc, "wgu", [32, D, 2 * D])
    bgu_d = dram_in(nc, "bgu", [32, 2 * D])
    wd_d = dram_in(nc, "wd", [32, D, D])
    bd_d = dram_in(nc, "bd", [32, D])
    consts_d = dram_in(nc, "consts", [128, 1024])
    xo_d = nc.dram_tensor("xo", [S_LEN, D], F32, kind="ExternalOutput").ap()
    with ExitStack() as stack:
        cx = new_ctx(nc, stack)
        block = stack.enter_context(nc.Block())
        cx.S.dma("sp", cx.consts[:], consts_d, writes=["consts"])
        phase_moe(cx, x_d, xo_d, c_d, adaw_d, adab_d, ffnn_d, wr_d, br_d, wgu_d, bgu_d, wd_d, bd_d, n_exp=n_exp)
        print("instructions", cx.S.nins, "waits", cx.S.nwait)
        cx.S.emit(block)
    return nc


def phase_mlstm(cx, x_d, xo_d, c_d, adaw_d, adab_d, mixn_d, win_d, bi_d, bf_d, outn_d, wout_d, oT_d, sel_d):
    S, nc = cx.S, cx.nc
    common_setup(cx, 0, c_d, adaw_d, adab_d, mixn_d)
    hT = T(cx, "hT", [128, 8, S_LEN], BF16)
    OG = T(cx, "OG", [128, 1024])
    S.dma("pool", OG[:], outn_d.partition_broadcast(128), writes=["OG"])
    cx.sel3 = T(cx, "sel3", [128, 8, 128], BF16)
    cx.r3 = T(cx, "r3", [32, S_LEN], BF16)
    cx.ucol = T(cx, "ucol", [128, 256])
    emcol = T(cx, "emcol", [128, 256])
    with ExitStack() as st2:
        cx.xt = [st2.enter_context(nc.sbuf_tensor(uname("xt%d" % i), [128, 1024], F32)) for i in range(2)]
        cx.hh = [st2.enter_context(nc.sbuf_tensor(uname("hh%d" % i), [128, 1024], F32)) for i in range(2)]
        cx.jk = st2.enter_context(nc.sbuf_tensor(uname("jk"), [128, 1024], F32))
        cx.nss = [st2.enter_context(nc.sbuf_tensor(uname("nss%d" % i), [128, 4], F32)) for i in range(2)]
        cx.ncnt = 0
        jk_ = cx.jk
        sel3_ = cx.sel3
        S.dma("pool", jk_[:], sel_d, writes=["jk"])
        S.op("dve", lambda e: e.tensor_copy(out=sel3_[:].rearrange("p a b -> p (a b)"), in_=jk_[:]), reads=["jk"], writes=["sel3"])
        for t in range(NT):
            norm_tile(cx, t, x_d, hT, t * 128)
        S.barrier()
    with ExitStack() as st2:
        def TT(name, shape, dt=F32):
            return st2.enter_context(nc.sbuf_tensor(uname(name), shape, dt))
        wif = TT("wif", [128, 8, 16])
        wifb = TT("wifb", [128, 8, 16], BF16)
        bcol = TT("bcol", [8, 4])
        gi_t = TT("gi", [32, S_LEN])
        gf = TT("gf", [8, S_LEN])
        Ft_t = TT("Ft", [32, S_LEN])
        S.op("pool", lambda e: e.memset(gi_t[:], 0.0), writes=["g0"])
        S.op("pool", lambda e: e.memset(Ft_t[:], 0.0), writes=["Ft"])
        S.op("pool", lambda e: e.memset(cx.r3[:], 0.0), writes=["r3"])
        gi = gi_t[0:8, :]
        Ft = Ft_t[0:8, :]
        ones = TT("ones", [8, S_LEN])
        cm = TT("cm", [8, S_LEN])
        rb = [TT("rb%d" % i, [8, S_LEN], BF16) for i in range(3)]
        S.dma("sp", wif[:], win_d[:, 3072:3088].rearrange("(kc p) j -> p kc j", p=128), writes=["wif"])
        S.op("dve", lambda e: e.tensor_copy(out=wifb[:], in_=wif[:]), reads=["wif"], writes=["wifb"])
        S.dma("sp", bcol[:, 0:1], bi_d.rearrange("o (p a) -> (o p) a", a=1), writes=["bcol"])
        S.dma("sp", bcol[:, 1:2], bf_d.rearrange("o (p a) -> (o p) a", a=1), writes=["bcol"])
        S.op("dve", lambda e: e.tensor_scalar(out=bcol[:, 2:4], in0=bcol[:, 0:2], scalar1=1.0 / 15, scalar2=None, op0=ALU.mult), reads=["bcol"], writes=["bcol"])
        S.op("pool", lambda e: e.memset(ones[:], 1.0), writes=["ones"])
        for blk in range(8):
            for g in range(2):
                for kc in range(8):
                    S.op("pe", lambda e, g=g, kc=kc, blk=blk: e.matmul(cx.ps[g][0:8, :], lhsT=wifb[:, kc, g * 8:(g + 1) * 8], rhs=hT[:, kc, blk * 512:(blk + 1) * 512], start=(kc == 0), stop=(kc == 7)),
                         reads=["wifb"] + [("hT", blk * 4 + q_) for q_ in range(4)], writes=["ps%d" % g], sig=(kc == 7))
                dst = gi if g == 0 else gf
                S.op("act", lambda e, g=g, blk=blk, dst=dst: e.activation(out=dst[:, blk * 512:(blk + 1) * 512], in_=cx.ps[g][0:8, :], func=AF.Tanh, scale=1.0 / 15, bias=bcol[:, 2 + g:3 + g]),
                     reads=["bcol"], writes=["g%d" % g, "ps%d" % g])
        S.op("dve", lambda e: e.tensor_scalar(out=gi[:], in0=gi[:], scalar1=15.0, scalar2=None, op0=ALU.mult), reads=["g0"], writes=["g0"])
        S.op("act", lambda e: e.activation(out=gf[:], in_=gf[:], func=AF.Exp, scale=-15.0), reads=["g1"], writes=["g1"])
        S.op("act", lambda e: e.activation(out=gf[:], in_=gf[:], func=AF.Ln, bias=1.0), reads=["g1"], writes=["g1"])
        S.op("dve", lambda e: e.tensor_tensor_scan(out=Ft[:], data0=ones[:], data1=gf[:], initial=0.0, op0=ALU.mult, op1=ALU.subtract), reads=["ones", "g1"], writes=["Ft"])
        S.op("dve", lambda e: e.tensor_tensor(out=gi[:], in0=gi[:], in1=Ft[:], op=ALU.subtract), reads=["g0", "Ft"], writes=["g0"])
        S.op("dve", lambda e: e.tensor_tensor_scan(out=cm[:], data0=ones[:], data1=gi[:], initial=0.0, op0=ALU.mult, op1=ALU.max), reads=["ones", "g0"], writes=["cm"])
        S.op("dve", lambda e: e.tensor_tensor(out=Ft[:], in0=Ft[:], in1=cm[:], op=ALU.add), reads=["Ft", "cm"], writes=["Ft"])
        S.op("act", lambda e: e.activation(out=Ft[:], in_=Ft[:], func=AF.Exp, scale=-1.0), reads=["Ft"], writes=["Ft"])
        S.op("dve", lambda e: e.tensor_scalar(out=cm[:], in0=cm[:], scalar1=-1.0, scalar2=None, op0=ALU.mult), reads=["cm"], writes=["cm"])
        for j in range(3):
            S.op("dve", lambda e, j=j: e.tensor_copy(out=rb[j][:], in_=cm[:]), reads=["cm"], writes=["rb%d" % j])
            if j < 2:
                S.op("dve", lambda e, j=j: e.tensor_tensor(out=cm[:], in0=cm[:], in1=rb[j][:], op=ALU.subtract), reads=["cm", "rb%d" % j], writes=["cm"])
            S.dma("sp", cx.r3[8 * j:8 * j + 8, :], rb[j][:], reads=["rb%d" % j], writes=["r3"])
        for which, src, dst, key in ((0, gi, cx.ucol, "ucol"), (1, Ft, emcol, "emcol")):
            for t in range(NT):
                S.op("pe", lambda e, which=which, src=src, t=t: e.transpose(cx.ps[2 + which][:, t * 8:(t + 1) * 8], src[:, t * 128:(t + 1) * 128], cx.ident[0:8, 0:8]),
                     reads=["g0" if which == 0 else "Ft", "consts"], writes=["ps%d" % (2 + which)])
            S.op("dve", lambda e, which=which, dst=dst: e.tensor_copy(out=dst[:], in_=cx.ps[2 + which][:, 0:256]), reads=[], writes=[key, "ps%d" % (2 + which)])
        dbg = getattr(cx, "dbg", None)
        if dbg:
            S.dma("sp", dbg["ucol"], cx.ucol[:], reads=["ucol"], writes=["dbg1"])
            S.dma("sp", dbg["emcol"], emcol[:], reads=["emcol"], writes=["dbg2"])
            S.dma("sp", dbg["r3"], cx.r3[:], reads=["r3"], writes=["dbg3"])
        S.barrier()
    with ExitStack() as st2:
        def TT(name, shape, dt=F32):
            return st2.enter_context(nc.sbuf_tensor(uname(name), shape, dt))
        wst = TT("wst", [128, 8, 384])
        wb = [TT("wb%d" % i, [128, 8, 384], BF16) for i in range(2)]
        QKT = [TT("QKT%d" % i, [128, 2, S_LEN], BF16) for i in range(2)]
        V = [TT("V%d" % i, [128, NT, 132], BF16) for i in range(2)]
        og = [TT("og0", [128, NT, 128], BF16)] * 2
        cx.PT = [TT("PT%d" % i, [128, 512], BF16) for i in range(2)]
        cx.DT = [TT("DT%d" % i, [128, 512]) for i in range(2)]
        if getattr(cx, "dbg", None) is not None:
            cx.dbgt = TT("dbgt", [128, 512])
        qk = [TT("qk%d" % i, [128, 128]) for i in range(2)]
        ot = [TT("ot%d" % i, [128, 128]) for i in range(2)]
        ojk = TT("ojk", [128, 128])
        osb = [TT("osb%d" % i, [128, 132]) for i in range(2)]
        oss = [TT("oss%d" % i, [128, 8]) for i in range(2)]
        oTb = [TT("oTb0", [128, S_LEN], BF16)] * 2
        for i in range(2):
            S.op("pool", lambda e, i=i: e.memset(V[i][:, :, 128:129], 1.0), writes=[("V", i)])
        cx.stcnt = 0
        fcnt = [0]
        for h in range(8):
            hb = h % 2
            load_cast_w(cx, wb[hb], wst, "wst", "wb%d" % hb,
                        [(win_d[:, h * 64:(h + 1) * 64], 0, 64), (win_d[:, 512 + h * 64:512 + (h + 1) * 64], 64, 64),
                         (win_d[:, 1024 + h * 128:1024 + (h + 1) * 128], 128, 128), (win_d[:, 2048 + h * 128:2048 + (h + 1) * 128], 256, 128)], q="pool", cast_eng="pool")
            for t in range(NT):
                p = t % 2
                P1 = cx.ps[4 + p]
                kp = "ps%d" % (4 + p)
                for kc in range(8):
                    S.op("pe", lambda e, P1=P1, kc=kc, t=t, hb=hb: e.matmul(P1[:, 0:384], lhsT=hT[:, kc, t * 128:(t + 1) * 128], rhs=wb[hb][:, kc, :], start=(kc == 0), stop=(kc == 7)),
                         reads=[("hT", t), "wb%d" % hb], writes=[kp], sig=(kc == 7))
                S.op("dve", lambda e, P1=P1, p=p: e.tensor_copy(out=qk[p][:], in_=P1[:, 0:128]), reads=[], writes=["qk%d" % p, kp])
                S.op("dve", lambda e, P1=P1, t=t, hb=hb: e.tensor_copy(out=V[hb][:, t, 0:128], in_=P1[:, 128:256]), reads=[], writes=[("V", hb), kp])
                S.op("act", lambda e, P1=P1, t=t, hb=hb: e.activation(out=og[hb][:, t, :], in_=P1[:, 256:384], func=AF.Sigmoid), reads=[], writes=[("og", 0), kp])
                for j in range(2):
                    S.op("pe", lambda e, j=j, p=p: e.transpose(cx.ps[6][0:64, j * 128:(j + 1) * 128], qk[p][:, j * 64:(j + 1) * 64], cx.ident[:]), reads=["qk%d" % p, "consts"], writes=["ps6"], sig=(j == 1))
                S.op("act", lambda e, t=t, hb=hb: e.activation(out=QKT[hb][0:64, :, t * 128:(t + 1) * 128], in_=cx.ps[6][0:64, 0:256].rearrange("p (a b) -> p a b", a=2), func=AF.Copy),
                     reads=[], writes=[("QK", hb), "ps6"])

            def finish(Tq, c, oap_ps, okey, h=h, hb=hb):
                f = fcnt[0] % 2
                fcnt[0] += 1
                kos, kot, kob = "oss%d" % f, "ot%d" % f, "osb%d" % f
                oap = osb[f]
                if f == 0:
                    S.op("act", lambda e: e.activation(out=oap[:, 0:129], in_=oap_ps, func=AF.Copy), reads=[], writes=[kob, okey])
                else:
                    S.op("dve", lambda e: e.tensor_copy(out=oap[:, 0:129], in_=oap_ps), reads=[], writes=[kob, okey])
                S.op("act", lambda e: e.activation(out=oss[f][:, 0:1], in_=oap[:, 128:129], func=AF.Abs), reads=[kob], writes=[kos])
                S.op("dve", lambda e: e.tensor_tensor(out=oss[f][:, 0:1], in0=oss[f][:, 0:1], in1=emcol[:, Tq * 8 + h:Tq * 8 + h + 1], op=ALU.max), reads=[kos, "emcol"], writes=[kos])
                S.op("dve", lambda e: e.reciprocal(out=oss[f][:, 1:2], in_=oss[f][:, 0:1]), reads=[kos], writes=[kos])
                S.op("act", lambda e: e.activation(out=ot[f][:], in_=oap[:, 0:128], func=AF.Copy, scale=oss[f][:, 1:2]), reads=[kob, kos], writes=[kot])
                dbg = getattr(cx, "dbg", None)
                dump = dbg is not None and h == 0 and Tq == 12
                if dump:
                    S.dma("sp", dbg["osb"], oap[:, 0:132], reads=[kob], writes=["dbg4"])
                    S.dma("sp", dbg["ot_a"], ot[f][:], reads=[kot], writes=["dbg5"])
                S.op("act", lambda e: e.activation(out=ojk[:], in_=ot[f][:], func=AF.Square, accum_out=oss[f][:, 2:3]), reads=[kot], writes=["ojk", kos])
                S.op("dve", lambda e: e.tensor_scalar(out=oss[f][:, 3:4], in0=oss[f][:, 2:3], scalar1=1.0 / 128, scalar2=EPS, op0=ALU.mult, op1=ALU.add), reads=[kos], writes=[kos])
                S.op("act", lambda e: e.activation(out=oss[f][:, 3:4], in_=oss[f][:, 3:4], func=AF.Sqrt), reads=[kos], writes=[kos])
                S.op("dve", lambda e: e.reciprocal(out=oss[f][:, 4:5], in_=oss[f][:, 3:4]), reads=[kos], writes=[kos])
                S.op("dve", lambda e: e.scalar_tensor_tensor(out=ot[f][:], in0=ot[f][:], scalar=oss[f][:, 4:5], in1=OG[:, h * 128:(h + 1) * 128], op0=ALU.mult, op1=ALU.mult), reads=[kot, kos, "OG"], writes=[kot])
                if dump:
                    S.dma("sp", dbg["ot_b"], ot[f][:], reads=[kot], writes=["dbg6"])
                    S.dma("sp", dbg["oss"], oss[f][:], reads=[kos], writes=["dbg7"])
                    S.dma("sp", dbg["og"], og[hb][:, Tq, :], reads=[("og", 0)], writes=["dbg8"])
                S.op("pool", lambda e: e.tensor_tensor(out=ot[f][:], in0=ot[f][:], in1=og[hb][:, Tq, :], op=ALU.mult), reads=[kot, ("og", 0)], writes=[kot])
                if dump:
                    S.dma("sp", dbg["ot_c"], ot[f][:], reads=[kot], writes=["dbg9"])
                S.op("pe", lambda e: e.transpose(cx.ps[7][:, 0:128], ot[f][:], cx.ident[:]), reads=[kot, "consts"], writes=["ps7"])
                S.op("act", lambda e: e.activation(out=oTb[hb][:, Tq * 128:(Tq + 1) * 128], in_=cx.ps[7][:, 0:128], func=AF.Copy), reads=[], writes=[("oTb", 0), "ps7"])

            flash_head(cx, h, "ml", QKT[hb][:, 0, :], QKT[hb][:, 1, :], V[hb], finish)
            S.dma("sp", oT_d[h], oTb[hb][:], reads=[("oTb", 0)], writes=["oT_d"])
        S.barrier()
    out_proj(cx, x_d, xo_d, wout_d, oT_d)


def build_ml_prog():
    nc = bass.Bass("TRN2", target_bir_lowering=False)
    x_d = dram_in(nc, "x", [S_LEN, D])
    c_d = dram_in(nc, "c", [1, D])
    adaw_d = dram_in(nc, "adaw", [D, 6 * D])
    adab_d = dram_in(nc, "adab", [1, 6 * D])
    mixn_d = dram_in(nc, "mixn", [1, D])
    win_d = dram_in(nc, "win", [D, 3088])
    bi_d = dram_in(nc, "bi", [1, 8])
    bf_d = dram_in(nc, "bf", [1, 8])
    outn_d = dram_in(nc, "outn", [1, D])
    wout_d = dram_in(nc, "wout", [D, D])
    consts_d = dram_in(nc, "consts", [128, 1024])
    sel_d = dram_in(nc, "sel", [128, 1024])
    xo_d = nc.dram_tensor("xo", [S_LEN, D], F32, kind="ExternalOutput").ap()
    oT_d = nc.dram_tensor("oT_d", [8, 128, S_LEN], BF16, kind="Internal").ap()
    with ExitStack() as stack:
        cx = new_ctx(nc, stack)
        block = stack.enter_context(nc.Block())
        cx.S.dma("sp", cx.consts[:], consts_d, writes=["consts"])
        phase_mlstm(cx, x_d, xo_d, c_d, adaw_d, adab_d, mixn_d, win_d, bi_d, bf_d, outn_d, wout_d, oT_d, sel_d)
        print("instructions", cx.S.nins, "waits", cx.S.nwait)
        cx.S.emit(block)
    return nc


PHASES = ("attn", "moe0", "mlstm", "moe1")


def build_prog(phases, n_exp=32, sliced=False, debug=False):
    nc = bass.Bass("TRN2", target_bir_lowering=False)
    d = {}
    def inp(name, shape, dt=F32):
        d[name] = dram_in(nc, name, shape, dt)
        return d[name]
    x_d = inp("x", [S_LEN, D])
    c_d = inp("c", [1, D])
    NL = 1 if sliced else 2
    adaw_d = inp("ada_w", [NL, D, 6 * D])
    adab_d = inp("ada_b", [NL, 6 * D])
    consts_d = inp("consts", [128, 1024])
    if "attn" in phases:
        pos_d = inp("positions", [1, S_LEN], I32)
        inp("mix_norm", [NL, D])
        inp("att_w_in", [1, D, 3 * D]); inp("att_w_out", [1, D, D])
        for nm in ("att_q_norm", "att_k_norm", "att_lam_q1", "att_lam_k1", "att_lam_q2", "att_lam_k2"):
            inp(nm, [1, 64])
        inp("att_sub_norm", [1, 128])
    if "mlstm" in phases:
        if "mix_norm" not in d:
            inp("mix_norm", [NL, D])
        inp("ml_w_in", [1, D, 3088]); inp("ml_b_igate", [1, 8]); inp("ml_b_fgate", [1, 8])
        inp("ml_out_norm", [1, D]); inp("ml_w_out", [1, D, D]); inp("sel", [128, 1024])
    if "moe0" in phases or "moe1" in phases:
        inp("ffn_norm", [NL, D]); inp("router_w", [NL, D, 32]); inp("router_b", [NL, 32])
        inp("moe_w_gate_up", [NL, 32, D, 2 * D]); inp("moe_b_gate_up", [NL, 32, 2 * D])
        inp("moe_w_down", [NL, 32, D, D]); inp("moe_b_down", [NL, 32, D])
    xo_d = nc.dram_tensor("xo", [S_LEN, D], F32, kind="ExternalOutput").ap()
    oT_d = nc.dram_tensor("oT_d", [8, 128, S_LEN], BF16, kind="ExternalOutput" if debug else "Internal").ap()
    xs = [x_d]
    for i in range(len(phases) - 1):
        xs.append(nc.dram_tensor("xmid%d" % i, [S_LEN, D], F32, kind="ExternalOutput" if debug else "Internal").ap())
    xs.append(xo_d)
    with ExitStack() as stack:
        cx = new_ctx(nc, stack)
        block = stack.enter_context(nc.Block())
        if debug:
            cx.dbg = {"ucol": nc.dram_tensor("dbg_ucol", [128, 256], F32, kind="ExternalOutput").ap(),
                      "emcol": nc.dram_tensor("dbg_emcol", [128, 256], F32, kind="ExternalOutput").ap(),
                      "r3": nc.dram_tensor("dbg_r3", [32, S_LEN], BF16, kind="ExternalOutput").ap(),
                      "osb": nc.dram_tensor("dbg_osb", [128, 132], F32, kind="ExternalOutput").ap(),
                      "ot_a": nc.dram_tensor("dbg_ot_a", [128, 128], F32, kind="ExternalOutput").ap(),
                      "ot_b": nc.dram_tensor("dbg_ot_b", [128, 128], F32, kind="ExternalOutput").ap(),
                      "ot_c": nc.dram_tensor("dbg_ot_c", [128, 128], F32, kind="ExternalOutput").ap(),
                      "oss": nc.dram_tensor("dbg_oss", [128, 8], F32, kind="ExternalOutput").ap(),
                      "og": nc.dram_tensor("dbg_og", [128, 128], BF16, kind="ExternalOutput").ap(),
                      "DT0": nc.dram_tensor("dbg_DT0", [128, 512], F32, kind="ExternalOutput").ap(),
                      "DT11": nc.dram_tensor("dbg_DT11", [128, 512], F32, kind="ExternalOutput").ap(),
                      "RB0": nc.dram_tensor("dbg_RB0", [128, 512], F32, kind="ExternalOutput").ap(),
                      "RB11": nc.dram_tensor("dbg_RB11", [128, 512], F32, kind="ExternalOutput").ap(),
                      "sel3": nc.dram_tensor("dbg_sel3", [32, 128], BF16, kind="ExternalOutput").ap()}
        cx.S.dma("sp", cx.consts[:], consts_d, writes=["consts"])
        for i, ph in enumerate(phases):
            xin, xout = xs[i], xs[i + 1]
            with ExitStack() as pst:
                cx.stack = pst
                if ph == "attn":
                    phase_attn(cx, xin, xout, c_d, pos_d, adaw_d[0], adab_d[0:1, :], d["mix_norm"][0:1, :], d["att_w_in"][0], d["att_w_out"][0],
                               d["att_q_norm"], d["att_k_norm"], [d["att_lam_q1"], d["att_lam_k1"], d["att_lam_q2"], d["att_lam_k2"]], d["att_sub_norm"], oT_d)
                elif ph == "mlstm":
                    L = 0 if sliced else 1
                    phase_mlstm(cx, xin, xout, c_d, adaw_d[L], adab_d[L:L + 1, :], d["mix_norm"][L:L + 1, :], d["ml_w_in"][0], d["ml_b_igate"], d["ml_b_fgate"],
                                d["ml_out_norm"], d["ml_w_out"][0], oT_d, d["sel"])
                else:
                    L = 0 if sliced else int(ph[-1])
                    phase_moe(cx, xin, xout, c_d, adaw_d[L], adab_d[L:L + 1, :], d["ffn_norm"][L:L + 1, :], d["router_w"][L], d["router_b"][L:L + 1, :],
                              d["moe_w_gate_up"][L], d["moe_b_gate_up"][L], d["moe_w_down"][L], d["moe_b_down"][L], n_exp=n_exp)
                cx.S.barrier()
            cx.stack = stack
        cx.S.emit(block)
    return nc, list(d.keys())


LAUNCH_GROUPS = [("attn",), ("moe0",), ("mlstm",), ("moe1",)]
PHASE_LAYER = {"attn": 0, "moe0": 0, "mlstm": 1, "moe1": 1}
PER_LAYER = ("ada_w", "ada_b", "mix_norm", "ffn_norm", "router_w", "router_b", "moe_w_gate_up", "moe_b_gate_up", "moe_w_down", "moe_b_down")
FUSED = True


def kernel(**inputs):
    n = 8
    shared = {k: np.ascontiguousarray(v) for k, v in inputs.items() if k not in ("x", "c", "positions")}
    shared["consts"] = make_consts()
    shared["sel"] = make_sel()
    xcur = [np.ascontiguousarray(inputs["x"][b]) for b in range(n)]
    groups = [PHASES] if FUSED else LAUNCH_GROUPS
    progs = {}
    for grp in groups:
        sliced = not FUSED
        pkey = ("moe0",) if (sliced and grp[0].startswith("moe")) else grp
        if pkey not in progs:
            progs[pkey] = build_prog(pkey, sliced=sliced)
        nc, names = progs[pkey]
        L = PHASE_LAYER[grp[0]]
        in_maps = []
        for b in range(n):
            m = {}
            for k in names:
                if k == "x":
                    m[k] = xcur[b]
                elif k == "c":
                    m[k] = np.ascontiguousarray(inputs["c"][b:b + 1])
                elif k == "positions":
                    m[k] = np.ascontiguousarray(inputs["positions"][b:b + 1]).astype(np.int32)
                elif sliced and k in PER_LAYER:
                    m[k] = np.ascontiguousarray(shared[k][L:L + 1])
                else:
                    m[k] = shared[k]
            in_maps.append(m)
        res = run_bass_kernel_spmd(nc, in_maps, core_ids=list(range(n)))
        xcur = [np.ascontiguousarray(res.results[b]["xo"]) for b in range(n)]
    return np.stack(xcur, axis=0).astype(np.float32)
```

```python
import numpy as np
import concourse.bass as bass
import concourse.mybir as mybir
from concourse.bass_utils import run_bass_kernel_spmd

F32 = mybir.dt.float32
BF16 = mybir.dt.bfloat16
I32 = mybir.dt.int32
AF = mybir.ActivationFunctionType
ALU = mybir.AluOpType
AX = mybir.AxisListType

ENGMAP = {"pe": "tensor", "act": "scalar", "dve": "vector", "pool": "gpsimd", "sp": "sync"}


class Sched:
    def __init__(self, nc, stack, ndma=8):
        self.nc = nc
        self.prog = {k: [] for k in ENGMAP}
        self.sem = {}
        self.cnt = {k: 0 for k in ENGMAP}
        self.dsem = {}
        self.drr = {k: 0 for k in ENGMAP}
        for k in ENGMAP:
            self.sem[k] = stack.enter_context(nc.semaphore("s_" + k))
        for k in ("sp", "pool", "act"):
            self.dsem[k] = [[stack.enter_context(nc.semaphore("d_%s%d" % (k, j))), 0] for j in range(ndma)]
        self.lastw = {}
        self.readers = {}
        self.seen = {}
        self.nwait = 0
        self.nins = 0

    def _deps(self, eng, reads, writes):
        best = {}
        def add(tok):
            s, v, src = tok
            if src == eng and eng == "pe":
                return
            key = id(s)
            if key not in best or best[key][1] < v:
                best[key] = (s, v)
        for k in reads:
            if k in self.lastw:
                add(self.lastw[k])
        for k in writes:
            if k in self.lastw:
                add(self.lastw[k])
            for t in self.readers.get(k, ()):
                add(t)
        for key, (s, v) in best.items():
            sk = (eng, key)
            if self.seen.get(sk, 0) >= v:
                continue
            self.seen[sk] = v
            self.prog[eng].append(lambda e, s=s, v=v: e.wait_ge(s, v))
            self.nwait += 1

    def _commit(self, tok, reads, writes):
        for k in reads:
            self.readers.setdefault(k, []).append(tok)
        for k in writes:
            self.lastw[k] = tok
            self.readers[k] = []

    def op(self, eng, fn, reads=(), writes=(), sig=True):
        self._deps(eng, reads, writes)
        sem = self.sem[eng]
        if sig:
            self.cnt[eng] += 1
            tok = (sem, self.cnt[eng], eng)
            self.prog[eng].append(lambda e, fn=fn, sem=sem: fn(e).then_inc(sem, 1))
        else:
            tok = (sem, self.cnt[eng] + 1, eng)
            self.prog[eng].append(lambda e, fn=fn: fn(e))
        self.nins += 1
        self._commit(tok, reads, writes)
        return tok

    def dma(self, q, out, in_, reads=(), writes=(), **kw):
        self._deps(q, reads, writes)
        j = self.drr[q]
        self.drr[q] = (j + 1) % len(self.dsem[q])
        ent = self.dsem[q][j]
        sem, c = ent
        if c > 0:
            sk = (q, id(sem))
            if self.seen.get(sk, 0) < 16 * c:
                self.seen[sk] = 16 * c
                self.prog[q].append(lambda e, s=sem, v=16 * c: e.wait_ge(s, v))
        ent[1] = c + 1
        tok = (sem, 16 * (c + 1), "dma_" + q)
        self.prog[q].append(lambda e, out=out, in_=in_, sem=sem, kw=kw: e.dma_start(out=out, in_=in_, **kw).then_inc(sem, 16))
        self.nins += 1
        self._commit(tok, reads, writes)
        return tok

    def barrier(self):
        toks = []
        for k in ENGMAP:
            if self.cnt[k] > 0:
                toks.append((self.sem[k], self.cnt[k], k))
        for q in self.dsem:
            for sem, c in self.dsem[q]:
                if c > 0:
                    toks.append((sem, 16 * c, "dma"))
        for eng in ENGMAP:
            for s_, v, src in toks:
                if src == eng and eng == "pe":
                    continue
                sk = (eng, id(s_))
                if self.seen.get(sk, 0) >= v:
                    continue
                self.seen[sk] = v
                self.prog[eng].append(lambda e, s_=s_, v=v: e.wait_ge(s_, v))
        self.lastw = {}
        self.readers = {}

    def wait_all(self, eng, keys):
        self._deps(eng, list(keys), [])

    def emit(self, block):
        for k, name in ENGMAP.items():
            lst = self.prog[k]
            def body(e, lst=lst):
                for f in lst:
                    f(e)
            getattr(block, name)(body)


import math
from contextlib import ExitStack
import numpy as np

S_LEN = 4096
D = 1024
NT = S_LEN // 128
EPS = 1e-6
PI = math.pi


def make_consts():
    c = np.zeros((128, 1024), np.float32)
    c[:, 0:128] = np.eye(128, dtype=np.float32)
    s = np.arange(128)[:, None]
    l = np.arange(128)[None, :]
    c[:, 128:256] = (s <= l).astype(np.float32)
    c[:, 256:384] = np.where(s <= l, 0.0, -30000.0)
    inv = (10000.0 ** (-np.arange(0, 64, 2, dtype=np.float32) / 64)).astype(np.float32)
    c[:, 384:416] = inv[None, :]
    for h in range(8):
        for j in range(3):
            c[8 * j + h, 416 + 0:416 + 0] = 0
    return c


def make_sel():
    sel = np.zeros((128, 8, 128), np.float32)
    for h in range(8):
        for j in range(3):
            sel[8 * j + h, h, :] = 1.0
    return sel.reshape(128, 1024)


class Ctx:
    pass


_uid = [0]


def uname(name):
    _uid[0] += 1
    return "s%d_%s" % (_uid[0], name)


def T(cx, name, shape, dt=F32):
    return cx.stack.enter_context(cx.nc.sbuf_tensor(uname(name), shape, dt))


def common_setup(cx, layer_half, c_d, adaw_d, adab_d, norm_d):
    S, nc = cx.S, cx.nc
    with ExitStack() as st:
        def TT(name, shape, dt=F32):
            return st.enter_context(nc.sbuf_tensor(uname(name), shape, dt))
        rowc = TT("rowc", [128, 1024])
        crep = TT("crep", [128, 8, 128])
        stg = [TT("adst%d" % i, [128, 3072]) for i in range(2)]
        adab = TT("adab", [128, 3072])
        nrm = TT("nrmb", [128, 1024])
        c0 = layer_half * 3072
        S.dma("sp", rowc[:], c_d.partition_broadcast(128), writes=["rowc"])
        S.dma("sp", adab[:], adab_d[:, c0:c0 + 3072].partition_broadcast(128), writes=["adab"])
        S.dma("sp", nrm[:], norm_d.partition_broadcast(128), writes=["nrmb"])
        S.op("act", lambda e: e.activation(out=crep[:].rearrange("p a b -> p (a b)"), in_=rowc[:], func=AF.Sigmoid), reads=["rowc"], writes=["crep"])
        S.op("dve", lambda e: e.tensor_tensor(out=rowc[:], in0=rowc[:], in1=crep[:].rearrange("p a b -> p (a b)"), op=ALU.mult), reads=["crep", "rowc"], writes=["rowc"])
        for kc in range(8):
            b = cx.ps[kc // 4]
            S.op("pe", lambda e, kc=kc, b=b: e.transpose(b[:, (kc % 4) * 128:(kc % 4 + 1) * 128], rowc[:, kc * 128:(kc + 1) * 128], cx.ident[:]),
                 reads=["rowc", "consts"], writes=["ps%d" % (kc // 4)])
        for hb in range(2):
            S.op("dve", lambda e, hb=hb: e.tensor_copy(out=crep[:, hb * 4:(hb + 1) * 4, :].rearrange("p a b -> p (a b)"), in_=cx.ps[hb][:, :]),
                 reads=[], writes=["crep", "ps%d" % hb])
        for kc in range(8):
            sb = stg[kc % 2]
            S.dma("sp" if kc % 2 == 0 else "pool", sb[:], adaw_d[kc * 128:(kc + 1) * 128, c0:c0 + 3072], writes=["adst%d" % (kc % 2)])
            for n in range(6):
                S.op("pe", lambda e, kc=kc, n=n, sb=sb: e.matmul(cx.ps[n][:, :], lhsT=crep[:, kc, :], rhs=sb[:, n * 512:(n + 1) * 512], start=(kc == 0), stop=(kc == 7)),
                     reads=["crep", "adst%d" % (kc % 2)], writes=["ps%d" % n], sig=(kc == 7 or n == 5))
        for n in range(6):
            dst = (cx.SH, cx.A, cx.G)[n // 2]
            S.op("dve", lambda e, n=n, dst=dst: e.tensor_tensor(out=dst[:, (n % 2) * 512:(n % 2 + 1) * 512], in0=cx.ps[n][:, :], in1=adab[:, n * 512:(n + 1) * 512], op=ALU.add),
                 reads=["adab"], writes=["mod%d" % (n // 2), "ps%d" % n])
        S.op("dve", lambda e: e.scalar_tensor_tensor(out=cx.A[:], in0=cx.A[:], scalar=1.0, in1=nrm[:], op0=ALU.add, op1=ALU.mult),
             reads=["mod1", "nrmb"], writes=["mod1"])
        S.barrier()


def norm_tile(cx, t, x_d, hT, tok0, want_f32T=None):
    S, nc = cx.S, cx.nc
    i = cx.ncnt % 2
    cx.ncnt += 1
    xt, hh, jk = cx.xt[i], cx.hh[i], cx.jk
    ss = cx.nss[i]
    kx, kh, ks = "xt%d" % i, "hh%d" % i, "nss%d" % i
    S.dma("sp", xt[:], x_d[t * 128:(t + 1) * 128, :], writes=[kx])
    S.op("act", lambda e: e.activation(out=jk[:], in_=xt[:], func=AF.Square, accum_out=ss[:, 0:1]), reads=[kx], writes=["jk", ks])
    S.op("dve", lambda e: e.tensor_scalar(out=ss[:, 1:2], in0=ss[:, 0:1], scalar1=1.0 / D, scalar2=EPS, op0=ALU.mult, op1=ALU.add), reads=[ks], writes=[ks])
    S.op("act", lambda e: e.activation(out=ss[:, 2:3], in_=ss[:, 1:2], func=AF.Sqrt), reads=[ks], writes=[ks])
    S.op("dve", lambda e: e.reciprocal(out=ss[:, 3:4], in_=ss[:, 2:3]), reads=[ks], writes=[ks])
    S.op("dve", lambda e: e.scalar_tensor_tensor(out=hh[:], in0=xt[:], scalar=ss[:, 3:4], in1=cx.A[:], op0=ALU.mult, op1=ALU.mult), reads=[kx, ks, "mod1"], writes=[kh])
    S.op("dve", lambda e: e.tensor_tensor(out=hh[:], in0=hh[:], in1=cx.SH[:], op=ALU.add), reads=[kh, "mod0"], writes=[kh])
    for kc in range(8):
        b = 6 + kc // 4
        S.op("pe", lambda e, kc=kc, b=b: e.transpose(cx.ps[b][:, (kc % 4) * 128:(kc % 4 + 1) * 128], hh[:, kc * 128:(kc + 1) * 128], cx.ident[:]),
             reads=[kh, "consts"], writes=["ps%d" % b], sig=(kc % 4 == 3))
    for hb in range(2):
        eng = "act" if hb == 0 else "dve"
        outap = hT[:, hb * 4:(hb + 1) * 4, tok0:tok0 + 128]
        inap = cx.ps[6 + hb][:, :].rearrange("p (a b) -> p a b", a=4)
        if eng == "act":
            S.op("act", lambda e, o=outap, i_=inap: e.activation(out=o, in_=i_, func=AF.Copy), reads=[], writes=[("hT", tok0 // 128), "ps%d" % (6 + hb)])
        else:
            S.op("dve", lambda e, o=outap, i_=inap: e.tensor_copy(out=o, in_=i_), reads=[], writes=[("hT", tok0 // 128), "ps%d" % (6 + hb)])
        if want_f32T is not None:
            o2 = want_f32T[:, hb * 4:(hb + 1) * 4, :]
            S.op("pool" if False else "dve", lambda e, o=o2, i_=inap: e.tensor_copy(out=o, in_=i_), reads=[], writes=["hTf", "ps%d" % (6 + hb)])


def flash_head(cx, h, mode, QT, KT, V, finish_tile):
    S = cx.S
    ncomp = 2 if mode == "att" else 1
    for qb in range(8):
        for c in range(ncomp):
            pb = 0 if mode == "ml" else c * 64
            nk = 4 * qb + 4
            for kt in range(nk):
                j0 = max(0, kt - 4 * qb)
                c0 = j0 * 128
                sb = cx.stcnt % 2
                cx.stcnt += 1
                stp = cx.ps[sb]
                pt = cx.PT[sb]
                kst, kpt = "ps%d" % sb, "PT%d" % sb
                S.op("pe", lambda e, stp=stp, pb=pb, kt=kt, qb=qb, c0=c0: e.matmul(
                    stp[:, c0:512], lhsT=KT[pb:pb + 64, kt * 128:(kt + 1) * 128], rhs=QT[pb:pb + 64, qb * 512 + c0:qb * 512 + 512], start=True, stop=True),
                    reads=[("QK", h % 2)], writes=[kst])
                if mode == "att":
                    S.op("act", lambda e, stp=stp, pt=pt, c0=c0: e.activation(out=pt[:, c0:512], in_=stp[:, c0:512], func=AF.Exp, scale=0.125),
                         reads=[], writes=[kpt, kst])
                    if kt >= 4 * qb:
                        S.op("pool", lambda e, pt=pt, c0=c0: e.tensor_tensor(out=pt[:, c0:c0 + 128], in0=pt[:, c0:c0 + 128], in1=cx.mask01b[:], op=ALU.mult),
                             reads=[kpt, "consts2"], writes=[kpt])
                else:
                    rb = cx.ps[4 + sb]
                    krb, kdt = "ps%d" % (4 + sb), "DT%d" % sb
                    dt_ = cx.DT[sb]
                    S.op("pe", lambda e, rb=rb, qb=qb, c0=c0: e.matmul(rb[:, c0:512], lhsT=cx.sel3[0:24, h, :], rhs=cx.r3[0:24, qb * 512 + c0:qb * 512 + 512], start=True, stop=True),
                         reads=["r3", "sel3"], writes=[krb])
                    ucol = cx.ucol[:, kt * 8 + h:kt * 8 + h + 1]
                    if kt >= 4 * qb:
                        S.op("dve", lambda e, rb=rb, dt_=dt_, c0=c0: e.tensor_tensor(out=dt_[:, c0:c0 + 128], in0=rb[:, c0:c0 + 128], in1=cx.maskneg[:], op=ALU.add),
                             reads=["consts"], writes=[kdt, krb])
                        S.op("act", lambda e, dt_=dt_, c0=c0, ucol=ucol: e.activation(out=dt_[:, c0:c0 + 128], in_=dt_[:, c0:c0 + 128], func=AF.Exp, bias=ucol),
                             reads=[kdt, "ucol"], writes=[kdt])
                        if c0 + 128 < 512:
                            S.op("act", lambda e, rb=rb, dt_=dt_, c0=c0, ucol=ucol: e.activation(out=dt_[:, c0 + 128:512], in_=rb[:, c0 + 128:512], func=AF.Exp, bias=ucol),
                                 reads=["ucol"], writes=[kdt, krb])
                    else:
                        S.op("act", lambda e, rb=rb, dt_=dt_, ucol=ucol: e.activation(out=dt_[:, 0:512], in_=rb[:, 0:512], func=AF.Exp, bias=ucol),
                             reads=["ucol"], writes=[kdt, krb])
                    dbg = getattr(cx, "dbg", None)
                    if dbg is not None and h == 0 and qb == 3 and kt in (0, 11):
                        S.dma("sp", dbg["DT%d" % kt], dt_[:, :], reads=[kdt], writes=["dbgDT%d" % kt])
                        S.op("dve", lambda e, rb=rb: e.tensor_copy(out=cx.dbgt[:], in_=rb[:, :]), reads=[], writes=["dbgt", krb])
                        S.dma("sp", dbg["RB%d" % kt], cx.dbgt[:], reads=["dbgt"], writes=["dbgRB%d" % kt])
                        if kt == 0:
                            S.dma("sp", dbg["sel3"], cx.sel3[0:32, 0, :], reads=["sel3"], writes=["dbgsel"])
                    S.op("dve", lambda e, stp=stp, pt=pt, dt_=dt_, c0=c0: e.scalar_tensor_tensor(out=pt[:, c0:512], in0=stp[:, c0:512], scalar=0.125, in1=dt_[:, c0:512], op0=ALU.mult, op1=ALU.mult),
                         reads=[kdt], writes=[kpt, kst])
                for i in range(j0, 4):
                    ob = cx.ps[2 + i // 2]
                    oap = ob[:, (i % 2) * 256:(i % 2) * 256 + 129]
                    last = (kt == 4 * qb + i)
                    S.op("pe", lambda e, oap=oap, pt=pt, i=i, kt=kt, last=last: e.matmul(oap, lhsT=pt[:, i * 128:(i + 1) * 128], rhs=V[:, kt, 0:129], start=(kt == 0 and i % 2 == 0), stop=last, skip_group_check=True),
                         reads=[kpt, ("V", h % 2)], writes=["ps%d" % (2 + i // 2)], sig=(last or i == 3))
            for i in range(4):
                ob = cx.ps[2 + i // 2]
                oap = ob[:, (i % 2) * 256:(i % 2) * 256 + 129]
                finish_tile(4 * qb + i, c, oap, "ps%d" % (2 + i // 2))


def rstd_ops(cx, ssap, n, inv_n, key):
    S = cx.S
    S.op("dve", lambda e: e.tensor_scalar(out=ssap[:, n:2 * n], in0=ssap[:, 0:n], scalar1=inv_n, scalar2=EPS, op0=ALU.mult, op1=ALU.add), reads=[key], writes=[key])
    S.op("act", lambda e: e.activation(out=ssap[:, n:2 * n], in_=ssap[:, n:2 * n], func=AF.Sqrt), reads=[key], writes=[key])
    S.op("dve", lambda e: e.reciprocal(out=ssap[:, 2 * n:3 * n], in_=ssap[:, n:2 * n]), reads=[key], writes=[key])


def load_cast_w(cx, dst, stage, kstage, kdst, srcs, q="sp", cast_eng="pool"):
    S = cx.S
    for (ap, off, w) in srcs:
        S.dma(q, stage[:, :, off:off + w], ap.rearrange("(kc p) j -> p kc j", p=128), writes=[kstage])
    if cast_eng == "act":
        S.op("act", lambda e: e.activation(out=dst[:], in_=stage[:], func=AF.Copy), reads=[kstage], writes=[kdst])
    else:
        S.op(cast_eng, lambda e: e.tensor_copy(out=dst[:], in_=stage[:]), reads=[kstage], writes=[kdst])


def phase_attn(cx, x_d, xo_d, c_d, pos_d, adaw_d, adab_d, mixn_d, win_d, wout_d, qn_d, kn_d, lam_ds, subn_d, oT_d):
    S, nc = cx.S, cx.nc
    st = cx.stack
    lam_init = 0.2
    common_setup(cx, 0, c_d, adaw_d, adab_d, mixn_d)
    hT = T(cx, "hT", [128, 8, S_LEN], BF16)
    GQK = T(cx, "GQK", [128, 256])
    for j, d_ in enumerate((qn_d, qn_d, kn_d, kn_d)):
        S.dma("pool", GQK[:, j * 64:(j + 1) * 64], d_.partition_broadcast(128), writes=["GQK"])
    lamt = T(cx, "lamt", [128, 4, 64])
    for j, d_ in enumerate(lam_ds):
        S.dma("pool", lamt[:, j, :], d_.partition_broadcast(128), writes=["lamt"])
    lams = T(cx, "lams", [128, 8])
    S.op("dve", lambda e: e.tensor_tensor(out=lamt[:, 0, :], in0=lamt[:, 0, :], in1=lamt[:, 1, :], op=ALU.mult), reads=["lamt"], writes=["lamt"])
    S.op("dve", lambda e: e.tensor_tensor(out=lamt[:, 2, :], in0=lamt[:, 2, :], in1=lamt[:, 3, :], op=ALU.mult), reads=["lamt"], writes=["lamt"])
    S.op("dve", lambda e: e.reduce_sum(out=lams[:, 0:1], in_=lamt[:, 0, :], axis=AX.X), reads=["lamt"], writes=["lams"])
    S.op("dve", lambda e: e.reduce_sum(out=lams[:, 1:2], in_=lamt[:, 2, :], axis=AX.X), reads=["lamt"], writes=["lams"])
    S.op("act", lambda e: e.activation(out=lams[:, 2:4], in_=lams[:, 0:2], func=AF.Exp), reads=["lams"], writes=["lams"])
    S.op("dve", lambda e: e.tensor_tensor(out=lams[:, 4:5], in0=lams[:, 3:4], in1=lams[:, 2:3], op=ALU.subtract), reads=["lams"], writes=["lams"])
    S.op("dve", lambda e: e.tensor_scalar(out=lams[:, 5:6], in0=lams[:, 4:5], scalar1=-lam_init, scalar2=None, op0=ALU.add), reads=["lams"], writes=["lams"])
    neglam = lams[:, 5:6]
    subg = T(cx, "subg", [128, 2])
    S.dma("pool", subg[:, 0:1], subn_d.rearrange("o (p a) -> (o p) a", a=1), writes=["subg"])
    S.op("dve", lambda e: e.tensor_scalar(out=subg[:, 1:2], in0=subg[:, 0:1], scalar1=1.0 - lam_init, scalar2=None, op0=ALU.mult), reads=["subg"], writes=["subg"])
    cosT = T(cx, "cosT", [128, NT, 32])
    sinT = T(cx, "sinT", [128, NT, 32])
    with ExitStack() as st2:
        posi = st2.enter_context(nc.sbuf_tensor(uname("posi"), [32, 128], I32))
        posf = st2.enter_context(nc.sbuf_tensor(uname("posf"), [32, 128], F32))
        post = st2.enter_context(nc.sbuf_tensor(uname("post"), [128, 32], F32))
        ang = st2.enter_context(nc.sbuf_tensor(uname("ang"), [128, NT, 32], F32))
        tmpf = st2.enter_context(nc.sbuf_tensor(uname("tmpf"), [128, NT, 32], F32))
        tmpi = st2.enter_context(nc.sbuf_tensor(uname("tmpi"), [128, NT, 32], I32))
        S.dma("sp", posi[:], pos_d.rearrange("o (t p) -> (o t) p", p=128), writes=["posi"])
        S.op("dve", lambda e: e.tensor_copy(out=posf[:], in_=posi[:]), reads=["posi"], writes=["posf"])
        S.op("pe", lambda e: e.transpose(cx.ps[0][:, 0:32], posf[:], cx.ident[0:32, 0:32]), reads=["posf", "consts"], writes=["ps0"])
        S.op("dve", lambda e: e.tensor_copy(out=post[:], in_=cx.ps[0][:, 0:32]), reads=[], writes=["post", "ps0"])
        S.op("dve", lambda e: e.tensor_tensor(out=ang[:], in0=post[:].unsqueeze(2).to_broadcast([128, NT, 32]), in1=cx.invf[:].unsqueeze(1).to_broadcast([128, NT, 32]), op=ALU.mult),
             reads=["post", "consts"], writes=["ang"])
        for which, dstT in ((0, sinT), (1, cosT)):
            off = 0.0 if which == 0 else PI / 2
            S.op("dve", lambda e, off=off: e.tensor_scalar(out=tmpf[:], in0=ang[:], scalar1=off, scalar2=1.0 / (2 * PI), op0=ALU.add, op1=ALU.mult), reads=["ang"], writes=["tmpf"])
            S.op("dve", lambda e: e.tensor_copy(out=tmpi[:], in_=tmpf[:]), reads=["tmpf"], writes=["tmpi"])
            S.op("dve", lambda e: e.tensor_copy(out=tmpf[:], in_=tmpi[:]), reads=["tmpi"], writes=["tmpf"])
            S.op("dve", lambda e: e.scalar_tensor_tensor(out=tmpf[:], in0=tmpf[:], scalar=-2 * PI, in1=ang[:], op0=ALU.mult, op1=ALU.add), reads=["tmpf", "ang"], writes=["tmpf"])
            S.op("dve", lambda e, off=off: e.tensor_scalar(out=tmpf[:], in0=tmpf[:], scalar1=off, scalar2=PI, op0=ALU.add, op1=ALU.min), reads=["tmpf"], writes=["tmpf"])
            S.op("dve", lambda e: e.tensor_scalar(out=tmpf[:], in0=tmpf[:], scalar1=-PI, scalar2=None, op0=ALU.max), reads=["tmpf"], writes=["tmpf"])
            S.op("act", lambda e, dstT=dstT: e.activation(out=dstT[:], in_=tmpf[:], func=AF.Sin), reads=["tmpf"], writes=["rope"])
        S.barrier()
    with ExitStack() as st2:
        cx.xt = [st2.enter_context(nc.sbuf_tensor(uname("xt%d" % i), [128, 1024], F32)) for i in range(2)]
        cx.hh = [st2.enter_context(nc.sbuf_tensor(uname("hh%d" % i), [128, 1024], F32)) for i in range(2)]
        cx.jk = st2.enter_context(nc.sbuf_tensor(uname("jk"), [128, 1024], F32))
        cx.nss = [st2.enter_context(nc.sbuf_tensor(uname("nss%d" % i), [128, 4], F32)) for i in range(2)]
        cx.ncnt = 0
        for t in range(NT):
            norm_tile(cx, t, x_d, hT, t * 128)
        S.barrier()
    with ExitStack() as st2:
        def TT(name, shape, dt=F32):
            return st2.enter_context(nc.sbuf_tensor(uname(name), shape, dt))
        wst = TT("wst", [128, 8, 384])
        wb = [TT("wb%d" % i, [128, 8, 384], BF16) for i in range(2)]
        QKT = [TT("QKT%d" % i, [128, 2, S_LEN], BF16) for i in range(2)]
        V = [TT("V%d" % i, [128, NT, 132], BF16) for i in range(2)]
        cx.PT = [TT("PT%d" % i, [128, 512], BF16) for i in range(2)]
        cx.mask01b = TT("mask01b", [128, 128], BF16)
        sq = [TT("sq%d" % i, [128, 256]) for i in range(2)]
        qn = [TT("qn%d" % i, [128, 256]) for i in range(2)]
        qr = [TT("qr%d" % i, [128, 256]) for i in range(2)]
        t1 = [TT("t1_%d" % i, [128, 128]) for i in range(2)]
        t2 = [TT("t2_%d" % i, [128, 128]) for i in range(2)]
        pss = [TT("pss%d" % i, [128, 12]) for i in range(2)]
        o0 = TT("o0", [128, 4, 128])
        ot = [TT("ot%d" % i, [128, 128]) for i in range(2)]
        ojk = TT("ojk", [128, 128])
        osb = [TT("osb%d" % i, [128, 132]) for i in range(2)]
        oss = [TT("oss%d" % i, [128, 4]) for i in range(2)]
        oTb = [TT("oTb%d" % i, [128, S_LEN], BF16) for i in range(2)]
        S.op("dve", lambda e: e.tensor_copy(out=cx.mask01b[:], in_=cx.mask01[:]), reads=["consts"], writes=["consts2"])
        for i in range(2):
            S.op("pool", lambda e, i=i: e.memset(V[i][:, :, 128:129], 1.0), writes=[("V", i)])
        cx.stcnt = 0
        fcnt = [0]
        for h in range(8):
            hb = h % 2
            load_cast_w(cx, wb[hb], wst, "wst", "wb%d" % hb,
                        [(win_d[:, o_ * 1024 + h * 128:o_ * 1024 + (h + 1) * 128], o_ * 128, 128) for o_ in range(3)], q="pool", cast_eng="pool")
            for t in range(NT):
                p = t % 2
                P1 = cx.ps[4 + p]
                kp = "ps%d" % (4 + p)
                for kc in range(8):
                    S.op("pe", lambda e, P1=P1, kc=kc, t=t, hb=hb: e.matmul(P1[:, 0:384], lhsT=hT[:, kc, t * 128:(t + 1) * 128], rhs=wb[hb][:, kc, :], start=(kc == 0), stop=(kc == 7)),
                         reads=[("hT", t), "wb%d" % hb], writes=[kp], sig=(kc == 7))
                ksq, kqn, kqr, kt1, kt2, kss = "sq%d" % p, "qn%d" % p, "qr%d" % p, "t1_%d" % p, "t2_%d" % p, "pss%d" % p
                S.op("act", lambda e, P1=P1, p=p: e.activation(out=sq[p][:], in_=P1[:, 0:256], func=AF.Square), reads=[], writes=[ksq, kp])
                S.op("dve", lambda e, p=p: e.reduce_sum(out=pss[p][:, 0:4], in_=sq[p][:].rearrange("p (a b) -> p a b", a=4), axis=AX.X), reads=[ksq], writes=[kss])
                rstd_ops(cx, pss[p], 4, 1.0 / 64, kss)
                S.op("dve", lambda e, P1=P1, p=p: e.tensor_tensor(out=qn[p][:].rearrange("p (a b) -> p a b", a=4), in0=P1[:, 0:256].rearrange("p (a b) -> p a b", a=4),
                                                             in1=pss[p][:, 8:12].unsqueeze(2).to_broadcast([128, 4, 64]), op=ALU.mult), reads=[kss], writes=[kqn, kp])
                S.op("pool", lambda e, p=p: e.tensor_tensor(out=qn[p][:], in0=qn[p][:], in1=GQK[:], op=ALU.mult), reads=[kqn, "GQK"], writes=[kqn])
                q4 = qn[p][:].rearrange("p (a b) -> p a b", a=4)
                r4 = qr[p][:].rearrange("p (a b) -> p a b", a=4)
                cb = cosT[:, t, :].unsqueeze(1).to_broadcast([128, 4, 32])
                sb_ = sinT[:, t, :].unsqueeze(1).to_broadcast([128, 4, 32])
                a4 = t1[p][:].rearrange("p (a b) -> p a b", a=4)
                b4 = t2[p][:].rearrange("p (a b) -> p a b", a=4)
                S.op("dve", lambda e, a4=a4, q4=q4, cb=cb: e.tensor_tensor(out=a4, in0=q4[:, :, 0:32], in1=cb, op=ALU.mult), reads=[kqn, "rope"], writes=[kt1])
                S.op("pool", lambda e, b4=b4, q4=q4, sb_=sb_: e.tensor_tensor(out=b4, in0=q4[:, :, 32:64], in1=sb_, op=ALU.mult), reads=[kqn, "rope"], writes=[kt2])
                S.op("dve", lambda e, a4=a4, b4=b4, r4=r4: e.tensor_tensor(out=r4[:, :, 0:32], in0=a4, in1=b4, op=ALU.subtract), reads=[kt1, kt2], writes=[kqr])
                S.op("pool", lambda e, a4=a4, q4=q4, cb=cb: e.tensor_tensor(out=a4, in0=q4[:, :, 32:64], in1=cb, op=ALU.mult), reads=[kqn, "rope", kqr], writes=[kt1])
                S.op("dve", lambda e, b4=b4, q4=q4, sb_=sb_: e.tensor_tensor(out=b4, in0=q4[:, :, 0:32], in1=sb_, op=ALU.mult), reads=[kqn, "rope", kqr], writes=[kt2])
                S.op("pool", lambda e, a4=a4, b4=b4, r4=r4: e.tensor_tensor(out=r4[:, :, 32:64], in0=a4, in1=b4, op=ALU.add), reads=[kt1, kt2], writes=[kqr])
                for j in range(2):
                    S.op("pe", lambda e, j=j, p=p: e.transpose(cx.ps[6][:, j * 128:(j + 1) * 128], qr[p][:, j * 128:(j + 1) * 128], cx.ident[:]), reads=[kqr, "consts"], writes=["ps6"], sig=(j == 1))
                S.op("act", lambda e, t=t, hb=hb: e.activation(out=QKT[hb][:, :, t * 128:(t + 1) * 128], in_=cx.ps[6][:, 0:256].rearrange("p (a b) -> p a b", a=2), func=AF.Copy),
                     reads=[], writes=[("QK", hb), "ps6"])
                S.op("dve", lambda e, P1=P1, t=t, hb=hb: e.tensor_copy(out=V[hb][:, t, 0:128], in_=P1[:, 256:384]), reads=[], writes=[("V", hb), kp])

            def finish(Tq, c, oap_ps, okey, h=h, hb=hb):
                i = Tq % 4
                f = fcnt[0] % 2
                fcnt[0] += 1
                kos, kot, kob = "oss%d" % f, "ot%d" % f, "osb%d" % f
                oap = osb[f]
                if f == 0:
                    S.op("act", lambda e: e.activation(out=oap[:, 0:129], in_=oap_ps, func=AF.Copy), reads=[], writes=[kob, okey])
                else:
                    S.op("dve", lambda e: e.tensor_copy(out=oap[:, 0:129], in_=oap_ps), reads=[], writes=[kob, okey])
                S.op("dve", lambda e: e.reciprocal(out=oss[f][:, 0:1], in_=oap[:, 128:129]), reads=[kob], writes=[kos])
                if c == 0:
                    S.op("act", lambda e: e.activation(out=o0[:, i, :], in_=oap[:, 0:128], func=AF.Copy, scale=oss[f][:, 0:1]), reads=[kob, kos], writes=[("o0", i)])
                    return
                S.op("act", lambda e: e.activation(out=ot[f][:], in_=oap[:, 0:128], func=AF.Copy, scale=oss[f][:, 0:1]), reads=[kob, kos], writes=[kot])
                S.op("dve", lambda e: e.scalar_tensor_tensor(out=ot[f][:], in0=ot[f][:], scalar=neglam, in1=o0[:, i, :], op0=ALU.mult, op1=ALU.add), reads=[kot, ("o0", i), "lams"], writes=[kot])
                S.op("act", lambda e: e.activation(out=ojk[:], in_=ot[f][:], func=AF.Square, accum_out=oss[f][:, 1:2]), reads=[kot], writes=["ojk", kos])
                S.op("dve", lambda e: e.tensor_scalar(out=oss[f][:, 2:3], in0=oss[f][:, 1:2], scalar1=1.0 / 128, scalar2=EPS, op0=ALU.mult, op1=ALU.add), reads=[kos], writes=[kos])
                S.op("act", lambda e: e.activation(out=oss[f][:, 2:3], in_=oss[f][:, 2:3], func=AF.Sqrt), reads=[kos], writes=[kos])
                S.op("dve", lambda e: e.reciprocal(out=oss[f][:, 3:4], in_=oss[f][:, 2:3]), reads=[kos], writes=[kos])
                S.op("dve", lambda e: e.tensor_scalar(out=ot[f][:], in0=ot[f][:], scalar1=oss[f][:, 3:4], scalar2=None, op0=ALU.mult), reads=[kot, kos], writes=[kot])
                S.op("pe", lambda e: e.transpose(cx.ps[7][:, 0:128], ot[f][:], cx.ident[:]), reads=[kot, "consts"], writes=["ps7"])
                S.op("act", lambda e: e.activation(out=oTb[hb][:, Tq * 128:(Tq + 1) * 128], in_=cx.ps[7][:, 0:128], func=AF.Copy, scale=subg[:, 1:2]), reads=["subg"], writes=[("oTb", hb), "ps7"])

            flash_head(cx, h, "att", QKT[hb][:, 0, :], QKT[hb][:, 1, :], V[hb], finish)
            S.dma("sp", oT_d[h], oTb[hb][:], reads=[("oTb", hb)], writes=["oT_d"])
        S.barrier()
    out_proj(cx, x_d, xo_d, wout_d, oT_d)


def out_proj(cx, x_d, xo_d, wout_d, oT_d):
    S, nc = cx.S, cx.nc
    with ExitStack() as st2:
        def TT(name, shape, dt=F32):
            return st2.enter_context(nc.sbuf_tensor(uname(name), shape, dt))
        wst = TT("wost", [128, 8, 1024])
        wo = TT("wo", [128, 8, 1024], BF16)
        oTt = [TT("oTt%d" % i, [128, 8, 128], BF16) for i in range(2)]
        xt = [TT("xo%d" % i, [128, 1024]) for i in range(2)]
        yt = [TT("yo%d" % i, [128, 1024]) for i in range(2)]
        load_cast_w(cx, wo, wst, "wost", "wo", [(wout_d, 0, 1024)], q="pool", cast_eng="pool")
        for t in range(NT):
            p = t % 2
            S.dma("sp", oTt[p][:], oT_d[:, :, t * 128:(t + 1) * 128].rearrange("h p j -> p h j"), reads=["oT_d"], writes=["oTt%d" % p])
            S.dma("sp", xt[p][:], x_d[t * 128:(t + 1) * 128, :], writes=["xo%d" % p])
            for n in range(2):
                pb = cx.ps[2 * p + n]
                kp = "ps%d" % (2 * p + n)
                for h in range(8):
                    S.op("pe", lambda e, pb=pb, h=h, p=p, n=n: e.matmul(pb[:, :], lhsT=oTt[p][:, h, :], rhs=wo[:, h, n * 512:(n + 1) * 512], start=(h == 0), stop=(h == 7)),
                         reads=["oTt%d" % p, "wo"], writes=[kp], sig=(h == 7))
                S.op("dve", lambda e, pb=pb, p=p, n=n: e.tensor_tensor(out=yt[p][:, n * 512:(n + 1) * 512], in0=pb[:, :], in1=cx.G[:, n * 512:(n + 1) * 512], op=ALU.mult),
                     reads=["mod2"], writes=["yo%d" % p, kp])
            S.op("pool", lambda e, p=p: e.tensor_tensor(out=yt[p][:], in0=yt[p][:], in1=xt[p][:], op=ALU.add), reads=["yo%d" % p, "xo%d" % p], writes=["yo%d" % p])
            S.dma("sp", xo_d[t * 128:(t + 1) * 128, :], yt[p][:], reads=["yo%d" % p], writes=["xo_d"])
        S.barrier()


def new_ctx(nc, stack):
    cx = Ctx()
    cx.nc = nc
    cx.stack = stack
    cx.S = Sched(nc, stack)
    cx.ps = [stack.enter_context(nc.psum_tensor("psb%d" % i, [128, 512], F32)) for i in range(8)]
    cx.consts = T(cx, "consts", [128, 1024])
    cx.ident = cx.consts[:, 0:128]
    cx.mask01 = cx.consts[:, 128:256]
    cx.maskneg = cx.consts[:, 256:384]
    cx.invf = cx.consts[:, 384:416]
    cx.SH = T(cx, "SH", [128, 1024])
    cx.A = T(cx, "A", [128, 1024])
    cx.G = T(cx, "G", [128, 1024])
    return cx


def dram_in(nc, name, shape, dt=F32):
    return nc.dram_tensor(name, list(shape), dt, kind="ExternalInput").ap()


def build_attn_prog():
    nc = bass.Bass("TRN2", target_bir_lowering=False)
    x_d = dram_in(nc, "x", [S_LEN, D])
    c_d = dram_in(nc, "c", [1, D])
    pos_d = dram_in(nc, "pos", [1, S_LEN], I32)
    adaw_d = dram_in(nc, "adaw", [D, 6 * D])
    adab_d = dram_in(nc, "adab", [1, 6 * D])
    mixn_d = dram_in(nc, "mixn", [1, D])
    win_d = dram_in(nc, "win", [D, 3 * D])
    wout_d = dram_in(nc, "wout", [D, D])
    qn_d = dram_in(nc, "qn", [1, 64])
    kn_d = dram_in(nc, "kn", [1, 64])
    lam_ds = [dram_in(nc, "lam%d" % i, [1, 64]) for i in range(4)]
    subn_d = dram_in(nc, "subn", [1, 128])
    consts_d = dram_in(nc, "consts", [128, 1024])
    xo_d = nc.dram_tensor("xo", [S_LEN, D], F32, kind="ExternalOutput").ap()
    oT_d = nc.dram_tensor("oT_d", [8, 128, S_LEN], BF16, kind="Internal").ap()
    with ExitStack() as stack:
        cx = new_ctx(nc, stack)
        block = stack.enter_context(nc.Block())
        cx.S.dma("sp", cx.consts[:], consts_d, writes=["consts"])
        phase_attn(cx, x_d, xo_d, c_d, pos_d, adaw_d, adab_d, mixn_d, win_d, wout_d, qn_d, kn_d, lam_ds, subn_d, oT_d)
        print("instructions", cx.S.nins, "waits", cx.S.nwait)
        cx.S.emit(block)
    return nc


def phase_moe(cx, x_d, xo_d, c_d, adaw_d, adab_d, ffnn_d, wr_d, br_d, wgu_d, bgu_d, wd_d, bd_d, n_exp=32):
    S, nc = cx.S, cx.nc
    common_setup(cx, 1, c_d, adaw_d, adab_d, ffnn_d)
    QT_ = 1024
    NQ = S_LEN // QT_
    with ExitStack() as st2:
        def TT(name, shape, dt=F32):
            return st2.enter_context(nc.sbuf_tensor(uname(name), shape, dt))
        biasT = TT("biasT", [128, 16, 32])
        with ExitStack() as st3:
            bgr = st3.enter_context(nc.sbuf_tensor(uname("bgr"), [32, 2048], F32))
            S.dma("pool", bgr[:], bgu_d, writes=["bgr"])
            bgr3 = bgr[:].rearrange("e (f two) -> e f two", two=2)
            for j in range(16):
                src = bgr3[:, (j % 8) * 128:(j % 8 + 1) * 128, j // 8]
                S.op("pe", lambda e, j=j, src=src: e.transpose(cx.ps[j // 8][:, (j % 8) * 32:(j % 8) * 32 + 32], src, cx.ident[0:32, 0:32]), reads=["bgr", "consts"], writes=["ps%d" % (j // 8)])
            for g in range(2):
                S.op("dve", lambda e, g=g: e.tensor_copy(out=biasT[:, g * 8:(g + 1) * 8, :], in_=cx.ps[g][:, 0:256].rearrange("p (a b) -> p a b", a=8)), reads=[], writes=["biasT", "ps%d" % g])
            S.op("dve", lambda e: e.tensor_scalar(out=biasT[:, 8:16, :], in0=biasT[:, 8:16, :], scalar1=1.0, scalar2=None, op0=ALU.add), reads=["biasT"], writes=["biasT"])
            S.barrier()
        hT = TT("hTm", [128, 8, QT_], BF16)
        hTf = TT("hTf", [128, 8, 128])
        acc = TT("acc", [128, 8, 1024])
        wr = TT("wr", [128, 8, 32])
        brb = TT("brb", [128, 32])
        bd = TT("bd", [32, 1024])
        gates = TT("gates", [128, 8, 32])
        gT = TT("gT", [32, 128])
        rt = TT("rt", [128, 96])
        r8 = TT("r8", [128, 16])
        wgu = [TT("wgu%d" % i, [128, 8, 1024], BF16) for i in range(2)]
        wdb = [TT("wdb%d" % i, [128, 4, 1024], BF16) for i in range(2)]
        NSTG = 4
        stg = [TT("stg%d" % i, [128, 1024]) for i in range(NSTG)]
        actT = TT("actT", [128, 4, 512], BF16)
        gt = [TT("gt%d" % i, [128, 512]) for i in range(2)]
        sg = [TT("sg%d" % i, [128, 512]) for i in range(2)]
        lt = [TT("lt%d" % i, [128, 512]) for i in range(2)]
        cx.xt = [TT("xt%d" % i, [128, 1024]) for i in range(2)]
        cx.hh = [TT("hh%d" % i, [128, 1024]) for i in range(2)]
        cx.jk = TT("jk", [128, 1024])
        cx.nss = [TT("nss%d" % i, [128, 4]) for i in range(2)]
        cx.ncnt = 0
        S.dma("pool", wr[:], wr_d.rearrange("(kc p) e -> p kc e", p=128), writes=["wr"])
        S.dma("pool", brb[:], br_d.partition_broadcast(128), writes=["brb"])
        S.dma("pool", bd[:], bd_d, writes=["bd"])
        NU = n_exp * 2

        def load_dma(seq, s_):
            u = seq % NU
            ex_, hf = u // 2, u % 2
            sb = (seq * 12 + s_) % NSTG
            ks = "stg%d" % sb
            if s_ < 8:
                S.dma("sp", stg[sb][:], wgu_d[ex_, s_ * 128:(s_ + 1) * 128, hf * 1024:(hf + 1) * 1024], writes=[ks])
            else:
                fc = s_ - 8
                S.dma("sp", stg[sb][:], wd_d[ex_, hf * 512 + fc * 128:hf * 512 + (fc + 1) * 128, :], writes=[ks])

        def load_cast(seq, s_):
            wb2 = seq % 2
            sb = (seq * 12 + s_) % NSTG
            ks = "stg%d" % sb
            if s_ < 8:
                s3 = stg[sb][:].rearrange("p (f two) -> p f two", two=2)
                S.op("act", lambda e: e.activation(out=wgu[wb2][:, s_, 0:512], in_=s3[:, :, 0], func=AF.Copy), reads=[ks], writes=["wgu%d" % wb2])
                S.op("pool", lambda e: e.tensor_copy(out=wgu[wb2][:, s_, 512:1024], in_=s3[:, :, 1]), reads=[ks], writes=["wgu%d" % wb2])
            else:
                fc = s_ - 8
                S.op("act", lambda e: e.activation(out=wdb[wb2][:, fc, :], in_=stg[sb][:], func=AF.Copy), reads=[ks], writes=["wdb%d" % wb2])

        for qtr in range(NQ):
            for tl in range(8):
                t = qtr * 8 + tl
                norm_tile(cx, t, x_d, hT, tl * 128, want_f32T=hTf)
                for kc in range(8):
                    S.op("pe", lambda e, kc=kc: e.matmul(cx.ps[5][:, 0:32], lhsT=hTf[:, kc, :], rhs=wr[:, kc, :], start=(kc == 0), stop=(kc == 7)), reads=["hTf", "wr"], writes=["ps5"], sig=(kc == 7))
                S.op("dve", lambda e: e.tensor_tensor(out=rt[:, 0:32], in0=cx.ps[5][:, 0:32], in1=brb[:], op=ALU.add), reads=["brb"], writes=["rt", "ps5"])
                S.op("dve", lambda e: e.max(out=r8[:, 0:8], in_=rt[:, 0:32]), reads=["rt"], writes=["r8"])
                S.op("dve", lambda e: e.tensor_scalar(out=rt[:, 32:64], in0=rt[:, 0:32], scalar1=r8[:, 3:4], scalar2=None, op0=ALU.is_ge), reads=["rt", "r8"], writes=["rt"])
                S.op("dve", lambda e: e.tensor_scalar(out=r8[:, 8:9], in0=r8[:, 0:1], scalar1=-1.0, scalar2=None, op0=ALU.mult), reads=["r8"], writes=["r8"])
                S.op("act", lambda e: e.activation(out=rt[:, 64:96], in_=rt[:, 0:32], func=AF.Exp, bias=r8[:, 8:9]), reads=["rt", "r8"], writes=["rt"])
                S.op("dve", lambda e: e.tensor_tensor(out=rt[:, 64:96], in0=rt[:, 64:96], in1=rt[:, 32:64], op=ALU.mult), reads=["rt"], writes=["rt"])
                S.op("dve", lambda e: e.reduce_sum(out=r8[:, 9:10], in_=rt[:, 64:96], axis=AX.X), reads=["rt"], writes=["r8"])
                S.op("dve", lambda e: e.reciprocal(out=r8[:, 10:11], in_=r8[:, 9:10]), reads=["r8"], writes=["r8"])
                S.op("dve", lambda e, tl=tl: e.tensor_scalar(out=gates[:, tl, :], in0=rt[:, 64:96], scalar1=r8[:, 10:11], scalar2=None, op0=ALU.mult), reads=["rt", "r8"], writes=["gates"])
                S.op("pe", lambda e, tl=tl: e.transpose(cx.ps[5][0:32, 128:256], gates[:, tl, :], cx.ident[:]), reads=["gates", "consts"], writes=["ps5"])
                S.op("dve", lambda e: e.tensor_copy(out=gT[:], in_=cx.ps[5][0:32, 128:256]), reads=[], writes=["gT", "ps5"])
                for n in range(2):
                    S.op("pe", lambda e, n=n: e.matmul(cx.ps[4][:, :], lhsT=gT[:], rhs=bd[:, n * 512:(n + 1) * 512], start=True, stop=True), reads=["gT", "bd"], writes=["ps4"])
                    S.op("act", lambda e, tl=tl, n=n: e.activation(out=acc[:, tl, n * 512:(n + 1) * 512], in_=cx.ps[4][:, :], func=AF.Copy), reads=[], writes=[("acc", tl), "ps4"])
            for u in range(NU):
                ex, hf = u // 2, u % 2
                seq = qtr * NU + u
                wb_ = seq % 2
                if seq == 0:
                    for s_ in range(12):
                        load_dma(0, s_)
                        load_cast(0, s_)
                nxt = seq + 1 if seq + 1 < NQ * NU else None
                slot = [0]

                def tick(nxt=nxt, slot=slot):
                    s_ = slot[0]
                    slot[0] += 1
                    if nxt is None:
                        return
                    if s_ < 12:
                        load_dma(nxt, s_)
                    if 2 <= s_ < 14:
                        load_cast(nxt, s_ - 2)
                for tb in range(QT_ // 512):
                    for j in range(4):
                        p = j % 2
                        jj = hf * 4 + j
                        psg, psl = cx.ps[2 * p], cx.ps[2 * p + 1]
                        kg, kl = "ps%d" % (2 * p), "ps%d" % (2 * p + 1)
                        for (pp, kk, off) in ((psg, kg, 0), (psl, kl, 512)):
                            for kc in range(8):
                                S.op("pe", lambda e, pp=pp, kc=kc, off=off, j=j, tb=tb, wb_=wb_: e.matmul(pp[:, :], lhsT=wgu[wb_][:, kc, off + j * 128:off + (j + 1) * 128], rhs=hT[:, kc, tb * 512:(tb + 1) * 512], start=(kc == 0), stop=(kc == 7)),
                                     reads=["wgu%d" % wb_] + [("hT", tb * 4 + q_) for q_ in range(4)], writes=[kk], sig=(kc == 7))
                        S.op("dve", lambda e, p=p, psg=psg, jj=jj, ex=ex: e.tensor_scalar(out=gt[p][:], in0=psg[:, :], scalar1=biasT[:, jj, ex:ex + 1], scalar2=7.0, op0=ALU.add, op1=ALU.min), reads=["biasT"], writes=["gt%d" % p, kg])
                        S.op("act", lambda e, p=p: e.activation(out=sg[p][:], in_=gt[p][:], func=AF.Sigmoid, scale=1.702), reads=["gt%d" % p], writes=["sg%d" % p])
                        S.op("dve", lambda e, p=p, psl=psl, jj=jj, ex=ex: e.tensor_scalar(out=lt[p][:], in0=psl[:, :], scalar1=biasT[:, 8 + jj, ex:ex + 1], scalar2=8.0, op0=ALU.add, op1=ALU.min), reads=["biasT"], writes=["lt%d" % p, kl])
                        S.op("pool", lambda e, p=p: e.tensor_tensor(out=gt[p][:], in0=gt[p][:], in1=sg[p][:], op=ALU.mult), reads=["gt%d" % p, "sg%d" % p], writes=["gt%d" % p])
                        S.op("dve", lambda e, p=p, j=j: e.scalar_tensor_tensor(out=actT[:, j, :], in0=lt[p][:], scalar=-6.0, in1=gt[p][:], op0=ALU.max, op1=ALU.mult), reads=["gt%d" % p, "lt%d" % p], writes=[("actT", j)])
                        tick()
                    for tt in range(4):
                        tl = tb * 4 + tt
                        for n in range(2):
                            pb = cx.ps[4 + n]
                            kp = "ps%d" % (4 + n)
                            for fc in range(4):
                                S.op("pe", lambda e, pb=pb, fc=fc, tt=tt, n=n, wb_=wb_: e.matmul(pb[:, :], lhsT=actT[:, fc, tt * 128:(tt + 1) * 128], rhs=wdb[wb_][:, fc, n * 512:(n + 1) * 512], start=(fc == 0), stop=(fc == 3)),
                                     reads=[("actT", fc), "wdb%d" % wb_], writes=[kp], sig=(fc == 3))
                            S.op("dve", lambda e, pb=pb, tl=tl, n=n, ex=ex: e.scalar_tensor_tensor(out=acc[:, tl, n * 512:(n + 1) * 512], in0=pb[:, :], scalar=gates[:, tl, ex:ex + 1], in1=acc[:, tl, n * 512:(n + 1) * 512], op0=ALU.mult, op1=ALU.add),
                                 reads=["gates"], writes=[("acc", tl), kp])
                        tick()
            for tl in range(8):
                t = qtr * 8 + tl
                p = tl % 2
                xtp = cx.xt[p]
                S.dma("sp", xtp[:], x_d[t * 128:(t + 1) * 128, :], writes=["xt%d" % p])
                S.op("dve", lambda e, tl=tl: e.tensor_tensor(out=acc[:, tl, :], in0=acc[:, tl, :], in1=cx.G[:], op=ALU.mult), reads=["mod2"], writes=[("acc", tl)])
                S.op("dve", lambda e, tl=tl, xtp=xtp: e.tensor_tensor(out=acc[:, tl, :], in0=acc[:, tl, :], in1=xtp[:], op=ALU.add), reads=["xt%d" % p], writes=[("acc", tl)])
                S.dma("sp", xo_d[t * 128:(t + 1) * 128, :], acc[:, tl, :], reads=[("acc", tl)], writes=["xo_d"])
        S.barrier()


def build_moe_prog(n_exp=32):
    nc = bass.Bass("TRN2", target_bir_lowering=False)
    x_d = dram_in(nc, "x", [S_LEN, D])
    c_d = dram_in(nc, "c", [1, D])
    adaw_d = dram_in(nc, "adaw", [D, 6 * D])
    adab_d = dram_in(nc, "adab", [1, 6 * D])
    ffnn_d = dram_in(nc, "ffnn", [1, D])
    wr_d = dram_in(nc, "wr", [D, 32])
    br_d = dram_in(nc, "br", [1, 32])
    wgu_d = dram_in(nc, "wgu", [32, D, 2 * D])
    bgu_d = dram_in(nc, "bgu", [32, 2 * D])
    wd_d = dram_in(nc, "wd", [32, D, D])
    bd_d = dram_in(nc, "bd", [32, D])
    consts_d = dram_in(nc, "consts", [128, 1024])
    xo_d = nc.dram_tensor("xo", [S_LEN, D], F32, kind="ExternalOutput").ap()
    with ExitStack() as stack:
        cx = new_ctx(nc, stack)
        block = stack.enter_context(nc.Block())
        cx.S.dma("sp", cx.consts[:], consts_d, writes=["consts"])
        phase_moe(cx, x_d, xo_d, c_d, adaw_d, adab_d, ffnn_d, wr_d, br_d, wgu_d, bgu_d, wd_d, bd_d, n_exp=n_exp)
        print("instructions", cx.S.nins, "waits", cx.S.nwait)
        cx.S.emit(block)
    return nc


def phase_mlstm(cx, x_d, xo_d, c_d, adaw_d, adab_d, mixn_d, win_d, bi_d, bf_d, outn_d, wout_d, oT_d, sel_d):
    S, nc = cx.S, cx.nc
    common_setup(cx, 0, c_d, adaw_d, adab_d, mixn_d)
    hT = T(cx, "hT", [128, 8, S_LEN], BF16)
    OG = T(cx, "OG", [128, 1024])
    S.dma("pool", OG[:], outn_d.partition_broadcast(128), writes=["OG"])
    cx.sel3 = T(cx, "sel3", [128, 8, 128], BF16)
    cx.r3 = T(cx, "r3", [32, S_LEN], BF16)
    cx.ucol = T(cx, "ucol", [128, 256])
    emcol = T(cx, "emcol", [128, 256])
    with ExitStack() as st2:
        cx.xt = [st2.enter_context(nc.sbuf_tensor(uname("xt%d" % i), [128, 1024], F32)) for i in range(2)]
        cx.hh = [st2.enter_context(nc.sbuf_tensor(uname("hh%d" % i), [128, 1024], F32)) for i in range(2)]
        cx.jk = st2.enter_context(nc.sbuf_tensor(uname("jk"), [128, 1024], F32))
        cx.nss = [st2.enter_context(nc.sbuf_tensor(uname("nss%d" % i), [128, 4], F32)) for i in range(2)]
        cx.ncnt = 0
        jk_ = cx.jk
        sel3_ = cx.sel3
        S.dma("pool", jk_[:], sel_d, writes=["jk"])
        S.op("dve", lambda e: e.tensor_copy(out=sel3_[:].rearrange("p a b -> p (a b)"), in_=jk_[:]), reads=["jk"], writes=["sel3"])
        for t in range(NT):
            norm_tile(cx, t, x_d, hT, t * 128)
        S.barrier()
    with ExitStack() as st2:
        def TT(name, shape, dt=F32):
            return st2.enter_context(nc.sbuf_tensor(uname(name), shape, dt))
        wif = TT("wif", [128, 8, 16])
        wifb = TT("wifb", [128, 8, 16], BF16)
        bcol = TT("bcol", [8, 4])
        gi_t = TT("gi", [32, S_LEN])
        gf = TT("gf", [8, S_LEN])
        Ft_t = TT("Ft", [32, S_LEN])
        S.op("pool", lambda e: e.memset(gi_t[:], 0.0), writes=["g0"])
        S.op("pool", lambda e: e.memset(Ft_t[:], 0.0), writes=["Ft"])
        S.op("pool", lambda e: e.memset(cx.r3[:], 0.0), writes=["r3"])
        gi = gi_t[0:8, :]
        Ft = Ft_t[0:8, :]
        ones = TT("ones", [8, S_LEN])
        cm = TT("cm", [8, S_LEN])
        rb = [TT("rb%d" % i, [8, S_LEN], BF16) for i in range(3)]
        S.dma("sp", wif[:], win_d[:, 3072:3088].rearrange("(kc p) j -> p kc j", p=128), writes=["wif"])
        S.op("dve", lambda e: e.tensor_copy(out=wifb[:], in_=wif[:]), reads=["wif"], writes=["wifb"])
        S.dma("sp", bcol[:, 0:1], bi_d.rearrange("o (p a) -> (o p) a", a=1), writes=["bcol"])
        S.dma("sp", bcol[:, 1:2], bf_d.rearrange("o (p a) -> (o p) a", a=1), writes=["bcol"])
        S.op("dve", lambda e: e.tensor_scalar(out=bcol[:, 2:4], in0=bcol[:, 0:2], scalar1=1.0 / 15, scalar2=None, op0=ALU.mult), reads=["bcol"], writes=["bcol"])
        S.op("pool", lambda e: e.memset(ones[:], 1.0), writes=["ones"])
        for blk in range(8):
            for g in range(2):
                for kc in range(8):
                    S.op("pe", lambda e, g=g, kc=kc, blk=blk: e.matmul(cx.ps[g][0:8, :], lhsT=wifb[:, kc, g * 8:(g + 1) * 8], rhs=hT[:, kc, blk * 512:(blk + 1) * 512], start=(kc == 0), stop=(kc == 7)),
                         reads=["wifb"] + [("hT", blk * 4 + q_) for q_ in range(4)], writes=["ps%d" % g], sig=(kc == 7))
                dst = gi if g == 0 else gf
                S.op("act", lambda e, g=g, blk=blk, dst=dst: e.activation(out=dst[:, blk * 512:(blk + 1) * 512], in_=cx.ps[g][0:8, :], func=AF.Tanh, scale=1.0 / 15, bias=bcol[:, 2 + g:3 + g]),
                     reads=["bcol"], writes=["g%d" % g, "ps%d" % g])
        S.op("dve", lambda e: e.tensor_scalar(out=gi[:], in0=gi[:], scalar1=15.0, scalar2=None, op0=ALU.mult), reads=["g0"], writes=["g0"])
        S.op("act", lambda e: e.activation(out=gf[:], in_=gf[:], func=AF.Exp, scale=-15.0), reads=["g1"], writes=["g1"])
        S.op("act", lambda e: e.activation(out=gf[:], in_=gf[:], func=AF.Ln, bias=1.0), reads=["g1"], writes=["g1"])
        S.op("dve", lambda e: e.tensor_tensor_scan(out=Ft[:], data0=ones[:], data1=gf[:], initial=0.0, op0=ALU.mult, op1=ALU.subtract), reads=["ones", "g1"], writes=["Ft"])
        S.op("dve", lambda e: e.tensor_tensor(out=gi[:], in0=gi[:], in1=Ft[:], op=ALU.subtract), reads=["g0", "Ft"], writes=["g0"])
        S.op("dve", lambda e: e.tensor_tensor_scan(out=cm[:], data0=ones[:], data1=gi[:], initial=0.0, op0=ALU.mult, op1=ALU.max), reads=["ones", "g0"], writes=["cm"])
        S.op("dve", lambda e: e.tensor_tensor(out=Ft[:], in0=Ft[:], in1=cm[:], op=ALU.add), reads=["Ft", "cm"], writes=["Ft"])
        S.op("act", lambda e: e.activation(out=Ft[:], in_=Ft[:], func=AF.Exp, scale=-1.0), reads=["Ft"], writes=["Ft"])
        S.op("dve", lambda e: e.tensor_scalar(out=cm[:], in0=cm[:], scalar1=-1.0, scalar2=None, op0=ALU.mult), reads=["cm"], writes=["cm"])
        for j in range(3):
            S.op("dve", lambda e, j=j: e.tensor_copy(out=rb[j][:], in_=cm[:]), reads=["cm"], writes=["rb%d" % j])
            if j < 2:
                S.op("dve", lambda e, j=j: e.tensor_tensor(out=cm[:], in0=cm[:], in1=rb[j][:], op=ALU.subtract), reads=["cm", "rb%d" % j], writes=["cm"])
            S.dma("sp", cx.r3[8 * j:8 * j + 8, :], rb[j][:], reads=["rb%d" % j], writes=["r3"])
        for which, src, dst, key in ((0, gi, cx.ucol, "ucol"), (1, Ft, emcol, "emcol")):
            for t in range(NT):
                S.op("pe", lambda e, which=which, src=src, t=t: e.transpose(cx.ps[2 + which][:, t * 8:(t + 1) * 8], src[:, t * 128:(t + 1) * 128], cx.ident[0:8, 0:8]),
                     reads=["g0" if which == 0 else "Ft", "consts"], writes=["ps%d" % (2 + which)])
            S.op("dve", lambda e, which=which, dst=dst: e.tensor_copy(out=dst[:], in_=cx.ps[2 + which][:, 0:256]), reads=[], writes=[key, "ps%d" % (2 + which)])
        dbg = getattr(cx, "dbg", None)
        if dbg:
            S.dma("sp", dbg["ucol"], cx.ucol[:], reads=["ucol"], writes=["dbg1"])
            S.dma("sp", dbg["emcol"], emcol[:], reads=["emcol"], writes=["dbg2"])
            S.dma("sp", dbg["r3"], cx.r3[:], reads=["r3"], writes=["dbg3"])
        S.barrier()
    with ExitStack() as st2:
        def TT(name, shape, dt=F32):
            return st2.enter_context(nc.sbuf_tensor(uname(name), shape, dt))
        wst = TT("wst", [128, 8, 384])
        wb = [TT("wb%d" % i, [128, 8, 384], BF16) for i in range(2)]
        QKT = [TT("QKT%d" % i, [128, 2, S_LEN], BF16) for i in range(2)]
        V = [TT("V%d" % i, [128, NT, 132], BF16) for i in range(2)]
        og = [TT("og0", [128, NT, 128], BF16)] * 2
        cx.PT = [TT("PT%d" % i, [128, 512], BF16) for i in range(2)]
        cx.DT = [TT("DT%d" % i, [128, 512]) for i in range(2)]
        if getattr(cx, "dbg", None) is not None:
            cx.dbgt = TT("dbgt", [128, 512])
        qk = [TT("qk%d" % i, [128, 128]) for i in range(2)]
        ot = [TT("ot%d" % i, [128, 128]) for i in range(2)]
        ojk = TT("ojk", [128, 128])
        osb = [TT("osb%d" % i, [128, 132]) for i in range(2)]
        oss = [TT("oss%d" % i, [128, 8]) for i in range(2)]
        oTb = [TT("oTb0", [128, S_LEN], BF16)] * 2
        for i in range(2):
            S.op("pool", lambda e, i=i: e.memset(V[i][:, :, 128:129], 1.0), writes=[("V", i)])
        cx.stcnt = 0
        fcnt = [0]
        for h in range(8):
            hb = h % 2
            load_cast_w(cx, wb[hb], wst, "wst", "wb%d" % hb,
                        [(win_d[:, h * 64:(h + 1) * 64], 0, 64), (win_d[:, 512 + h * 64:512 + (h + 1) * 64], 64, 64),
                         (win_d[:, 1024 + h * 128:1024 + (h + 1) * 128], 128, 128), (win_d[:, 2048 + h * 128:2048 + (h + 1) * 128], 256, 128)], q="pool", cast_eng="pool")
            for t in range(NT):
                p = t % 2
                P1 = cx.ps[4 + p]
                kp = "ps%d" % (4 + p)
                for kc in range(8):
                    S.op("pe", lambda e, P1=P1, kc=kc, t=t, hb=hb: e.matmul(P1[:, 0:384], lhsT=hT[:, kc, t * 128:(t + 1) * 128], rhs=wb[hb][:, kc, :], start=(kc == 0), stop=(kc == 7)),
                         reads=[("hT", t), "wb%d" % hb], writes=[kp], sig=(kc == 7))
                S.op("dve", lambda e, P1=P1, p=p: e.tensor_copy(out=qk[p][:], in_=P1[:, 0:128]), reads=[], writes=["qk%d" % p, kp])
                S.op("dve", lambda e, P1=P1, t=t, hb=hb: e.tensor_copy(out=V[hb][:, t, 0:128], in_=P1[:, 128:256]), reads=[], writes=[("V", hb), kp])
                S.op("act", lambda e, P1=P1, t=t, hb=hb: e.activation(out=og[hb][:, t, :], in_=P1[:, 256:384], func=AF.Sigmoid), reads=[], writes=[("og", 0), kp])
                for j in range(2):
                    S.op("pe", lambda e, j=j, p=p: e.transpose(cx.ps[6][0:64, j * 128:(j + 1) * 128], qk[p][:, j * 64:(j + 1) * 64], cx.ident[:]), reads=["qk%d" % p, "consts"], writes=["ps6"], sig=(j == 1))
                S.op("act", lambda e, t=t, hb=hb: e.activation(out=QKT[hb][0:64, :, t * 128:(t + 1) * 128], in_=cx.ps[6][0:64, 0:256].rearrange("p (a b) -> p a b", a=2), func=AF.Copy),
                     reads=[], writes=[("QK", hb), "ps6"])

            def finish(Tq, c, oap_ps, okey, h=h, hb=hb):
                f = fcnt[0] % 2
                fcnt[0] += 1
                kos, kot, kob = "oss%d" % f, "ot%d" % f, "osb%d" % f
                oap = osb[f]
                if f == 0:
                    S.op("act", lambda e: e.activation(out=oap[:, 0:129], in_=oap_ps, func=AF.Copy), reads=[], writes=[kob, okey])
                else:
                    S.op("dve", lambda e: e.tensor_copy(out=oap[:, 0:129], in_=oap_ps), reads=[], writes=[kob, okey])
                S.op("act", lambda e: e.activation(out=oss[f][:, 0:1], in_=oap[:, 128:129], func=AF.Abs), reads=[kob], writes=[kos])
                S.op("dve", lambda e: e.tensor_tensor(out=oss[f][:, 0:1], in0=oss[f][:, 0:1], in1=emcol[:, Tq * 8 + h:Tq * 8 + h + 1], op=ALU.max), reads=[kos, "emcol"], writes=[kos])
                S.op("dve", lambda e: e.reciprocal(out=oss[f][:, 1:2], in_=oss[f][:, 0:1]), reads=[kos], writes=[kos])
                S.op("act", lambda e: e.activation(out=ot[f][:], in_=oap[:, 0:128], func=AF.Copy, scale=oss[f][:, 1:2]), reads=[kob, kos], writes=[kot])
                dbg = getattr(cx, "dbg", None)
                dump = dbg is not None and h == 0 and Tq == 12
                if dump:
                    S.dma("sp", dbg["osb"], oap[:, 0:132], reads=[kob], writes=["dbg4"])
                    S.dma("sp", dbg["ot_a"], ot[f][:], reads=[kot], writes=["dbg5"])
                S.op("act", lambda e: e.activation(out=ojk[:], in_=ot[f][:], func=AF.Square, accum_out=oss[f][:, 2:3]), reads=[kot], writes=["ojk", kos])
                S.op("dve", lambda e: e.tensor_scalar(out=oss[f][:, 3:4], in0=oss[f][:, 2:3], scalar1=1.0 / 128, scalar2=EPS, op0=ALU.mult, op1=ALU.add), reads=[kos], writes=[kos])
                S.op("act", lambda e: e.activation(out=oss[f][:, 3:4], in_=oss[f][:, 3:4], func=AF.Sqrt), reads=[kos], writes=[kos])
                S.op("dve", lambda e: e.reciprocal(out=oss[f][:, 4:5], in_=oss[f][:, 3:4]), reads=[kos], writes=[kos])
                S.op("dve", lambda e: e.scalar_tensor_tensor(out=ot[f][:], in0=ot[f][:], scalar=oss[f][:, 4:5], in1=OG[:, h * 128:(h + 1) * 128], op0=ALU.mult, op1=ALU.mult), reads=[kot, kos, "OG"], writes=[kot])
                if dump:
                    S.dma("sp", dbg["ot_b"], ot[f][:], reads=[kot], writes=["dbg6"])
                    S.dma("sp", dbg["oss"], oss[f][:], reads=[kos], writes=["dbg7"])
                    S.dma("sp", dbg["og"], og[hb][:, Tq, :], reads=[("og", 0)], writes=["dbg8"])
                S.op("pool", lambda e: e.tensor_tensor(out=ot[f][:], in0=ot[f][:], in1=og[hb][:, Tq, :], op=ALU.mult), reads=[kot, ("og", 0)], writes=[kot])
                if dump:
                    S.dma("sp", dbg["ot_c"], ot[f][:], reads=[kot], writes=["dbg9"])
                S.op("pe", lambda e: e.transpose(cx.ps[7][:, 0:128], ot[f][:], cx.ident[:]), reads=[kot, "consts"], writes=["ps7"])
                S.op("act", lambda e: e.activation(out=oTb[hb][:, Tq * 128:(Tq + 1) * 128], in_=cx.ps[7][:, 0:128], func=AF.Copy), reads=[], writes=[("oTb", 0), "ps7"])

            flash_head(cx, h, "ml", QKT[hb][:, 0, :], QKT[hb][:, 1, :], V[hb], finish)
            S.dma("sp", oT_d[h], oTb[hb][:], reads=[("oTb", 0)], writes=["oT_d"])
        S.barrier()
    out_proj(cx, x_d, xo_d, wout_d, oT_d)


def build_ml_prog():
    nc = bass.Bass("TRN2", target_bir_lowering=False)
    x_d = dram_in(nc, "x", [S_LEN, D])
    c_d = dram_in(nc, "c", [1, D])
    adaw_d = dram_in(nc, "adaw", [D, 6 * D])
    adab_d = dram_in(nc, "adab", [1, 6 * D])
    mixn_d = dram_in(nc, "mixn", [1, D])
    win_d = dram_in(nc, "win", [D, 3088])
    bi_d = dram_in(nc, "bi", [1, 8])
    bf_d = dram_in(nc, "bf", [1, 8])
    outn_d = dram_in(nc, "outn", [1, D])
    wout_d = dram_in(nc, "wout", [D, D])
    consts_d = dram_in(nc, "consts", [128, 1024])
    sel_d = dram_in(nc, "sel", [128, 1024])
    xo_d = nc.dram_tensor("xo", [S_LEN, D], F32, kind="ExternalOutput").ap()
    oT_d = nc.dram_tensor("oT_d", [8, 128, S_LEN], BF16, kind="Internal").ap()
    with ExitStack() as stack:
        cx = new_ctx(nc, stack)
        block = stack.enter_context(nc.Block())
        cx.S.dma("sp", cx.consts[:], consts_d, writes=["consts"])
        phase_mlstm(cx, x_d, xo_d, c_d, adaw_d, adab_d, mixn_d, win_d, bi_d, bf_d, outn_d, wout_d, oT_d, sel_d)
        print("instructions", cx.S.nins, "waits", cx.S.nwait)
        cx.S.emit(block)
    return nc


PHASES = ("attn", "moe0", "mlstm", "moe1")


def build_prog(phases, n_exp=32, sliced=False, debug=False):
    nc = bass.Bass("TRN2", target_bir_lowering=False)
    d = {}
    def inp(name, shape, dt=F32):
        d[name] = dram_in(nc, name, shape, dt)
        return d[name]
    x_d = inp("x", [S_LEN, D])
    c_d = inp("c", [1, D])
    NL = 1 if sliced else 2
    adaw_d = inp("ada_w", [NL, D, 6 * D])
    adab_d = inp("ada_b", [NL, 6 * D])
    consts_d = inp("consts", [128, 1024])
    if "attn" in phases:
        pos_d = inp("positions", [1, S_LEN], I32)
        inp("mix_norm", [NL, D])
        inp("att_w_in", [1, D, 3 * D]); inp("att_w_out", [1, D, D])
        for nm in ("att_q_norm", "att_k_norm", "att_lam_q1", "att_lam_k1", "att_lam_q2", "att_lam_k2"):
            inp(nm, [1, 64])
        inp("att_sub_norm", [1, 128])
    if "mlstm" in phases:
        if "mix_norm" not in d:
            inp("mix_norm", [NL, D])
        inp("ml_w_in", [1, D, 3088]); inp("ml_b_igate", [1, 8]); inp("ml_b_fgate", [1, 8])
        inp("ml_out_norm", [1, D]); inp("ml_w_out", [1, D, D]); inp("sel", [128, 1024])
    if "moe0" in phases or "moe1" in phases:
        inp("ffn_norm", [NL, D]); inp("router_w", [NL, D, 32]); inp("router_b", [NL, 32])
        inp("moe_w_gate_up", [NL, 32, D, 2 * D]); inp("moe_b_gate_up", [NL, 32, 2 * D])
        inp("moe_w_down", [NL, 32, D, D]); inp("moe_b_down", [NL, 32, D])
    xo_d = nc.dram_tensor("xo", [S_LEN, D], F32, kind="ExternalOutput").ap()
    oT_d = nc.dram_tensor("oT_d", [8, 128, S_LEN], BF16, kind="ExternalOutput" if debug else "Internal").ap()
    xs = [x_d]
    for i in range(len(phases) - 1):
        xs.append(nc.dram_tensor("xmid%d" % i, [S_LEN, D], F32, kind="ExternalOutput" if debug else "Internal").ap())
    xs.append(xo_d)
    with ExitStack() as stack:
        cx = new_ctx(nc, stack)
        block = stack.enter_context(nc.Block())
        if debug:
            cx.dbg = {"ucol": nc.dram_tensor("dbg_ucol", [128, 256], F32, kind="ExternalOutput").ap(),
                      "emcol": nc.dram_tensor("dbg_emcol", [128, 256], F32, kind="ExternalOutput").ap(),
                      "r3": nc.dram_tensor("dbg_r3", [32, S_LEN], BF16, kind="ExternalOutput").ap(),
                      "osb": nc.dram_tensor("dbg_osb", [128, 132], F32, kind="ExternalOutput").ap(),
                      "ot_a": nc.dram_tensor("dbg_ot_a", [128, 128], F32, kind="ExternalOutput").ap(),
                      "ot_b": nc.dram_tensor("dbg_ot_b", [128, 128], F32, kind="ExternalOutput").ap(),
                      "ot_c": nc.dram_tensor("dbg_ot_c", [128, 128], F32, kind="ExternalOutput").ap(),
                      "oss": nc.dram_tensor("dbg_oss", [128, 8], F32, kind="ExternalOutput").ap(),
                      "og": nc.dram_tensor("dbg_og", [128, 128], BF16, kind="ExternalOutput").ap(),
                      "DT0": nc.dram_tensor("dbg_DT0", [128, 512], F32, kind="ExternalOutput").ap(),
                      "DT11": nc.dram_tensor("dbg_DT11", [128, 512], F32, kind="ExternalOutput").ap(),
                      "RB0": nc.dram_tensor("dbg_RB0", [128, 512], F32, kind="ExternalOutput").ap(),
                      "RB11": nc.dram_tensor("dbg_RB11", [128, 512], F32, kind="ExternalOutput").ap(),
                      "sel3": nc.dram_tensor("dbg_sel3", [32, 128], BF16, kind="ExternalOutput").ap()}
        cx.S.dma("sp", cx.consts[:], consts_d, writes=["consts"])
        for i, ph in enumerate(phases):
            xin, xout = xs[i], xs[i + 1]
            with ExitStack() as pst:
                cx.stack = pst
                if ph == "attn":
                    phase_attn(cx, xin, xout, c_d, pos_d, adaw_d[0], adab_d[0:1, :], d["mix_norm"][0:1, :], d["att_w_in"][0], d["att_w_out"][0],
                               d["att_q_norm"], d["att_k_norm"], [d["att_lam_q1"], d["att_lam_k1"], d["att_lam_q2"], d["att_lam_k2"]], d["att_sub_norm"], oT_d)
                elif ph == "mlstm":
                    L = 0 if sliced else 1
                    phase_mlstm(cx, xin, xout, c_d, adaw_d[L], adab_d[L:L + 1, :], d["mix_norm"][L:L + 1, :], d["ml_w_in"][0], d["ml_b_igate"], d["ml_b_fgate"],
                                d["ml_out_norm"], d["ml_w_out"][0], oT_d, d["sel"])
                else:
                    L = 0 if sliced else int(ph[-1])
                    phase_moe(cx, xin, xout, c_d, adaw_d[L], adab_d[L:L + 1, :], d["ffn_norm"][L:L + 1, :], d["router_w"][L], d["router_b"][L:L + 1, :],
                              d["moe_w_gate_up"][L], d["moe_b_gate_up"][L], d["moe_w_down"][L], d["moe_b_down"][L], n_exp=n_exp)
                cx.S.barrier()
            cx.stack = stack
        cx.S.emit(block)
    return nc, list(d.keys())


LAUNCH_GROUPS = [("attn",), ("moe0",), ("mlstm",), ("moe1",)]
PHASE_LAYER = {"attn": 0, "moe0": 0, "mlstm": 1, "moe1": 1}
PER_LAYER = ("ada_w", "ada_b", "mix_norm", "ffn_norm", "router_w", "router_b", "moe_w_gate_up", "moe_b_gate_up", "moe_w_down", "moe_b_down")
FUSED = True


def kernel(**inputs):
    n = 8
    shared = {k: np.ascontiguousarray(v) for k, v in inputs.items() if k not in ("x", "c", "positions")}
    shared["consts"] = make_consts()
    shared["sel"] = make_sel()
    xcur = [np.ascontiguousarray(inputs["x"][b]) for b in range(n)]
    groups = [PHASES] if FUSED else LAUNCH_GROUPS
    progs = {}
    for grp in groups:
        sliced = not FUSED
        pkey = ("moe0",) if (sliced and grp[0].startswith("moe")) else grp
        if pkey not in progs:
            progs[pkey] = build_prog(pkey, sliced=sliced)
        nc, names = progs[pkey]
        L = PHASE_LAYER[grp[0]]
        in_maps = []
        for b in range(n):
            m = {}
            for k in names:
                if k == "x":
                    m[k] = xcur[b]
                elif k == "c":
                    m[k] = np.ascontiguousarray(inputs["c"][b:b + 1])
                elif k == "positions":
                    m[k] = np.ascontiguousarray(inputs["positions"][b:b + 1]).astype(np.int32)
                elif sliced and k in PER_LAYER:
                    m[k] = np.ascontiguousarray(shared[k][L:L + 1])
                else:
                    m[k] = shared[k]
            in_maps.append(m)
        res = run_bass_kernel_spmd(nc, in_maps, core_ids=list(range(n)))
        xcur = [np.ascontiguousarray(res.results[b]["xo"]) for b in range(n)]
    return np.stack(xcur, axis=0).astype(np.float32)
```

```python
import numpy as np
import concourse.bass as bass
import concourse.mybir as mybir
from concourse.bass_utils import run_bass_kernel_spmd

F32 = mybir.dt.float32
BF16 = mybir.dt.bfloat16
I32 = mybir.dt.int32
AF = mybir.ActivationFunctionType
ALU = mybir.AluOpType
AX = mybir.AxisListType

ENGMAP = {"pe": "tensor", "act": "scalar", "dve": "vector", "pool": "gpsimd", "sp": "sync"}


class Sched:
    def __init__(self, nc, stack, ndma=8):
        self.nc = nc
        self.prog = {k: [] for k in ENGMAP}
        self.sem = {}
        self.cnt = {k: 0 for k in ENGMAP}
        self.dsem = {}
        self.drr = {k: 0 for k in ENGMAP}
        for k in ENGMAP:
            self.sem[k] = stack.enter_context(nc.semaphore("s_" + k))
        for k in ("sp", "pool", "act"):
            self.dsem[k] = [[stack.enter_context(nc.semaphore("d_%s%d" % (k, j))), 0] for j in range(ndma)]
        self.lastw = {}
        self.readers = {}
        self.seen = {}
        self.nwait = 0
        self.nins = 0

    def _deps(self, eng, reads, writes):
        best = {}
        def add(tok):
            s, v, src = tok
            if src == eng and eng == "pe":
                return
            key = id(s)
            if key not in best or best[key][1] < v:
                best[key] = (s, v)
        for k in reads:
            if k in self.lastw:
                add(self.lastw[k])
        for k in writes:
            if k in self.lastw:
                add(self.lastw[k])
            for t in self.readers.get(k, ()):
                add(t)
        for key, (s, v) in best.items():
            sk = (eng, key)
            if self.seen.get(sk, 0) >= v:
                continue
            self.seen[sk] = v
            self.prog[eng].append(lambda e, s=s, v=v: e.wait_ge(s, v))
            self.nwait += 1

    def _commit(self, tok, reads, writes):
        for k in reads:
            self.readers.setdefault(k, []).append(tok)
        for k in writes:
            self.lastw[k] = tok
            self.readers[k] = []

    def op(self, eng, fn, reads=(), writes=(), sig=True):
        self._deps(eng, reads, writes)
        sem = self.sem[eng]
        if sig:
            self.cnt[eng] += 1
            tok = (sem, self.cnt[eng], eng)
            self.prog[eng].append(lambda e, fn=fn, sem=sem: fn(e).then_inc(sem, 1))
        else:
            tok = (sem, self.cnt[eng] + 1, eng)
            self.prog[eng].append(lambda e, fn=fn: fn(e))
        self.nins += 1
        self._commit(tok, reads, writes)
        return tok

    def dma(self, q, out, in_, reads=(), writes=(), **kw):
        self._deps(q, reads, writes)
        j = self.drr[q]
        self.drr[q] = (j + 1) % len(self.dsem[q])
        ent = self.dsem[q][j]
        sem, c = ent
        if c > 0:
            sk = (q, id(sem))
            if self.seen.get(sk, 0) < 16 * c:
                self.seen[sk] = 16 * c
                self.prog[q].append(lambda e, s=sem, v=16 * c: e.wait_ge(s, v))
        ent[1] = c + 1
        tok = (sem, 16 * (c + 1), "dma_" + q)
        self.prog[q].append(lambda e, out=out, in_=in_, sem=sem, kw=kw: e.dma_start(out=out, in_=in_, **kw).then_inc(sem, 16))
        self.nins += 1
        self._commit(tok, reads, writes)
        return tok

    def barrier(self):
        toks = []
        for k in ENGMAP:
            if self.cnt[k] > 0:
                toks.append((self.sem[k], self.cnt[k], k))
        for q in self.dsem:
            for sem, c in self.dsem[q]:
                if c > 0:
                    toks.append((sem, 16 * c, "dma"))
        for eng in ENGMAP:
            for s_, v, src in toks:
                if src == eng and eng == "pe":
                    continue
                sk = (eng, id(s_))
                if self.seen.get(sk, 0) >= v:
                    continue
                self.seen[sk] = v
                self.prog[eng].append(lambda e, s_=s_, v=v: e.wait_ge(s_, v))
        self.lastw = {}
        self.readers = {}

    def wait_all(self, eng, keys):
        self._deps(eng, list(keys), [])

    def emit(self, block):
        for k, name in ENGMAP.items():
            lst = self.prog[k]
            def body(e, lst=lst):
                for f in lst:
                    f(e)
            getattr(block, name)(body)


import math
from contextlib import ExitStack
import numpy as np

S_LEN = 4096
D = 1024
NT = S_LEN // 128
EPS = 1e-6
PI = math.pi


def make_consts():
    c = np.zeros((128, 1024), np.float32)
    c[:, 0:128] = np.eye(128, dtype=np.float32)
    s = np.arange(128)[:, None]
    l = np.arange(128)[None, :]
    c[:, 128:256] = (s <= l).astype(np.float32)
    c[:, 256:384] = np.where(s <= l, 0.0, -30000.0)
    inv = (10000.0 ** (-np.arange(0, 64, 2, dtype=np.float32) / 64)).astype(np.float32)
    c[:, 384:416] = inv[None, :]
    for h in range(8):
        for j in range(3):
            c[8 * j + h, 416 + 0:416 + 0] = 0
    return c


def make_sel():
    sel = np.zeros((128, 8, 128), np.float32)
    for h in range(8):
        for j in range(3):
            sel[8 * j + h, h, :] = 1.0
    return sel.reshape(128, 1024)


class Ctx:
    pass


_uid = [0]


def uname(name):
    _uid[0] += 1
    return "s%d_%s" % (_uid[0], name)


def T(cx, name, shape, dt=F32):
    return cx.stack.enter_context(cx.nc.sbuf_tensor(uname(name), shape, dt))


def common_setup(cx, layer_half, c_d, adaw_d, adab_d, norm_d):
    S, nc = cx.S, cx.nc
    with ExitStack() as st:
        def TT(name, shape, dt=F32):
            return st.enter_context(nc.sbuf_tensor(uname(name), shape, dt))
        rowc = TT("rowc", [128, 1024])
        crep = TT("crep", [128, 8, 128])
        stg = [TT("adst%d" % i, [128, 3072]) for i in range(2)]
        adab = TT("adab", [128, 3072])
        nrm = TT("nrmb", [128, 1024])
        c0 = layer_half * 3072
        S.dma("sp", rowc[:], c_d.partition_broadcast(128), writes=["rowc"])
        S.dma("sp", adab[:], adab_d[:, c0:c0 + 3072].partition_broadcast(128), writes=["adab"])
        S.dma("sp", nrm[:], norm_d.partition_broadcast(128), writes=["nrmb"])
        S.op("act", lambda e: e.activation(out=crep[:].rearrange("p a b -> p (a b)"), in_=rowc[:], func=AF.Sigmoid), reads=["rowc"], writes=["crep"])
        S.op("dve", lambda e: e.tensor_tensor(out=rowc[:], in0=rowc[:], in1=crep[:].rearrange("p a b -> p (a b)"), op=ALU.mult), reads=["crep", "rowc"], writes=["rowc"])
        for kc in range(8):
            b = cx.ps[kc // 4]
            S.op("pe", lambda e, kc=kc, b=b: e.transpose(b[:, (kc % 4) * 128:(kc % 4 + 1) * 128], rowc[:, kc * 128:(kc + 1) * 128], cx.ident[:]),
                 reads=["rowc", "consts"], writes=["ps%d" % (kc // 4)])
        for hb in range(2):
            S.op("dve", lambda e, hb=hb: e.tensor_copy(out=crep[:, hb * 4:(hb + 1) * 4, :].rearrange("p a b -> p (a b)"), in_=cx.ps[hb][:, :]),
                 reads=[], writes=["crep", "ps%d" % hb])
        for kc in range(8):
            sb = stg[kc % 2]
            S.dma("sp" if kc % 2 == 0 else "pool", sb[:], adaw_d[kc * 128:(kc + 1) * 128, c0:c0 + 3072], writes=["adst%d" % (kc % 2)])
            for n in range(6):
                S.op("pe", lambda e, kc=kc, n=n, sb=sb: e.matmul(cx.ps[n][:, :], lhsT=crep[:, kc, :], rhs=sb[:, n * 512:(n + 1) * 512], start=(kc == 0), stop=(kc == 7)),
                     reads=["crep", "adst%d" % (kc % 2)], writes=["ps%d" % n], sig=(kc == 7 or n == 5))
        for n in range(6):
            dst = (cx.SH, cx.A, cx.G)[n // 2]
            S.op("dve", lambda e, n=n, dst=dst: e.tensor_tensor(out=dst[:, (n % 2) * 512:(n % 2 + 1) * 512], in0=cx.ps[n][:, :], in1=adab[:, n * 512:(n + 1) * 512], op=ALU.add),
                 reads=["adab"], writes=["mod%d" % (n // 2), "ps%d" % n])
        S.op("dve", lambda e: e.scalar_tensor_tensor(out=cx.A[:], in0=cx.A[:], scalar=1.0, in1=nrm[:], op0=ALU.add, op1=ALU.mult),
             reads=["mod1", "nrmb"], writes=["mod1"])
        S.barrier()


def norm_tile(cx, t, x_d, hT, tok0, want_f32T=None):
    S, nc = cx.S, cx.nc
    i = cx.ncnt % 2
    cx.ncnt += 1
    xt, hh, jk = cx.xt[i], cx.hh[i], cx.jk
    ss = cx.nss[i]
    kx, kh, ks = "xt%d" % i, "hh%d" % i, "nss%d" % i
    S.dma("sp", xt[:], x_d[t * 128:(t + 1) * 128, :], writes=[kx])
    S.op("act", lambda e: e.activation(out=jk[:], in_=xt[:], func=AF.Square, accum_out=ss[:, 0:1]), reads=[kx], writes=["jk", ks])
    S.op("dve", lambda e: e.tensor_scalar(out=ss[:, 1:2], in0=ss[:, 0:1], scalar1=1.0 / D, scalar2=EPS, op0=ALU.mult, op1=ALU.add), reads=[ks], writes=[ks])
    S.op("act", lambda e: e.activation(out=ss[:, 2:3], in_=ss[:, 1:2], func=AF.Sqrt), reads=[ks], writes=[ks])
    S.op("dve", lambda e: e.reciprocal(out=ss[:, 3:4], in_=ss[:, 2:3]), reads=[ks], writes=[ks])
    S.op("dve", lambda e: e.scalar_tensor_tensor(out=hh[:], in0=xt[:], scalar=ss[:, 3:4], in1=cx.A[:], op0=ALU.mult, op1=ALU.mult), reads=[kx, ks, "mod1"], writes=[kh])
    S.op("dve", lambda e: e.tensor_tensor(out=hh[:], in0=hh[:], in1=cx.SH[:], op=ALU.add), reads=[kh, "mod0"], writes=[kh])
    for kc in range(8):
        b = 6 + kc // 4
        S.op("pe", lambda e, kc=kc, b=b: e.transpose(cx.ps[b][:, (kc % 4) * 128:(kc % 4 + 1) * 128], hh[:, kc * 128:(kc + 1) * 128], cx.ident[:]),
             reads=[kh, "consts"], writes=["ps%d" % b], sig=(kc % 4 == 3))
    for hb in range(2):
        eng = "act" if hb == 0 else "dve"
        outap = hT[:, hb * 4:(hb + 1) * 4, tok0:tok0 + 128]
        inap = cx.ps[6 + hb][:, :].rearrange("p (a b) -> p a b", a=4)
        if eng == "act":
            S.op("act", lambda e, o=outap, i_=inap: e.activation(out=o, in_=i_, func=AF.Copy), reads=[], writes=[("hT", tok0 // 128), "ps%d" % (6 + hb)])
        else:
            S.op("dve", lambda e, o=outap, i_=inap: e.tensor_copy(out=o, in_=i_), reads=[], writes=[("hT", tok0 // 128), "ps%d" % (6 + hb)])
        if want_f32T is not None:
            o2 = want_f32T[:, hb * 4:(hb + 1) * 4, :]
            S.op("pool" if False else "dve", lambda e, o=o2, i_=inap: e.tensor_copy(out=o, in_=i_), reads=[], writes=["hTf", "ps%d" % (6 + hb)])


def flash_head(cx, h, mode, QT, KT, V, finish_tile):
    S = cx.S
    ncomp = 2 if mode == "att" else 1
    sel3_, r3_ = getattr(cx, "sel3", None), getattr(cx, "r3", None)
    for qb in range(8):
        for c in range(ncomp):
            pb = 0 if mode == "ml" else c * 64
            nk = 4 * qb + 4
            sbs = {}

            def emit_st(kt, qb=qb, pb=pb, sbs=sbs):
                j0 = max(0, kt - 4 * qb)
                c0 = j0 * 128
                sb = cx.stcnt % 2
                cx.stcnt += 1
                sbs[kt] = sb
                stp = cx.ps[sb]
                S.op("pe", lambda e: e.matmul(stp[:, c0:512], lhsT=KT[pb:pb + 64, kt * 128:(kt + 1) * 128], rhs=QT[pb:pb + 64, qb * 512 + c0:qb * 512 + 512], start=True, stop=True),
                     reads=[("QK", h % 2)], writes=["ps%d" % sb])
                if mode == "ml":
                    rb = cx.ps[4 + sb]
                    S.op("pe", lambda e: e.matmul(rb[:, c0:512], lhsT=sel3_[0:24, h, :], rhs=r3_[0:24, qb * 512 + c0:qb * 512 + 512], start=True, stop=True),
                         reads=["r3", "sel3"], writes=["ps%d" % (4 + sb)])

            def emit_post(kt, qb=qb, sbs=sbs):
                j0 = max(0, kt - 4 * qb)
                c0 = j0 * 128
                sb = sbs[kt]
                stp = cx.ps[sb]
                pt = cx.PT[sb]
                kst, kpt = "ps%d" % sb, "PT%d" % sb
                if mode == "att":
                    S.op("act", lambda e: e.activation(out=pt[:, c0:512], in_=stp[:, c0:512], func=AF.Exp, scale=0.125), reads=[], writes=[kpt, kst])
                    if kt >= 4 * qb:
                        S.op("pool", lambda e: e.tensor_tensor(out=pt[:, c0:c0 + 128], in0=pt[:, c0:c0 + 128], in1=cx.mask01b[:], op=ALU.mult),
                             reads=[kpt, "consts2"], writes=[kpt])
                else:
                    rb = cx.ps[4 + sb]
                    krb, kdt = "ps%d" % (4 + sb), "DT%d" % sb
                    dt_ = cx.DT[sb]
                    ucol = cx.ucol[:, kt * 8 + h:kt * 8 + h + 1]
                    if kt >= 4 * qb:
                        S.op("dve", lambda e: e.tensor_tensor(out=dt_[:, c0:c0 + 128], in0=rb[:, c0:c0 + 128], in1=cx.maskneg[:], op=ALU.add), reads=["consts"], writes=[kdt, krb])
                        S.op("act", lambda e: e.activation(out=dt_[:, c0:c0 + 128], in_=dt_[:, c0:c0 + 128], func=AF.Exp, bias=ucol), reads=[kdt, "ucol"], writes=[kdt])
                        if c0 + 128 < 512:
                            S.op("act", lambda e: e.activation(out=dt_[:, c0 + 128:512], in_=rb[:, c0 + 128:512], func=AF.Exp, bias=ucol), reads=["ucol"], writes=[kdt, krb])
                    else:
                        S.op("act", lambda e: e.activation(out=dt_[:, 0:512], in_=rb[:, 0:512], func=AF.Exp, bias=ucol), reads=["ucol"], writes=[kdt, krb])
                    S.op("dve", lambda e: e.scalar_tensor_tensor(out=pt[:, c0:512], in0=stp[:, c0:512], scalar=0.125, in1=dt_[:, c0:512], op0=ALU.mult, op1=ALU.mult),
                         reads=[kdt], writes=[kpt, kst])

            def emit_pv(kt, qb=qb, sbs=sbs):
                j0 = max(0, kt - 4 * qb)
                sb = sbs[kt]
                pt = cx.PT[sb]
                kpt = "PT%d" % sb
                for i in range(j0, 4):
                    ob = cx.ps[2 + i // 2]
                    oap = ob[:, (i % 2) * 256:(i % 2) * 256 + 129]
                    last = (kt == 4 * qb + i)
                    S.op("pe", lambda e, oap=oap, i=i, last=last: e.matmul(oap, lhsT=pt[:, i * 128:(i + 1) * 128], rhs=V[:, kt, 0:129], start=(kt == 0 and i % 2 == 0), stop=last, skip_group_check=True),
                         reads=[kpt, ("V", h % 2)], writes=["ps%d" % (2 + i // 2)], sig=(last or i == 3))

            emit_st(0)
            for kt in range(nk):
                if kt + 1 < nk:
                    emit_st(kt + 1)
                emit_post(kt)
                emit_pv(kt)
            for i in range(4):
                ob = cx.ps[2 + i // 2]
                oap = ob[:, (i % 2) * 256:(i % 2) * 256 + 129]
                finish_tile(4 * qb + i, c, oap, "ps%d" % (2 + i // 2))


def rstd_ops(cx, ssap, n, inv_n, key):
    S = cx.S
    S.op("dve", lambda e: e.tensor_scalar(out=ssap[:, n:2 * n], in0=ssap[:, 0:n], scalar1=inv_n, scalar2=EPS, op0=ALU.mult, op1=ALU.add), reads=[key], writes=[key])
    S.op("act", lambda e: e.activation(out=ssap[:, n:2 * n], in_=ssap[:, n:2 * n], func=AF.Sqrt), reads=[key], writes=[key])
    S.op("dve", lambda e: e.reciprocal(out=ssap[:, 2 * n:3 * n], in_=ssap[:, n:2 * n]), reads=[key], writes=[key])


def load_cast_w(cx, dst, stage, kstage, kdst, srcs, q="sp", cast_eng="pool"):
    S = cx.S
    for (ap, off, w) in srcs:
        S.dma(q, stage[:, :, off:off + w], ap.rearrange("(kc p) j -> p kc j", p=128), writes=[kstage])
    if cast_eng == "act":
        S.op("act", lambda e: e.activation(out=dst[:], in_=stage[:], func=AF.Copy), reads=[kstage], writes=[kdst])
    else:
        S.op(cast_eng, lambda e: e.tensor_copy(out=dst[:], in_=stage[:]), reads=[kstage], writes=[kdst])


def phase_attn(cx, x_d, xo_d, c_d, pos_d, adaw_d, adab_d, mixn_d, win_d, wout_d, qn_d, kn_d, lam_ds, subn_d, oT_d):
    S, nc = cx.S, cx.nc
    st = cx.stack
    lam_init = 0.2
    common_setup(cx, 0, c_d, adaw_d, adab_d, mixn_d)
    hT = T(cx, "hT", [128, 8, S_LEN], BF16)
    GQK = T(cx, "GQK", [128, 256])
    for j, d_ in enumerate((qn_d, qn_d, kn_d, kn_d)):
        S.dma("pool", GQK[:, j * 64:(j + 1) * 64], d_.partition_broadcast(128), writes=["GQK"])
    lamt = T(cx, "lamt", [128, 4, 64])
    for j, d_ in enumerate(lam_ds):
        S.dma("pool", lamt[:, j, :], d_.partition_broadcast(128), writes=["lamt"])
    lams = T(cx, "lams", [128, 8])
    S.op("dve", lambda e: e.tensor_tensor(out=lamt[:, 0, :], in0=lamt[:, 0, :], in1=lamt[:, 1, :], op=ALU.mult), reads=["lamt"], writes=["lamt"])
    S.op("dve", lambda e: e.tensor_tensor(out=lamt[:, 2, :], in0=lamt[:, 2, :], in1=lamt[:, 3, :], op=ALU.mult), reads=["lamt"], writes=["lamt"])
    S.op("dve", lambda e: e.reduce_sum(out=lams[:, 0:1], in_=lamt[:, 0, :], axis=AX.X), reads=["lamt"], writes=["lams"])
    S.op("dve", lambda e: e.reduce_sum(out=lams[:, 1:2], in_=lamt[:, 2, :], axis=AX.X), reads=["lamt"], writes=["lams"])
    S.op("act", lambda e: e.activation(out=lams[:, 2:4], in_=lams[:, 0:2], func=AF.Exp), reads=["lams"], writes=["lams"])
    S.op("dve", lambda e: e.tensor_tensor(out=lams[:, 4:5], in0=lams[:, 3:4], in1=lams[:, 2:3], op=ALU.subtract), reads=["lams"], writes=["lams"])
    S.op("dve", lambda e: e.tensor_scalar(out=lams[:, 5:6], in0=lams[:, 4:5], scalar1=-lam_init, scalar2=None, op0=ALU.add), reads=["lams"], writes=["lams"])
    neglam = lams[:, 5:6]
    subg = T(cx, "subg", [128, 2])
    S.dma("pool", subg[:, 0:1], subn_d.rearrange("o (p a) -> (o p) a", a=1), writes=["subg"])
    S.op("dve", lambda e: e.tensor_scalar(out=subg[:, 1:2], in0=subg[:, 0:1], scalar1=1.0 - lam_init, scalar2=None, op0=ALU.mult), reads=["subg"], writes=["subg"])
    cosT = T(cx, "cosT", [128, NT, 32])
    sinT = T(cx, "sinT", [128, NT, 32])
    with ExitStack() as st2:
        posi = st2.enter_context(nc.sbuf_tensor(uname("posi"), [32, 128], I32))
        posf = st2.enter_context(nc.sbuf_tensor(uname("posf"), [32, 128], F32))
        post = st2.enter_context(nc.sbuf_tensor(uname("post"), [128, 32], F32))
        ang = st2.enter_context(nc.sbuf_tensor(uname("ang"), [128, NT, 32], F32))
        tmpf = st2.enter_context(nc.sbuf_tensor(uname("tmpf"), [128, NT, 32], F32))
        tmpi = st2.enter_context(nc.sbuf_tensor(uname("tmpi"), [128, NT, 32], I32))
        S.dma("sp", posi[:], pos_d.rearrange("o (t p) -> (o t) p", p=128), writes=["posi"])
        S.op("dve", lambda e: e.tensor_copy(out=posf[:], in_=posi[:]), reads=["posi"], writes=["posf"])
        S.op("pe", lambda e: e.transpose(cx.ps[0][:, 0:32], posf[:], cx.ident[0:32, 0:32]), reads=["posf", "consts"], writes=["ps0"])
        S.op("dve", lambda e: e.tensor_copy(out=post[:], in_=cx.ps[0][:, 0:32]), reads=[], writes=["post", "ps0"])
        S.op("dve", lambda e: e.tensor_tensor(out=ang[:], in0=post[:].unsqueeze(2).to_broadcast([128, NT, 32]), in1=cx.invf[:].unsqueeze(1).to_broadcast([128, NT, 32]), op=ALU.mult),
             reads=["post", "consts"], writes=["ang"])
        for which, dstT in ((0, sinT), (1, cosT)):
            off = 0.0 if which == 0 else PI / 2
            S.op("dve", lambda e, off=off: e.tensor_scalar(out=tmpf[:], in0=ang[:], scalar1=off, scalar2=1.0 / (2 * PI), op0=ALU.add, op1=ALU.mult), reads=["ang"], writes=["tmpf"])
            S.op("dve", lambda e: e.tensor_copy(out=tmpi[:], in_=tmpf[:]), reads=["tmpf"], writes=["tmpi"])
            S.op("dve", lambda e: e.tensor_copy(out=tmpf[:], in_=tmpi[:]), reads=["tmpi"], writes=["tmpf"])
            S.op("dve", lambda e: e.scalar_tensor_tensor(out=tmpf[:], in0=tmpf[:], scalar=-2 * PI, in1=ang[:], op0=ALU.mult, op1=ALU.add), reads=["tmpf", "ang"], writes=["tmpf"])
            S.op("dve", lambda e, off=off: e.tensor_scalar(out=tmpf[:], in0=tmpf[:], scalar1=off, scalar2=PI, op0=ALU.add, op1=ALU.min), reads=["tmpf"], writes=["tmpf"])
            S.op("dve", lambda e: e.tensor_scalar(out=tmpf[:], in0=tmpf[:], scalar1=-PI, scalar2=None, op0=ALU.max), reads=["tmpf"], writes=["tmpf"])
            S.op("act", lambda e, dstT=dstT: e.activation(out=dstT[:], in_=tmpf[:], func=AF.Sin), reads=["tmpf"], writes=["rope"])
        S.barrier()
    with ExitStack() as st2:
        cx.xt = [st2.enter_context(nc.sbuf_tensor(uname("xt%d" % i), [128, 1024], F32)) for i in range(2)]
        cx.hh = [st2.enter_context(nc.sbuf_tensor(uname("hh%d" % i), [128, 1024], F32)) for i in range(2)]
        cx.jk = st2.enter_context(nc.sbuf_tensor(uname("jk"), [128, 1024], F32))
        cx.nss = [st2.enter_context(nc.sbuf_tensor(uname("nss%d" % i), [128, 4], F32)) for i in range(2)]
        cx.ncnt = 0
        for t in range(NT):
            norm_tile(cx, t, x_d, hT, t * 128)
        S.barrier()
    with ExitStack() as st2:
        def TT(name, shape, dt=F32):
            return st2.enter_context(nc.sbuf_tensor(uname(name), shape, dt))
        wst = TT("wst", [128, 8, 384])
        wb = [TT("wb%d" % i, [128, 8, 384], BF16) for i in range(2)]
        QKT = [TT("QKT%d" % i, [128, 2, S_LEN], BF16) for i in range(2)]
        V = [TT("V%d" % i, [128, NT, 132], BF16) for i in range(2)]
        cx.PT = [TT("PT%d" % i, [128, 512], BF16) for i in range(2)]
        cx.mask01b = TT("mask01b", [128, 128], BF16)
        sq = [TT("sq%d" % i, [128, 256]) for i in range(2)]
        qn = [TT("qn%d" % i, [128, 256]) for i in range(2)]
        qr = [TT("qr%d" % i, [128, 256]) for i in range(2)]
        t1 = [TT("t1_%d" % i, [128, 128]) for i in range(2)]
        t2 = [TT("t2_%d" % i, [128, 128]) for i in range(2)]
        pss = [TT("pss%d" % i, [128, 12]) for i in range(2)]
        o0 = TT("o0", [128, 4, 128])
        ot = [TT("ot%d" % i, [128, 128]) for i in range(2)]
        ojk = TT("ojk", [128, 128])
        osb = [TT("osb%d" % i, [128, 132]) for i in range(2)]
        oss = [TT("oss%d" % i, [128, 4]) for i in range(2)]
        oTb = [TT("oTb%d" % i, [128, S_LEN], BF16) for i in range(2)]
        S.op("dve", lambda e: e.tensor_copy(out=cx.mask01b[:], in_=cx.mask01[:]), reads=["consts"], writes=["consts2"])
        for i in range(2):
            S.op("pool", lambda e, i=i: e.memset(V[i][:, :, 128:129], 1.0), writes=[("V", i)])
        cx.stcnt = 0
        fcnt = [0]
        for h in range(8):
            hb = h % 2
            load_cast_w(cx, wb[hb], wst, "wst", "wb%d" % hb,
                        [(win_d[:, o_ * 1024 + h * 128:o_ * 1024 + (h + 1) * 128], o_ * 128, 128) for o_ in range(3)], q="pool", cast_eng="pool")
            def proj(t, hb=hb):
                p = t % 2
                P1 = cx.ps[4 + p]
                kp = "ps%d" % (4 + p)
                for kc in range(8):
                    S.op("pe", lambda e, P1=P1, kc=kc, t=t, hb=hb: e.matmul(P1[:, 0:384], lhsT=hT[:, kc, t * 128:(t + 1) * 128], rhs=wb[hb][:, kc, :], start=(kc == 0), stop=(kc == 7)),
                         reads=[("hT", t), "wb%d" % hb], writes=[kp], sig=(kc == 7))

            def rest(t, hb=hb):
                p = t % 2
                P1 = cx.ps[4 + p]
                kp = "ps%d" % (4 + p)
                ksq, kqn, kqr, kt1, kt2, kss = "sq%d" % p, "qn%d" % p, "qr%d" % p, "t1_%d" % p, "t2_%d" % p, "pss%d" % p
                S.op("act", lambda e, P1=P1, p=p: e.activation(out=sq[p][:], in_=P1[:, 0:256], func=AF.Square), reads=[], writes=[ksq, kp])
                S.op("dve", lambda e, p=p: e.reduce_sum(out=pss[p][:, 0:4], in_=sq[p][:].rearrange("p (a b) -> p a b", a=4), axis=AX.X), reads=[ksq], writes=[kss])
                rstd_ops(cx, pss[p], 4, 1.0 / 64, kss)
                S.op("dve", lambda e, P1=P1, p=p: e.tensor_tensor(out=qn[p][:].rearrange("p (a b) -> p a b", a=4), in0=P1[:, 0:256].rearrange("p (a b) -> p a b", a=4),
                                                             in1=pss[p][:, 8:12].unsqueeze(2).to_broadcast([128, 4, 64]), op=ALU.mult), reads=[kss], writes=[kqn, kp])
                S.op("pool", lambda e, p=p: e.tensor_tensor(out=qn[p][:], in0=qn[p][:], in1=GQK[:], op=ALU.mult), reads=[kqn, "GQK"], writes=[kqn])
                q4 = qn[p][:].rearrange("p (a b) -> p a b", a=4)
                r4 = qr[p][:].rearrange("p (a b) -> p a b", a=4)
                cb = cosT[:, t, :].unsqueeze(1).to_broadcast([128, 4, 32])
                sb_ = sinT[:, t, :].unsqueeze(1).to_broadcast([128, 4, 32])
                a4 = t1[p][:].rearrange("p (a b) -> p a b", a=4)
                b4 = t2[p][:].rearrange("p (a b) -> p a b", a=4)
                S.op("dve", lambda e, a4=a4, q4=q4, cb=cb: e.tensor_tensor(out=a4, in0=q4[:, :, 0:32], in1=cb, op=ALU.mult), reads=[kqn, "rope"], writes=[kt1])
                S.op("pool", lambda e, b4=b4, q4=q4, sb_=sb_: e.tensor_tensor(out=b4, in0=q4[:, :, 32:64], in1=sb_, op=ALU.mult), reads=[kqn, "rope"], writes=[kt2])
                S.op("dve", lambda e, a4=a4, b4=b4, r4=r4: e.tensor_tensor(out=r4[:, :, 0:32], in0=a4, in1=b4, op=ALU.subtract), reads=[kt1, kt2], writes=[kqr])
                S.op("pool", lambda e, a4=a4, q4=q4, cb=cb: e.tensor_tensor(out=a4, in0=q4[:, :, 32:64], in1=cb, op=ALU.mult), reads=[kqn, "rope", kqr], writes=[kt1])
                S.op("dve", lambda e, b4=b4, q4=q4, sb_=sb_: e.tensor_tensor(out=b4, in0=q4[:, :, 0:32], in1=sb_, op=ALU.mult), reads=[kqn, "rope", kqr], writes=[kt2])
                S.op("pool", lambda e, a4=a4, b4=b4, r4=r4: e.tensor_tensor(out=r4[:, :, 32:64], in0=a4, in1=b4, op=ALU.add), reads=[kt1, kt2], writes=[kqr])
                for j in range(2):
                    S.op("pe", lambda e, j=j, p=p: e.transpose(cx.ps[6][:, j * 128:(j + 1) * 128], qr[p][:, j * 128:(j + 1) * 128], cx.ident[:]), reads=[kqr, "consts"], writes=["ps6"], sig=(j == 1))
                S.op("act", lambda e, t=t, hb=hb: e.activation(out=QKT[hb][:, :, t * 128:(t + 1) * 128], in_=cx.ps[6][:, 0:256].rearrange("p (a b) -> p a b", a=2), func=AF.Copy),
                     reads=[], writes=[("QK", hb), "ps6"])
                S.op("dve", lambda e, P1=P1, t=t, hb=hb: e.tensor_copy(out=V[hb][:, t, 0:128], in_=P1[:, 256:384]), reads=[], writes=[("V", hb), kp])

            proj(0)
            for t in range(NT):
                if t + 1 < NT:
                    proj(t + 1)
                rest(t)

            def finish(Tq, c, oap_ps, okey, h=h, hb=hb):
                i = Tq % 4
                f = fcnt[0] % 2
                fcnt[0] += 1
                kos, kot, kob = "oss%d" % f, "ot%d" % f, "osb%d" % f
                oap = osb[f]
                if f == 0:
                    S.op("act", lambda e: e.activation(out=oap[:, 0:129], in_=oap_ps, func=AF.Copy), reads=[], writes=[kob, okey])
                else:
                    S.op("dve", lambda e: e.tensor_copy(out=oap[:, 0:129], in_=oap_ps), reads=[], writes=[kob, okey])
                S.op("dve", lambda e: e.reciprocal(out=oss[f][:, 0:1], in_=oap[:, 128:129]), reads=[kob], writes=[kos])
                if c == 0:
                    S.op("act", lambda e: e.activation(out=o0[:, i, :], in_=oap[:, 0:128], func=AF.Copy, scale=oss[f][:, 0:1]), reads=[kob, kos], writes=[("o0", i)])
                    return
                S.op("act", lambda e: e.activation(out=ot[f][:], in_=oap[:, 0:128], func=AF.Copy, scale=oss[f][:, 0:1]), reads=[kob, kos], writes=[kot])
                S.op("dve", lambda e: e.scalar_tensor_tensor(out=ot[f][:], in0=ot[f][:], scalar=neglam, in1=o0[:, i, :], op0=ALU.mult, op1=ALU.add), reads=[kot, ("o0", i), "lams"], writes=[kot])
                S.op("act", lambda e: e.activation(out=ojk[:], in_=ot[f][:], func=AF.Square, accum_out=oss[f][:, 1:2]), reads=[kot], writes=["ojk", kos])
                S.op("dve", lambda e: e.tensor_scalar(out=oss[f][:, 2:3], in0=oss[f][:, 1:2], scalar1=1.0 / 128, scalar2=EPS, op0=ALU.mult, op1=ALU.add), reads=[kos], writes=[kos])
                S.op("act", lambda e: e.activation(out=oss[f][:, 2:3], in_=oss[f][:, 2:3], func=AF.Sqrt), reads=[kos], writes=[kos])
                S.op("dve", lambda e: e.reciprocal(out=oss[f][:, 3:4], in_=oss[f][:, 2:3]), reads=[kos], writes=[kos])
                S.op("dve", lambda e: e.tensor_scalar(out=ot[f][:], in0=ot[f][:], scalar1=oss[f][:, 3:4], scalar2=None, op0=ALU.mult), reads=[kot, kos], writes=[kot])
                S.op("pe", lambda e: e.transpose(cx.ps[7][:, 0:128], ot[f][:], cx.ident[:]), reads=[kot, "consts"], writes=["ps7"])
                S.op("act", lambda e: e.activation(out=oTb[hb][:, Tq * 128:(Tq + 1) * 128], in_=cx.ps[7][:, 0:128], func=AF.Copy, scale=subg[:, 1:2]), reads=["subg"], writes=[("oTb", hb), "ps7"])

            flash_head(cx, h, "att", QKT[hb][:, 0, :], QKT[hb][:, 1, :], V[hb], finish)
            S.dma("sp", oT_d[h], oTb[hb][:], reads=[("oTb", hb)], writes=["oT_d"])
        S.barrier()
    out_proj(cx, x_d, xo_d, wout_d, oT_d)


def out_proj(cx, x_d, xo_d, wout_d, oT_d):
    S, nc = cx.S, cx.nc
    with ExitStack() as st2:
        def TT(name, shape, dt=F32):
            return st2.enter_context(nc.sbuf_tensor(uname(name), shape, dt))
        wst = TT("wost", [128, 8, 1024])
        wo = TT("wo", [128, 8, 1024], BF16)
        oTt = [TT("oTt%d" % i, [128, 8, 128], BF16) for i in range(2)]
        xt = [TT("xo%d" % i, [128, 1024]) for i in range(2)]
        yt = [TT("yo%d" % i, [128, 1024]) for i in range(2)]
        load_cast_w(cx, wo, wst, "wost", "wo", [(wout_d, 0, 1024)], q="pool", cast_eng="pool")
        for t in range(NT):
            p = t % 2
            S.dma("sp", oTt[p][:], oT_d[:, :, t * 128:(t + 1) * 128].rearrange("h p j -> p h j"), reads=["oT_d"], writes=["oTt%d" % p])
            S.dma("sp", xt[p][:], x_d[t * 128:(t + 1) * 128, :], writes=["xo%d" % p])
            for n in range(2):
                pb = cx.ps[2 * p + n]
                kp = "ps%d" % (2 * p + n)
                for h in range(8):
                    S.op("pe", lambda e, pb=pb, h=h, p=p, n=n: e.matmul(pb[:, :], lhsT=oTt[p][:, h, :], rhs=wo[:, h, n * 512:(n + 1) * 512], start=(h == 0), stop=(h == 7)),
                         reads=["oTt%d" % p, "wo"], writes=[kp], sig=(h == 7))
                S.op("dve", lambda e, pb=pb, p=p, n=n: e.tensor_tensor(out=yt[p][:, n * 512:(n + 1) * 512], in0=pb[:, :], in1=cx.G[:, n * 512:(n + 1) * 512], op=ALU.mult),
                     reads=["mod2"], writes=["yo%d" % p, kp])
            S.op("pool", lambda e, p=p: e.tensor_tensor(out=yt[p][:], in0=yt[p][:], in1=xt[p][:], op=ALU.add), reads=["yo%d" % p, "xo%d" % p], writes=["yo%d" % p])
            S.dma("sp", xo_d[t * 128:(t + 1) * 128, :], yt[p][:], reads=["yo%d" % p], writes=["xo_d"])
        S.barrier()


def new_ctx(nc, stack):
    cx = Ctx()
    cx.nc = nc
    cx.stack = stack
    cx.S = Sched(nc, stack)
    cx.ps = [stack.enter_context(nc.psum_tensor("psb%d" % i, [128, 512], F32)) for i in range(8)]
    cx.consts = T(cx, "consts", [128, 1024])
    cx.ident = cx.consts[:, 0:128]
    cx.mask01 = cx.consts[:, 128:256]
    cx.maskneg = cx.consts[:, 256:384]
    cx.invf = cx.consts[:, 384:416]
    cx.SH = T(cx, "SH", [128, 1024])
    cx.A = T(cx, "A", [128, 1024])
    cx.G = T(cx, "G", [128, 1024])
    return cx


def dram_in(nc, name, shape, dt=F32):
    return nc.dram_tensor(name, list(shape), dt, kind="ExternalInput").ap()


def build_attn_prog():
    nc = bass.Bass("TRN2", target_bir_lowering=False)
    x_d = dram_in(nc, "x", [S_LEN, D])
    c_d = dram_in(nc, "c", [1, D])
    pos_d = dram_in(nc, "pos", [1, S_LEN], I32)
    adaw_d = dram_in(nc, "adaw", [D, 6 * D])
    adab_d = dram_in(nc, "adab", [1, 6 * D])
    mixn_d = dram_in(nc, "mixn", [1, D])
    win_d = dram_in(nc, "win", [D, 3 * D])
    wout_d = dram_in(nc, "wout", [D, D])
    qn_d = dram_in(nc, "qn", [1, 64])
    kn_d = dram_in(nc, "kn", [1, 64])
    lam_ds = [dram_in(nc, "lam%d" % i, [1, 64]) for i in range(4)]
    subn_d = dram_in(nc, "subn", [1, 128])
    consts_d = dram_in(nc, "consts", [128, 1024])
    xo_d = nc.dram_tensor("xo", [S_LEN, D], F32, kind="ExternalOutput").ap()
    oT_d = nc.dram_tensor("oT_d", [8, 128, S_LEN], BF16, kind="Internal").ap()
    with ExitStack() as stack:
        cx = new_ctx(nc, stack)
        block = stack.enter_context(nc.Block())
        cx.S.dma("sp", cx.consts[:], consts_d, writes=["consts"])
        phase_attn(cx, x_d, xo_d, c_d, pos_d, adaw_d, adab_d, mixn_d, win_d, wout_d, qn_d, kn_d, lam_ds, subn_d, oT_d)
        print("instructions", cx.S.nins, "waits", cx.S.nwait)
        cx.S.emit(block)
    return nc


def phase_moe(cx, x_d, xo_d, c_d, adaw_d, adab_d, ffnn_d, wr_d, br_d, wgu_d, bgu_d, wd_d, bd_d, n_exp=32):
    S, nc = cx.S, cx.nc
    common_setup(cx, 1, c_d, adaw_d, adab_d, ffnn_d)
    QT_ = 1024
    NQ = S_LEN // QT_
    with ExitStack() as st2:
        def TT(name, shape, dt=F32):
            return st2.enter_context(nc.sbuf_tensor(uname(name), shape, dt))
        biasT = TT("biasT", [128, 16, 32])
        with ExitStack() as st3:
            bgr = st3.enter_context(nc.sbuf_tensor(uname("bgr"), [32, 2048], F32))
            S.dma("pool", bgr[:], bgu_d, writes=["bgr"])
            bgr3 = bgr[:].rearrange("e (f two) -> e f two", two=2)
            for j in range(16):
                src = bgr3[:, (j % 8) * 128:(j % 8 + 1) * 128, j // 8]
                S.op("pe", lambda e, j=j, src=src: e.transpose(cx.ps[j // 8][:, (j % 8) * 32:(j % 8) * 32 + 32], src, cx.ident[0:32, 0:32]), reads=["bgr", "consts"], writes=["ps%d" % (j // 8)])
            for g in range(2):
                S.op("dve", lambda e, g=g: e.tensor_copy(out=biasT[:, g * 8:(g + 1) * 8, :], in_=cx.ps[g][:, 0:256].rearrange("p (a b) -> p a b", a=8)), reads=[], writes=["biasT", "ps%d" % g])
            S.op("dve", lambda e: e.tensor_scalar(out=biasT[:, 8:16, :], in0=biasT[:, 8:16, :], scalar1=1.0, scalar2=None, op0=ALU.add), reads=["biasT"], writes=["biasT"])
            S.barrier()
        hT = TT("hTm", [128, 8, QT_], BF16)
        hTf = TT("hTf", [128, 8, 128])
        acc = TT("acc", [128, 8, 1024])
        wr = TT("wr", [128, 8, 32])
        brb = TT("brb", [128, 32])
        bd = TT("bd", [32, 1024])
        gates = TT("gates", [128, 8, 32])
        gT = TT("gT", [32, 128])
        rt = TT("rt", [128, 96])
        r8 = TT("r8", [128, 16])
        wgu = [TT("wgu%d" % i, [128, 8, 1024], BF16) for i in range(2)]
        wdb = [TT("wdb%d" % i, [128, 4, 1024], BF16) for i in range(2)]
        NSTG = 4
        stg = [TT("stg%d" % i, [128, 1024]) for i in range(NSTG)]
        actT = TT("actT", [128, 4, 512], BF16)
        gt = [TT("gt%d" % i, [128, 512]) for i in range(2)]
        sg = [TT("sg%d" % i, [128, 512]) for i in range(2)]
        lt = [TT("lt%d" % i, [128, 512]) for i in range(2)]
        cx.xt = [TT("xt%d" % i, [128, 1024]) for i in range(2)]
        cx.hh = [TT("hh%d" % i, [128, 1024]) for i in range(2)]
        cx.jk = TT("jk", [128, 1024])
        cx.nss = [TT("nss%d" % i, [128, 4]) for i in range(2)]
        cx.ncnt = 0
        S.dma("pool", wr[:], wr_d.rearrange("(kc p) e -> p kc e", p=128), writes=["wr"])
        S.dma("pool", brb[:], br_d.partition_broadcast(128), writes=["brb"])
        S.dma("pool", bd[:], bd_d, writes=["bd"])
        NU = n_exp * 2

        def load_dma(seq, s_):
            u = seq % NU
            ex_, hf = u // 2, u % 2
            sb = (seq * 12 + s_) % NSTG
            ks = "stg%d" % sb
            if s_ < 8:
                S.dma("sp", stg[sb][:], wgu_d[ex_, s_ * 128:(s_ + 1) * 128, hf * 1024:(hf + 1) * 1024], writes=[ks])
            else:
                fc = s_ - 8
                S.dma("sp", stg[sb][:], wd_d[ex_, hf * 512 + fc * 128:hf * 512 + (fc + 1) * 128, :], writes=[ks])

        def load_cast(seq, s_):
            wb2 = seq % 2
            sb = (seq * 12 + s_) % NSTG
            ks = "stg%d" % sb
            if s_ < 8:
                s3 = stg[sb][:].rearrange("p (f two) -> p f two", two=2)
                S.op("act", lambda e: e.activation(out=wgu[wb2][:, s_, 0:512], in_=s3[:, :, 0], func=AF.Copy), reads=[ks], writes=["wgu%d" % wb2])
                S.op("pool", lambda e: e.tensor_copy(out=wgu[wb2][:, s_, 512:1024], in_=s3[:, :, 1]), reads=[ks], writes=["wgu%d" % wb2])
            else:
                fc = s_ - 8
                S.op("act", lambda e: e.activation(out=wdb[wb2][:, fc, :], in_=stg[sb][:], func=AF.Copy), reads=[ks], writes=["wdb%d" % wb2])

        for qtr in range(NQ):
            for tl in range(8):
                t = qtr * 8 + tl
                norm_tile(cx, t, x_d, hT, tl * 128, want_f32T=hTf)
                for kc in range(8):
                    S.op("pe", lambda e, kc=kc: e.matmul(cx.ps[5][:, 0:32], lhsT=hTf[:, kc, :], rhs=wr[:, kc, :], start=(kc == 0), stop=(kc == 7)), reads=["hTf", "wr"], writes=["ps5"], sig=(kc == 7))
                S.op("dve", lambda e: e.tensor_tensor(out=rt[:, 0:32], in0=cx.ps[5][:, 0:32], in1=brb[:], op=ALU.add), reads=["brb"], writes=["rt", "ps5"])
                S.op("dve", lambda e: e.max(out=r8[:, 0:8], in_=rt[:, 0:32]), reads=["rt"], writes=["r8"])
                S.op("dve", lambda e: e.tensor_scalar(out=rt[:, 32:64], in0=rt[:, 0:32], scalar1=r8[:, 3:4], scalar2=None, op0=ALU.is_ge), reads=["rt", "r8"], writes=["rt"])
                S.op("dve", lambda e: e.tensor_scalar(out=r8[:, 8:9], in0=r8[:, 0:1], scalar1=-1.0, scalar2=None, op0=ALU.mult), reads=["r8"], writes=["r8"])
                S.op("act", lambda e: e.activation(out=rt[:, 64:96], in_=rt[:, 0:32], func=AF.Exp, bias=r8[:, 8:9]), reads=["rt", "r8"], writes=["rt"])
                S.op("dve", lambda e: e.tensor_tensor(out=rt[:, 64:96], in0=rt[:, 64:96], in1=rt[:, 32:64], op=ALU.mult), reads=["rt"], writes=["rt"])
                S.op("dve", lambda e: e.reduce_sum(out=r8[:, 9:10], in_=rt[:, 64:96], axis=AX.X), reads=["rt"], writes=["r8"])
                S.op("dve", lambda e: e.reciprocal(out=r8[:, 10:11], in_=r8[:, 9:10]), reads=["r8"], writes=["r8"])
                S.op("dve", lambda e, tl=tl: e.tensor_scalar(out=gates[:, tl, :], in0=rt[:, 64:96], scalar1=r8[:, 10:11], scalar2=None, op0=ALU.mult), reads=["rt", "r8"], writes=["gates"])
                S.op("pe", lambda e, tl=tl: e.transpose(cx.ps[5][0:32, 128:256], gates[:, tl, :], cx.ident[:]), reads=["gates", "consts"], writes=["ps5"])
                S.op("dve", lambda e: e.tensor_copy(out=gT[:], in_=cx.ps[5][0:32, 128:256]), reads=[], writes=["gT", "ps5"])
                for n in range(2):
                    S.op("pe", lambda e, n=n: e.matmul(cx.ps[4][:, :], lhsT=gT[:], rhs=bd[:, n * 512:(n + 1) * 512], start=True, stop=True), reads=["gT", "bd"], writes=["ps4"])
                    S.op("act", lambda e, tl=tl, n=n: e.activation(out=acc[:, tl, n * 512:(n + 1) * 512], in_=cx.ps[4][:, :], func=AF.Copy), reads=[], writes=[("acc", tl), "ps4"])
            for u in range(NU):
                ex, hf = u // 2, u % 2
                seq = qtr * NU + u
                wb_ = seq % 2
                if seq == 0:
                    for s_ in range(12):
                        load_dma(0, s_)
                        load_cast(0, s_)
                nxt = seq + 1 if seq + 1 < NQ * NU else None
                slot = [0]

                def tick(nxt=nxt, slot=slot):
                    s_ = slot[0]
                    slot[0] += 1
                    if nxt is None:
                        return
                    if s_ < 12:
                        load_dma(nxt, s_)
                    if 2 <= s_ < 14:
                        load_cast(nxt, s_ - 2)
                for tb in range(QT_ // 512):
                    for j in range(4):
                        p = j % 2
                        jj = hf * 4 + j
                        psg, psl = cx.ps[2 * p], cx.ps[2 * p + 1]
                        kg, kl = "ps%d" % (2 * p), "ps%d" % (2 * p + 1)
                        for (pp, kk, off) in ((psg, kg, 0), (psl, kl, 512)):
                            for kc in range(8):
                                S.op("pe", lambda e, pp=pp, kc=kc, off=off, j=j, tb=tb, wb_=wb_: e.matmul(pp[:, :], lhsT=wgu[wb_][:, kc, off + j * 128:off + (j + 1) * 128], rhs=hT[:, kc, tb * 512:(tb + 1) * 512], start=(kc == 0), stop=(kc == 7)),
                                     reads=["wgu%d" % wb_] + [("hT", tb * 4 + q_) for q_ in range(4)], writes=[kk], sig=(kc == 7))
                        S.op("dve", lambda e, p=p, psg=psg, jj=jj, ex=ex: e.tensor_scalar(out=gt[p][:], in0=psg[:, :], scalar1=biasT[:, jj, ex:ex + 1], scalar2=7.0, op0=ALU.add, op1=ALU.min), reads=["biasT"], writes=["gt%d" % p, kg])
                        S.op("act", lambda e, p=p: e.activation(out=sg[p][:], in_=gt[p][:], func=AF.Sigmoid, scale=1.702), reads=["gt%d" % p], writes=["sg%d" % p])
                        S.op("dve", lambda e, p=p, psl=psl, jj=jj, ex=ex: e.tensor_scalar(out=lt[p][:], in0=psl[:, :], scalar1=biasT[:, 8 + jj, ex:ex + 1], scalar2=8.0, op0=ALU.add, op1=ALU.min), reads=["biasT"], writes=["lt%d" % p, kl])
                        S.op("pool", lambda e, p=p: e.tensor_tensor(out=gt[p][:], in0=gt[p][:], in1=sg[p][:], op=ALU.mult), reads=["gt%d" % p, "sg%d" % p], writes=["gt%d" % p])
                        S.op("dve", lambda e, p=p, j=j: e.scalar_tensor_tensor(out=actT[:, j, :], in0=lt[p][:], scalar=-6.0, in1=gt[p][:], op0=ALU.max, op1=ALU.mult), reads=["gt%d" % p, "lt%d" % p], writes=[("actT", j)])
                        tick()
                    for tt in range(4):
                        tl = tb * 4 + tt
                        for n in range(2):
                            pb = cx.ps[4 + n]
                            kp = "ps%d" % (4 + n)
                            for fc in range(4):
                                S.op("pe", lambda e, pb=pb, fc=fc, tt=tt, n=n, wb_=wb_: e.matmul(pb[:, :], lhsT=actT[:, fc, tt * 128:(tt + 1) * 128], rhs=wdb[wb_][:, fc, n * 512:(n + 1) * 512], start=(fc == 0), stop=(fc == 3)),
                                     reads=[("actT", fc), "wdb%d" % wb_], writes=[kp], sig=(fc == 3))
                            S.op("dve", lambda e, pb=pb, tl=tl, n=n, ex=ex: e.scalar_tensor_tensor(out=acc[:, tl, n * 512:(n + 1) * 512], in0=pb[:, :], scalar=gates[:, tl, ex:ex + 1], in1=acc[:, tl, n * 512:(n + 1) * 512], op0=ALU.mult, op1=ALU.add),
                                 reads=["gates"], writes=[("acc", tl), kp])
                        tick()
            for tl in range(8):
                t = qtr * 8 + tl
                p = tl % 2
                xtp = cx.xt[p]
                S.dma("sp", xtp[:], x_d[t * 128:(t + 1) * 128, :], writes=["xt%d" % p])
                S.op("dve", lambda e, tl=tl: e.tensor_tensor(out=acc[:, tl, :], in0=acc[:, tl, :], in1=cx.G[:], op=ALU.mult), reads=["mod2"], writes=[("acc", tl)])
                S.op("dve", lambda e, tl=tl, xtp=xtp: e.tensor_tensor(out=acc[:, tl, :], in0=acc[:, tl, :], in1=xtp[:], op=ALU.add), reads=["xt%d" % p], writes=[("acc", tl)])
                S.dma("sp", xo_d[t * 128:(t + 1) * 128, :], acc[:, tl, :], reads=[("acc", tl)], writes=["xo_d"])
        S.barrier()


def build_moe_prog(n_exp=32):
    nc = bass.Bass("TRN2", target_bir_lowering=False)
    x_d = dram_in(nc, "x", [S_LEN, D])
    c_d = dram_in(nc, "c", [1, D])
    adaw_d = dram_in(nc, "adaw", [D, 6 * D])
    adab_d = dram_in(nc, "adab", [1, 6 * D])
    ffnn_d = dram_in(nc, "ffnn", [1, D])
    wr_d = dram_in(nc, "wr", [D, 32])
    br_d = dram_in(nc, "br", [1, 32])
    wgu_d = dram_in(nc, "wgu", [32, D, 2 * D])
    bgu_d = dram_in(nc, "bgu", [32, 2 * D])
    wd_d = dram_in(nc, "wd", [32, D, D])
    bd_d = dram_in(nc, "bd", [32, D])
    consts_d = dram_in(nc, "consts", [128, 1024])
    xo_d = nc.dram_tensor("xo", [S_LEN, D], F32, kind="ExternalOutput").ap()
    with ExitStack() as stack:
        cx = new_ctx(nc, stack)
        block = stack.enter_context(nc.Block())
        cx.S.dma("sp", cx.consts[:], consts_d, writes=["consts"])
        phase_moe(cx, x_d, xo_d, c_d, adaw_d, adab_d, ffnn_d, wr_d, br_d, wgu_d, bgu_d, wd_d, bd_d, n_exp=n_exp)
        print("instructions", cx.S.nins, "waits", cx.S.nwait)
        cx.S.emit(block)
    return nc


def phase_mlstm(cx, x_d, xo_d, c_d, adaw_d, adab_d, mixn_d, win_d, bi_d, bf_d, outn_d, wout_d, oT_d, sel_d):
    S, nc = cx.S, cx.nc
    common_setup(cx, 0, c_d, adaw_d, adab_d, mixn_d)
    hT = T(cx, "hT", [128, 8, S_LEN], BF16)
    OG = T(cx, "OG", [128, 1024])
    S.dma("pool", OG[:], outn_d.partition_broadcast(128), writes=["OG"])
    cx.sel3 = T(cx, "sel3", [128, 8, 128], BF16)
    cx.r3 = T(cx, "r3", [32, S_LEN], BF16)
    cx.ucol = T(cx, "ucol", [128, 256])
    emcol = T(cx, "emcol", [128, 256])
    with ExitStack() as st2:
        cx.xt = [st2.enter_context(nc.sbuf_tensor(uname("xt%d" % i), [128, 1024], F32)) for i in range(2)]
        cx.hh = [st2.enter_context(nc.sbuf_tensor(uname("hh%d" % i), [128, 1024], F32)) for i in range(2)]
        cx.jk = st2.enter_context(nc.sbuf_tensor(uname("jk"), [128, 1024], F32))
        cx.nss = [st2.enter_context(nc.sbuf_tensor(uname("nss%d" % i), [128, 4], F32)) for i in range(2)]
        cx.ncnt = 0
        jk_ = cx.jk
        sel3_ = cx.sel3
        S.dma("pool", jk_[:], sel_d, writes=["jk"])
        S.op("dve", lambda e: e.tensor_copy(out=sel3_[:].rearrange("p a b -> p (a b)"), in_=jk_[:]), reads=["jk"], writes=["sel3"])
        for t in range(NT):
            norm_tile(cx, t, x_d, hT, t * 128)
        S.barrier()
    with ExitStack() as st2:
        def TT(name, shape, dt=F32):
            return st2.enter_context(nc.sbuf_tensor(uname(name), shape, dt))
        wif = TT("wif", [128, 8, 16])
        wifb = TT("wifb", [128, 8, 16], BF16)
        bcol = TT("bcol", [8, 4])
        gi_t = TT("gi", [32, S_LEN])
        gf = TT("gf", [8, S_LEN])
        Ft_t = TT("Ft", [32, S_LEN])
        S.op("pool", lambda e: e.memset(gi_t[:], 0.0), writes=["g0"])
        S.op("pool", lambda e: e.memset(Ft_t[:], 0.0), writes=["Ft"])
        S.op("pool", lambda e: e.memset(cx.r3[:], 0.0), writes=["r3"])
        gi = gi_t[0:8, :]
        Ft = Ft_t[0:8, :]
        ones = TT("ones", [8, S_LEN])
        cm = TT("cm", [8, S_LEN])
        rb = [TT("rb%d" % i, [8, S_LEN], BF16) for i in range(3)]
        S.dma("sp", wif[:], win_d[:, 3072:3088].rearrange("(kc p) j -> p kc j", p=128), writes=["wif"])
        S.op("dve", lambda e: e.tensor_copy(out=wifb[:], in_=wif[:]), reads=["wif"], writes=["wifb"])
        S.dma("sp", bcol[:, 0:1], bi_d.rearrange("o (p a) -> (o p) a", a=1), writes=["bcol"])
        S.dma("sp", bcol[:, 1:2], bf_d.rearrange("o (p a) -> (o p) a", a=1), writes=["bcol"])
        S.op("dve", lambda e: e.tensor_scalar(out=bcol[:, 2:4], in0=bcol[:, 0:2], scalar1=1.0 / 15, scalar2=None, op0=ALU.mult), reads=["bcol"], writes=["bcol"])
        S.op("pool", lambda e: e.memset(ones[:], 1.0), writes=["ones"])
        for blk in range(8):
            for g in range(2):
                for kc in range(8):
                    S.op("pe", lambda e, g=g, kc=kc, blk=blk: e.matmul(cx.ps[g][0:8, :], lhsT=wifb[:, kc, g * 8:(g + 1) * 8], rhs=hT[:, kc, blk * 512:(blk + 1) * 512], start=(kc == 0), stop=(kc == 7)),
                         reads=["wifb"] + [("hT", blk * 4 + q_) for q_ in range(4)], writes=["ps%d" % g], sig=(kc == 7))
                dst = gi if g == 0 else gf
                S.op("act", lambda e, g=g, blk=blk, dst=dst: e.activation(out=dst[:, blk * 512:(blk + 1) * 512], in_=cx.ps[g][0:8, :], func=AF.Tanh, scale=1.0 / 15, bias=bcol[:, 2 + g:3 + g]),
                     reads=["bcol"], writes=["g%d" % g, "ps%d" % g])
        S.op("dve", lambda e: e.tensor_scalar(out=gi[:], in0=gi[:], scalar1=15.0, scalar2=None, op0=ALU.mult), reads=["g0"], writes=["g0"])
        S.op("act", lambda e: e.activation(out=gf[:], in_=gf[:], func=AF.Exp, scale=-15.0), reads=["g1"], writes=["g1"])
        S.op("act", lambda e: e.activation(out=gf[:], in_=gf[:], func=AF.Ln, bias=1.0), reads=["g1"], writes=["g1"])
        S.op("dve", lambda e: e.tensor_tensor_scan(out=Ft[:], data0=ones[:], data1=gf[:], initial=0.0, op0=ALU.mult, op1=ALU.subtract), reads=["ones", "g1"], writes=["Ft"])
        S.op("dve", lambda e: e.tensor_tensor(out=gi[:], in0=gi[:], in1=Ft[:], op=ALU.subtract), reads=["g0", "Ft"], writes=["g0"])
        S.op("dve", lambda e: e.tensor_tensor_scan(out=cm[:], data0=ones[:], data1=gi[:], initial=0.0, op0=ALU.mult, op1=ALU.max), reads=["ones", "g0"], writes=["cm"])
        S.op("dve", lambda e: e.tensor_tensor(out=Ft[:], in0=Ft[:], in1=cm[:], op=ALU.add), reads=["Ft", "cm"], writes=["Ft"])
        S.op("act", lambda e: e.activation(out=Ft[:], in_=Ft[:], func=AF.Exp, scale=-1.0), reads=["Ft"], writes=["Ft"])
        S.op("dve", lambda e: e.tensor_scalar(out=cm[:], in0=cm[:], scalar1=-1.0, scalar2=None, op0=ALU.mult), reads=["cm"], writes=["cm"])
        for j in range(3):
            S.op("dve", lambda e, j=j: e.tensor_copy(out=rb[j][:], in_=cm[:]), reads=["cm"], writes=["rb%d" % j])
            if j < 2:
                S.op("dve", lambda e, j=j: e.tensor_tensor(out=cm[:], in0=cm[:], in1=rb[j][:], op=ALU.subtract), reads=["cm", "rb%d" % j], writes=["cm"])
            S.dma("sp", cx.r3[8 * j:8 * j + 8, :], rb[j][:], reads=["rb%d" % j], writes=["r3"])
        for which, src, dst, key in ((0, gi, cx.ucol, "ucol"), (1, Ft, emcol, "emcol")):
            for t in range(NT):
                S.op("pe", lambda e, which=which, src=src, t=t: e.transpose(cx.ps[2 + which][:, t * 8:(t + 1) * 8], src[:, t * 128:(t + 1) * 128], cx.ident[0:8, 0:8]),
                     reads=["g0" if which == 0 else "Ft", "consts"], writes=["ps%d" % (2 + which)])
            S.op("dve", lambda e, which=which, dst=dst: e.tensor_copy(out=dst[:], in_=cx.ps[2 + which][:, 0:256]), reads=[], writes=[key, "ps%d" % (2 + which)])
        dbg = getattr(cx, "dbg", None)
        if dbg:
            S.dma("sp", dbg["ucol"], cx.ucol[:], reads=["ucol"], writes=["dbg1"])
            S.dma("sp", dbg["emcol"], emcol[:], reads=["emcol"], writes=["dbg2"])
            S.dma("sp", dbg["r3"], cx.r3[:], reads=["r3"], writes=["dbg3"])
        S.barrier()
    with ExitStack() as st2:
        def TT(name, shape, dt=F32):
            return st2.enter_context(nc.sbuf_tensor(uname(name), shape, dt))
        wst = TT("wst", [128, 8, 384])
        wb = [TT("wb%d" % i, [128, 8, 384], BF16) for i in range(2)]
        QKT = [TT("QKT%d" % i, [128, 2, S_LEN], BF16) for i in range(2)]
        V = [TT("V%d" % i, [128, NT, 132], BF16) for i in range(2)]
        og = [TT("og0", [128, NT, 128], BF16)] * 2
        cx.PT = [TT("PT%d" % i, [128, 512], BF16) for i in range(2)]
        cx.DT = [TT("DT%d" % i, [128, 512]) for i in range(2)]
        if getattr(cx, "dbg", None) is not None:
            cx.dbgt = TT("dbgt", [128, 512])
        qk = [TT("qk%d" % i, [128, 128]) for i in range(2)]
        ot = [TT("ot%d" % i, [128, 128]) for i in range(2)]
        ojk = TT("ojk", [128, 128])
        osb = [TT("osb%d" % i, [128, 132]) for i in range(2)]
        oss = [TT("oss%d" % i, [128, 8]) for i in range(2)]
        oTb = [TT("oTb0", [128, S_LEN], BF16)] * 2
        for i in range(2):
            S.op("pool", lambda e, i=i: e.memset(V[i][:, :, 128:129], 1.0), writes=[("V", i)])
        cx.stcnt = 0
        fcnt = [0]
        for h in range(8):
            hb = h % 2
            load_cast_w(cx, wb[hb], wst, "wst", "wb%d" % hb,
                        [(win_d[:, h * 64:(h + 1) * 64], 0, 64), (win_d[:, 512 + h * 64:512 + (h + 1) * 64], 64, 64),
                         (win_d[:, 1024 + h * 128:1024 + (h + 1) * 128], 128, 128), (win_d[:, 2048 + h * 128:2048 + (h + 1) * 128], 256, 128)], q="pool", cast_eng="pool")
            for t in range(NT):
                p = t % 2
                P1 = cx.ps[4 + p]
                kp = "ps%d" % (4 + p)
                for kc in range(8):
                    S.op("pe", lambda e, P1=P1, kc=kc, t=t, hb=hb: e.matmul(P1[:, 0:384], lhsT=hT[:, kc, t * 128:(t + 1) * 128], rhs=wb[hb][:, kc, :], start=(kc == 0), stop=(kc == 7)),
                         reads=[("hT", t), "wb%d" % hb], writes=[kp], sig=(kc == 7))
                S.op("dve", lambda e, P1=P1, p=p: e.tensor_copy(out=qk[p][:], in_=P1[:, 0:128]), reads=[], writes=["qk%d" % p, kp])
                S.op("dve", lambda e, P1=P1, t=t, hb=hb: e.tensor_copy(out=V[hb][:, t, 0:128], in_=P1[:, 128:256]), reads=[], writes=[("V", hb), kp])
                S.op("act", lambda e, P1=P1, t=t, hb=hb: e.activation(out=og[hb][:, t, :], in_=P1[:, 256:384], func=AF.Sigmoid), reads=[], writes=[("og", 0), kp])
                for j in range(2):
                    S.op("pe", lambda e, j=j, p=p: e.transpose(cx.ps[6][0:64, j * 128:(j + 1) * 128], qk[p][:, j * 64:(j + 1) * 64], cx.ident[:]), reads=["qk%d" % p, "consts"], writes=["ps6"], sig=(j == 1))
                S.op("act", lambda e, t=t, hb=hb: e.activation(out=QKT[hb][0:64, :, t * 128:(t + 1) * 128], in_=cx.ps[6][0:64, 0:256].rearrange("p (a b) -> p a b", a=2), func=AF.Copy),
                     reads=[], writes=[("QK", hb), "ps6"])

            def finish(Tq, c, oap_ps, okey, h=h, hb=hb):
                f = fcnt[0] % 2
                fcnt[0] += 1
                kos, kot, kob = "oss%d" % f, "ot%d" % f, "osb%d" % f
                oap = osb[f]
                if f == 0:
                    S.op("act", lambda e: e.activation(out=oap[:, 0:129], in_=oap_ps, func=AF.Copy), reads=[], writes=[kob, okey])
                else:
                    S.op("dve", lambda e: e.tensor_copy(out=oap[:, 0:129], in_=oap_ps), reads=[], writes=[kob, okey])
                S.op("act", lambda e: e.activation(out=oss[f][:, 0:1], in_=oap[:, 128:129], func=AF.Abs), reads=[kob], writes=[kos])
                S.op("dve", lambda e: e.tensor_tensor(out=oss[f][:, 0:1], in0=oss[f][:, 0:1], in1=emcol[:, Tq * 8 + h:Tq * 8 + h + 1], op=ALU.max), reads=[kos, "emcol"], writes=[kos])
                S.op("dve", lambda e: e.reciprocal(out=oss[f][:, 1:2], in_=oss[f][:, 0:1]), reads=[kos], writes=[kos])
                S.op("act", lambda e: e.activation(out=ot[f][:], in_=oap[:, 0:128], func=AF.Copy, scale=oss[f][:, 1:2]), reads=[kob, kos], writes=[kot])
                dbg = getattr(cx, "dbg", None)
                dump = dbg is not None and h == 0 and Tq == 12
                if dump:
                    S.dma("sp", dbg["osb"], oap[:, 0:132], reads=[kob], writes=["dbg4"])
                    S.dma("sp", dbg["ot_a"], ot[f][:], reads=[kot], writes=["dbg5"])
                S.op("act", lambda e: e.activation(out=ojk[:], in_=ot[f][:], func=AF.Square, accum_out=oss[f][:, 2:3]), reads=[kot], writes=["ojk", kos])
                S.op("dve", lambda e: e.tensor_scalar(out=oss[f][:, 3:4], in0=oss[f][:, 2:3], scalar1=1.0 / 128, scalar2=EPS, op0=ALU.mult, op1=ALU.add), reads=[kos], writes=[kos])
                S.op("act", lambda e: e.activation(out=oss[f][:, 3:4], in_=oss[f][:, 3:4], func=AF.Sqrt), reads=[kos], writes=[kos])
                S.op("dve", lambda e: e.reciprocal(out=oss[f][:, 4:5], in_=oss[f][:, 3:4]), reads=[kos], writes=[kos])
                S.op("dve", lambda e: e.scalar_tensor_tensor(out=ot[f][:], in0=ot[f][:], scalar=oss[f][:, 4:5], in1=OG[:, h * 128:(h + 1) * 128], op0=ALU.mult, op1=ALU.mult), reads=[kot, kos, "OG"], writes=[kot])
                if dump:
                    S.dma("sp", dbg["ot_b"], ot[f][:], reads=[kot], writes=["dbg6"])
                    S.dma("sp", dbg["oss"], oss[f][:], reads=[kos], writes=["dbg7"])
                    S.dma("sp", dbg["og"], og[hb][:, Tq, :], reads=[("og", 0)], writes=["dbg8"])
                S.op("pool", lambda e: e.tensor_tensor(out=ot[f][:], in0=ot[f][:], in1=og[hb][:, Tq, :], op=ALU.mult), reads=[kot, ("og", 0)], writes=[kot])
                if dump:
                    S.dma("sp", dbg["ot_c"], ot[f][:], reads=[kot], writes=["dbg9"])
                S.op("pe", lambda e: e.transpose(cx.ps[7][:, 0:128], ot[f][:], cx.ident[:]), reads=[kot, "consts"], writes=["ps7"])
                S.op("act", lambda e: e.activation(out=oTb[hb][:, Tq * 128:(Tq + 1) * 128], in_=cx.ps[7][:, 0:128], func=AF.Copy), reads=[], writes=[("oTb", 0), "ps7"])

            flash_head(cx, h, "ml", QKT[hb][:, 0, :], QKT[hb][:, 1, :], V[hb], finish)
            S.dma("sp", oT_d[h], oTb[hb][:], reads=[("oTb", 0)], writes=["oT_d"])
        S.barrier()
    out_proj(cx, x_d, xo_d, wout_d, oT_d)


def build_ml_prog():
    nc = bass.Bass("TRN2", target_bir_lowering=False)
    x_d = dram_in(nc, "x", [S_LEN, D])
    c_d = dram_in(nc, "c", [1, D])
    adaw_d = dram_in(nc, "adaw", [D, 6 * D])
    adab_d = dram_in(nc, "adab", [1, 6 * D])
    mixn_d = dram_in(nc, "mixn", [1, D])
    win_d = dram_in(nc, "win", [D, 3088])
    bi_d = dram_in(nc, "bi", [1, 8])
    bf_d = dram_in(nc, "bf", [1, 8])
    outn_d = dram_in(nc, "outn", [1, D])
    wout_d = dram_in(nc, "wout", [D, D])
    consts_d = dram_in(nc, "consts", [128, 1024])
    sel_d = dram_in(nc, "sel", [128, 1024])
    xo_d = nc.dram_tensor("xo", [S_LEN, D], F32, kind="ExternalOutput").ap()
    oT_d = nc.dram_tensor("oT_d", [8, 128, S_LEN], BF16, kind="Internal").ap()
    with ExitStack() as stack:
        cx = new_ctx(nc, stack)
        block = stack.enter_context(nc.Block())
        cx.S.dma("sp", cx.consts[:], consts_d, writes=["consts"])
        phase_mlstm(cx, x_d, xo_d, c_d, adaw_d, adab_d, mixn_d, win_d, bi_d, bf_d, outn_d, wout_d, oT_d, sel_d)
        print("instructions", cx.S.nins, "waits", cx.S.nwait)
        cx.S.emit(block)
    return nc


PHASES = ("attn", "moe0", "mlstm", "moe1")


def build_prog(phases, n_exp=32, sliced=False, debug=False):
    nc = bass.Bass("TRN2", target_bir_lowering=False)
    d = {}
    def inp(name, shape, dt=F32):
        d[name] = dram_in(nc, name, shape, dt)
        return d[name]
    x_d = inp("x", [S_LEN, D])
    c_d = inp("c", [1, D])
    NL = 1 if sliced else 2
    adaw_d = inp("ada_w", [NL, D, 6 * D])
    adab_d = inp("ada_b", [NL, 6 * D])
    consts_d = inp("consts", [128, 1024])
    if "attn" in phases:
        pos_d = inp("positions", [1, S_LEN], I32)
        inp("mix_norm", [NL, D])
        inp("att_w_in", [1, D, 3 * D]); inp("att_w_out", [1, D, D])
        for nm in ("att_q_norm", "att_k_norm", "att_lam_q1", "att_lam_k1", "att_lam_q2", "att_lam_k2"):
            inp(nm, [1, 64])
        inp("att_sub_norm", [1, 128])
    if "mlstm" in phases:
        if "mix_norm" not in d:
            inp("mix_norm", [NL, D])
        inp("ml_w_in", [1, D, 3088]); inp("ml_b_igate", [1, 8]); inp("ml_b_fgate", [1, 8])
        inp("ml_out_norm", [1, D]); inp("ml_w_out", [1, D, D]); inp("sel", [128, 1024])
    if "moe0" in phases or "moe1" in phases:
        inp("ffn_norm", [NL, D]); inp("router_w", [NL, D, 32]); inp("router_b", [NL, 32])
        inp("moe_w_gate_up", [NL, 32, D, 2 * D]); inp("moe_b_gate_up", [NL, 32, 2 * D])
        inp("moe_w_down", [NL, 32, D, D]); inp("moe_b_down", [NL, 32, D])
    xo_d = nc.dram_tensor("xo", [S_LEN, D], F32, kind="ExternalOutput").ap()
    oT_d = nc.dram_tensor("oT_d", [8, 128, S_LEN], BF16, kind="ExternalOutput" if debug else "Internal").ap()
    xs = [x_d]
    for i in range(len(phases) - 1):
        xs.append(nc.dram_tensor("xmid%d" % i, [S_LEN, D], F32, kind="ExternalOutput" if debug else "Internal").ap())
    xs.append(xo_d)
    with ExitStack() as stack:
        cx = new_ctx(nc, stack)
        block = stack.enter_context(nc.Block())
        if debug:
            cx.dbg = {"ucol": nc.dram_tensor("dbg_ucol", [128, 256], F32, kind="ExternalOutput").ap(),
                      "emcol": nc.dram_tensor("dbg_emcol", [128, 256], F32, kind="ExternalOutput").ap(),
                      "r3": nc.dram_tensor("dbg_r3", [32, S_LEN], BF16, kind="ExternalOutput").ap(),
                      "osb": nc.dram_tensor("dbg_osb", [128, 132], F32, kind="ExternalOutput").ap(),
                      "ot_a": nc.dram_tensor("dbg_ot_a", [128, 128], F32, kind="ExternalOutput").ap(),
                      "ot_b": nc.dram_tensor("dbg_ot_b", [128, 128], F32, kind="ExternalOutput").ap(),
                      "ot_c": nc.dram_tensor("dbg_ot_c", [128, 128], F32, kind="ExternalOutput").ap(),
                      "oss": nc.dram_tensor("dbg_oss", [128, 8], F32, kind="ExternalOutput").ap(),
                      "og": nc.dram_tensor("dbg_og", [128, 128], BF16, kind="ExternalOutput").ap(),
                      "DT0": nc.dram_tensor("dbg_DT0", [128, 512], F32, kind="ExternalOutput").ap(),
                      "DT11": nc.dram_tensor("dbg_DT11", [128, 512], F32, kind="ExternalOutput").ap(),
                      "RB0": nc.dram_tensor("dbg_RB0", [128, 512], F32, kind="ExternalOutput").ap(),
                      "RB11": nc.dram_tensor("dbg_RB11", [128, 512], F32, kind="ExternalOutput").ap(),
                      "sel3": nc.dram_tensor("dbg_sel3", [32, 128], BF16, kind="ExternalOutput").ap()}
        cx.S.dma("sp", cx.consts[:], consts_d, writes=["consts"])
        for i, ph in enumerate(phases):
            xin, xout = xs[i], xs[i + 1]
            with ExitStack() as pst:
                cx.stack = pst
                if ph == "attn":
                    phase_attn(cx, xin, xout, c_d, pos_d, adaw_d[0], adab_d[0:1, :], d["mix_norm"][0:1, :], d["att_w_in"][0], d["att_w_out"][0],
                               d["att_q_norm"], d["att_k_norm"], [d["att_lam_q1"], d["att_lam_k1"], d["att_lam_q2"], d["att_lam_k2"]], d["att_sub_norm"], oT_d)
                elif ph == "mlstm":
                    L = 0 if sliced else 1
                    phase_mlstm(cx, xin, xout, c_d, adaw_d[L], adab_d[L:L + 1, :], d["mix_norm"][L:L + 1, :], d["ml_w_in"][0], d["ml_b_igate"], d["ml_b_fgate"],
                                d["ml_out_norm"], d["ml_w_out"][0], oT_d, d["sel"])
                else:
                    L = 0 if sliced else int(ph[-1])
                    phase_moe(cx, xin, xout, c_d, adaw_d[L], adab_d[L:L + 1, :], d["ffn_norm"][L:L + 1, :], d["router_w"][L], d["router_b"][L:L + 1, :],
                              d["moe_w_gate_up"][L], d["moe_b_gate_up"][L], d["moe_w_down"][L], d["moe_b_down"][L], n_exp=n_exp)
                cx.S.barrier()
            cx.stack = stack
        cx.S.emit(block)
    return nc, list(d.keys())


LAUNCH_GROUPS = [("attn",), ("moe0",), ("mlstm",), ("moe1",)]
PHASE_LAYER = {"attn": 0, "moe0": 0, "mlstm": 1, "moe1": 1}
PER_LAYER = ("ada_w", "ada_b", "mix_norm", "ffn_norm", "router_w", "router_b", "moe_w_gate_up", "moe_b_gate_up", "moe_w_down", "moe_b_down")
FUSED = True


def kernel(**inputs):
    n = 8
    shared = {k: np.ascontiguousarray(v) for k, v in inputs.items() if k not in ("x", "c", "positions")}
    shared["consts"] = make_consts()
    shared["sel"] = make_sel()
    xcur = [np.ascontiguousarray(inputs["x"][b]) for b in range(n)]
    groups = [PHASES] if FUSED else LAUNCH_GROUPS
    progs = {}
    for grp in groups:
        sliced = not FUSED
        pkey = ("moe0",) if (sliced and grp[0].startswith("moe")) else grp
        if pkey not in progs:
            progs[pkey] = build_prog(pkey, sliced=sliced)
        nc, names = progs[pkey]
        L = PHASE_LAYER[grp[0]]
        in_maps = []
        for b in range(n):
            m = {}
            for k in names:
                if k == "x":
                    m[k] = xcur[b]
                elif k == "c":
                    m[k] = np.ascontiguousarray(inputs["c"][b:b + 1])
                elif k == "positions":
                    m[k] = np.ascontiguousarray(inputs["positions"][b:b + 1]).astype(np.int32)
                elif sliced and k in PER_LAYER:
                    m[k] = np.ascontiguousarray(shared[k][L:L + 1])
                else:
                    m[k] = shared[k]
            in_maps.append(m)
        res = run_bass_kernel_spmd(nc, in_maps, core_ids=list(range(n)))
        xcur = [np.ascontiguousarray(res.results[b]["xo"]) for b in range(n)]
    return np.stack(xcur, axis=0).astype(np.float32)
```

```python
import numpy as np
import concourse.bass as bass
import concourse.mybir as mybir
from concourse.bass_utils import run_bass_kernel_spmd

F32 = mybir.dt.float32
BF16 = mybir.dt.bfloat16
I32 = mybir.dt.int32
AF = mybir.ActivationFunctionType
ALU = mybir.AluOpType
AX = mybir.AxisListType

ENGMAP = {"pe": "tensor", "act": "scalar", "dve": "vector", "pool": "gpsimd", "sp": "sync"}


class Sched:
    def __init__(self, nc, stack, ndma=8):
        self.nc = nc
        self.prog = {k: [] for k in ENGMAP}
        self.sem = {}
        self.cnt = {k: 0 for k in ENGMAP}
        self.dsem = {}
        self.drr = {k: 0 for k in ENGMAP}
        for k in ENGMAP:
            self.sem[k] = stack.enter_context(nc.semaphore("s_" + k))
        for k in ("sp", "pool", "act"):
            self.dsem[k] = [[stack.enter_context(nc.semaphore("d_%s%d" % (k, j))), 0] for j in range(ndma)]
        self.lastw = {}
        self.readers = {}
        self.seen = {}
        self.nwait = 0
        self.nins = 0

    def _deps(self, eng, reads, writes):
        best = {}
        def add(tok):
            s, v, src = tok
            if src == eng and eng == "pe":
                return
            key = id(s)
            if key not in best or best[key][1] < v:
                best[key] = (s, v)
        for k in reads:
            if k in self.lastw:
                add(self.lastw[k])
        for k in writes:
            if k in self.lastw:
                add(self.lastw[k])
            for t in self.readers.get(k, ()):
                add(t)
        for key, (s, v) in best.items():
            sk = (eng, key)
            if self.seen.get(sk, 0) >= v:
                continue
            self.seen[sk] = v
            self.prog[eng].append(lambda e, s=s, v=v: e.wait_ge(s, v))
            self.nwait += 1

    def _commit(self, tok, reads, writes):
        for k in reads:
            self.readers.setdefault(k, []).append(tok)
        for k in writes:
            self.lastw[k] = tok
            self.readers[k] = []

    def op(self, eng, fn, reads=(), writes=(), sig=True):
        self._deps(eng, reads, writes)
        sem = self.sem[eng]
        if sig:
            self.cnt[eng] += 1
            tok = (sem, self.cnt[eng], eng)
            self.prog[eng].append(lambda e, fn=fn, sem=sem: fn(e).then_inc(sem, 1))
        else:
            tok = (sem, self.cnt[eng] + 1, eng)
            self.prog[eng].append(lambda e, fn=fn: fn(e))
        self.nins += 1
        self._commit(tok, reads, writes)
        return tok

    def dma(self, q, out, in_, reads=(), writes=(), **kw):
        self._deps(q, reads, writes)
        j = self.drr[q]
        self.drr[q] = (j + 1) % len(self.dsem[q])
        ent = self.dsem[q][j]
        sem, c = ent
        if c > 0:
            sk = (q, id(sem))
            if self.seen.get(sk, 0) < 16 * c:
                self.seen[sk] = 16 * c
                self.prog[q].append(lambda e, s=sem, v=16 * c: e.wait_ge(s, v))
        ent[1] = c + 1
        tok = (sem, 16 * (c + 1), "dma_" + q)
        self.prog[q].append(lambda e, out=out, in_=in_, sem=sem, kw=kw: e.dma_start(out=out, in_=in_, **kw).then_inc(sem, 16))
        self.nins += 1
        self._commit(tok, reads, writes)
        return tok

    def barrier(self):
        toks = []
        for k in ENGMAP:
            if self.cnt[k] > 0:
                toks.append((self.sem[k], self.cnt[k], k))
        for q in self.dsem:
            for sem, c in self.dsem[q]:
                if c > 0:
                    toks.append((sem, 16 * c, "dma"))
        for eng in ENGMAP:
            for s_, v, src in toks:
                if src == eng and eng == "pe":
                    continue
                sk = (eng, id(s_))
                if self.seen.get(sk, 0) >= v:
                    continue
                self.seen[sk] = v
                self.prog[eng].append(lambda e, s_=s_, v=v: e.wait_ge(s_, v))
        self.lastw = {}
        self.readers = {}

    def wait_all(self, eng, keys):
        self._deps(eng, list(keys), [])

    def emit(self, block):
        for k, name in ENGMAP.items():
            lst = self.prog[k]
            def body(e, lst=lst):
                for f in lst:
                    f(e)
            getattr(block, name)(body)


import math
from contextlib import ExitStack
import numpy as np

S_LEN = 4096
D = 1024
NT = S_LEN // 128
EPS = 1e-6
PI = math.pi


def make_consts():
    c = np.zeros((128, 1024), np.float32)
    c[:, 0:128] = np.eye(128, dtype=np.float32)
    s = np.arange(128)[:, None]
    l = np.arange(128)[None, :]
    c[:, 128:256] = (s <= l).astype(np.float32)
    c[:, 256:384] = np.where(s <= l, 0.0, -30000.0)
    inv = (10000.0 ** (-np.arange(0, 64, 2, dtype=np.float32) / 64)).astype(np.float32)
    c[:, 384:416] = inv[None, :]
    for h in range(8):
        for j in range(3):
            c[8 * j + h, 416 + 0:416 + 0] = 0
    return c


def make_sel():
    sel = np.zeros((128, 8, 128), np.float32)
    for h in range(8):
        for j in range(3):
            sel[8 * j + h, h, :] = 1.0
    return sel.reshape(128, 1024)


class Ctx:
    pass


_uid = [0]


def uname(name):
    _uid[0] += 1
    return "s%d_%s" % (_uid[0], name)


def T(cx, name, shape, dt=F32):
    return cx.stack.enter_context(cx.nc.sbuf_tensor(uname(name), shape, dt))


def common_setup(cx, layer_half, c_d, adaw_d, adab_d, norm_d):
    S, nc = cx.S, cx.nc
    with ExitStack() as st:
        def TT(name, shape, dt=F32):
            return st.enter_context(nc.sbuf_tensor(uname(name), shape, dt))
        rowc = TT("rowc", [128, 1024])
        crep = TT("crep", [128, 8, 128])
        stg = [TT("adst%d" % i, [128, 3072]) for i in range(2)]
        adab = TT("adab", [128, 3072])
        nrm = TT("nrmb", [128, 1024])
        c0 = layer_half * 3072
        S.dma("sp", rowc[:], c_d.partition_broadcast(128), writes=["rowc"])
        S.dma("sp", adab[:], adab_d[:, c0:c0 + 3072].partition_broadcast(128), writes=["adab"])
        S.dma("sp", nrm[:], norm_d.partition_broadcast(128), writes=["nrmb"])
        S.op("act", lambda e: e.activation(out=crep[:].rearrange("p a b -> p (a b)"), in_=rowc[:], func=AF.Sigmoid), reads=["rowc"], writes=["crep"])
        S.op("dve", lambda e: e.tensor_tensor(out=rowc[:], in0=rowc[:], in1=crep[:].rearrange("p a b -> p (a b)"), op=ALU.mult), reads=["crep", "rowc"], writes=["rowc"])
        for kc in range(8):
            b = cx.ps[kc // 4]
            S.op("pe", lambda e, kc=kc, b=b: e.transpose(b[:, (kc % 4) * 128:(kc % 4 + 1) * 128], rowc[:, kc * 128:(kc + 1) * 128], cx.ident[:]),
                 reads=["rowc", "consts"], writes=["ps%d" % (kc // 4)])
        for hb in range(2):
            S.op("dve", lambda e, hb=hb: e.tensor_copy(out=crep[:, hb * 4:(hb + 1) * 4, :].rearrange("p a b -> p (a b)"), in_=cx.ps[hb][:, :]),
                 reads=[], writes=["crep", "ps%d" % hb])
        for kc in range(8):
            sb = stg[kc % 2]
            S.dma("sp" if kc % 2 == 0 else "pool", sb[:], adaw_d[kc * 128:(kc + 1) * 128, c0:c0 + 3072], writes=["adst%d" % (kc % 2)])
            for n in range(6):
                S.op("pe", lambda e, kc=kc, n=n, sb=sb: e.matmul(cx.ps[n][:, :], lhsT=crep[:, kc, :], rhs=sb[:, n * 512:(n + 1) * 512], start=(kc == 0), stop=(kc == 7)),
                     reads=["crep", "adst%d" % (kc % 2)], writes=["ps%d" % n], sig=(kc == 7 or n == 5))
        for n in range(6):
            dst = (cx.SH, cx.A, cx.G)[n // 2]
            S.op("dve", lambda e, n=n, dst=dst: e.tensor_tensor(out=dst[:, (n % 2) * 512:(n % 2 + 1) * 512], in0=cx.ps[n][:, :], in1=adab[:, n * 512:(n + 1) * 512], op=ALU.add),
                 reads=["adab"], writes=["mod%d" % (n // 2), "ps%d" % n])
        S.op("dve", lambda e: e.scalar_tensor_tensor(out=cx.A[:], in0=cx.A[:], scalar=1.0, in1=nrm[:], op0=ALU.add, op1=ALU.mult),
             reads=["mod1", "nrmb"], writes=["mod1"])
        S.barrier()


def norm_tile(cx, t, x_d, hT, tok0, want_f32T=None):
    S, nc = cx.S, cx.nc
    i = cx.ncnt % 2
    cx.ncnt += 1
    xt, hh, jk = cx.xt[i], cx.hh[i], cx.jk
    ss = cx.nss[i]
    kx, kh, ks = "xt%d" % i, "hh%d" % i, "nss%d" % i
    S.dma("sp", xt[:], x_d[t * 128:(t + 1) * 128, :], writes=[kx])
    S.op("act", lambda e: e.activation(out=jk[:], in_=xt[:], func=AF.Square, accum_out=ss[:, 0:1]), reads=[kx], writes=["jk", ks])
    S.op("dve", lambda e: e.tensor_scalar(out=ss[:, 1:2], in0=ss[:, 0:1], scalar1=1.0 / D, scalar2=EPS, op0=ALU.mult, op1=ALU.add), reads=[ks], writes=[ks])
    S.op("act", lambda e: e.activation(out=ss[:, 2:3], in_=ss[:, 1:2], func=AF.Sqrt), reads=[ks], writes=[ks])
    S.op("dve", lambda e: e.reciprocal(out=ss[:, 3:4], in_=ss[:, 2:3]), reads=[ks], writes=[ks])
    S.op("dve", lambda e: e.scalar_tensor_tensor(out=hh[:], in0=xt[:], scalar=ss[:, 3:4], in1=cx.A[:], op0=ALU.mult, op1=ALU.mult), reads=[kx, ks, "mod1"], writes=[kh])
    S.op("dve", lambda e: e.tensor_tensor(out=hh[:], in0=hh[:], in1=cx.SH[:], op=ALU.add), reads=[kh, "mod0"], writes=[kh])
    for kc in range(8):
        b = 6 + kc // 4
        S.op("pe", lambda e, kc=kc, b=b: e.transpose(cx.ps[b][:, (kc % 4) * 128:(kc % 4 + 1) * 128], hh[:, kc * 128:(kc + 1) * 128], cx.ident[:]),
             reads=[kh, "consts"], writes=["ps%d" % b], sig=(kc % 4 == 3))
    for hb in range(2):
        eng = "act" if hb == 0 else "dve"
        outap = hT[:, hb * 4:(hb + 1) * 4, tok0:tok0 + 128]
        inap = cx.ps[6 + hb][:, :].rearrange("p (a b) -> p a b", a=4)
        if eng == "act":
            S.op("act", lambda e, o=outap, i_=inap: e.activation(out=o, in_=i_, func=AF.Copy), reads=[], writes=[("hT", tok0 // 128), "ps%d" % (6 + hb)])
        else:
            S.op("dve", lambda e, o=outap, i_=inap: e.tensor_copy(out=o, in_=i_), reads=[], writes=[("hT", tok0 // 128), "ps%d" % (6 + hb)])
        if want_f32T is not None:
            o2 = want_f32T[:, hb * 4:(hb + 1) * 4, :]
            S.op("pool" if False else "dve", lambda e, o=o2, i_=inap: e.tensor_copy(out=o, in_=i_), reads=[], writes=["hTf", "ps%d" % (6 + hb)])


def flash_head(cx, h, mode, QT, KT, V, finish_tile):
    S = cx.S
    ncomp = 2 if mode == "att" else 1
    sel3_, r3_ = getattr(cx, "sel3", None), getattr(cx, "r3", None)
    for qb in range(8):
        for c in range(ncomp):
            pb = 0 if mode == "ml" else c * 64
            nk = 4 * qb + 4
            sbs = {}

            def emit_st(kt, qb=qb, pb=pb, sbs=sbs):
                j0 = max(0, kt - 4 * qb)
                c0 = j0 * 128
                nrot = 3 if mode == "att" else 2
                sb = cx.stcnt % nrot
                cx.stcnt += 1
                sbs[kt] = sb
                bank = (0, 1, 4)[sb]
                stp = cx.ps[bank]
                S.op("pe", lambda e: e.matmul(stp[:, c0:512], lhsT=KT[pb:pb + 64, kt * 128:(kt + 1) * 128], rhs=QT[pb:pb + 64, qb * 512 + c0:qb * 512 + 512], start=True, stop=True),
                     reads=[("QK", h % 2)], writes=["ps%d" % bank])
                if mode == "ml":
                    rb = cx.ps[4 + sb]
                    S.op("pe", lambda e: e.matmul(rb[:, c0:512], lhsT=sel3_[0:24, h, :], rhs=r3_[0:24, qb * 512 + c0:qb * 512 + 512], start=True, stop=True),
                         reads=["r3", "sel3"], writes=["ps%d" % (4 + sb)])

            def emit_post(kt, qb=qb, sbs=sbs):
                j0 = max(0, kt - 4 * qb)
                c0 = j0 * 128
                sb = sbs[kt]
                stp = cx.ps[(0, 1, 4)[sb]]
                pt = cx.PT[sb]
                kst, kpt = "ps%d" % (0, 1, 4)[sb], "PT%d" % sb
                if mode == "att":
                    S.op("act", lambda e: e.activation(out=pt[:, c0:512], in_=stp[:, c0:512], func=AF.Exp, scale=0.125), reads=[], writes=[kpt, kst])
                    if kt >= 4 * qb:
                        S.op("pool", lambda e: e.tensor_tensor(out=pt[:, c0:c0 + 128], in0=pt[:, c0:c0 + 128], in1=cx.mask01b[:], op=ALU.mult),
                             reads=[kpt, "consts2"], writes=[kpt])
                else:
                    rb = cx.ps[4 + sb]
                    krb, kdt = "ps%d" % (4 + sb), "DT%d" % sb
                    dt_ = cx.DT[sb]
                    ucol = cx.ucol[:, kt * 8 + h:kt * 8 + h + 1]
                    if kt >= 4 * qb:
                        S.op("dve", lambda e: e.tensor_tensor(out=dt_[:, c0:c0 + 128], in0=rb[:, c0:c0 + 128], in1=cx.maskneg[:], op=ALU.add), reads=["consts"], writes=[kdt, krb])
                        S.op("act", lambda e: e.activation(out=dt_[:, c0:c0 + 128], in_=dt_[:, c0:c0 + 128], func=AF.Exp, bias=ucol), reads=[kdt, "ucol"], writes=[kdt])
                        if c0 + 128 < 512:
                            S.op("act", lambda e: e.activation(out=dt_[:, c0 + 128:512], in_=rb[:, c0 + 128:512], func=AF.Exp, bias=ucol), reads=["ucol"], writes=[kdt, krb])
                    else:
                        S.op("act", lambda e: e.activation(out=dt_[:, 0:512], in_=rb[:, 0:512], func=AF.Exp, bias=ucol), reads=["ucol"], writes=[kdt, krb])
                    S.op("dve", lambda e: e.scalar_tensor_tensor(out=pt[:, c0:512], in0=stp[:, c0:512], scalar=0.125, in1=dt_[:, c0:512], op0=ALU.mult, op1=ALU.mult),
                         reads=[kdt], writes=[kpt, kst])

            def emit_pv(kt, qb=qb, sbs=sbs):
                j0 = max(0, kt - 4 * qb)
                sb = sbs[kt]
                pt = cx.PT[sb]
                kpt = "PT%d" % sb
                for i in range(j0, 4):
                    ob = cx.ps[2 + i // 2]
                    oap = ob[:, (i % 2) * 256:(i % 2) * 256 + 129]
                    last = (kt == 4 * qb + i)
                    S.op("pe", lambda e, oap=oap, i=i, last=last: e.matmul(oap, lhsT=pt[:, i * 128:(i + 1) * 128], rhs=V[:, kt, 0:129], start=(kt == 0 and i % 2 == 0), stop=last, skip_group_check=True),
                         reads=[kpt, ("V", h % 2)], writes=["ps%d" % (2 + i // 2)], sig=(last or i == 3))

            depth = 2 if mode == "att" else 1
            for kk in range(min(depth, nk)):
                emit_st(kk)
            for kt in range(nk):
                if kt + depth < nk:
                    emit_st(kt + depth)
                emit_post(kt)
                emit_pv(kt)
            for i in range(4):
                ob = cx.ps[2 + i // 2]
                oap = ob[:, (i % 2) * 256:(i % 2) * 256 + 129]
                finish_tile(4 * qb + i, c, oap, "ps%d" % (2 + i // 2))


def rstd_ops(cx, ssap, n, inv_n, key):
    S = cx.S
    S.op("dve", lambda e: e.tensor_scalar(out=ssap[:, n:2 * n], in0=ssap[:, 0:n], scalar1=inv_n, scalar2=EPS, op0=ALU.mult, op1=ALU.add), reads=[key], writes=[key])
    S.op("act", lambda e: e.activation(out=ssap[:, n:2 * n], in_=ssap[:, n:2 * n], func=AF.Sqrt), reads=[key], writes=[key])
    S.op("dve", lambda e: e.reciprocal(out=ssap[:, 2 * n:3 * n], in_=ssap[:, n:2 * n]), reads=[key], writes=[key])


def load_cast_w(cx, dst, stage, kstage, kdst, srcs, q="sp", cast_eng="pool"):
    S = cx.S
    for (ap, off, w) in srcs:
        S.dma(q, stage[:, :, off:off + w], ap.rearrange("(kc p) j -> p kc j", p=128), writes=[kstage])
    if cast_eng == "act":
        S.op("act", lambda e: e.activation(out=dst[:], in_=stage[:], func=AF.Copy), reads=[kstage], writes=[kdst])
    else:
        S.op(cast_eng, lambda e: e.tensor_copy(out=dst[:], in_=stage[:]), reads=[kstage], writes=[kdst])


def phase_attn(cx, x_d, xo_d, c_d, pos_d, adaw_d, adab_d, mixn_d, win_d, wout_d, qn_d, kn_d, lam_ds, subn_d, oT_d):
    S, nc = cx.S, cx.nc
    st = cx.stack
    lam_init = 0.2
    common_setup(cx, 0, c_d, adaw_d, adab_d, mixn_d)
    hT = T(cx, "hT", [128, 8, S_LEN], BF16)
    GQK = T(cx, "GQK", [128, 256])
    for j, d_ in enumerate((qn_d, qn_d, kn_d, kn_d)):
        S.dma("pool", GQK[:, j * 64:(j + 1) * 64], d_.partition_broadcast(128), writes=["GQK"])
    lamt = T(cx, "lamt", [128, 4, 64])
    for j, d_ in enumerate(lam_ds):
        S.dma("pool", lamt[:, j, :], d_.partition_broadcast(128), writes=["lamt"])
    lams = T(cx, "lams", [128, 8])
    S.op("dve", lambda e: e.tensor_tensor(out=lamt[:, 0, :], in0=lamt[:, 0, :], in1=lamt[:, 1, :], op=ALU.mult), reads=["lamt"], writes=["lamt"])
    S.op("dve", lambda e: e.tensor_tensor(out=lamt[:, 2, :], in0=lamt[:, 2, :], in1=lamt[:, 3, :], op=ALU.mult), reads=["lamt"], writes=["lamt"])
    S.op("dve", lambda e: e.reduce_sum(out=lams[:, 0:1], in_=lamt[:, 0, :], axis=AX.X), reads=["lamt"], writes=["lams"])
    S.op("dve", lambda e: e.reduce_sum(out=lams[:, 1:2], in_=lamt[:, 2, :], axis=AX.X), reads=["lamt"], writes=["lams"])
    S.op("act", lambda e: e.activation(out=lams[:, 2:4], in_=lams[:, 0:2], func=AF.Exp), reads=["lams"], writes=["lams"])
    S.op("dve", lambda e: e.tensor_tensor(out=lams[:, 4:5], in0=lams[:, 3:4], in1=lams[:, 2:3], op=ALU.subtract), reads=["lams"], writes=["lams"])
    S.op("dve", lambda e: e.tensor_scalar(out=lams[:, 5:6], in0=lams[:, 4:5], scalar1=-lam_init, scalar2=None, op0=ALU.add), reads=["lams"], writes=["lams"])
    neglam = lams[:, 5:6]
    subg = T(cx, "subg", [128, 2])
    S.dma("pool", subg[:, 0:1], subn_d.rearrange("o (p a) -> (o p) a", a=1), writes=["subg"])
    S.op("dve", lambda e: e.tensor_scalar(out=subg[:, 1:2], in0=subg[:, 0:1], scalar1=1.0 - lam_init, scalar2=None, op0=ALU.mult), reads=["subg"], writes=["subg"])
    cosT = T(cx, "cosT", [128, NT, 32])
    sinT = T(cx, "sinT", [128, NT, 32])
    with ExitStack() as st2:
        posi = st2.enter_context(nc.sbuf_tensor(uname("posi"), [32, 128], I32))
        posf = st2.enter_context(nc.sbuf_tensor(uname("posf"), [32, 128], F32))
        post = st2.enter_context(nc.sbuf_tensor(uname("post"), [128, 32], F32))
        ang = st2.enter_context(nc.sbuf_tensor(uname("ang"), [128, NT, 32], F32))
        tmpf = st2.enter_context(nc.sbuf_tensor(uname("tmpf"), [128, NT, 32], F32))
        tmpi = st2.enter_context(nc.sbuf_tensor(uname("tmpi"), [128, NT, 32], I32))
        S.dma("sp", posi[:], pos_d.rearrange("o (t p) -> (o t) p", p=128), writes=["posi"])
        S.op("dve", lambda e: e.tensor_copy(out=posf[:], in_=posi[:]), reads=["posi"], writes=["posf"])
        S.op("pe", lambda e: e.transpose(cx.ps[0][:, 0:32], posf[:], cx.ident[0:32, 0:32]), reads=["posf", "consts"], writes=["ps0"])
        S.op("dve", lambda e: e.tensor_copy(out=post[:], in_=cx.ps[0][:, 0:32]), reads=[], writes=["post", "ps0"])
        S.op("dve", lambda e: e.tensor_tensor(out=ang[:], in0=post[:].unsqueeze(2).to_broadcast([128, NT, 32]), in1=cx.invf[:].unsqueeze(1).to_broadcast([128, NT, 32]), op=ALU.mult),
             reads=["post", "consts"], writes=["ang"])
        for which, dstT in ((0, sinT), (1, cosT)):
            off = 0.0 if which == 0 else PI / 2
            S.op("dve", lambda e, off=off: e.tensor_scalar(out=tmpf[:], in0=ang[:], scalar1=off, scalar2=1.0 / (2 * PI), op0=ALU.add, op1=ALU.mult), reads=["ang"], writes=["tmpf"])
            S.op("dve", lambda e: e.tensor_copy(out=tmpi[:], in_=tmpf[:]), reads=["tmpf"], writes=["tmpi"])
            S.op("dve", lambda e: e.tensor_copy(out=tmpf[:], in_=tmpi[:]), reads=["tmpi"], writes=["tmpf"])
            S.op("dve", lambda e: e.scalar_tensor_tensor(out=tmpf[:], in0=tmpf[:], scalar=-2 * PI, in1=ang[:], op0=ALU.mult, op1=ALU.add), reads=["tmpf", "ang"], writes=["tmpf"])
            S.op("dve", lambda e, off=off: e.tensor_scalar(out=tmpf[:], in0=tmpf[:], scalar1=off, scalar2=PI, op0=ALU.add, op1=ALU.min), reads=["tmpf"], writes=["tmpf"])
            S.op("dve", lambda e: e.tensor_scalar(out=tmpf[:], in0=tmpf[:], scalar1=-PI, scalar2=None, op0=ALU.max), reads=["tmpf"], writes=["tmpf"])
            S.op("act", lambda e, dstT=dstT: e.activation(out=dstT[:], in_=tmpf[:], func=AF.Sin), reads=["tmpf"], writes=["rope"])
        S.barrier()
    with ExitStack() as st2:
        cx.xt = [st2.enter_context(nc.sbuf_tensor(uname("xt%d" % i), [128, 1024], F32)) for i in range(2)]
        cx.hh = [st2.enter_context(nc.sbuf_tensor(uname("hh%d" % i), [128, 1024], F32)) for i in range(2)]
        cx.jk = st2.enter_context(nc.sbuf_tensor(uname("jk"), [128, 1024], F32))
        cx.nss = [st2.enter_context(nc.sbuf_tensor(uname("nss%d" % i), [128, 4], F32)) for i in range(2)]
        cx.ncnt = 0
        for t in range(NT):
            norm_tile(cx, t, x_d, hT, t * 128)
        S.barrier()
    with ExitStack() as st2:
        def TT(name, shape, dt=F32):
            return st2.enter_context(nc.sbuf_tensor(uname(name), shape, dt))
        wst = TT("wst", [128, 8, 384])
        wb = [TT("wb%d" % i, [128, 8, 384], BF16) for i in range(2)]
        QKT = [TT("QKT%d" % i, [128, 2, S_LEN], BF16) for i in range(2)]
        V = [TT("V%d" % i, [128, NT, 132], BF16) for i in range(2)]
        cx.PT = [TT("PT%d" % i, [128, 512], BF16) for i in range(3)]
        cx.mask01b = TT("mask01b", [128, 128], BF16)
        sq = [TT("sq%d" % i, [128, 256]) for i in range(2)]
        qn = [TT("qn%d" % i, [128, 256]) for i in range(2)]
        qr = [TT("qr%d" % i, [128, 256]) for i in range(2)]
        t1 = [TT("t1_%d" % i, [128, 128]) for i in range(2)]
        t2 = [TT("t2_%d" % i, [128, 128]) for i in range(2)]
        pss = [TT("pss%d" % i, [128, 12]) for i in range(2)]
        o0 = TT("o0", [128, 4, 128])
        ot = [TT("ot%d" % i, [128, 128]) for i in range(2)]
        ojk = TT("ojk", [128, 128])
        osb = [TT("osb%d" % i, [128, 132]) for i in range(2)]
        oss = [TT("oss%d" % i, [128, 4]) for i in range(2)]
        oTb = [TT("oTb%d" % i, [128, S_LEN], BF16) for i in range(2)]
        S.op("dve", lambda e: e.tensor_copy(out=cx.mask01b[:], in_=cx.mask01[:]), reads=["consts"], writes=["consts2"])
        for i in range(2):
            S.op("pool", lambda e, i=i: e.memset(V[i][:, :, 128:129], 1.0), writes=[("V", i)])
        cx.stcnt = 0
        fcnt = [0]
        for h in range(8):
            hb = h % 2
            load_cast_w(cx, wb[hb], wst, "wst", "wb%d" % hb,
                        [(win_d[:, o_ * 1024 + h * 128:o_ * 1024 + (h + 1) * 128], o_ * 128, 128) for o_ in range(3)], q="pool", cast_eng="pool")
            def proj(t, hb=hb):
                p = t % 2
                P1 = cx.ps[4 + p]
                kp = "ps%d" % (4 + p)
                for kc in range(8):
                    S.op("pe", lambda e, P1=P1, kc=kc, t=t, hb=hb: e.matmul(P1[:, 0:384], lhsT=hT[:, kc, t * 128:(t + 1) * 128], rhs=wb[hb][:, kc, :], start=(kc == 0), stop=(kc == 7)),
                         reads=[("hT", t), "wb%d" % hb], writes=[kp], sig=(kc == 7))

            def rest(t, hb=hb):
                p = t % 2
                P1 = cx.ps[4 + p]
                kp = "ps%d" % (4 + p)
                ksq, kqn, kqr, kt1, kt2, kss = "sq%d" % p, "qn%d" % p, "qr%d" % p, "t1_%d" % p, "t2_%d" % p, "pss%d" % p
                S.op("act", lambda e, P1=P1, p=p: e.activation(out=sq[p][:], in_=P1[:, 0:256], func=AF.Square), reads=[], writes=[ksq, kp])
                S.op("dve", lambda e, p=p: e.reduce_sum(out=pss[p][:, 0:4], in_=sq[p][:].rearrange("p (a b) -> p a b", a=4), axis=AX.X), reads=[ksq], writes=[kss])
                rstd_ops(cx, pss[p], 4, 1.0 / 64, kss)
                S.op("dve", lambda e, P1=P1, p=p: e.tensor_tensor(out=qn[p][:].rearrange("p (a b) -> p a b", a=4), in0=P1[:, 0:256].rearrange("p (a b) -> p a b", a=4),
                                                             in1=pss[p][:, 8:12].unsqueeze(2).to_broadcast([128, 4, 64]), op=ALU.mult), reads=[kss], writes=[kqn, kp])
                S.op("pool", lambda e, p=p: e.tensor_tensor(out=qn[p][:], in0=qn[p][:], in1=GQK[:], op=ALU.mult), reads=[kqn, "GQK"], writes=[kqn])
                q4 = qn[p][:].rearrange("p (a b) -> p a b", a=4)
                r4 = qr[p][:].rearrange("p (a b) -> p a b", a=4)
                cb = cosT[:, t, :].unsqueeze(1).to_broadcast([128, 4, 32])
                sb_ = sinT[:, t, :].unsqueeze(1).to_broadcast([128, 4, 32])
                a4 = t1[p][:].rearrange("p (a b) -> p a b", a=4)
                b4 = t2[p][:].rearrange("p (a b) -> p a b", a=4)
                S.op("dve", lambda e, a4=a4, q4=q4, cb=cb: e.tensor_tensor(out=a4, in0=q4[:, :, 0:32], in1=cb, op=ALU.mult), reads=[kqn, "rope"], writes=[kt1])
                S.op("pool", lambda e, b4=b4, q4=q4, sb_=sb_: e.tensor_tensor(out=b4, in0=q4[:, :, 32:64], in1=sb_, op=ALU.mult), reads=[kqn, "rope"], writes=[kt2])
                S.op("dve", lambda e, a4=a4, b4=b4, r4=r4: e.tensor_tensor(out=r4[:, :, 0:32], in0=a4, in1=b4, op=ALU.subtract), reads=[kt1, kt2], writes=[kqr])
                S.op("pool", lambda e, a4=a4, q4=q4, cb=cb: e.tensor_tensor(out=a4, in0=q4[:, :, 32:64], in1=cb, op=ALU.mult), reads=[kqn, "rope", kqr], writes=[kt1])
                S.op("dve", lambda e, b4=b4, q4=q4, sb_=sb_: e.tensor_tensor(out=b4, in0=q4[:, :, 0:32], in1=sb_, op=ALU.mult), reads=[kqn, "rope", kqr], writes=[kt2])
                S.op("pool", lambda e, a4=a4, b4=b4, r4=r4: e.tensor_tensor(out=r4[:, :, 32:64], in0=a4, in1=b4, op=ALU.add), reads=[kt1, kt2], writes=[kqr])
                for j in range(2):
                    S.op("pe", lambda e, j=j, p=p: e.transpose(cx.ps[6][:, j * 128:(j + 1) * 128], qr[p][:, j * 128:(j + 1) * 128], cx.ident[:]), reads=[kqr, "consts"], writes=["ps6"], sig=(j == 1))
                S.op("act", lambda e, t=t, hb=hb: e.activation(out=QKT[hb][:, :, t * 128:(t + 1) * 128], in_=cx.ps[6][:, 0:256].rearrange("p (a b) -> p a b", a=2), func=AF.Copy),
                     reads=[], writes=[("QK", hb), "ps6"])
                S.op("dve", lambda e, P1=P1, t=t, hb=hb: e.tensor_copy(out=V[hb][:, t, 0:128], in_=P1[:, 256:384]), reads=[], writes=[("V", hb), kp])

            proj(0)
            for t in range(NT):
                if t + 1 < NT:
                    proj(t + 1)
                rest(t)

            def finish(Tq, c, oap_ps, okey, h=h, hb=hb):
                i = Tq % 4
                f = fcnt[0] % 2
                fcnt[0] += 1
                kos, kot, kob = "oss%d" % f, "ot%d" % f, "osb%d" % f
                oap = osb[f]
                if f == 0:
                    S.op("act", lambda e: e.activation(out=oap[:, 0:129], in_=oap_ps, func=AF.Copy), reads=[], writes=[kob, okey])
                else:
                    S.op("dve", lambda e: e.tensor_copy(out=oap[:, 0:129], in_=oap_ps), reads=[], writes=[kob, okey])
                S.op("dve", lambda e: e.reciprocal(out=oss[f][:, 0:1], in_=oap[:, 128:129]), reads=[kob], writes=[kos])
                if c == 0:
                    S.op("act", lambda e: e.activation(out=o0[:, i, :], in_=oap[:, 0:128], func=AF.Copy, scale=oss[f][:, 0:1]), reads=[kob, kos], writes=[("o0", i)])
                    return
                S.op("act", lambda e: e.activation(out=ot[f][:], in_=oap[:, 0:128], func=AF.Copy, scale=oss[f][:, 0:1]), reads=[kob, kos], writes=[kot])
                S.op("dve", lambda e: e.scalar_tensor_tensor(out=ot[f][:], in0=ot[f][:], scalar=neglam, in1=o0[:, i, :], op0=ALU.mult, op1=ALU.add), reads=[kot, ("o0", i), "lams"], writes=[kot])
                S.op("act", lambda e: e.activation(out=ojk[:], in_=ot[f][:], func=AF.Square, accum_out=oss[f][:, 1:2]), reads=[kot], writes=["ojk", kos])
                S.op("dve", lambda e: e.tensor_scalar(out=oss[f][:, 2:3], in0=oss[f][:, 1:2], scalar1=1.0 / 128, scalar2=EPS, op0=ALU.mult, op1=ALU.add), reads=[kos], writes=[kos])
                S.op("act", lambda e: e.activation(out=oss[f][:, 2:3], in_=oss[f][:, 2:3], func=AF.Sqrt), reads=[kos], writes=[kos])
                S.op("dve", lambda e: e.reciprocal(out=oss[f][:, 3:4], in_=oss[f][:, 2:3]), reads=[kos], writes=[kos])
                S.op("dve", lambda e: e.tensor_scalar(out=ot[f][:], in0=ot[f][:], scalar1=oss[f][:, 3:4], scalar2=None, op0=ALU.mult), reads=[kot, kos], writes=[kot])
                S.op("pe", lambda e: e.transpose(cx.ps[7][:, 0:128], ot[f][:], cx.ident[:]), reads=[kot, "consts"], writes=["ps7"])
                S.op("act", lambda e: e.activation(out=oTb[hb][:, Tq * 128:(Tq + 1) * 128], in_=cx.ps[7][:, 0:128], func=AF.Copy, scale=subg[:, 1:2]), reads=["subg"], writes=[("oTb", hb), "ps7"])

            flash_head(cx, h, "att", QKT[hb][:, 0, :], QKT[hb][:, 1, :], V[hb], finish)
            S.dma("sp", oT_d[h], oTb[hb][:], reads=[("oTb", hb)], writes=["oT_d"])
        S.barrier()
    out_proj(cx, x_d, xo_d, wout_d, oT_d)


def out_proj(cx, x_d, xo_d, wout_d, oT_d):
    S, nc = cx.S, cx.nc
    with ExitStack() as st2:
        def TT(name, shape, dt=F32):
            return st2.enter_context(nc.sbuf_tensor(uname(name), shape, dt))
        wst = TT("wost", [128, 8, 1024])
        wo = TT("wo", [128, 8, 1024], BF16)
        oTt = [TT("oTt%d" % i, [128, 8, 128], BF16) for i in range(2)]
        xt = [TT("xo%d" % i, [128, 1024]) for i in range(2)]
        yt = [TT("yo%d" % i, [128, 1024]) for i in range(2)]
        load_cast_w(cx, wo, wst, "wost", "wo", [(wout_d, 0, 1024)], q="pool", cast_eng="pool")
        for t in range(NT):
            p = t % 2
            S.dma("sp", oTt[p][:], oT_d[:, :, t * 128:(t + 1) * 128].rearrange("h p j -> p h j"), reads=["oT_d"], writes=["oTt%d" % p])
            S.dma("sp", xt[p][:], x_d[t * 128:(t + 1) * 128, :], writes=["xo%d" % p])
            for n in range(2):
                pb = cx.ps[2 * p + n]
                kp = "ps%d" % (2 * p + n)
                for h in range(8):
                    S.op("pe", lambda e, pb=pb, h=h, p=p, n=n: e.matmul(pb[:, :], lhsT=oTt[p][:, h, :], rhs=wo[:, h, n * 512:(n + 1) * 512], start=(h == 0), stop=(h == 7)),
                         reads=["oTt%d" % p, "wo"], writes=[kp], sig=(h == 7))
                S.op("dve", lambda e, pb=pb, p=p, n=n: e.tensor_tensor(out=yt[p][:, n * 512:(n + 1) * 512], in0=pb[:, :], in1=cx.G[:, n * 512:(n + 1) * 512], op=ALU.mult),
                     reads=["mod2"], writes=["yo%d" % p, kp])
            S.op("pool", lambda e, p=p: e.tensor_tensor(out=yt[p][:], in0=yt[p][:], in1=xt[p][:], op=ALU.add), reads=["yo%d" % p, "xo%d" % p], writes=["yo%d" % p])
            S.dma("sp", xo_d[t * 128:(t + 1) * 128, :], yt[p][:], reads=["yo%d" % p], writes=["xo_d"])
        S.barrier()


def new_ctx(nc, stack):
    cx = Ctx()
    cx.nc = nc
    cx.stack = stack
    cx.S = Sched(nc, stack)
    cx.ps = [stack.enter_context(nc.psum_tensor("psb%d" % i, [128, 512], F32)) for i in range(8)]
    cx.consts = T(cx, "consts", [128, 1024])
    cx.ident = cx.consts[:, 0:128]
    cx.mask01 = cx.consts[:, 128:256]
    cx.maskneg = cx.consts[:, 256:384]
    cx.invf = cx.consts[:, 384:416]
    cx.SH = T(cx, "SH", [128, 1024])
    cx.A = T(cx, "A", [128, 1024])
    cx.G = T(cx, "G", [128, 1024])
    return cx


def dram_in(nc, name, shape, dt=F32):
    return nc.dram_tensor(name, list(shape), dt, kind="ExternalInput").ap()


def build_attn_prog():
    nc = bass.Bass("TRN2", target_bir_lowering=False)
    x_d = dram_in(nc, "x", [S_LEN, D])
    c_d = dram_in(nc, "c", [1, D])
    pos_d = dram_in(nc, "pos", [1, S_LEN], I32)
    adaw_d = dram_in(nc, "adaw", [D, 6 * D])
    adab_d = dram_in(nc, "adab", [1, 6 * D])
    mixn_d = dram_in(nc, "mixn", [1, D])
    win_d = dram_in(nc, "win", [D, 3 * D])
    wout_d = dram_in(nc, "wout", [D, D])
    qn_d = dram_in(nc, "qn", [1, 64])
    kn_d = dram_in(nc, "kn", [1, 64])
    lam_ds = [dram_in(nc, "lam%d" % i, [1, 64]) for i in range(4)]
    subn_d = dram_in(nc, "subn", [1, 128])
    consts_d = dram_in(nc, "consts", [128, 1024])
    xo_d = nc.dram_tensor("xo", [S_LEN, D], F32, kind="ExternalOutput").ap()
    oT_d = nc.dram_tensor("oT_d", [8, 128, S_LEN], BF16, kind="Internal").ap()
    with ExitStack() as stack:
        cx = new_ctx(nc, stack)
        block = stack.enter_context(nc.Block())
        cx.S.dma("sp", cx.consts[:], consts_d, writes=["consts"])
        phase_attn(cx, x_d, xo_d, c_d, pos_d, adaw_d, adab_d, mixn_d, win_d, wout_d, qn_d, kn_d, lam_ds, subn_d, oT_d)
        print("instructions", cx.S.nins, "waits", cx.S.nwait)
        cx.S.emit(block)
    return nc


def phase_moe(cx, x_d, xo_d, c_d, adaw_d, adab_d, ffnn_d, wr_d, br_d, wgu_d, bgu_d, wd_d, bd_d, n_exp=32):
    S, nc = cx.S, cx.nc
    common_setup(cx, 1, c_d, adaw_d, adab_d, ffnn_d)
    QT_ = 1024
    NQ = S_LEN // QT_
    with ExitStack() as st2:
        def TT(name, shape, dt=F32):
            return st2.enter_context(nc.sbuf_tensor(uname(name), shape, dt))
        biasT = TT("biasT", [128, 16, 32])
        with ExitStack() as st3:
            bgr = st3.enter_context(nc.sbuf_tensor(uname("bgr"), [32, 2048], F32))
            S.dma("pool", bgr[:], bgu_d, writes=["bgr"])
            bgr3 = bgr[:].rearrange("e (f two) -> e f two", two=2)
            for j in range(16):
                src = bgr3[:, (j % 8) * 128:(j % 8 + 1) * 128, j // 8]
                S.op("pe", lambda e, j=j, src=src: e.transpose(cx.ps[j // 8][:, (j % 8) * 32:(j % 8) * 32 + 32], src, cx.ident[0:32, 0:32]), reads=["bgr", "consts"], writes=["ps%d" % (j // 8)])
            for g in range(2):
                S.op("dve", lambda e, g=g: e.tensor_copy(out=biasT[:, g * 8:(g + 1) * 8, :], in_=cx.ps[g][:, 0:256].rearrange("p (a b) -> p a b", a=8)), reads=[], writes=["biasT", "ps%d" % g])
            S.op("dve", lambda e: e.tensor_scalar(out=biasT[:, 8:16, :], in0=biasT[:, 8:16, :], scalar1=1.0, scalar2=None, op0=ALU.add), reads=["biasT"], writes=["biasT"])
            S.barrier()
        hT = TT("hTm", [128, 8, QT_], BF16)
        hTf = TT("hTf", [128, 8, 128])
        acc = TT("acc", [128, 8, 1024])
        wr = TT("wr", [128, 8, 32])
        brb = TT("brb", [128, 32])
        bd = TT("bd", [32, 1024])
        gates = TT("gates", [128, 8, 32])
        gT = TT("gT", [32, 128])
        rt = TT("rt", [128, 96])
        r8 = TT("r8", [128, 16])
        wgu = [TT("wgu%d" % i, [128, 8, 1024], BF16) for i in range(2)]
        wdb = [TT("wdb%d" % i, [128, 4, 1024], BF16) for i in range(2)]
        NSTG = 4
        stg = [TT("stg%d" % i, [128, 1024]) for i in range(NSTG)]
        actT = [TT("actT%d" % i, [128, 4, 512], BF16) for i in range(2)]
        gt = [TT("gt%d" % i, [128, 512]) for i in range(2)]
        sg = [TT("sg%d" % i, [128, 512]) for i in range(2)]
        lt = [TT("lt%d" % i, [128, 512]) for i in range(2)]
        cx.xt = [TT("xt%d" % i, [128, 1024]) for i in range(2)]
        cx.hh = [TT("hh%d" % i, [128, 1024]) for i in range(2)]
        cx.jk = TT("jk", [128, 1024])
        cx.nss = [TT("nss%d" % i, [128, 4]) for i in range(2)]
        cx.ncnt = 0
        S.dma("pool", wr[:], wr_d.rearrange("(kc p) e -> p kc e", p=128), writes=["wr"])
        S.dma("pool", brb[:], br_d.partition_broadcast(128), writes=["brb"])
        S.dma("pool", bd[:], bd_d, writes=["bd"])
        NU = n_exp * 2

        def load_dma(seq, s_):
            u = seq % NU
            ex_, hf = u // 2, u % 2
            sb = (seq * 12 + s_) % NSTG
            ks = "stg%d" % sb
            if s_ < 8:
                S.dma("sp", stg[sb][:], wgu_d[ex_, s_ * 128:(s_ + 1) * 128, hf * 1024:(hf + 1) * 1024], writes=[ks])
            else:
                fc = s_ - 8
                S.dma("sp", stg[sb][:], wd_d[ex_, hf * 512 + fc * 128:hf * 512 + (fc + 1) * 128, :], writes=[ks])

        def load_cast(seq, s_):
            wb2 = seq % 2
            sb = (seq * 12 + s_) % NSTG
            ks = "stg%d" % sb
            if s_ < 8:
                s3 = stg[sb][:].rearrange("p (f two) -> p f two", two=2)
                S.op("act", lambda e: e.activation(out=wgu[wb2][:, s_, 0:512], in_=s3[:, :, 0], func=AF.Copy), reads=[ks], writes=["wgu%d" % wb2])
                S.op("pool", lambda e: e.tensor_copy(out=wgu[wb2][:, s_, 512:1024], in_=s3[:, :, 1]), reads=[ks], writes=["wgu%d" % wb2])
            else:
                fc = s_ - 8
                S.op("act", lambda e: e.activation(out=wdb[wb2][:, fc, :], in_=stg[sb][:], func=AF.Copy), reads=[ks], writes=["wdb%d" % wb2])

        for qtr in range(NQ):
            for tl in range(8):
                t = qtr * 8 + tl
                norm_tile(cx, t, x_d, hT, tl * 128, want_f32T=hTf)
                for kc in range(8):
                    S.op("pe", lambda e, kc=kc: e.matmul(cx.ps[5][:, 0:32], lhsT=hTf[:, kc, :], rhs=wr[:, kc, :], start=(kc == 0), stop=(kc == 7)), reads=["hTf", "wr"], writes=["ps5"], sig=(kc == 7))
                S.op("dve", lambda e: e.tensor_tensor(out=rt[:, 0:32], in0=cx.ps[5][:, 0:32], in1=brb[:], op=ALU.add), reads=["brb"], writes=["rt", "ps5"])
                S.op("dve", lambda e: e.max(out=r8[:, 0:8], in_=rt[:, 0:32]), reads=["rt"], writes=["r8"])
                S.op("dve", lambda e: e.tensor_scalar(out=rt[:, 32:64], in0=rt[:, 0:32], scalar1=r8[:, 3:4], scalar2=None, op0=ALU.is_ge), reads=["rt", "r8"], writes=["rt"])
                S.op("dve", lambda e: e.tensor_scalar(out=r8[:, 8:9], in0=r8[:, 0:1], scalar1=-1.0, scalar2=None, op0=ALU.mult), reads=["r8"], writes=["r8"])
                S.op("act", lambda e: e.activation(out=rt[:, 64:96], in_=rt[:, 0:32], func=AF.Exp, bias=r8[:, 8:9]), reads=["rt", "r8"], writes=["rt"])
                S.op("dve", lambda e: e.tensor_tensor(out=rt[:, 64:96], in0=rt[:, 64:96], in1=rt[:, 32:64], op=ALU.mult), reads=["rt"], writes=["rt"])
                S.op("dve", lambda e: e.reduce_sum(out=r8[:, 9:10], in_=rt[:, 64:96], axis=AX.X), reads=["rt"], writes=["r8"])
                S.op("dve", lambda e: e.reciprocal(out=r8[:, 10:11], in_=r8[:, 9:10]), reads=["r8"], writes=["r8"])
                S.op("dve", lambda e, tl=tl: e.tensor_scalar(out=gates[:, tl, :], in0=rt[:, 64:96], scalar1=r8[:, 10:11], scalar2=None, op0=ALU.mult), reads=["rt", "r8"], writes=["gates"])
                S.op("pe", lambda e, tl=tl: e.transpose(cx.ps[5][0:32, 128:256], gates[:, tl, :], cx.ident[:]), reads=["gates", "consts"], writes=["ps5"])
                S.op("dve", lambda e: e.tensor_copy(out=gT[:], in_=cx.ps[5][0:32, 128:256]), reads=[], writes=["gT", "ps5"])
                for n in range(2):
                    S.op("pe", lambda e, n=n: e.matmul(cx.ps[4][:, :], lhsT=gT[:], rhs=bd[:, n * 512:(n + 1) * 512], start=True, stop=True), reads=["gT", "bd"], writes=["ps4"])
                    S.op("act", lambda e, tl=tl, n=n: e.activation(out=acc[:, tl, n * 512:(n + 1) * 512], in_=cx.ps[4][:, :], func=AF.Copy), reads=[], writes=[("acc", tl), "ps4"])
            for u in range(NU):
                ex, hf = u // 2, u % 2
                seq = qtr * NU + u
                wb_ = seq % 2
                if seq == 0:
                    for s_ in range(12):
                        load_dma(0, s_)
                        load_cast(0, s_)
                nxt = seq + 1 if seq + 1 < NQ * NU else None
                slot = [0]

                def tick(nxt=nxt, slot=slot):
                    s_ = slot[0]
                    slot[0] += 1
                    if nxt is None:
                        return
                    if s_ < 12:
                        load_dma(nxt, s_)
                    if 2 <= s_ < 14:
                        load_cast(nxt, s_ - 2)
                def gate_up(tb, u=u, ex=ex, hf=hf, wb_=wb_, tick=tick):
                    at = actT[tb % 2]
                    for j in range(4):
                        p = j % 2
                        jj = hf * 4 + j
                        psg, psl = cx.ps[2 * p], cx.ps[2 * p + 1]
                        kg, kl = "ps%d" % (2 * p), "ps%d" % (2 * p + 1)
                        for (pp, kk, off) in ((psg, kg, 0), (psl, kl, 512)):
                            for kc in range(8):
                                S.op("pe", lambda e, pp=pp, kc=kc, off=off, j=j: e.matmul(pp[:, :], lhsT=wgu[wb_][:, kc, off + j * 128:off + (j + 1) * 128], rhs=hT[:, kc, tb * 512:(tb + 1) * 512], start=(kc == 0), stop=(kc == 7)),
                                     reads=["wgu%d" % wb_] + [("hT", tb * 4 + q_) for q_ in range(4)], writes=[kk], sig=(kc == 7))
                        S.op("dve", lambda e, p=p, psg=psg, jj=jj: e.tensor_scalar(out=gt[p][:], in0=psg[:, :], scalar1=biasT[:, jj, ex:ex + 1], scalar2=7.0, op0=ALU.add, op1=ALU.min), reads=["biasT"], writes=["gt%d" % p, kg])
                        S.op("act", lambda e, p=p: e.activation(out=sg[p][:], in_=gt[p][:], func=AF.Sigmoid, scale=1.702), reads=["gt%d" % p], writes=["sg%d" % p])
                        S.op("dve", lambda e, p=p, psl=psl, jj=jj: e.tensor_scalar(out=lt[p][:], in0=psl[:, :], scalar1=biasT[:, 8 + jj, ex:ex + 1], scalar2=8.0, op0=ALU.add, op1=ALU.min), reads=["biasT"], writes=["lt%d" % p, kl])
                        S.op("pool", lambda e, p=p: e.tensor_tensor(out=gt[p][:], in0=gt[p][:], in1=sg[p][:], op=ALU.mult), reads=["gt%d" % p, "sg%d" % p], writes=["gt%d" % p])
                        S.op("dve", lambda e, p=p, j=j: e.scalar_tensor_tensor(out=at[:, j, :], in0=lt[p][:], scalar=-6.0, in1=gt[p][:], op0=ALU.max, op1=ALU.mult), reads=["gt%d" % p, "lt%d" % p], writes=[("actT", tb % 2, j)])
                        tick()

                def down(tb, u=u, ex=ex, wb_=wb_, tick=tick):
                    at = actT[tb % 2]
                    for tt in range(4):
                        tl = tb * 4 + tt
                        for n in range(2):
                            pb = cx.ps[4 + n]
                            kp = "ps%d" % (4 + n)
                            for fc in range(4):
                                S.op("pe", lambda e, pb=pb, fc=fc, tt=tt, n=n: e.matmul(pb[:, :], lhsT=at[:, fc, tt * 128:(tt + 1) * 128], rhs=wdb[wb_][:, fc, n * 512:(n + 1) * 512], start=(fc == 0), stop=(fc == 3)),
                                     reads=[("actT", tb % 2, fc), "wdb%d" % wb_], writes=[kp], sig=(fc == 3))
                            S.op("dve", lambda e, pb=pb, tl=tl, n=n: e.scalar_tensor_tensor(out=acc[:, tl, n * 512:(n + 1) * 512], in0=pb[:, :], scalar=gates[:, tl, ex:ex + 1], in1=acc[:, tl, n * 512:(n + 1) * 512], op0=ALU.mult, op1=ALU.add),
                                 reads=["gates"], writes=[("acc", tl), kp])
                        tick()

                gate_up(0)
                gate_up(1)
                down(0)
                down(1)
            for tl in range(8):
                t = qtr * 8 + tl
                p = tl % 2
                xtp = cx.xt[p]
                S.dma("sp", xtp[:], x_d[t * 128:(t + 1) * 128, :], writes=["xt%d" % p])
                S.op("dve", lambda e, tl=tl: e.tensor_tensor(out=acc[:, tl, :], in0=acc[:, tl, :], in1=cx.G[:], op=ALU.mult), reads=["mod2"], writes=[("acc", tl)])
                S.op("dve", lambda e, tl=tl, xtp=xtp: e.tensor_tensor(out=acc[:, tl, :], in0=acc[:, tl, :], in1=xtp[:], op=ALU.add), reads=["xt%d" % p], writes=[("acc", tl)])
                S.dma("sp", xo_d[t * 128:(t + 1) * 128, :], acc[:, tl, :], reads=[("acc", tl)], writes=["xo_d"])
        S.barrier()


def build_moe_prog(n_exp=32):
    nc = bass.Bass("TRN2", target_bir_lowering=False)
    x_d = dram_in(nc, "x", [S_LEN, D])
    c_d = dram_in(nc, "c", [1, D])
    adaw_d = dram_in(nc, "adaw", [D, 6 * D])
    adab_d = dram_in(nc, "adab", [1, 6 * D])
    ffnn_d = dram_in(nc, "ffnn", [1, D])
    wr_d = dram_in(nc, "wr", [D, 32])
    br_d = dram_in(nc, "br", [1, 32])
    wgu_d = dram_in(nc, "wgu", [32, D, 2 * D])
    bgu_d = dram_in(nc, "bgu", [32, 2 * D])
    wd_d = dram_in(nc, "wd", [32, D, D])
    bd_d = dram_in(nc, "bd", [32, D])
    consts_d = dram_in(nc, "consts", [128, 1024])
    xo_d = nc.dram_tensor("xo", [S_LEN, D], F32, kind="ExternalOutput").ap()
    with ExitStack() as stack:
        cx = new_ctx(nc, stack)
        block = stack.enter_context(nc.Block())
        cx.S.dma("sp", cx.consts[:], consts_d, writes=["consts"])
        phase_moe(cx, x_d, xo_d, c_d, adaw_d, adab_d, ffnn_d, wr_d, br_d, wgu_d, bgu_d, wd_d, bd_d, n_exp=n_exp)
        print("instructions", cx.S.nins, "waits", cx.S.nwait)
        cx.S.emit(block)
    return nc


def phase_mlstm(cx, x_d, xo_d, c_d, adaw_d, adab_d, mixn_d, win_d, bi_d, bf_d, outn_d, wout_d, oT_d, sel_d):
    S, nc = cx.S, cx.nc
    common_setup(cx, 0, c_d, adaw_d, adab_d, mixn_d)
    hT = T(cx, "hT", [128, 8, S_LEN], BF16)
    OG = T(cx, "OG", [128, 1024])
    S.dma("pool", OG[:], outn_d.partition_broadcast(128), writes=["OG"])
    cx.sel3 = T(cx, "sel3", [128, 8, 128], BF16)
    cx.r3 = T(cx, "r3", [32, S_LEN], BF16)
    cx.ucol = T(cx, "ucol", [128, 256])
    emcol = T(cx, "emcol", [128, 256])
    with ExitStack() as st2:
        cx.xt = [st2.enter_context(nc.sbuf_tensor(uname("xt%d" % i), [128, 1024], F32)) for i in range(2)]
        cx.hh = [st2.enter_context(nc.sbuf_tensor(uname("hh%d" % i), [128, 1024], F32)) for i in range(2)]
        cx.jk = st2.enter_context(nc.sbuf_tensor(uname("jk"), [128, 1024], F32))
        cx.nss = [st2.enter_context(nc.sbuf_tensor(uname("nss%d" % i), [128, 4], F32)) for i in range(2)]
        cx.ncnt = 0
        jk_ = cx.jk
        sel3_ = cx.sel3
        S.dma("pool", jk_[:], sel_d, writes=["jk"])
        S.op("dve", lambda e: e.tensor_copy(out=sel3_[:].rearrange("p a b -> p (a b)"), in_=jk_[:]), reads=["jk"], writes=["sel3"])
        for t in range(NT):
            norm_tile(cx, t, x_d, hT, t * 128)
        S.barrier()
    with ExitStack() as st2:
        def TT(name, shape, dt=F32):
            return st2.enter_context(nc.sbuf_tensor(uname(name), shape, dt))
        wif = TT("wif", [128, 8, 16])
        wifb = TT("wifb", [128, 8, 16], BF16)
        bcol = TT("bcol", [8, 4])
        gi_t = TT("gi", [32, S_LEN])
        gf = TT("gf", [8, S_LEN])
        Ft_t = TT("Ft", [32, S_LEN])
        S.op("pool", lambda e: e.memset(gi_t[:], 0.0), writes=["g0"])
        S.op("pool", lambda e: e.memset(Ft_t[:], 0.0), writes=["Ft"])
        S.op("pool", lambda e: e.memset(cx.r3[:], 0.0), writes=["r3"])
        gi = gi_t[0:8, :]
        Ft = Ft_t[0:8, :]
        ones = TT("ones", [8, S_LEN])
        cm = TT("cm", [8, S_LEN])
        rb = [TT("rb%d" % i, [8, S_LEN], BF16) for i in range(3)]
        S.dma("sp", wif[:], win_d[:, 3072:3088].rearrange("(kc p) j -> p kc j", p=128), writes=["wif"])
        S.op("dve", lambda e: e.tensor_copy(out=wifb[:], in_=wif[:]), reads=["wif"], writes=["wifb"])
        S.dma("sp", bcol[:, 0:1], bi_d.rearrange("o (p a) -> (o p) a", a=1), writes=["bcol"])
        S.dma("sp", bcol[:, 1:2], bf_d.rearrange("o (p a) -> (o p) a", a=1), writes=["bcol"])
        S.op("dve", lambda e: e.tensor_scalar(out=bcol[:, 2:4], in0=bcol[:, 0:2], scalar1=1.0 / 15, scalar2=None, op0=ALU.mult), reads=["bcol"], writes=["bcol"])
        S.op("pool", lambda e: e.memset(ones[:], 1.0), writes=["ones"])
        for blk in range(8):
            for g in range(2):
                for kc in range(8):
                    S.op("pe", lambda e, g=g, kc=kc, blk=blk: e.matmul(cx.ps[g][0:8, :], lhsT=wifb[:, kc, g * 8:(g + 1) * 8], rhs=hT[:, kc, blk * 512:(blk + 1) * 512], start=(kc == 0), stop=(kc == 7)),
                         reads=["wifb"] + [("hT", blk * 4 + q_) for q_ in range(4)], writes=["ps%d" % g], sig=(kc == 7))
                dst = gi if g == 0 else gf
                S.op("act", lambda e, g=g, blk=blk, dst=dst: e.activation(out=dst[:, blk * 512:(blk + 1) * 512], in_=cx.ps[g][0:8, :], func=AF.Tanh, scale=1.0 / 15, bias=bcol[:, 2 + g:3 + g]),
                     reads=["bcol"], writes=["g%d" % g, "ps%d" % g])
        S.op("dve", lambda e: e.tensor_scalar(out=gi[:], in0=gi[:], scalar1=15.0, scalar2=None, op0=ALU.mult), reads=["g0"], writes=["g0"])
        S.op("act", lambda e: e.activation(out=gf[:], in_=gf[:], func=AF.Exp, scale=-15.0), reads=["g1"], writes=["g1"])
        S.op("act", lambda e: e.activation(out=gf[:], in_=gf[:], func=AF.Ln, bias=1.0), reads=["g1"], writes=["g1"])
        S.op("dve", lambda e: e.tensor_tensor_scan(out=Ft[:], data0=ones[:], data1=gf[:], initial=0.0, op0=ALU.mult, op1=ALU.subtract), reads=["ones", "g1"], writes=["Ft"])
        S.op("dve", lambda e: e.tensor_tensor(out=gi[:], in0=gi[:], in1=Ft[:], op=ALU.subtract), reads=["g0", "Ft"], writes=["g0"])
        S.op("dve", lambda e: e.tensor_tensor_scan(out=cm[:], data0=ones[:], data1=gi[:], initial=0.0, op0=ALU.mult, op1=ALU.max), reads=["ones", "g0"], writes=["cm"])
        S.op("dve", lambda e: e.tensor_tensor(out=Ft[:], in0=Ft[:], in1=cm[:], op=ALU.add), reads=["Ft", "cm"], writes=["Ft"])
        S.op("act", lambda e: e.activation(out=Ft[:], in_=Ft[:], func=AF.Exp, scale=-1.0), reads=["Ft"], writes=["Ft"])
        S.op("dve", lambda e: e.tensor_scalar(out=cm[:], in0=cm[:], scalar1=-1.0, scalar2=None, op0=ALU.mult), reads=["cm"], writes=["cm"])
        for j in range(3):
            S.op("dve", lambda e, j=j: e.tensor_copy(out=rb[j][:], in_=cm[:]), reads=["cm"], writes=["rb%d" % j])
            if j < 2:
                S.op("dve", lambda e, j=j: e.tensor_tensor(out=cm[:], in0=cm[:], in1=rb[j][:], op=ALU.subtract), reads=["cm", "rb%d" % j], writes=["cm"])
            S.dma("sp", cx.r3[8 * j:8 * j + 8, :], rb[j][:], reads=["rb%d" % j], writes=["r3"])
        for which, src, dst, key in ((0, gi, cx.ucol, "ucol"), (1, Ft, emcol, "emcol")):
            for t in range(NT):
                S.op("pe", lambda e, which=which, src=src, t=t: e.transpose(cx.ps[2 + which][:, t * 8:(t + 1) * 8], src[:, t * 128:(t + 1) * 128], cx.ident[0:8, 0:8]),
                     reads=["g0" if which == 0 else "Ft", "consts"], writes=["ps%d" % (2 + which)])
            S.op("dve", lambda e, which=which, dst=dst: e.tensor_copy(out=dst[:], in_=cx.ps[2 + which][:, 0:256]), reads=[], writes=[key, "ps%d" % (2 + which)])
        dbg = getattr(cx, "dbg", None)
        if dbg:
            S.dma("sp", dbg["ucol"], cx.ucol[:], reads=["ucol"], writes=["dbg1"])
            S.dma("sp", dbg["emcol"], emcol[:], reads=["emcol"], writes=["dbg2"])
            S.dma("sp", dbg["r3"], cx.r3[:], reads=["r3"], writes=["dbg3"])
        S.barrier()
    with ExitStack() as st2:
        def TT(name, shape, dt=F32):
            return st2.enter_context(nc.sbuf_tensor(uname(name), shape, dt))
        wst = TT("wst", [128, 8, 384])
        wb = [TT("wb%d" % i, [128, 8, 384], BF16) for i in range(2)]
        QKT = [TT("QKT%d" % i, [128, 2, S_LEN], BF16) for i in range(2)]
        V = [TT("V%d" % i, [128, NT, 132], BF16) for i in range(2)]
        og = [TT("og0", [128, NT, 128], BF16)] * 2
        cx.PT = [TT("PT%d" % i, [128, 512], BF16) for i in range(2)]
        cx.DT = [TT("DT%d" % i, [128, 512]) for i in range(2)]
        if getattr(cx, "dbg", None) is not None:
            cx.dbgt = TT("dbgt", [128, 512])
        qk = [TT("qk%d" % i, [128, 128]) for i in range(2)]
        ot = [TT("ot%d" % i, [128, 128]) for i in range(2)]
        ojk = TT("ojk", [128, 128])
        osb = [TT("osb%d" % i, [128, 132]) for i in range(2)]
        oss = [TT("oss%d" % i, [128, 8]) for i in range(2)]
        oTb = [TT("oTb0", [128, S_LEN], BF16)] * 2
        for i in range(2):
            S.op("pool", lambda e, i=i: e.memset(V[i][:, :, 128:129], 1.0), writes=[("V", i)])
        cx.stcnt = 0
        fcnt = [0]
        for h in range(8):
            hb = h % 2
            load_cast_w(cx, wb[hb], wst, "wst", "wb%d" % hb,
                        [(win_d[:, h * 64:(h + 1) * 64], 0, 64), (win_d[:, 512 + h * 64:512 + (h + 1) * 64], 64, 64),
                         (win_d[:, 1024 + h * 128:1024 + (h + 1) * 128], 128, 128), (win_d[:, 2048 + h * 128:2048 + (h + 1) * 128], 256, 128)], q="pool", cast_eng="pool")
            for t in range(NT):
                p = t % 2
                P1 = cx.ps[4 + p]
                kp = "ps%d" % (4 + p)
                for kc in range(8):
                    S.op("pe", lambda e, P1=P1, kc=kc, t=t, hb=hb: e.matmul(P1[:, 0:384], lhsT=hT[:, kc, t * 128:(t + 1) * 128], rhs=wb[hb][:, kc, :], start=(kc == 0), stop=(kc == 7)),
                         reads=[("hT", t), "wb%d" % hb], writes=[kp], sig=(kc == 7))
                S.op("dve", lambda e, P1=P1, p=p: e.tensor_copy(out=qk[p][:], in_=P1[:, 0:128]), reads=[], writes=["qk%d" % p, kp])
                S.op("dve", lambda e, P1=P1, t=t, hb=hb: e.tensor_copy(out=V[hb][:, t, 0:128], in_=P1[:, 128:256]), reads=[], writes=[("V", hb), kp])
                S.op("act", lambda e, P1=P1, t=t, hb=hb: e.activation(out=og[hb][:, t, :], in_=P1[:, 256:384], func=AF.Sigmoid), reads=[], writes=[("og", 0), kp])
                for j in range(2):
                    S.op("pe", lambda e, j=j, p=p: e.transpose(cx.ps[6][0:64, j * 128:(j + 1) * 128], qk[p][:, j * 64:(j + 1) * 64], cx.ident[:]), reads=["qk%d" % p, "consts"], writes=["ps6"], sig=(j == 1))
                S.op("act", lambda e, t=t, hb=hb: e.activation(out=QKT[hb][0:64, :, t * 128:(t + 1) * 128], in_=cx.ps[6][0:64, 0:256].rearrange("p (a b) -> p a b", a=2), func=AF.Copy),
                     reads=[], writes=[("QK", hb), "ps6"])

            def finish(Tq, c, oap_ps, okey, h=h, hb=hb):
                f = fcnt[0] % 2
                fcnt[0] += 1
                kos, kot, kob = "oss%d" % f, "ot%d" % f, "osb%d" % f
                oap = osb[f]
                if f == 0:
                    S.op("act", lambda e: e.activation(out=oap[:, 0:129], in_=oap_ps, func=AF.Copy), reads=[], writes=[kob, okey])
                else:
                    S.op("dve", lambda e: e.tensor_copy(out=oap[:, 0:129], in_=oap_ps), reads=[], writes=[kob, okey])
                S.op("act", lambda e: e.activation(out=oss[f][:, 0:1], in_=oap[:, 128:129], func=AF.Abs), reads=[kob], writes=[kos])
                S.op("dve", lambda e: e.tensor_tensor(out=oss[f][:, 0:1], in0=oss[f][:, 0:1], in1=emcol[:, Tq * 8 + h:Tq * 8 + h + 1], op=ALU.max), reads=[kos, "emcol"], writes=[kos])
                S.op("dve", lambda e: e.reciprocal(out=oss[f][:, 1:2], in_=oss[f][:, 0:1]), reads=[kos], writes=[kos])
                S.op("act", lambda e: e.activation(out=ot[f][:], in_=oap[:, 0:128], func=AF.Copy, scale=oss[f][:, 1:2]), reads=[kob, kos], writes=[kot])
                dbg = getattr(cx, "dbg", None)
                dump = dbg is not None and h == 0 and Tq == 12
                if dump:
                    S.dma("sp", dbg["osb"], oap[:, 0:132], reads=[kob], writes=["dbg4"])
                    S.dma("sp", dbg["ot_a"], ot[f][:], reads=[kot], writes=["dbg5"])
                S.op("act", lambda e: e.activation(out=ojk[:], in_=ot[f][:], func=AF.Square, accum_out=oss[f][:, 2:3]), reads=[kot], writes=["ojk", kos])
                S.op("dve", lambda e: e.tensor_scalar(out=oss[f][:, 3:4], in0=oss[f][:, 2:3], scalar1=1.0 / 128, scalar2=EPS, op0=ALU.mult, op1=ALU.add), reads=[kos], writes=[kos])
                S.op("act", lambda e: e.activation(out=oss[f][:, 3:4], in_=oss[f][:, 3:4], func=AF.Sqrt), reads=[kos], writes=[kos])
                S.op("dve", lambda e: e.reciprocal(out=oss[f][:, 4:5], in_=oss[f][:, 3:4]), reads=[kos], writes=[kos])
                S.op("dve", lambda e: e.scalar_tensor_tensor(out=ot[f][:], in0=ot[f][:], scalar=oss[f][:, 4:5], in1=OG[:, h * 128:(h + 1) * 128], op0=ALU.mult, op1=ALU.mult), reads=[kot, kos, "OG"], writes=[kot])
                if dump:
                    S.dma("sp", dbg["ot_b"], ot[f][:], reads=[kot], writes=["dbg6"])
                    S.dma("sp", dbg["oss"], oss[f][:], reads=[kos], writes=["dbg7"])
                    S.dma("sp", dbg["og"], og[hb][:, Tq, :], reads=[("og", 0)], writes=["dbg8"])
                S.op("pool", lambda e: e.tensor_tensor(out=ot[f][:], in0=ot[f][:], in1=og[hb][:, Tq, :], op=ALU.mult), reads=[kot, ("og", 0)], writes=[kot])
                if dump:
                    S.dma("sp", dbg["ot_c"], ot[f][:], reads=[kot], writes=["dbg9"])
                S.op("pe", lambda e: e.transpose(cx.ps[7][:, 0:128], ot[f][:], cx.ident[:]), reads=[kot, "consts"], writes=["ps7"])
                S.op("act", lambda e: e.activation(out=oTb[hb][:, Tq * 128:(Tq + 1) * 128], in_=cx.ps[7][:, 0:128], func=AF.Copy), reads=[], writes=[("oTb", 0), "ps7"])

            flash_head(cx, h, "ml", QKT[hb][:, 0, :], QKT[hb][:, 1, :], V[hb], finish)
            S.dma("sp", oT_d[h], oTb[hb][:], reads=[("oTb", 0)], writes=["oT_d"])
        S.barrier()
    out_proj(cx, x_d, xo_d, wout_d, oT_d)


def build_ml_prog():
    nc = bass.Bass("TRN2", target_bir_lowering=False)
    x_d = dram_in(nc, "x", [S_LEN, D])
    c_d = dram_in(nc, "c", [1, D])
    adaw_d = dram_in(nc, "adaw", [D, 6 * D])
    adab_d = dram_in(nc, "adab", [1, 6 * D])
    mixn_d = dram_in(nc, "mixn", [1, D])
    win_d = dram_in(nc, "win", [D, 3088])
    bi_d = dram_in(nc, "bi", [1, 8])
    bf_d = dram_in(nc, "bf", [1, 8])
    outn_d = dram_in(nc, "outn", [1, D])
    wout_d = dram_in(nc, "wout", [D, D])
    consts_d = dram_in(nc, "consts", [128, 1024])
    sel_d = dram_in(nc, "sel", [128, 1024])
    xo_d = nc.dram_tensor("xo", [S_LEN, D], F32, kind="ExternalOutput").ap()
    oT_d = nc.dram_tensor("oT_d", [8, 128, S_LEN], BF16, kind="Internal").ap()
    with ExitStack() as stack:
        cx = new_ctx(nc, stack)
        block = stack.enter_context(nc.Block())
        cx.S.dma("sp", cx.consts[:], consts_d, writes=["consts"])
        phase_mlstm(cx, x_d, xo_d, c_d, adaw_d, adab_d, mixn_d, win_d, bi_d, bf_d, outn_d, wout_d, oT_d, sel_d)
        print("instructions", cx.S.nins, "waits", cx.S.nwait)
        cx.S.emit(block)
    return nc


PHASES = ("attn", "moe0", "mlstm", "moe1")


def build_prog(phases, n_exp=32, sliced=False, debug=False):
    nc = bass.Bass("TRN2", target_bir_lowering=False)
    d = {}
    def inp(name, shape, dt=F32):
        d[name] = dram_in(nc, name, shape, dt)
        return d[name]
    x_d = inp("x", [S_LEN, D])
    c_d = inp("c", [1, D])
    NL = 1 if sliced else 2
    adaw_d = inp("ada_w", [NL, D, 6 * D])
    adab_d = inp("ada_b", [NL, 6 * D])
    consts_d = inp("consts", [128, 1024])
    if "attn" in phases:
        pos_d = inp("positions", [1, S_LEN], I32)
        inp("mix_norm", [NL, D])
        inp("att_w_in", [1, D, 3 * D]); inp("att_w_out", [1, D, D])
        for nm in ("att_q_norm", "att_k_norm", "att_lam_q1", "att_lam_k1", "att_lam_q2", "att_lam_k2"):
            inp(nm, [1, 64])
        inp("att_sub_norm", [1, 128])
    if "mlstm" in phases:
        if "mix_norm" not in d:
            inp("mix_norm", [NL, D])
        inp("ml_w_in", [1, D, 3088]); inp("ml_b_igate", [1, 8]); inp("ml_b_fgate", [1, 8])
        inp("ml_out_norm", [1, D]); inp("ml_w_out", [1, D, D]); inp("sel", [128, 1024])
    if "moe0" in phases or "moe1" in phases:
        inp("ffn_norm", [NL, D]); inp("router_w", [NL, D, 32]); inp("router_b", [NL, 32])
        inp("moe_w_gate_up", [NL, 32, D, 2 * D]); inp("moe_b_gate_up", [NL, 32, 2 * D])
        inp("moe_w_down", [NL, 32, D, D]); inp("moe_b_down", [NL, 32, D])
    xo_d = nc.dram_tensor("xo", [S_LEN, D], F32, kind="ExternalOutput").ap()
    oT_d = nc.dram_tensor("oT_d", [8, 128, S_LEN], BF16, kind="ExternalOutput" if debug else "Internal").ap()
    xs = [x_d]
    for i in range(len(phases) - 1):
        xs.append(nc.dram_tensor("xmid%d" % i, [S_LEN, D], F32, kind="ExternalOutput" if debug else "Internal").ap())
    xs.append(xo_d)
    with ExitStack() as stack:
        cx = new_ctx(nc, stack)
        block = stack.enter_context(nc.Block())
        if debug:
            cx.dbg = {"ucol": nc.dram_tensor("dbg_ucol", [128, 256], F32, kind="ExternalOutput").ap(),
                      "emcol": nc.dram_tensor("dbg_emcol", [128, 256], F32, kind="ExternalOutput").ap(),
                      "r3": nc.dram_tensor("dbg_r3", [32, S_LEN], BF16, kind="ExternalOutput").ap(),
                      "osb": nc.dram_tensor("dbg_osb", [128, 132], F32, kind="ExternalOutput").ap(),
                      "ot_a": nc.dram_tensor("dbg_ot_a", [128, 128], F32, kind="ExternalOutput").ap(),
                      "ot_b": nc.dram_tensor("dbg_ot_b", [128, 128], F32, kind="ExternalOutput").ap(),
                      "ot_c": nc.dram_tensor("dbg_ot_c", [128, 128], F32, kind="ExternalOutput").ap(),
                      "oss": nc.dram_tensor("dbg_oss", [128, 8], F32, kind="ExternalOutput").ap(),
                      "og": nc.dram_tensor("dbg_og", [128, 128], BF16, kind="ExternalOutput").ap(),
                      "DT0": nc.dram_tensor("dbg_DT0", [128, 512], F32, kind="ExternalOutput").ap(),
                      "DT11": nc.dram_tensor("dbg_DT11", [128, 512], F32, kind="ExternalOutput").ap(),
                      "RB0": nc.dram_tensor("dbg_RB0", [128, 512], F32, kind="ExternalOutput").ap(),
                      "RB11": nc.dram_tensor("dbg_RB11", [128, 512], F32, kind="ExternalOutput").ap(),
                      "sel3": nc.dram_tensor("dbg_sel3", [32, 128], BF16, kind="ExternalOutput").ap()}
        cx.S.dma("sp", cx.consts[:], consts_d, writes=["consts"])
        for i, ph in enumerate(phases):
            xin, xout = xs[i], xs[i + 1]
            with ExitStack() as pst:
                cx.stack = pst
                if ph == "attn":
                    phase_attn(cx, xin, xout, c_d, pos_d, adaw_d[0], adab_d[0:1, :], d["mix_norm"][0:1, :], d["att_w_in"][0], d["att_w_out"][0],
                               d["att_q_norm"], d["att_k_norm"], [d["att_lam_q1"], d["att_lam_k1"], d["att_lam_q2"], d["att_lam_k2"]], d["att_sub_norm"], oT_d)
                elif ph == "mlstm":
                    L = 0 if sliced else 1
                    phase_mlstm(cx, xin, xout, c_d, adaw_d[L], adab_d[L:L + 1, :], d["mix_norm"][L:L + 1, :], d["ml_w_in"][0], d["ml_b_igate"], d["ml_b_fgate"],
                                d["ml_out_norm"], d["ml_w_out"][0], oT_d, d["sel"])
                else:
                    L = 0 if sliced else int(ph[-1])
                    phase_moe(cx, xin, xout, c_d, adaw_d[L], adab_d[L:L + 1, :], d["ffn_norm"][L:L + 1, :], d["router_w"][L], d["router_b"][L:L + 1, :],
                              d["moe_w_gate_up"][L], d["moe_b_gate_up"][L], d["moe_w_down"][L], d["moe_b_down"][L], n_exp=n_exp)
                cx.S.barrier()
            cx.stack = stack
        cx.S.emit(block)
    return nc, list(d.keys())


LAUNCH_GROUPS = [("attn",), ("moe0",), ("mlstm",), ("moe1",)]
PHASE_LAYER = {"attn": 0, "moe0": 0, "mlstm": 1, "moe1": 1}
PER_LAYER = ("ada_w", "ada_b", "mix_norm", "ffn_norm", "router_w", "router_b", "moe_w_gate_up", "moe_b_gate_up", "moe_w_down", "moe_b_down")
FUSED = True


def kernel(**inputs):
    n = 8
    shared = {k: np.ascontiguousarray(v) for k, v in inputs.items() if k not in ("x", "c", "positions")}
    shared["consts"] = make_consts()
    shared["sel"] = make_sel()
    xcur = [np.ascontiguousarray(inputs["x"][b]) for b in range(n)]
    groups = [PHASES] if FUSED else LAUNCH_GROUPS
    progs = {}
    for grp in groups:
        sliced = not FUSED
        pkey = ("moe0",) if (sliced and grp[0].startswith("moe")) else grp
        if pkey not in progs:
            progs[pkey] = build_prog(pkey, sliced=sliced)
        nc, names = progs[pkey]
        L = PHASE_LAYER[grp[0]]
        in_maps = []
        for b in range(n):
            m = {}
            for k in names:
                if k == "x":
                    m[k] = xcur[b]
                elif k == "c":
                    m[k] = np.ascontiguousarray(inputs["c"][b:b + 1])
                elif k == "positions":
                    m[k] = np.ascontiguousarray(inputs["positions"][b:b + 1]).astype(np.int32)
                elif sliced and k in PER_LAYER:
                    m[k] = np.ascontiguousarray(shared[k][L:L + 1])
                else:
                    m[k] = shared[k]
            in_maps.append(m)
        res = run_bass_kernel_spmd(nc, in_maps, core_ids=list(range(n)))
        xcur = [np.ascontiguousarray(res.results[b]["xo"]) for b in range(n)]
    return np.stack(xcur, axis=0).astype(np.float32)
```
